# Optimizing a Trainium2 kernel written in Bass

```python
import math, functools
import jax
import jax.numpy as jnp
from jax import lax
import numpy as np

D_MODEL = 1024
BATCH = 4
SEQ = 8192
DEPTH = 4

GRID_W = 64
CTX_LEN = 256
N_MIXERS = 3
ALPHA = (2 * DEPTH) ** 0.25
BETA = (8 * DEPTH) ** -0.25
LN_EPS = 1e-5
GN_EPS = 1e-5
RMS_EPS = 1e-6
LNX_EPS = 64e-5

RET_HEADS = 4
RET_DK = D_MODEL // RET_HEADS
RET_DV = 2 * RET_DK
RET_CHUNK = 128
ROPE_BASE = 10000.0

DN_QK_HEADS = 8
DN_V_HEADS = 16
DN_HEAD_DIM = 128
DN_CHUNK = 64
DN_CONV = 5

RWKV_HEAD = 64
RWKV_HEADS = D_MODEL // RWKV_HEAD
RWKV_DECAY_LORA = 64
RWKV_A_LORA = 64
RWKV_GATE_LORA = 128

FFN_DIM = 2816
N_EXPERTS = 8
TOP_K = 2
EXPERT_DIM = 3584
MOE_BLOCK = 256

N_RET = (DEPTH + 2) // 3
N_DN = (DEPTH + 1) // 3
N_RWKV = DEPTH // 3
N_DENSE = (DEPTH + 1) // 2
N_MOE = DEPTH // 2

RET_IN = 2 * RET_HEADS * RET_DK + 2 * RET_HEADS * RET_DV
DN_QK_W = DN_QK_HEADS * DN_HEAD_DIM
DN_V_W = DN_V_HEADS * DN_HEAD_DIM
DN_IN = 2 * DN_QK_W + 2 * DN_V_W + 4 * DN_V_HEADS

kernel_name = 'hybrid_retention_deltanet_rwkv7_moe_dit'


def layer_norm(x, g, b):
    xf = x.astype(jnp.float32)
    mu = jnp.mean(xf, -1, keepdims=True)
    var = jnp.mean(jnp.square(xf - mu), -1, keepdims=True)
    return ((xf - mu) * lax.rsqrt(var + LN_EPS) * g + b).astype(x.dtype)


def group_norm(x, eps):
    xf = x.astype(jnp.float32)
    mu = jnp.mean(xf, -1, keepdims=True)
    var = jnp.mean(jnp.square(xf - mu), -1, keepdims=True)
    return (xf - mu) * lax.rsqrt(var + eps)


def rms_norm(x, eps):
    xf = x.astype(jnp.float32)
    return xf * lax.rsqrt(jnp.mean(xf * xf, -1, keepdims=True) + eps)


def l2_normalize(x, eps=1e-6):
    xf = x.astype(jnp.float32)
    return (xf * lax.rsqrt(jnp.sum(xf * xf, -1, keepdims=True) + eps)).astype(x.dtype)


def rope_2d(x, row_pos, col_pos):
    half = x.shape[-1] // 2
    quarter = half // 2
    inv = ROPE_BASE ** (-jnp.arange(quarter, dtype=jnp.float32) / quarter)

    def rot(xa, pos):
        ang = pos.astype(jnp.float32)[:, None] * inv
        cos = jnp.cos(ang)[None, :, None, :]
        sin = jnp.sin(ang)[None, :, None, :]
        x1, x2 = xa[..., :quarter], xa[..., quarter:]
        return jnp.concatenate([x1 * cos - x2 * sin, x1 * sin + x2 * cos], -1)

    return jnp.concatenate([rot(x[..., :half], row_pos), rot(x[..., half:], col_pos)], -1).astype(x.dtype)


def centred_shift(x):
    p = jnp.pad(x, ((0, 0), (1, 1), (0, 0)))
    return 0.5 * (p[:, :-2] + p[:, 2:]) - x


def short_conv(x, w):
    ch = x.shape[-1]
    pad = (w.shape[0] - 1) // 2
    return lax.conv_general_dilated(x, w.astype(x.dtype)[:, None, :], (1,), [(pad, pad)],
                                    dimension_numbers=('NWC', 'WIO', 'NWC'), feature_group_count=ch)


def retention_scan(q, k, v, log_gamma, s0):
    b, h, t, _ = q.shape
    c = RET_CHUNK
    n = t // c
    pos = jnp.arange(c, dtype=jnp.float32)
    dist = pos[:, None] - pos[None, :]
    lower = dist >= 0
    inner = jnp.where(lower, jnp.exp(jnp.where(lower, dist, 0.0) * log_gamma[:, None, None]), 0.0)
    q_decay = jnp.exp((pos + 1.0) * log_gamma[:, None])[..., None]
    k_decay = jnp.exp((c - 1.0 - pos) * log_gamma[:, None])[..., None]
    chunk_decay = jnp.exp(c * log_gamma)[:, None, None]
    to_chunks = lambda a: jnp.moveaxis(a.reshape(b, h, n, c, a.shape[-1]), 2, 0)

    def step(s, inp):
        qc, kc, vc = inp
        scores = jnp.einsum('bhid,bhjd->bhij', qc, kc) * inner
        o = jnp.einsum('bhij,bhjv->bhiv', scores, vc) + jnp.einsum('bhid,bhdv->bhiv', qc * q_decay, s)
        s = s * chunk_decay + jnp.einsum('bhjd,bhjv->bhdv', kc * k_decay, vc)
        return s, o

    s, o = lax.scan(step, s0, (to_chunks(q), to_chunks(k), to_chunks(v)))
    return jnp.moveaxis(o, 0, 2).reshape(b, h, t, -1), s


def retention_mixer(h_ctx, h_lat, row_pos, col_pos, w_in, decay_logit, gn_g, w_out, with_ctx_out):
    hk = RET_HEADS * RET_DK
    hv = RET_HEADS * RET_DV

    def project(h, pos):
        b, t, _ = h.shape
        q, k, v, g = jnp.split(h @ w_in, [hk, 2 * hk, 2 * hk + hv], axis=-1)
        q = q.reshape(b, t, RET_HEADS, RET_DK)
        k = k.reshape(b, t, RET_HEADS, RET_DK) * (RET_DK ** -0.5)
        if pos is not None:
            q = rope_2d(q, *pos)
            k = rope_2d(k, *pos)
        v = v.reshape(b, t, RET_HEADS, RET_DV)
        bh = lambda a: jnp.swapaxes(a, 1, 2)
        return bh(q), bh(k), bh(v), g

    def finish(o, g):
        b, _, t, _ = o.shape
        o = group_norm(jnp.swapaxes(o, 1, 2), GN_EPS).reshape(b, t, hv) * gn_g
        return (jax.nn.silu(g) * o).astype(g.dtype) @ w_out

    log_gamma = jax.nn.log_sigmoid(decay_logit.astype(jnp.float32))
    qc, kc, vc, gc = project(h_ctx, None)
    ql, kl, vl, gl = project(h_lat, (row_pos, col_pos))
    s0 = jnp.zeros((h_lat.shape[0], RET_HEADS, RET_DK, RET_DV), jnp.float32)
    fl = lambda a: jnp.flip(a, axis=2)
    oc_f, sc_f = retention_scan(qc, kc, vc, log_gamma[0], s0)
    oc_b, sc_b = retention_scan(fl(qc), fl(kc), fl(vc), log_gamma[1], s0)
    ol_f, _ = retention_scan(ql, kl, vl, log_gamma[0], sc_f)
    ol_b, _ = retention_scan(fl(ql), fl(kl), fl(vl), log_gamma[1], sc_b)
    o_lat = finish(ol_f + fl(ol_b), gl)
    o_ctx = finish(oc_f + fl(oc_b), gc) if with_ctx_out else None
    return o_ctx, o_lat


def gdn_chunk_scan(q, k, v, g, beta, s0):
    b, h, t, _ = q.shape
    c = DN_CHUNK
    n = t // c
    f32 = jnp.float32
    ch = lambda a: a.astype(f32).reshape(b, h, n, c, *a.shape[3:])
    q, k, v, g, beta = ch(q), ch(k), ch(v), ch(g), ch(beta)
    gc = jnp.cumsum(g, axis=-1)
    idx = jnp.arange(c)
    incl = idx[:, None] >= idx[None, :]
    strict = idx[:, None] > idx[None, :]
    decay = jnp.exp(jnp.where(incl, gc[..., :, None] - gc[..., None, :], -jnp.inf))
    kb = k * beta[..., None]
    a_mat = jnp.where(strict, jnp.einsum('bhnid,bhnjd->bhnij', kb, k) * decay, 0.0) + jnp.eye(c, dtype=f32)
    u = lax.linalg.triangular_solve(a_mat, v * beta[..., None], left_side=True, lower=True, unit_diagonal=True)
    w = lax.linalg.triangular_solve(a_mat, kb * jnp.exp(gc)[..., None], left_side=True, lower=True, unit_diagonal=True)
    qk = jnp.einsum('bhnid,bhnjd->bhnij', q, k) * decay
    qg = q * jnp.exp(gc)[..., None]
    kt = k * jnp.exp(gc[..., -1:] - gc)[..., None]
    tail = jnp.exp(gc[..., -1])[..., None, None]
    sw = lambda a: jnp.moveaxis(a, 2, 0)

    def step(s, inp):
        qg_c, qk_c, u_c, w_c, kt_c, tail_c = inp
        v_new = u_c - jnp.einsum('bhcd,bhdv->bhcv', w_c, s)
        o = jnp.einsum('bhcd,bhdv->bhcv', qg_c, s) + jnp.einsum('bhij,bhjv->bhiv', qk_c, v_new)
        s = s * tail_c + jnp.einsum('bhcd,bhcv->bhdv', kt_c, v_new)
        return s, o

    s, o = lax.scan(step, s0, (sw(qg), sw(qk), sw(u), sw(w), sw(kt), sw(tail)))
    return jnp.moveaxis(o, 0, 2).reshape(b, h, t, -1), s


def deltanet_mixer(h_ctx, h_lat, w_in, conv_w, a_log, dt_bias, norm_g, w_out, with_ctx_out):
    rep = DN_V_HEADS // DN_QK_HEADS

    def project(h):
        b, t, _ = h.shape
        qkv, z, ab = jnp.split(h @ w_in, [2 * DN_QK_W + DN_V_W, 2 * DN_QK_W + 2 * DN_V_W], axis=-1)
        qkv = jax.nn.silu(short_conv(qkv, conv_w))
        q, k, v = jnp.split(qkv, [DN_QK_W, 2 * DN_QK_W], axis=-1)
        q = jnp.repeat(l2_normalize(q.reshape(b, t, DN_QK_HEADS, DN_HEAD_DIM)), rep, axis=2) * (DN_HEAD_DIM ** -0.5)
        k = jnp.repeat(l2_normalize(k.reshape(b, t, DN_QK_HEADS, DN_HEAD_DIM)), rep, axis=2)
        v = v.reshape(b, t, DN_V_HEADS, DN_HEAD_DIM)
        ab = ab.astype(jnp.float32).reshape(b, t, 2, 2, DN_V_HEADS)
        g = -jnp.exp(a_log.astype(jnp.float32)) * jax.nn.softplus(ab[:, :, :, 0] + dt_bias)
        beta = jax.nn.sigmoid(ab[:, :, :, 1])
        bh = lambda a: jnp.swapaxes(a, 1, 2)
        return bh(q), bh(k), bh(v), jnp.moveaxis(g, 1, -1), jnp.moveaxis(beta, 1, -1), z

    def finish(o, z):
        b, _, t, _ = o.shape
        o = rms_norm(jnp.swapaxes(o, 1, 2), RMS_EPS) * norm_g
        o = o * jax.nn.silu(z.reshape(b, t, DN_V_HEADS, DN_HEAD_DIM))
        return o.reshape(b, t, DN_V_W).astype(z.dtype) @ w_out

    qc, kc, vc, gcx, bcx, zc = project(h_ctx)
    ql, kl, vl, glt, blt, zl = project(h_lat)
    s0 = jnp.zeros((h_lat.shape[0], DN_V_HEADS, DN_HEAD_DIM, DN_HEAD_DIM), jnp.float32)
    fl = lambda a: jnp.flip(a, axis=2)
    oc_f, sc_f = gdn_chunk_scan(qc, kc, vc, gcx[:, 0], bcx[:, 0], s0)
    oc_b, sc_b = gdn_chunk_scan(fl(qc), fl(kc), fl(vc), fl(gcx[:, 1]), fl(bcx[:, 1]), s0)
    ol_f, _ = gdn_chunk_scan(ql, kl, vl, glt[:, 0], blt[:, 0], sc_f)
    ol_b, _ = gdn_chunk_scan(fl(ql), fl(kl), fl(vl), fl(glt[:, 1]), fl(blt[:, 1]), sc_b)
    o_lat = finish(ol_f + fl(ol_b), zl)
    o_ctx = finish(oc_f + fl(oc_b), zc) if with_ctx_out else None
    return o_ctx, o_lat


def rwkv7_scan(r, w, k, v, a, b, s0):
    tm = lambda u: jnp.moveaxis(u.astype(jnp.float32), 1, 0)

    def step(s, inp):
        r_t, w_t, k_t, v_t, a_t, b_t = inp
        sa = jnp.einsum('bhvk,bhk->bhv', s, a_t)
        s = s * w_t[:, :, None, :] + sa[..., None] * b_t[:, :, None, :] + v_t[..., None] * k_t[:, :, None, :]
        return s, jnp.einsum('bhvk,bhk->bhv', s, r_t)

    s, o = lax.scan(step, s0, (tm(r), tm(w), tm(k), tm(v), tm(a), tm(b)))
    return jnp.moveaxis(o, 0, 1), s


def rwkv7_mixer(h_ctx, h_lat, mix, w_rkv, w0, w1, w2, a0, a1, a2, g1, g2, k_k, k_a, r_k, lnx_g, w_out, with_ctx_out):
    heads = lambda u: u.reshape(*u.shape[:-1], RWKV_HEADS, RWKV_HEAD)

    def project(h):
        xx = centred_shift(h)
        r = (h + xx * mix[0]) @ w_rkv[0]
        k = (h + xx * mix[1]) @ w_rkv[1]
        v = (h + xx * mix[2]) @ w_rkv[2]
        xw, xa, xg = h + xx * mix[3], h + xx * mix[4], h + xx * mix[5]
        w_logit = w0[:, None, None, :] + jnp.einsum('zbtr,zrc->zbtc', jnp.tanh(jnp.einsum('btc,zcr->zbtr', xw, w1)), w2)
        decay = jnp.exp(-jnp.exp(-jax.nn.softplus(-w_logit.astype(jnp.float32)) - 0.5))
        a = jax.nn.sigmoid(a0[:, None, None, :] + jnp.einsum('zbtr,zrc->zbtc', jnp.einsum('btc,zcr->zbtr', xa, a1), a2))
        g = jax.nn.sigmoid(xg @ g1) @ g2
        kk = l2_normalize(heads(k * k_k))
        k_dir = heads(k[None] * (1.0 + (a - 1.0) * k_a))
        return heads(r), heads(decay), k_dir, heads(v), kk, heads(a), g

    def scan_args(r, dec, kd, v, kk, a, z):
        return r, dec[z], kd[z], v, -kk, kk * a[z]

    def finish(o, r, k_dir, v, g):
        b, t = o.shape[:2]
        bonus = jnp.sum(r[None] * k_dir * r_k, axis=(0, -1))[..., None] * v
        o = group_norm(o, LNX_EPS).reshape(b, t, D_MODEL) * lnx_g + bonus.reshape(b, t, D_MODEL)
        return (o * g).astype(g.dtype) @ w_out

    rc, dc, kdc, vc, kkc, ac, gc = project(h_ctx)
    rl, dl, kdl, vl, kkl, al, gl = project(h_lat)
    s0 = jnp.zeros((h_lat.shape[0], RWKV_HEADS, RWKV_HEAD, RWKV_HEAD), jnp.float32)
    fl = lambda u: jnp.flip(u, axis=1)
    oc_f, sc_f = rwkv7_scan(*scan_args(rc, dc, kdc, vc, kkc, ac, 0), s0)
    oc_b, sc_b = rwkv7_scan(*map(fl, scan_args(rc, dc, kdc, vc, kkc, ac, 1)), s0)
    ol_f, _ = rwkv7_scan(*scan_args(rl, dl, kdl, vl, kkl, al, 0), sc_f)
    ol_b, _ = rwkv7_scan(*map(fl, scan_args(rl, dl, kdl, vl, kkl, al, 1)), sc_b)
    o_lat = finish(ol_f + fl(ol_b), rl, kdl, vl, gl)
    o_ctx = finish(oc_f + fl(oc_b), rc, kdc, vc, gc) if with_ctx_out else None
    return o_ctx, o_lat


def swiglu(h, w_gu, w_down):
    gt, up = jnp.split(h @ w_gu, 2, axis=-1)
    return (jax.nn.silu(gt) * up) @ w_down


def moe_swiglu(h, w_router, w_gu, w_down):
    shp = h.shape
    x = h.reshape(-1, shp[-1])
    n = x.shape[0]
    logits = (x @ w_router).astype(jnp.float32)
    top_logit, top_e = lax.top_k(logits, TOP_K)
    gate = jax.nn.softmax(top_logit, axis=-1)
    flat_e = top_e.reshape(-1)
    flat_tok = jnp.repeat(jnp.arange(n, dtype=jnp.int32), TOP_K)
    flat_gate = gate.reshape(-1)
    order = jnp.argsort(flat_e)
    e_sorted = flat_e[order]
    counts = jnp.bincount(flat_e, length=N_EXPERTS)
    padded = (counts + MOE_BLOCK - 1) // MOE_BLOCK * MOE_BLOCK
    start = jnp.cumsum(counts) - counts
    pend = jnp.cumsum(padded)
    pstart = pend - padded
    slot = pstart[e_sorted] + jnp.arange(n * TOP_K) - start[e_sorted]
    n_slots = (n * TOP_K + MOE_BLOCK - 1) // MOE_BLOCK * MOE_BLOCK + N_EXPERTS * MOE_BLOCK
    n_blocks = n_slots // MOE_BLOCK
    slot_tok = jnp.full((n_slots,), n, jnp.int32).at[slot].set(flat_tok[order])
    slot_gate = jnp.zeros((n_slots,), jnp.float32).at[slot].set(flat_gate[order])
    block_e = jnp.minimum(jnp.sum(jnp.arange(n_blocks)[:, None] * MOE_BLOCK >= pend[None, :], axis=1), N_EXPERTS - 1)
    x_pad = jnp.concatenate([x, jnp.zeros((1, x.shape[-1]), x.dtype)], 0)
    xb = x_pad[slot_tok].reshape(n_blocks, MOE_BLOCK, -1)
    yb = lax.map(lambda args: swiglu(args[0], w_gu[args[1]], w_down[args[1]]), (xb, block_e))
    yb = yb.reshape(n_slots, -1)
    y = jnp.zeros((n + 1, yb.shape[-1]), yb.dtype).at[slot_tok].add(yb * slot_gate[:, None].astype(yb.dtype))
    return y[:n].reshape(shp)


def setup_inputs(seed: int = 0) -> dict:
    key = jax.random.key(seed)
    ks = iter(jax.random.split(key, 48))
    f32 = jnp.float32
    D = D_MODEL
    nrm = lambda shape, scale: scale * jax.random.normal(next(ks), shape, f32)
    uni = lambda shape, lo, hi: jax.random.uniform(next(ks), shape, f32, minval=lo, maxval=hi)

    ret_gamma = 1.0 - 2.0 ** (-5.0 - jnp.arange(RET_HEADS, dtype=f32))
    ret_logit = jnp.log(ret_gamma) - jnp.log1p(-ret_gamma)
    dt = jnp.exp(uni((N_DN, 2, DN_V_HEADS), math.log(1e-3), math.log(1e-1)))
    n_idx = jnp.arange(D, dtype=f32) / (D - 1)
    decay_speed = -7.0 + 5.0 * n_idx ** (0.85 + 0.5 ** 0.5)

    return {
        'x': nrm((BATCH, SEQ, D), 1.0),
        'c': nrm((BATCH, D), 1.0),
        'ctx': nrm((BATCH, CTX_LEN, D), 1.0),
        'c_ctx': nrm((D,), 1.0),
        'mod_w': nrm((DEPTH, D, 6 * D), 0.5 * D ** -0.5),
        'mod_b': nrm((DEPTH, 6 * D), 0.02),
        'ln_g': 1.0 + nrm((DEPTH, 2, D), 0.02),
        'ln_b': nrm((DEPTH, 2, D), 0.02),
        'ret_w_in': nrm((N_RET, D, RET_IN), D ** -0.5),
        'ret_decay': ret_logit + nrm((N_RET, 2, RET_HEADS), 0.01),
        'ret_gn_g': 1.0 + nrm((N_RET, RET_HEADS * RET_DV), 0.02),
        'ret_w_out': nrm((N_RET, RET_HEADS * RET_DV, D), BETA * (RET_HEADS * RET_DV) ** -0.5),
        'dn_w_in': nrm((N_DN, D, DN_IN), D ** -0.5),
        'dn_conv_w': nrm((N_DN, DN_CONV, 2 * DN_QK_W + DN_V_W), DN_CONV ** -0.5),
        'dn_a_log': jnp.log(uni((N_DN, 2, DN_V_HEADS), 1.0, 16.0)),
        'dn_dt_bias': dt + jnp.log(-jnp.expm1(-dt)),
        'dn_norm_g': 1.0 + nrm((N_DN, DN_HEAD_DIM), 0.02),
        'dn_w_out': nrm((N_DN, DN_V_W, D), BETA * DN_V_W ** -0.5),
        'rk_mix': uni((N_RWKV, 6, D), 0.0, 1.0),
        'rk_w_rkv': nrm((N_RWKV, 3, D, D), D ** -0.5),
        'rk_w0': decay_speed + 0.5 + nrm((N_RWKV, 2, D), 0.1),
        'rk_w1': nrm((N_RWKV, 2, D, RWKV_DECAY_LORA), D ** -0.5),
        'rk_w2': nrm((N_RWKV, 2, RWKV_DECAY_LORA, D), 0.1 * RWKV_DECAY_LORA ** -0.5),
        'rk_a0': nrm((N_RWKV, 2, D), 0.1),
        'rk_a1': nrm((N_RWKV, 2, D, RWKV_A_LORA), D ** -0.5),
        'rk_a2': nrm((N_RWKV, 2, RWKV_A_LORA, D), 0.3 * RWKV_A_LORA ** -0.5),
        'rk_g1': nrm((N_RWKV, D, RWKV_GATE_LORA), D ** -0.5),
        'rk_g2': nrm((N_RWKV, RWKV_GATE_LORA, D), RWKV_GATE_LORA ** -0.5),
        'rk_k_k': 0.85 + nrm((N_RWKV, D), 0.02),
        'rk_k_a': 1.0 + nrm((N_RWKV, D), 0.02),
        'rk_r_k': nrm((N_RWKV, RWKV_HEADS, RWKV_HEAD), 0.1),
        'rk_lnx_g': 1.0 + nrm((N_RWKV, D), 0.02),
        'rk_w_out': nrm((N_RWKV, D, D), BETA * D ** -0.5),
        'ffn_w_gu': nrm((N_DENSE, D, 2 * FFN_DIM), D ** -0.5),
        'ffn_w_down': nrm((N_DENSE, FFN_DIM, D), BETA * FFN_DIM ** -0.5),
        'moe_router': nrm((N_MOE, D, N_EXPERTS), D ** -0.5),
        'moe_w_gu': nrm((N_MOE, N_EXPERTS, D, 2 * EXPERT_DIM), D ** -0.5),
        'moe_w_down': nrm((N_MOE, N_EXPERTS, EXPERT_DIM, D), BETA * EXPERT_DIM ** -0.5),
    }


def reference(x, c, ctx, c_ctx, mod_w, mod_b, ln_g, ln_b,
              ret_w_in, ret_decay, ret_gn_g, ret_w_out,
              dn_w_in, dn_conv_w, dn_a_log, dn_dt_bias, dn_norm_g, dn_w_out,
              rk_mix, rk_w_rkv, rk_w0, rk_w1, rk_w2, rk_a0, rk_a1, rk_a2, rk_g1, rk_g2,
              rk_k_k, rk_k_a, rk_r_k, rk_lnx_g, rk_w_out,
              ffn_w_gu, ffn_w_down, moe_router, moe_w_gu, moe_w_down):
    t = x.shape[1]
    n_ctx = ctx.shape[1]
    rows = t // GRID_W
    row_pos = jnp.repeat(jnp.arange(rows), GRID_W)
    col_pos = jnp.tile(jnp.arange(GRID_W), rows)
    s_lat = jax.nn.silu(c)
    s_ctx = jax.nn.silu(c_ctx)
    lat, cx = x, ctx
    for i in range(DEPTH):
        last = i == DEPTH - 1
        m_lat = (s_lat @ mod_w[i] + mod_b[i])[:, None, :]
        m_ctx = (s_ctx @ mod_w[i] + mod_b[i])[None, None, :]
        sh1, sc1, ga1, sh2, sc2, ga2 = jnp.split(m_lat, 6, axis=-1)
        csh1, csc1, cga1, csh2, csc2, cga2 = jnp.split(m_ctx, 6, axis=-1)
        h_lat = lat * (1.0 + sc1) + sh1
        h_ctx = cx * (1.0 + csc1) + csh1
        kind, j = i % N_MIXERS, i // N_MIXERS
        if kind == 0:
            o_ctx, o_lat = retention_mixer(h_ctx, h_lat, row_pos, col_pos, ret_w_in[j], ret_decay[j],
                                           ret_gn_g[j], ret_w_out[j], not last)
        elif kind == 1:
            o_ctx, o_lat = deltanet_mixer(h_ctx, h_lat, dn_w_in[j], dn_conv_w[j], dn_a_log[j], dn_dt_bias[j],
                                          dn_norm_g[j], dn_w_out[j], not last)
        else:
            o_ctx, o_lat = rwkv7_mixer(h_ctx, h_lat, rk_mix[j], rk_w_rkv[j], rk_w0[j], rk_w1[j], rk_w2[j],
                                       rk_a0[j], rk_a1[j], rk_a2[j], rk_g1[j], rk_g2[j], rk_k_k[j], rk_k_a[j],
                                       rk_r_k[j], rk_lnx_g[j], rk_w_out[j], not last)
        if i % 2 == 0:
            channel = functools.partial(swiglu, w_gu=ffn_w_gu[i // 2], w_down=ffn_w_down[i // 2])
        else:
            channel = functools.partial(moe_swiglu, w_router=moe_router[i // 2], w_gu=moe_w_gu[i // 2],
                                        w_down=moe_w_down[i // 2])
        lat = layer_norm(ALPHA * lat + (1.0 + ga1) * o_lat, ln_g[i, 0], ln_b[i, 0])
        h_lat = lat * (1.0 + sc2) + sh2
        if last:
            lat = layer_norm(ALPHA * lat + (1.0 + ga2) * channel(h_lat), ln_g[i, 1], ln_b[i, 1])
        else:
            cx = layer_norm(ALPHA * cx + (1.0 + cga1) * o_ctx, ln_g[i, 0], ln_b[i, 0])
            h_ctx = cx * (1.0 + csc2) + csh2
            f = channel(jnp.concatenate([h_ctx, h_lat], axis=1))
            cx = layer_norm(ALPHA * cx + (1.0 + cga2) * f[:, :n_ctx], ln_g[i, 1], ln_b[i, 1])
            lat = layer_norm(ALPHA * lat + (1.0 + ga2) * f[:, n_ctx:], ln_g[i, 1], ln_b[i, 1])
    return lat
```

```python
import numpy as np
from contextlib import ExitStack
import concourse.bass as bass
import concourse.mybir as mybir
from concourse.bass_utils import run_bass_kernel_spmd

F32 = mybir.dt.float32
BF16 = mybir.dt.bfloat16
AF = mybir.ActivationFunctionType
ALU = mybir.AluOpType
AX = mybir.AxisListType

D = 1024
NCTX = 256
ALPHA = 8.0 ** 0.25
LN_EPS = 1e-5


class Sem:
    __slots__ = ("h", "total")

    def __init__(self, h):
        self.h = h
        self.total = 0


class Buf:
    __slots__ = ("t", "w", "r", "sem", "name")

    def __init__(self, t, name):
        self.t = t
        self.w = None
        self.r = {}
        self.sem = None
        self.name = name

    def __getitem__(self, k):
        return self.t[k]


class Em:
    def __init__(self, nc):
        self.nc = nc
        self.eng = {"pe": nc.tensor, "act": nc.scalar, "dve": nc.vector, "pool": nc.gpsimd, "sp": nc.sync}
        self.semobj = {k: Sem(nc.alloc_semaphore(name="s_" + k)) for k in self.eng}
        self.waited = {k: {} for k in self.eng}
        self.dma_sems = []
        self.free_dma_sems = []
        self.bufs = []
        self.nins = 0

    def sb(self, stack, name, shape, dt):
        self.uid = getattr(self, "uid", 0) + 1
        t = stack.enter_context(self.nc.sbuf_tensor("sb%d_%s" % (self.uid, name), list(shape), dt))
        b = Buf(t, name)
        self.bufs.append(b)
        return b

    def ps(self, stack, name, shape=(128, 512), dt=F32):
        t = stack.enter_context(self.nc.psum_tensor("pp_" + name, list(shape), dt))
        b = Buf(t, name)
        self.bufs.append(b)
        return b

    def _dma_sem(self):
        if self.free_dma_sems:
            return self.free_dma_sems.pop()
        s = Sem(self.nc.alloc_semaphore(name="d%d" % len(self.dma_sems)))
        self.dma_sems.append(s)
        return s

    def release(self, bufs):
        for b in bufs:
            if b.sem is not None:
                self.free_dma_sems.append(b.sem)
                b.sem = None
            if b in self.bufs:
                self.bufs.remove(b)

    def _wait(self, en, toks):
        E = self.eng[en]
        w = self.waited[en]
        own = self.semobj[en]
        best = {}
        for (s, v) in toks:
            if s is own and en in ("pe", "sp"):
                continue
            if best.get(s, 0) < v:
                best[s] = v
        for s, v in best.items():
            if w.get(s, 0) < v:
                E.wait_ge(s.h, v)
                w[s] = v

    def _deps(self, reads, writes):
        toks = []
        for b in reads:
            if b.w is not None:
                toks.append(b.w)
        for b in writes:
            if b.w is not None:
                toks.append(b.w)
            toks.extend(b.r.items())
        return toks

    @staticmethod
    def _mark(tok, reads, writes):
        s, v = tok
        for b in reads:
            if b.r.get(s, 0) < v:
                b.r[s] = v
        for b in writes:
            b.w = tok
            b.r = {}

    def op(self, en, fn, reads=(), writes=()):
        self._wait(en, self._deps(reads, writes))
        ins = fn(self.eng[en])
        s = self.semobj[en]
        s.total += 1
        ins.then_inc(s.h, 1)
        self._mark((s, s.total), reads, writes)
        self.nins += 1

    def dma(self, qn, out, in_, reads=(), writes=(), sem=None):
        self._wait(qn, self._deps(reads, writes))
        ins = self.eng[qn].dma_start(out=out, in_=in_)
        if sem is None:
            b = writes[0] if writes else reads[0]
            if b.sem is None:
                b.sem = self._dma_sem()
            sem = b.sem
        sem.total += 16
        ins.then_inc(sem.h, 16)
        self._mark((sem, sem.total), reads, writes)
        self.nins += 1

    def barrier(self):
        allsems = list(self.semobj.values()) + self.dma_sems
        for en, E in self.eng.items():
            w = self.waited[en]
            for s in allsems:
                if s.total > 0 and w.get(s, 0) < s.total:
                    E.wait_ge(s.h, s.total)
                    w[s] = s.total
        for b in self.bufs:
            b.w = None
            b.r = {}


def bc(ap_t, offset_elems, dims):
    from concourse.ap import AP
    return AP(ap_t, offset_elems, dims)


C_ID, C_J0, C_J1, C_MRET, C_TRI, C_BLK, C_SEL0, C_SEL1, C_MS, C_MI = range(10)
C_TRIS, C_ONES, C_B64 = 10, 11, 12
C_LVT = 13
C_LV = 19
NCONST = 25
NF32 = 13


def make_consts(z):
    p = np.arange(128)[:, None]
    f = np.arange(128)[None, :]
    blk = (p // 64) == (f // 64)
    cm = np.zeros((NCONST, 128, 128), np.float32)
    cm[C_ID] = np.eye(128)
    J = np.eye(128)[::-1]
    cm[C_J0] = J
    cm[C_MRET] = (p <= f)
    cm[C_TRI] = (p <= f) & blk
    cm[C_TRIS] = (p < f) & blk
    cm[C_BLK] = blk
    cm[C_SEL0] = (p < 64) & (f >= 0)
    cm[C_SEL1] = (p >= 64) & (f >= 0)
    cm[C_MS] = (p < f) & blk
    cm[C_MI] = (p <= f) & blk
    for k in range(6):
        m = ((p >> (k + 1)) == (f >> (k + 1))) & (((p >> k) & 1) == 1) & (((f >> k) & 1) == 0)
        cm[C_LV + k] = m
        cm[C_LVT + k] = m.T
    cm[C_ONES] = 1.0
    cm[C_B64] = blk
    return cm


class MK:
    def __init__(self, L, layers, ncores=4, direct=True, layer_ids=None):
        self.layer_ids = list(range(layers)) if layer_ids is None else list(layer_ids)
        self.ncores = ncores
        self.direct = direct
        self.pairs = [[2 * p, 2 * p + 1] for p in range(max(1, ncores // 2))]
        self.L = L
        self.layers = layers
        self.NLT = L // 128
        self.NT = 2 + self.NLT
        self.T = 256 + L
        self.HT = 256 + L // 2
        self.groups = [(0, 2, True)] + [(2 + 4 * g, 4, False) for g in range(self.NLT // 4)]
        self.half_groups = self.groups
        self.nc = bass.Bass("TRN2", target_bir_lowering=False)
        self.em = Em(self.nc)
        self.ext = {}
        self.units = {}
        self.cc_sem = Sem(self.nc.alloc_semaphore(name="cc"))
        self.em.dma_sems.append(self.cc_sem)
        self.gsem = Sem(self.nc.alloc_semaphore(name="gdma"))
        self.em.dma_sems.append(self.gsem)

    def xin(self, name, shape, dt=F32):
        t = self.nc.dram_tensor(name, list(shape), dt, kind="ExternalInput")
        self.ext[name] = (tuple(shape), dt)
        return t

    def dr(self, name, shape, dt):
        return self.nc.dram_tensor(name, list(shape), dt, kind="Internal")

    def ptile(self, t):
        return 1 - t if t < 2 else 2 + (self.NLT - 1 - (t - 2))

    def unit(self, name, K, N):
        em = self.em
        g = self.dr("wg_" + name, [K, N], BF16)
        self.units[name] = (g, K, N)
        if self.direct:
            full = self.xin("wf_" + name, [K, N])
            em.dma("pool", g.ap(), full.ap(), sem=self.gsem)
            return None
        sh = self.xin("w_" + name, [K // 8, N])
        sb = self.dr("ws_" + name, [K // 8, N], BF16)
        gq = self.dr("wq_" + name, [K // 2, N], BF16)
        em.dma("pool", sb.ap(), sh.ap(), sem=self.gsem)
        return (sb, gq, g)

    def gather_units(self, pend):
        em = self.em
        em.barrier()
        if self.direct:
            return
        for stage in range(2):
            for sb, gq, g in pend:
                rg = [[0, 1, 2, 3], [4, 5, 6, 7]] if stage == 0 else [[0, 4], [1, 5], [2, 6], [3, 7]]
                src, dst = (sb, gq) if stage == 0 else (gq, g)
                ins = self.nc.gpsimd.collective_compute("AllGather", ALU.bypass, replica_groups=rg,
                                                        ins=[src.ap()], outs=[dst.ap()])
                self.cc_sem.total += 1
                ins.then_inc(self.cc_sem.h, 1)
                self.nc.gpsimd.wait_ge(self.cc_sem.h, self.cc_sem.total)
            em.barrier()

    def pair_gather(self, src, dst):
        em = self.em
        em.barrier()
        ins = self.nc.gpsimd.collective_compute("AllGather", ALU.bypass, replica_groups=self.pairs,
                                                ins=[src.ap()], outs=[dst.ap()])
        self.cc_sem.total += 1
        ins.then_inc(self.cc_sem.h, 1)
        em.barrier()

    def wsrc(self, name, kc0, nkc, n0, nw):
        g, K, N = self.units[name]
        return g.ap()[kc0 * 128:(kc0 + nkc) * 128, n0:n0 + nw].rearrange("(c p) n -> p c n", p=128)

    def load_w(self, buf, name, kc0, nkc, n0, nw):
        self.em.dma("sp", buf[:, 0:nkc, 0:nw], self.wsrc(name, kc0, nkc, n0, nw), writes=[buf])

    def setup(self):
        nc, em = self.nc, self.em
        self.pst = ExitStack()
        st = self.pst
        self.cmf = em.sb(st, "cmf", [128, NF32, 128], F32)
        self.cmb = em.sb(st, "cmb", [128, NCONST, 128], BF16)
        cm = self.xin("cm", [128, NCONST, 128])
        em.dma("sp", self.cmf[:], cm.ap()[:, 0:NF32, :], writes=[self.cmf])
        em.dma("pool", self.cmb[:], cm.ap(), writes=[self.cmb])
        self.iota = em.sb(st, "iota", [128, 4], F32)
        em.dma("sp", self.iota[:], self.xin("iota", [128, 4]).ap(), writes=[self.iota])
        self.psb = [em.ps(st, "ps%d" % i) for i in range(8)]
        self.psk = 0
        self.xs = self.xin("xs", [self.T, D])
        self.cvec = self.xin("cvec", [128, 16])
        self.modb = self.xin("modb", [4, 128, 48])
        self.modbf = self.xin("modbf", [4, 6144])
        self.lng = self.xin("lng", [4, 2, D])
        self.lnb = self.xin("lnb", [4, 2, D])
        self.xsf = self.xin("xsf", [self.T, D])
        self.LAT2 = [self.dr("LAT0", [self.T, D], F32), self.dr("LAT1", [self.T, D], F32)]
        self.O2 = [self.dr("O0", [self.T, 2048], F32), self.dr("O1", [self.T, 2048], F32)]
        self.H2T = self.dr("H2T", [len(self.half_groups), 128, 8 * 512], BF16)
        em.dma("pool", self.LAT2[0].ap(), self.xs.ap(), sem=self.gsem)
        em.dma("pool", self.LAT2[1].ap(), self.xsf.ap(), sem=self.gsem)
        self.OUT = self.nc.dram_tensor("out", [self.T, D], F32, kind="ExternalOutput")
        self.flt = [em.sb(st, "flt%d" % q, [128, D], F32) for q in range(2)]
        self.fltk = 0

    def cf(self, i):
        return self.cmf[:, i, :]

    def store_lat(self, t, ou, out=False):
        em = self.em
        em.dma("pool", self.LAT2[0].ap()[t * 128:(t + 1) * 128, :], ou[:], reads=[ou])
        if out:
            em.dma("pool", self.OUT.ap()[t * 128:(t + 1) * 128, :], ou[:], reads=[ou])
        fl = self.flt[self.fltk % 2]
        self.fltk += 1
        for half in range(2):
            ps = self.pn()
            self.mm(ps, ps[:, :], self.cf(C_J0), ou[:, half * 512:(half + 1) * 512], True, True, [self.cmf, ou])
            if half:
                em.op("act", lambda e, ps=ps, fl=fl: e.activation(out=fl[:, 512:1024], in_=ps[:, :], func=AF.Copy), reads=[ps], writes=[fl])
            else:
                em.op("dve", lambda e, ps=ps, fl=fl: e.tensor_copy(out=fl[:, 0:512], in_=ps[:, :]), reads=[ps], writes=[fl])
        pt = self.ptile(t)
        em.dma("pool", self.LAT2[1].ap()[pt * 128:(pt + 1) * 128, :], fl[:], reads=[fl])

    def cb(self, i):
        return self.cmb[:, i, :]

    def pn(self):
        while True:
            b = self.psb[self.psk % 8]
            self.psk += 1
            if b not in getattr(self, "reserved", ()):
                return b

    def mm(self, ps, out, lhsT, rhs, start, stop, reads):
        self.em.op("pe", lambda e: e.matmul(out, lhsT=lhsT, rhs=rhs, start=start, stop=stop), reads=reads, writes=[ps])

    def phase_mod(self, i, st):
        em = self.em
        modLC = em.sb(st, "modLC%d" % i, [128, 48, 2], F32)
        rows = {n: em.sb(st, "%s_%d" % (n, i), [128, D], F32)
                for n in ("GA1L", "GA1C", "GA2L", "GA2C", "LNG0", "LNB0", "LNG1", "LNB1")}
        import os
        for s in range(2 if os.environ.get("MKA", "0") == "0" else 0):
            em.dma("sp", rows["LNG%d" % s][:], self.lng.ap()[i, s:s + 1, :].partition_broadcast(128), writes=[rows["LNG%d" % s]])
            em.dma("sp", rows["LNB%d" % s][:], self.lnb.ap()[i, s:s + 1, :].partition_broadcast(128), writes=[rows["LNB%d" % s]])
        import os
        dbg = int(os.environ.get("MKDBG", "99"))
        if dbg == 0:
            em.barrier()
            return modLC, rows
        with ExitStack() as ts:
            cv = em.sb(ts, "cv", [128, 16], F32)
            em.dma("sp", cv[:], self.cvec.ap(), writes=[cv])
            sT = em.sb(ts, "sT", [128, 8, 2], BF16)
            em.op("act", lambda e: e.activation(out=sT[:].rearrange("p c t -> p (c t)"), in_=cv[:], func=AF.Silu),
                  reads=[cv], writes=[sT])
            sBC = em.sb(ts, "sBC", [128, 8, 2, 128], BF16)
            em.op("dve", lambda e: e.tensor_copy(out=sBC[:], in_=sT[:].unsqueeze(3).broadcast_to([128, 8, 2, 128])),
                  reads=[sT], writes=[sBC])
            mb = em.sb(ts, "mb", [128, 48], F32)
            em.dma("sp", mb[:], self.modb.ap()[i], writes=[mb])
            brow = [em.sb(ts, "brow%d" % q, [128, D], F32) for q in range(2)]
            em.dma("sp", brow[0][:], self.modbf.ap()[i:i + 1, 2048:3072].partition_broadcast(128), writes=[brow[0]])
            em.dma("sp", brow[1][:], self.modbf.ap()[i:i + 1, 5120:6144].partition_broadcast(128), writes=[brow[1]])
            wb = [em.sb(ts, "wm%d" % q, [128, 8, 1536], BF16) for q in range(2)]
            psm = self.pn()
            self.reserved = [psm]
            if dbg == 1:
                em.barrier()
                return modLC, rows
            for piece in range(4):
                w = wb[piece % 2]
                self.load_w(w, "modw%d" % i, 0, 8, piece * 1536, 1536)
                if dbg == 2:
                    continue
                for oc in range(12):
                    g = piece * 12 + oc
                    for kc in range(8):
                        self.mm(psm, psm[:, g * 2:g * 2 + 2], w[:, kc, oc * 128:(oc + 1) * 128], sT[:, kc, :],
                                kc == 0, kc == 7, [w, sT])
                if piece in (1, 3):
                    for t, nm in ((0, "L"), (1, "C")):
                        dest = rows[("GA1" if piece == 1 else "GA2") + nm]
                        br = brow[0 if piece == 1 else 1]
                        for half in range(2):
                            pb = self.pn()
                            for kc in range(8):
                                self.mm(pb, pb[:, :], sBC[:, kc, t, :], w[:, kc, 512 + half * 512:1024 + half * 512],
                                        kc == 0, kc == 7, [w, sBC])
                            em.op("dve", lambda e, pb=pb, dest=dest, br=br, half=half: e.scalar_tensor_tensor(
                                out=dest[:, half * 512:(half + 1) * 512], in0=pb[:, :], scalar=1.0,
                                in1=br[:, half * 512:(half + 1) * 512], op0=ALU.add, op1=ALU.add),
                                reads=[pb, br], writes=[dest])
            em.op("dve", lambda e: e.tensor_tensor(out=modLC[:], in0=psm[:, 0:96].rearrange("p (c t) -> p c t", t=2),
                                                   in1=mb[:].unsqueeze(2).broadcast_to([128, 48, 2]), op=ALU.add),
                  reads=[psm, mb], writes=[modLC])
            self.reserved = []
            for c0 in (8, 32):
                em.op("dve", lambda e, c0=c0: e.tensor_scalar_add(out=modLC[:, c0:c0 + 8, :], in0=modLC[:, c0:c0 + 8, :], scalar1=1.0),
                      reads=[modLC], writes=[modLC])
            em.barrier()
            em.release([cv, sT, sBC, mb] + brow + wb)
        return modLC, rows

    def hT_tile(self, lat_tile, hT, col0, modLC, sub, isctx):
        em = self.em
        t = 1 if isctx else 0
        sh0, sc0 = (0, 8) if sub == 0 else (24, 32)
        for half in range(2):
            ps = self.pn()
            for c4 in range(4):
                c = half * 4 + c4
                em.op("pe", lambda e, ps=ps, c=c, c4=c4: e.transpose(out=ps[:, c4 * 128:(c4 + 1) * 128],
                                                                     in_=lat_tile[:, c * 128:(c + 1) * 128],
                                                                     identity=self.cf(C_ID)),
                      reads=[lat_tile, self.cmf], writes=[ps])
            for c4 in range(4):
                c = half * 4 + c4
                em.op("act", lambda e, ps=ps, c=c, c4=c4: e.activation(
                    out=hT[:, c, col0:col0 + 128], in_=ps[:, c4 * 128:(c4 + 1) * 128], func=AF.Identity,
                    scale=modLC[:, sc0 + c, t:t + 1], bias=modLC[:, sh0 + c, t:t + 1]),
                    reads=[ps, modLC], writes=[hT])

    def tail(self, pso, lat_tile, GA, LNG, LNB, tmp, outt, st6, mv, rs):
        em = self.em
        for half in range(2):
            sl = slice(half * 512, (half + 1) * 512)
            sb_, sap = pso[half] if isinstance(pso[half], tuple) else (pso[half], pso[half][:, :])
            em.op("dve", lambda e, sap=sap, sl=sl: e.tensor_tensor(out=tmp[:, sl], in0=sap, in1=GA[:, sl], op=ALU.mult),
                  reads=[sb_, GA], writes=[tmp])
        em.op("dve", lambda e: e.scalar_tensor_tensor(out=tmp[:], in0=lat_tile[:], scalar=ALPHA, in1=tmp[:], op0=ALU.mult, op1=ALU.add),
              reads=[lat_tile, tmp], writes=[tmp])
        self.layer_norm(tmp, LNG, LNB, outt, st6, mv, rs)

    def layer_norm(self, tmp, LNG, LNB, outt, st6, mv, rs):
        em = self.em
        for half in range(2):
            em.op("dve", lambda e, half=half: e.bn_stats(out=st6[:, half, :], in_=tmp[:, half * 512:(half + 1) * 512]),
                  reads=[tmp], writes=[st6])
        em.op("dve", lambda e: e.bn_aggr(out=mv[:], in_=st6[:]), reads=[st6], writes=[mv])
        em.op("act", lambda e: e.activation(out=rs[:], in_=mv[:, 1:2], func=AF.Sqrt, bias=LN_EPS, scale=1.0), reads=[mv], writes=[rs])
        em.op("dve", lambda e: e.reciprocal(out=rs[:], in_=rs[:]), reads=[rs], writes=[rs])
        em.op("dve", lambda e: e.tensor_scalar(out=tmp[:], in0=tmp[:], scalar1=mv[:, 0:1], scalar2=rs[:, 0:1],
                                               op0=ALU.subtract, op1=ALU.mult), reads=[tmp, mv, rs], writes=[tmp])
        em.op("pool", lambda e: e.tensor_tensor(out=tmp[:], in0=tmp[:], in1=LNG[:], op=ALU.mult), reads=[tmp, LNG], writes=[tmp])
        em.op("pool", lambda e: e.tensor_tensor(out=outt[:], in0=tmp[:], in1=LNB[:], op=ALU.add), reads=[tmp, LNB], writes=[outt])

    def phase_f(self, i, modLC, rows, factory, wout_name, KC):
        em = self.em
        with ExitStack() as st:
            make_yT, mbufs = factory(st)
            wout = em.sb(st, "wout", [128, KC, D], BF16)
            self.load_w(wout, wout_name, 0, KC, 0, D)
            yT = [em.sb(st, "yT%d" % q, [128, KC, 128], BF16) for q in range(2)]
            lat = [em.sb(st, "flat%d" % q, [128, D], F32) for q in range(2)]
            outt = [em.sb(st, "fout%d" % q, [128, D], F32) for q in range(2)]
            tmp = em.sb(st, "ftmp", [128, D], F32)
            st6 = em.sb(st, "fst6", [128, 2, 6], F32)
            mv = em.sb(st, "fmv", [128, 2], F32)
            rs = em.sb(st, "frs", [128, 1], F32)
            h2g = [em.sb(st, "h2g%d" % q, [128, 8, 512], BF16) for q in range(2)]
            k = 0
            for gi, (t0, ng, isctx) in enumerate(self.groups):
                own = gi < len(self.half_groups)
                sfx = "C" if isctx else "L"
                for ti in range(ng):
                    t = t0 + ti
                    y = yT[k % 2]
                    la = lat[k % 2]
                    ou = outt[k % 2]
                    k += 1
                    make_yT(t, isctx, y)
                    em.dma("sp", la[:], self.LAT2[0].ap()[t * 128:(t + 1) * 128, :], writes=[la])
                    pso = [self.pn(), self.pn()]
                    for half in range(2):
                        for kc in range(KC):
                            self.mm(pso[half], pso[half][:, :], y[:, kc, :], wout[:, kc, half * 512:(half + 1) * 512],
                                    kc == 0, kc == KC - 1, [y, wout])
                    self.tail(pso, la, rows["GA1" + sfx], rows["LNG0"], rows["LNB0"], tmp, ou, st6, mv, rs)
                    self.store_lat(t, ou)
                    if own:
                        self.hT_tile(ou, h2g[gi % 2], ti * 128, modLC, 1, isctx)
                if own:
                    em.dma("pool", self.H2T.ap()[gi].rearrange("p (c n) -> p c n", c=8), h2g[gi % 2][:], reads=[h2g[gi % 2]])
            em.barrier()
            em.release([wout, tmp, st6, mv, rs] + yT + lat + outt + h2g + mbufs)

    def ffn_tail_bufs(self, st):
        em = self.em
        d = dict(lat=[em.sb(st, "glat%d" % q, [128, D], F32) for q in range(2)],
                 outt=[em.sb(st, "gout%d" % q, [128, D], F32) for q in range(2)],
                 tmp=em.sb(st, "gtmp", [128, D], F32), st6=em.sb(st, "gst6", [128, 2, 6], F32),
                 mv=em.sb(st, "gmv", [128, 2], F32), rs=em.sb(st, "grs", [128, 1], F32))
        return d

    def ffn_store(self, t, ou):
        self.store_lat(t, ou, out=True)

    def ffn_dense(self, li, modLC, rows):
        em = self.em
        with ExitStack() as st:
            wd = em.sb(st, "wd", [128, 22, D], BF16)
            self.load_w(wd, "ffndn%d" % li, 0, 22, 0, D)
            wg = [em.sb(st, "wg%d" % q, [128, 8, 256], BF16) for q in range(2)]
            wu = [em.sb(st, "wu%d" % q, [128, 8, 256], BF16) for q in range(2)]
            h2 = [em.sb(st, "h2_%d" % q, [128, 8, 512], BF16) for q in range(2)]
            act = em.sb(st, "act", [128, 22, 512], BF16)
            sg = [em.sb(st, "sg%d" % q, [128, 512], F32) for q in range(2)]
            B = self.ffn_tail_bufs(st)
            k = 0
            for gi, (t0, ng, isctx) in enumerate(self.half_groups):
                N = ng * 128
                h = h2[gi % 2]
                em.dma("sp", h[:], self.H2T.ap()[gi].rearrange("p (c n) -> p c n", c=8), writes=[h])
                for s in range(11):
                    g_, u_ = wg[s % 2], wu[s % 2]
                    self.load_w(g_, "ffngu%d" % li, 0, 8, s * 256, 256)
                    self.load_w(u_, "ffngu%d" % li, 0, 8, 2816 + s * 256, 256)
                    for fc in range(2):
                        f = s * 2 + fc
                        pg, pu = self.pn(), self.pn()
                        for kc in range(8):
                            self.mm(pg, pg[:, :N], g_[:, kc, fc * 128:(fc + 1) * 128], h[:, kc, :N], kc == 0, kc == 7, [g_, h])
                        for kc in range(8):
                            self.mm(pu, pu[:, :N], u_[:, kc, fc * 128:(fc + 1) * 128], h[:, kc, :N], kc == 0, kc == 7, [u_, h])
                        s_ = sg[f % 2]
                        em.op("act", lambda e, pg=pg, s_=s_: e.activation(out=s_[:, :N], in_=pg[:, :N], func=AF.Silu), reads=[pg], writes=[s_])
                        em.op("dve", lambda e, pu=pu, s_=s_, f=f: e.tensor_tensor(out=act[:, f, :N], in0=s_[:, :N], in1=pu[:, :N], op=ALU.mult),
                              reads=[pu, s_], writes=[act])
                sfx = "C" if isctx else "L"
                for ti in range(ng):
                    t = t0 + ti
                    la, ou = B["lat"][k % 2], B["outt"][k % 2]
                    k += 1
                    em.dma("sp", la[:], self.LAT2[0].ap()[t * 128:(t + 1) * 128, :], writes=[la])
                    pso = [self.pn(), self.pn()]
                    for half in range(2):
                        for f in range(22):
                            self.mm(pso[half], pso[half][:, :], act[:, f, ti * 128:(ti + 1) * 128], wd[:, f, half * 512:(half + 1) * 512],
                                    f == 0, f == 21, [act, wd])
                    self.tail(pso, la, rows["GA2" + sfx], rows["LNG1"], rows["LNB1"], B["tmp"], ou, B["st6"], B["mv"], B["rs"])
                    self.ffn_store(t, ou)
            em.barrier()
            em.release([wd, act] + wg + wu + h2 + sg + B["lat"] + B["outt"] + [B["tmp"], B["st6"], B["mv"], B["rs"]])

    def phase_h(self):
        em = self.em
        self.pair_gather(self.FX, self.FXG)
        with ExitStack() as st:
            a = [[em.sb(st, "ha%d_%d" % (s, q), [128, D], F32) for s in range(2)] for q in range(2)]
            ou = [em.sb(st, "ho%d" % q, [128, D], F32) for q in range(2)]
            k = 0
            for t in range(2 + self.NLT // 2, self.NT):
                pt = self.ptile(t)
                aa, o_ = a[k % 2], ou[k % 2]
                k += 1
                for s in range(2):
                    em.dma("sp", aa[s][:], self.FXG.ap()[s * self.HT + pt * 128:s * self.HT + (pt + 1) * 128, :], writes=[aa[s]])
                for half in range(2):
                    ps = self.pn()
                    for s in range(2):
                        self.mm(ps, ps[:, :], self.cf(C_J0 + s), aa[s][:, half * 512:(half + 1) * 512], s == 0, s == 1, [self.cmf, aa[s]])
                    em.op("act" if half else "dve", lambda e, ps=ps, half=half, o_=o_: (
                        e.activation(out=o_[:, half * 512:(half + 1) * 512], in_=ps[:, :], func=AF.Copy) if half else
                        e.tensor_copy(out=o_[:, half * 512:(half + 1) * 512], in_=ps[:, :])), reads=[ps], writes=[o_])
                em.dma("pool", self.LAT.ap()[t * 128:(t + 1) * 128, :], o_[:], reads=[o_])
            em.barrier()
            em.release(a[0] + a[1] + ou)

    def ret_alloc(self):
        NT = self.NT
        self.rQT2 = [self.dr("rQT%d" % z, [NT, 128, 1024], BF16) for z in range(2)]
        self.rKT2 = [self.dr("rKT%d" % z, [NT, 128, 1024], BF16) for z in range(2)]
        self.rKTM2 = [self.dr("rKTM%d" % z, [NT, 128, 1024], BF16) for z in range(2)]
        self.rV2 = [self.dr("rV%d" % z, [NT, 128, 2048], BF16) for z in range(2)]
        self.rG = self.dr("rG", [NT, 128, 2048], BF16)
        self.rope2 = [self.xin("rope%d" % z, [NT, 128, 256]) for z in range(2)]

    def ret_phase_a(self, j, modLC, z):
        em = self.em
        self.LAT, self.rope = self.LAT2[z], self.rope2[z]
        self.rQT, self.rKT, self.rKTM, self.rV = self.rQT2[z], self.rKT2[z], self.rKTM2[z], self.rV2[z]
        with ExitStack() as st:
            wb = [em.sb(st, "rw%d" % q, [128, 8, 512], BF16) for q in range(2)]
            hT = [em.sb(st, "rhT%d" % q, [128, 8, 512], BF16) for q in range(2)]
            lat = [em.sb(st, "rlat%d" % q, [128, D], F32) for q in range(2)]
            qr = [em.sb(st, "rqr%d" % q, [128, 1024], BF16) for q in range(4)]
            kr = [em.sb(st, "rkr%d" % q, [128, 1024], BF16) for q in range(4)]
            vv = [em.sb(st, "rvv%d" % q, [128, 2048], BF16) for q in range(4)]
            gg = [em.sb(st, "rgg%d" % q, [128, 2048], BF16) for q in range(4)]
            rp = [em.sb(st, "rrp%d" % q, [128, 256], F32) for q in range(4)]
            cos4 = [em.sb(st, "rcos%d" % q, [128, 4, 64], F32) for q in range(4)]
            sin4 = [em.sb(st, "rsin%d" % q, [128, 4, 64], F32) for q in range(4)]
            xf = [em.sb(st, "rxf%d" % q, [128, 512], F32) for q in range(2)]
            t1 = [em.sb(st, "rt1%d" % q, [128, 512], F32) for q in range(2)]
            t2 = [em.sb(st, "rt2%d" % q, [128, 4, 64], F32) for q in range(2)]
            t3 = [em.sb(st, "rt3%d" % q, [128, 4, 64], F32) for q in range(2)]
            qT = [em.sb(st, "rqT%d" % q, [128, 1024], BF16) for q in range(2)]
            kk = 0
            rk = 0
            for gi, (t0, ng, isctx) in enumerate(self.groups):
                h = hT[gi % 2]
                for ti in range(ng):
                    la = lat[kk % 2]
                    kk += 1
                    em.dma("sp", la[:], self.LAT.ap()[(t0 + ti) * 128:(t0 + ti + 1) * 128, :], writes=[la])
                    self.hT_tile(la, h, ti * 128, modLC, 0, isctx)
                    em.dma("sp", rp[ti][:], self.rope.ap()[t0 + ti], writes=[rp[ti]])
                    em.op("pool", lambda e, ti=ti: e.tensor_copy(out=cos4[ti][:].rearrange("p (a b) f -> p a (b f)", a=2),
                                                                 in_=rp[ti][:, 0:128].unsqueeze(1).broadcast_to([128, 2, 128])),
                          reads=[rp[ti]], writes=[cos4[ti]])
                    em.op("pool", lambda e, ti=ti: e.tensor_copy(out=sin4[ti][:].rearrange("p (a b) f -> p a (b f)", a=2),
                                                                 in_=rp[ti][:, 128:256].unsqueeze(1).broadcast_to([128, 2, 128])),
                          reads=[rp[ti]], writes=[sin4[ti]])
                for nb in range(12 if z == 0 else 8):
                    w = wb[nb % 2]
                    self.load_w(w, "retin%d" % j, 0, 8, nb * 512, 512)
                    for ti in range(ng):
                        ps = self.pn()
                        for kc in range(8):
                            self.mm(ps, ps[:, :], h[:, kc, ti * 128:(ti + 1) * 128], w[:, kc, :], kc == 0, kc == 7, [h, w])
                        if nb < 4:
                            dest = (qr if nb < 2 else kr)[ti]
                            dsl = dest[:, (nb % 2) * 512:(nb % 2 + 1) * 512].rearrange("p (a s f) -> p a s f", a=4, s=2)
                            x_, t1_, t2_, t3_ = xf[rk % 2], t1[rk % 2], t2[rk % 2], t3[rk % 2]
                            rk += 1
                            sc = 1.0 if nb < 2 else 1.0 / 16.0
                            em.op("act", lambda e, ps=ps, x_=x_, sc=sc: e.activation(out=x_[:], in_=ps[:, :], func=AF.Copy, scale=sc),
                                  reads=[ps], writes=[x_])
                            X = x_[:].rearrange("p (a s f) -> p a s f", a=4, s=2)
                            T1 = t1_[:].rearrange("p (a s f) -> p a s f", a=4, s=2)
                            em.op("dve", lambda e, X=X, T1=T1, ti=ti: e.tensor_tensor(
                                out=T1, in0=X, in1=cos4[ti][:].unsqueeze(2).broadcast_to([128, 4, 2, 64]), op=ALU.mult),
                                reads=[x_, cos4[ti]], writes=[t1_])
                            em.op("pool", lambda e, X=X, t2_=t2_, ti=ti: e.tensor_tensor(out=t2_[:], in0=X[:, :, 1, :], in1=sin4[ti][:], op=ALU.mult),
                                  reads=[x_, sin4[ti]], writes=[t2_])
                            em.op("pool", lambda e, X=X, t3_=t3_, ti=ti: e.tensor_tensor(out=t3_[:], in0=X[:, :, 0, :], in1=sin4[ti][:], op=ALU.mult),
                                  reads=[x_, sin4[ti]], writes=[t3_])
                            em.op("dve", lambda e, dsl=dsl, T1=T1, t2_=t2_: e.tensor_tensor(out=dsl[:, :, 0, :], in0=T1[:, :, 0, :], in1=t2_[:], op=ALU.subtract),
                                  reads=[t1_, t2_], writes=[dest])
                            em.op("dve", lambda e, dsl=dsl, T1=T1, t3_=t3_: e.tensor_tensor(out=dsl[:, :, 1, :], in0=T1[:, :, 1, :], in1=t3_[:], op=ALU.add),
                                  reads=[t1_, t3_], writes=[dest])
                        else:
                            dest = (vv if nb < 8 else gg)[ti]
                            c0 = (nb % 4) * 512
                            if (nb + ti) % 2:
                                em.op("act", lambda e, ps=ps, dest=dest, c0=c0: e.activation(out=dest[:, c0:c0 + 512], in_=ps[:, :], func=AF.Copy),
                                      reads=[ps], writes=[dest])
                            else:
                                em.op("dve", lambda e, ps=ps, dest=dest, c0=c0: e.tensor_copy(out=dest[:, c0:c0 + 512], in_=ps[:, :]),
                                      reads=[ps], writes=[dest])
                for ti in range(ng):
                    t = t0 + ti
                    em.dma("pool", self.rKTM.ap()[t], kr[ti][:], reads=[kr[ti]])
                    em.dma("pool", self.rV.ap()[t], vv[ti][:], reads=[vv[ti]])
                    if z == 0:
                        em.dma("pool", self.rG.ap()[t], gg[ti][:], reads=[gg[ti]])
                    for src, dstT in ((qr[ti], self.rQT), (kr[ti], self.rKT)):
                        ps = self.pn()
                        pb = ps[:].bitcast(BF16)
                        for c in range(8):
                            em.op("pe", lambda e, pb=pb, src=src, c=c: e.transpose(out=pb[:, c * 128:(c + 1) * 128], in_=src[:, c * 128:(c + 1) * 128],
                                                                                 identity=self.cb(C_ID)), reads=[src, self.cmb], writes=[ps])
                        q_ = qT[kk % 2]
                        kk += 1
                        em.op("act", lambda e, pb=pb, q_=q_: e.activation(out=q_[:], in_=pb[:, 0:1024], func=AF.Copy), reads=[ps], writes=[q_])
                        em.dma("pool", dstT.ap()[t], q_[:], reads=[q_])
            em.barrier()
            em.release(wb + hT + lat + qr + kr + vv + gg + rp + cos4 + sin4 + xf + t1 + t2 + t3 + qT)

    def ret_phase_s(self, j, z):
        em = self.em
        dec_in = self.xin("retdec%d_%d" % (j, z), [1, 4])
        self.rQT, self.rKT, self.rKTM, self.rV, self.O = self.rQT2[z], self.rKT2[z], self.rKTM2[z], self.rV2[z], self.O2[z]
        with ExitStack() as st:
            dec = em.sb(st, "sdec", [128, 4], F32)
            em.dma("sp", dec[:], dec_in.ap().partition_broadcast(128), writes=[dec])
            lsp = em.sb(st, "slsp", [128, 4], F32)
            em.op("act", lambda e: e.activation(out=lsp[:], in_=dec[:], func=AF.Exp, scale=-1.0), reads=[dec], writes=[lsp])
            em.op("act", lambda e: e.activation(out=lsp[:], in_=lsp[:], func=AF.Ln, bias=1.0, scale=1.0), reads=[lsp], writes=[lsp])
            outsc = em.sb(st, "soutsc", [128, 4], F32)
            scsc = em.sb(st, "sscsc", [128, 4], F32)
            kdec = em.sb(st, "skdec", [128, 4], F32)
            gC = em.sb(st, "sgC", [128, 4], F32)
            io = self.iota
            em.op("act", lambda e: e.activation(out=outsc[:], in_=lsp[:], func=AF.Exp, scale=io[:, 1:2]), reads=[lsp, io], writes=[outsc])
            em.op("act", lambda e: e.activation(out=scsc[:], in_=lsp[:], func=AF.Exp, scale=io[:, 0:1]), reads=[lsp, io], writes=[scsc])
            em.op("act", lambda e: e.activation(out=kdec[:], in_=lsp[:], func=AF.Exp, scale=io[:, 3:4]), reads=[lsp, io], writes=[kdec])
            em.op("act", lambda e: e.activation(out=gC[:], in_=lsp[:], func=AF.Exp, scale=-128.0), reads=[lsp], writes=[gC])
            S = [[em.sb(st, "sS%d_%d" % (h, dc), [128, 512], F32) for dc in range(2)] for h in range(4)]
            Sb = [[em.sb(st, "sSb%d_%d" % (h, dc), [128, 512], BF16) for dc in range(2)] for h in range(4)]
            for h in range(4):
                for dc in range(2):
                    em.op("pool", lambda e, h=h, dc=dc: e.memset(S[h][dc][:], 0.0), writes=[S[h][dc]])
                    em.op("pool", lambda e, h=h, dc=dc: e.memset(Sb[h][dc][:], 0.0), writes=[Sb[h][dc]])
            qt = [em.sb(st, "sqt%d" % q, [128, 8, 128], BF16) for q in range(2)]
            kt = [em.sb(st, "skt%d" % q, [128, 8, 128], BF16) for q in range(2)]
            ktm = [em.sb(st, "sktm%d" % q, [128, 1024], BF16) for q in range(2)]
            v = [em.sb(st, "sv%d" % q, [128, 2048], BF16) for q in range(2)]
            ot = [em.sb(st, "sot%d" % q, [128, 2048], F32) for q in range(2)]
            scb = [em.sb(st, "sscb%d" % q, [128, 128], BF16) for q in range(2)]
            kd = [em.sb(st, "skd%d" % q, [128, 256], BF16) for q in range(2)]
            n = 0
            for t in range(self.NT):
                q_, k_, km_, v_, o_ = qt[t % 2], kt[t % 2], ktm[t % 2], v[t % 2], ot[t % 2]
                em.dma("sp", q_[:], self.rQT.ap()[t].rearrange("p (c n) -> p c n", c=8), writes=[q_])
                em.dma("sp", k_[:], self.rKT.ap()[t].rearrange("p (c n) -> p c n", c=8), writes=[k_])
                em.dma("sp", km_[:], self.rKTM.ap()[t], writes=[km_])
                em.dma("sp", v_[:], self.rV.ap()[t], writes=[v_])
                for h in range(4):
                    sc_, kd_ = scb[n % 2], kd[n % 2]
                    n += 1
                    ps = self.pn()
                    for dc in range(2):
                        self.mm(ps, ps[:, 0:128], k_[:, 2 * h + dc, :], q_[:, 2 * h + dc, :], dc == 0, dc == 1, [k_, q_])
                    em.op("dve", lambda e, ps=ps, sc_=sc_, h=h: e.scalar_tensor_tensor(
                        out=sc_[:], in0=ps[:, 0:128], scalar=scsc[:, h:h + 1], in1=self.cf(C_MRET), op0=ALU.mult, op1=ALU.mult),
                        reads=[ps, scsc, self.cmf], writes=[sc_])
                    po = self.pn()
                    vh = v_[:, h * 512:(h + 1) * 512]
                    self.mm(po, po[:, :], sc_[:], vh, True, False, [sc_, v_])
                    for dc in range(2):
                        self.mm(po, po[:, :], q_[:, 2 * h + dc, :], Sb[h][dc][:], False, dc == 1, [q_, Sb[h][dc]])
                    em.op("act", lambda e, po=po, o_=o_, h=h: e.activation(out=o_[:, h * 512:(h + 1) * 512], in_=po[:, :], func=AF.Identity,
                                                                        scale=outsc[:, h:h + 1]), reads=[po, outsc], writes=[o_])
                    em.op("pool", lambda e, kd_=kd_, km_=km_, h=h: e.tensor_scalar(out=kd_[:], in0=km_[:, h * 256:(h + 1) * 256],
                                                                               scalar1=kdec[:, h:h + 1], scalar2=None, op0=ALU.mult),
                          reads=[km_, kdec], writes=[kd_])
                    for dc in range(2):
                        pu = self.pn()
                        self.mm(pu, pu[:, :], kd_[:, dc * 128:(dc + 1) * 128], vh, True, True, [kd_, v_])
                        S_, Sb_ = S[h][dc], Sb[h][dc]
                        em.op("dve", lambda e, pu=pu, S_=S_, h=h: e.scalar_tensor_tensor(
                            out=S_[:], in0=S_[:], scalar=gC[:, h:h + 1], in1=pu[:, :], op0=ALU.mult, op1=ALU.add),
                            reads=[pu, S_, gC], writes=[S_])
                        em.op("act", lambda e, S_=S_, Sb_=Sb_: e.activation(out=Sb_[:], in_=S_[:], func=AF.Copy), reads=[S_], writes=[Sb_])
                em.dma("pool", self.O.ap()[t * 128:(t + 1) * 128, :], o_[:], reads=[o_])
            em.barrier()
            em.release([dec, lsp, outsc, scsc, kdec, gC] + sum(S, []) + sum(Sb, []) + qt + kt + ktm + v + ot + scb + kd)

    def ret_factory(self, j):
        em = self.em
        gn_in = self.xin("retgn%d" % j, [1, 2048])

        def factory(st):
            gng = em.sb(st, "ygng", [128, 2048], F32)
            em.dma("sp", gng[:], gn_in.ap().partition_broadcast(128), writes=[gng])
            oo = [em.sb(st, "yoo%d" % q, [128, 512], F32) for q in range(2)]
            p0 = [em.sb(st, "yp0%d" % q, [128, 512], F32) for q in range(2)]
            p1 = [em.sb(st, "yp1%d" % q, [128, 512], F32) for q in range(2)]
            gt = [em.sb(st, "ygt%d" % q, [128, 512], BF16) for q in range(2)]
            osum = em.sb(st, "yosum", [128, 512], F32)
            sg = em.sb(st, "ysg", [128, 512], F32)
            y = em.sb(st, "yy", [128, 2048], BF16)
            st6 = em.sb(st, "yst6", [128, 6], F32)
            mv = em.sb(st, "ymv", [128, 2], F32)
            rs = em.sb(st, "yrs", [128, 1], F32)
            cnt = [0]

            def make_yT(t, isctx, yT):
                pt = self.ptile(t)
                for h in range(4):
                    k = cnt[0]
                    cnt[0] += 1
                    o_, a0, a1, g_ = oo[k % 2], p0[k % 2], p1[k % 2], gt[k % 2]
                    cs = slice(h * 512, (h + 1) * 512)
                    em.dma("sp", o_[:], self.O2[0].ap()[t * 128:(t + 1) * 128, cs], writes=[o_])
                    em.dma("sp", a1[:], self.O2[1].ap()[pt * 128:(pt + 1) * 128, cs], writes=[a1])
                    em.dma("sp", g_[:], self.rG.ap()[t][:, cs], writes=[g_])
                    ps = self.pn()
                    self.mm(ps, ps[:, :], self.cf(C_J0), a1[:], True, True, [self.cmf, a1])
                    em.op("dve", lambda e, ps=ps, o_=o_: e.tensor_tensor(out=osum[:], in0=ps[:, :], in1=o_[:], op=ALU.add),
                          reads=[ps, o_], writes=[osum])
                    em.op("dve", lambda e: e.bn_stats(out=st6[:], in_=osum[:]), reads=[osum], writes=[st6])
                    em.op("dve", lambda e: e.bn_aggr(out=mv[:], in_=st6[:]), reads=[st6], writes=[mv])
                    em.op("act", lambda e: e.activation(out=rs[:], in_=mv[:, 1:2], func=AF.Sqrt, bias=1e-5, scale=1.0), reads=[mv], writes=[rs])
                    em.op("dve", lambda e: e.reciprocal(out=rs[:], in_=rs[:]), reads=[rs], writes=[rs])
                    em.op("dve", lambda e: e.tensor_scalar(out=osum[:], in0=osum[:], scalar1=mv[:, 0:1], scalar2=rs[:, 0:1],
                                                           op0=ALU.subtract, op1=ALU.mult), reads=[osum, mv, rs], writes=[osum])
                    em.op("act", lambda e, g_=g_: e.activation(out=sg[:], in_=g_[:], func=AF.Silu), reads=[g_], writes=[sg])
                    em.op("pool", lambda e, cs=cs: e.tensor_tensor(out=osum[:], in0=osum[:], in1=gng[:, cs], op=ALU.mult),
                          reads=[osum, gng], writes=[osum])
                    em.op("pool", lambda e, cs=cs: e.tensor_tensor(out=y[:, cs], in0=osum[:], in1=sg[:], op=ALU.mult),
                          reads=[osum, sg], writes=[y])
                for half in range(2):
                    ps = self.pn()
                    pb = ps[:].bitcast(BF16)
                    for c in range(8):
                        cc = half * 8 + c
                        em.op("pe", lambda e, pb=pb, c=c, cc=cc: e.transpose(out=pb[:, c * 128:(c + 1) * 128], in_=y[:, cc * 128:(cc + 1) * 128],
                                                                           identity=self.cb(C_ID)), reads=[y, self.cmb], writes=[ps])
                    em.op("act", lambda e, pb=pb, half=half: e.activation(
                        out=yT[:, half * 8:(half + 1) * 8, :].rearrange("p c n -> p (c n)"), in_=pb[:, 0:1024], func=AF.Copy),
                        reads=[ps], writes=[yT])
            return make_yT, [gng, osum, sg, y, st6, mv, rs] + oo + p0 + p1 + gt
        return factory

    def build(self):
        em = self.em
        self.setup()
        pend = []
        stop = getattr(self, "stop", 99)
        for i in self.layer_ids:
            pend.append(self.unit("modw%d" % i, 1024, 6144))
            kind, j = i % 3, i // 3
            if kind == 0:
                pend += [self.unit("retin%d" % j, 1024, 6144), self.unit("retout%d" % j, 2048, 1024)]
            elif kind == 1:
                pend += [self.unit("dnin", 1024, 6144), self.unit("dnout", 2048, 1024)]
            else:
                pend += [self.unit(n, 1024, 1024) for n in ("rkr", "rkk", "rkv", "rkout")]
                pend += [self.unit("rkg1", 1024, 128), self.unit("rkg2", 128, 1024)]
            if stop == 5 and i == self.layer_ids[-1]:
                pass
            elif i % 2 == 0 and not getattr(self, "force_moe", False):
                pend += [self.unit("ffngu%d" % (i // 2), 1024, 5632), self.unit("ffndn%d" % (i // 2), 2816, 1024)]
            else:
                for e_ in range(8):
                    pend += [self.unit("moegu%d_%d" % (i // 2, e_), 1024, 7168), self.unit("moedn%d_%d" % (i // 2, e_), 3584, 1024)]
        self.gather_units(pend)
        stop = getattr(self, "stop", 99)
        if self.layers >= 1:
            self.ret_alloc()
        if stop == 0:
            em.barrier()
            return self.nc
        for i in self.layer_ids:
            kind, j = i % 3, i // 3
            with ExitStack() as lst:
                modLC, rows = self.phase_mod(i, lst)
                if stop == 1:
                    return self.nc
                if kind == 0:
                    for z in range(2):
                        self.ret_phase_a(j, modLC, z)
                        self.ret_phase_s(j, z)
                    if stop == 3:
                        import os
                        which = os.environ.get("MKDUMP", "O0")
                        dbg = self.nc.dram_tensor("dbg", [self.T, 2048], F32, kind="ExternalOutput")
                        srcs = {"O0": self.O2[0].ap(), "O1": self.O2[1].ap(),
                                "V0": self.rV2[0].ap().rearrange("t p n -> (t p) n"),
                                "G": self.rG.ap().rearrange("t p n -> (t p) n")}
                        if which in srcs:
                            em.dma("pool", dbg.ap(), srcs[which], sem=self.gsem)
                        elif which == "K0":
                            em.dma("pool", dbg.ap()[:, 0:1024], self.rKTM2[0].ap().rearrange("t p n -> (t p) n"), sem=self.gsem)
                        elif which == "QT0":
                            em.dma("pool", dbg.ap()[:, 0:1024], self.rQT2[0].ap().rearrange("t p n -> (t p) n"), sem=self.gsem)
                        em.barrier()
                        return self.nc
                    self.phase_f(i, modLC, rows, self.ret_factory(j), "retout%d" % j, 16)
                elif kind == 1:
                    self.dn_layer(i, modLC, rows)
                else:
                    self.rk_layer(i, modLC, rows)
                if stop == 5 and i == self.layer_ids[-1]:
                    em.dma("pool", self.OUT.ap(), self.LAT2[0].ap(), sem=self.gsem)
                    em.barrier()
                    return self.nc
                if i % 2 == 0 and not getattr(self, "force_moe", False):
                    self.ffn_dense(i // 2, modLC, rows)
                else:
                    self.ffn_moe(i // 2, modLC, rows)
                em.barrier()
                em.release([modLC] + list(rows.values()))
        em.barrier()
        return self.nc


def rope_tables(L, z):
    NT = 2 + L // 128
    tab = np.zeros((NT, 128, 256), np.float32)
    tab[:2, :, 0:128] = 1.0
    inv = (10000.0 ** (-np.arange(64, dtype=np.float32) / 64)).astype(np.float32)
    n = np.arange(L)
    if z:
        n = n[::-1]
    row = (n // 64).astype(np.float32)[:, None] * inv[None, :]
    col = (n % 64).astype(np.float32)[:, None] * inv[None, :]
    full = np.concatenate([np.cos(row), np.cos(col), np.sin(row), np.sin(col)], 1).astype(np.float32)
    tab[2:] = full.reshape(L // 128, 128, 256)
    return tab


def host_inputs(mk, inp, core):
    b, z = core, 0
    L = mk.L
    f = lambda a: np.ascontiguousarray(a, dtype=np.float32)
    x = inp["x"][b][:L]
    cx = inp["ctx"][b]
    m = {}
    m["xs"] = f(np.concatenate([cx, x], 0))
    m["xsf"] = f(np.concatenate([cx[::-1], x[::-1]], 0))
    cv = np.stack([inp["c"][b], inp["c_ctx"]], -1).reshape(8, 128, 2).transpose(1, 0, 2).reshape(128, 16)
    m["cvec"] = f(cv)
    m["modb"] = f(inp["mod_b"].reshape(4, 48, 128).transpose(0, 2, 1))
    m["modbf"] = f(inp["mod_b"])
    m["lng"] = f(inp["ln_g"])
    m["lnb"] = f(inp["ln_b"])
    m["cm"] = f(make_consts(z).transpose(1, 0, 2))
    p = np.arange(128, dtype=np.float32)
    m["iota"] = f(np.stack([p + 1, -(p + 1), 0 * p, p - 127], 1))
    m["rope0"] = rope_tables(L, 0)
    m["rope1"] = rope_tables(L, 1)
    W = {}
    for i in range(4):
        W["modw%d" % i] = inp["mod_w"][i]
    for j in range(2):
        W["retin%d" % j] = inp["ret_w_in"][j]
        W["retout%d" % j] = inp["ret_w_out"][j]
        for zz in range(2):
            m["retdec%d_%d" % (j, zz)] = f(inp["ret_decay"][j][zz][None, :])
        m["retgn%d" % j] = f(inp["ret_gn_g"][j][None, :])
        W["ffngu%d" % j] = inp["ffn_w_gu"][j]
        W["ffndn%d" % j] = inp["ffn_w_down"][j]
        for e_ in range(8):
            W["moegu%d_%d" % (j, e_)] = inp["moe_w_gu"][j][e_]
            W["moedn%d_%d" % (j, e_)] = inp["moe_w_down"][j][e_]
    for j in range(2):
        m["moer%d" % j] = f(inp["moe_router"][j].reshape(8, 128, 8).transpose(1, 0, 2).reshape(128, 64))
    W["dnin"] = inp["dn_w_in"][0][:, :6144]
    W["dnout"] = inp["dn_w_out"][0]
    for q, nme in enumerate(("rkr", "rkk", "rkv")):
        W[nme] = inp["rk_w_rkv"][0][q]
    W["rkout"] = inp["rk_w_out"][0]
    W["rkg1"] = inp["rk_g1"][0]
    W["rkg2"] = inp["rk_g2"][0]
    for name in mk.ext:
        if name.startswith("w_"):
            w = W[name[2:]]
            k8 = w.shape[0] // 8
            m[name] = f(w[core * k8:(core + 1) * k8])
        elif name.startswith("wf_"):
            m[name] = f(W[name[3:]])
    host_extra(mk, inp, core, m)
    out = {}
    for name, (shape, dt) in mk.ext.items():
        a = m[name]
        assert tuple(a.shape) == tuple(shape), (name, a.shape, shape)
        out[name] = a
    return out


def host_extra(mk, inp, core, m):
    f = lambda a: np.ascontiguousarray(a, dtype=np.float32)
    wab = inp["dn_w_in"][0][:, 6144:]
    cw = inp["dn_conv_w"][0]
    for z in range(2):
        a = wab[:, z * 32:(z + 1) * 32]
        m["dnab%d" % z] = f(a.reshape(8, 128, 32).transpose(1, 0, 2).reshape(128, 256))
        m["dnalog%d" % z] = f(inp["dn_a_log"][0][z][None, :])
        m["dndtb%d" % z] = f(inp["dn_dt_bias"][0][z][None, :])
        c = cw if z == 0 else cw[::-1]
        m["dnconv%d" % z] = f(c.T.reshape(32, 128, 5).transpose(1, 0, 2).reshape(128, 160))
    m["dnng"] = f(inp["dn_norm_g"][0][None, :])
    fm8 = lambda v_: v_.reshape(8, 128).T
    m["rkmix"] = f(np.concatenate([fm8(inp["rk_mix"][0][q]) for q in range(6)], 1))
    m["rkvec"] = f(np.concatenate([fm8(inp["rk_k_k"][0]), fm8(inp["rk_k_a"][0]), fm8(inp["rk_r_k"][0].reshape(-1)), fm8(inp["rk_lnx_g"][0])], 1))
    lo = lambda w_: w_.reshape(8, 128, 64).transpose(1, 0, 2).reshape(128, 512)
    for z in range(2):
        m["rkw0_%d" % z] = f(inp["rk_w0"][0][z][None, :])
        m["rka0_%d" % z] = f(fm8(inp["rk_a0"][0][z]))
        m["rkw1_%d" % z] = f(lo(inp["rk_w1"][0][z]))
        m["rka1_%d" % z] = f(lo(inp["rk_a1"][0][z]))
        m["rkw2_%d" % z] = f(inp["rk_w2"][0][z])
        m["rka2_%d" % z] = f(inp["rk_a2"][0][z])


_CACHE = {}


def run_model(inp, L=8192, layers=4):
    key = (L, layers)
    if key not in _CACHE:
        mk = MK(L, layers)
        mk.build()
        _CACHE[key] = mk
    mk = _CACHE[key]
    nco = mk.ncores
    in_maps = [host_inputs(mk, inp, c) for c in range(nco)]
    res = run_bass_kernel_spmd(mk.nc, in_maps, core_ids=list(range(nco)))
    lat = np.zeros((4, L, D), np.float32)
    cx = np.zeros((4, NCTX, D), np.float32)
    for c in range(nco):
        o = res.results[c]["out"]
        lat[c] = o[256:]
        cx[c] = o[:256]
    return lat, cx


def kernel(**inputs):
    inp = {k: np.asarray(v) for k, v in inputs.items()}
    lat, _ = run_model(inp, 8192, 4)
    return lat


def ffn_moe(self, li, modLC, rows):
    em = self.em
    rin = self.xin("moer%d" % li, [128, 64])
    with ExitStack() as st:
        wr = em.sb(st, "mwr", [128, 8, 8], BF16)
        em.dma("pool", wr[:].rearrange("p c e -> p (c e)"), rin.ap(), writes=[wr])
        h2 = em.sb(st, "mh2", [128, 8, 512], BF16)
        wg = [em.sb(st, "mwg%d" % q, [128, 8, 256], BF16) for q in range(2)]
        wu = [em.sb(st, "mwu%d" % q, [128, 8, 256], BF16) for q in range(2)]
        wd = [em.sb(st, "mwd%d" % q, [128, 28, 512], BF16) for q in range(2)]
        act = em.sb(st, "mact", [128, 28, 512], BF16)
        acc = em.sb(st, "macc", [128, 4, D], F32)
        sg = [em.sb(st, "msg%d" % q, [128, 512], F32) for q in range(2)]
        lg = em.sb(st, "mlg", [128, 8], F32)
        eq = em.sb(st, "meq", [128, 8], F32)
        l2 = em.sb(st, "ml2", [128, 8], F32)
        ex = em.sb(st, "mex", [128, 8], F32)
        m1 = em.sb(st, "mm1", [128, 4], F32)
        gate = em.sb(st, "mgate", [128, 4, 8], F32)
        la = em.sb(st, "mlat", [128, D], F32)
        ou = em.sb(st, "mout", [128, D], F32)
        tmp = em.sb(st, "mtmp", [128, D], F32)
        st6 = em.sb(st, "mst6", [128, 2, 6], F32)
        mv = em.sb(st, "mmv", [128, 2], F32)
        rs = em.sb(st, "mrs", [128, 1], F32)
        nf = 0
        for gi, (t0, ng, isctx) in enumerate(self.groups):
            N = ng * 128
            em.dma("sp", h2[:], self.H2T.ap()[gi].rearrange("p (c n) -> p c n", c=8), writes=[h2])
            for ti in range(ng):
                ps = self.pn()
                for kc in range(8):
                    self.mm(ps, ps[:, 0:8], h2[:, kc, ti * 128:(ti + 1) * 128], wr[:, kc, :], kc == 0, kc == 7, [h2, wr])
                em.op("dve", lambda e, ps=ps: e.tensor_copy(out=lg[:], in_=ps[:, 0:8]), reads=[ps], writes=[lg])
                em.op("dve", lambda e: e.tensor_reduce(out=m1[:, 0:1], in_=lg[:], axis=AX.X, op=ALU.max), reads=[lg], writes=[m1])
                em.op("dve", lambda e: e.tensor_scalar(out=eq[:], in0=lg[:], scalar1=m1[:, 0:1], scalar2=None, op0=ALU.is_equal),
                      reads=[lg, m1], writes=[eq])
                em.op("dve", lambda e: e.scalar_tensor_tensor(out=l2[:], in0=eq[:], scalar=-1e30, in1=lg[:], op0=ALU.mult, op1=ALU.add),
                      reads=[eq, lg], writes=[l2])
                em.op("dve", lambda e: e.tensor_reduce(out=m1[:, 1:2], in_=l2[:], axis=AX.X, op=ALU.max), reads=[l2], writes=[m1])
                em.op("dve", lambda e: e.tensor_scalar(out=eq[:], in0=lg[:], scalar1=m1[:, 1:2], scalar2=None, op0=ALU.is_ge),
                      reads=[lg, m1], writes=[eq])
                em.op("dve", lambda e: e.tensor_scalar(out=m1[:, 2:3], in0=m1[:, 0:1], scalar1=-1.0, scalar2=None, op0=ALU.mult),
                      reads=[m1], writes=[m1])
                em.op("act", lambda e: e.activation(out=ex[:], in_=lg[:], func=AF.Exp, bias=m1[:, 2:3], scale=1.0), reads=[lg, m1], writes=[ex])
                em.op("dve", lambda e: e.tensor_tensor(out=ex[:], in0=ex[:], in1=eq[:], op=ALU.mult), reads=[ex, eq], writes=[ex])
                em.op("dve", lambda e: e.tensor_reduce(out=m1[:, 3:4], in_=ex[:], axis=AX.X, op=ALU.add), reads=[ex], writes=[m1])
                em.op("dve", lambda e: e.reciprocal(out=m1[:, 3:4], in_=m1[:, 3:4]), reads=[m1], writes=[m1])
                em.op("dve", lambda e, ti=ti: e.tensor_scalar(out=gate[:, ti, :], in0=ex[:], scalar1=m1[:, 3:4], scalar2=None, op0=ALU.mult),
                      reads=[ex, m1], writes=[gate])
            for e_ in range(8):
                for half in range(2):
                    self.load_w(wd[half], "moedn%d_%d" % (li, e_), 0, 28, half * 512, 512)
                for s in range(14):
                    g_, u_ = wg[s % 2], wu[s % 2]
                    self.load_w(g_, "moegu%d_%d" % (li, e_), 0, 8, s * 256, 256)
                    self.load_w(u_, "moegu%d_%d" % (li, e_), 0, 8, 3584 + s * 256, 256)
                    for fc in range(2):
                        f = s * 2 + fc
                        pg, pu = self.pn(), self.pn()
                        for kc in range(8):
                            self.mm(pg, pg[:, :N], g_[:, kc, fc * 128:(fc + 1) * 128], h2[:, kc, :N], kc == 0, kc == 7, [g_, h2])
                        for kc in range(8):
                            self.mm(pu, pu[:, :N], u_[:, kc, fc * 128:(fc + 1) * 128], h2[:, kc, :N], kc == 0, kc == 7, [u_, h2])
                        s_ = sg[nf % 2]
                        nf += 1
                        em.op("act", lambda e, pg=pg, s_=s_: e.activation(out=s_[:, :N], in_=pg[:, :N], func=AF.Silu), reads=[pg], writes=[s_])
                        em.op("dve", lambda e, pu=pu, s_=s_, f=f: e.tensor_tensor(out=act[:, f, :N], in0=s_[:, :N], in1=pu[:, :N], op=ALU.mult),
                              reads=[pu, s_], writes=[act])
                for ti in range(ng):
                    for half in range(2):
                        ps = self.pn()
                        for f in range(28):
                            self.mm(ps, ps[:, :], act[:, f, ti * 128:(ti + 1) * 128], wd[half][:, f, :], f == 0, f == 27, [act, wd[half]])
                        asl = acc[:, ti, half * 512:(half + 1) * 512]
                        if e_ == 0:
                            em.op("dve", lambda e, ps=ps, asl=asl, ti=ti, e_=e_: e.tensor_scalar(
                                out=asl, in0=ps[:, :], scalar1=gate[:, ti, e_:e_ + 1], scalar2=None, op0=ALU.mult),
                                reads=[ps, gate], writes=[acc])
                        else:
                            em.op("dve", lambda e, ps=ps, asl=asl, ti=ti, e_=e_: e.scalar_tensor_tensor(
                                out=asl, in0=ps[:, :], scalar=gate[:, ti, e_:e_ + 1], in1=asl, op0=ALU.mult, op1=ALU.add),
                                reads=[ps, gate, acc], writes=[acc])
            sfx = "C" if isctx else "L"
            for ti in range(ng):
                t = t0 + ti
                em.dma("sp", la[:], self.LAT2[0].ap()[t * 128:(t + 1) * 128, :], writes=[la])
                srcs = [(acc, acc[:, ti, 0:512]), (acc, acc[:, ti, 512:1024])]
                self.tail(srcs, la, rows["GA2" + sfx], rows["LNG1"], rows["LNB1"], tmp, ou, st6, mv, rs)
                self.ffn_store(t, ou)
        em.barrier()
        em.release([wr, h2, act, acc, lg, eq, l2, ex, m1, gate, la, ou, tmp, st6, mv, rs] + wg + wu + wd + sg)


MK.ffn_moe = ffn_moe


def dn_alloc(self):
    NT, T = self.NT, self.T
    self.dPRE = self.dr("dPRE", [32, 128, T], F32)
    self.dQT = [self.dr("dQT%d" % z, [8, 128, T], BF16) for z in range(2)]
    self.dKT = [self.dr("dKT%d" % z, [8, 128, T], BF16) for z in range(2)]
    self.dKM = [self.dr("dKM%d" % z, [NT, 128, 1024], BF16) for z in range(2)]
    self.dV = [self.dr("dV%d" % z, [NT, 128, 2048], BF16) for z in range(2)]
    self.dGB = [self.dr("dGB%d" % z, [NT, 128, 48], F32) for z in range(2)]
    self.dn_in = dict(
        ab=[self.xin("dnab%d" % z, [128, 8 * 32]) for z in range(2)],
        alog=[self.xin("dnalog%d" % z, [1, 16]) for z in range(2)],
        dtb=[self.xin("dndtb%d" % z, [1, 16]) for z in range(2)],
        conv=[self.xin("dnconv%d" % z, [128, 32 * 5]) for z in range(2)],
        ng=self.xin("dnng", [1, 128]))


def dn_phase_a1(self, modLC, z):
    em = self.em
    LAT = self.LAT2[z]
    with ExitStack() as st:
        wb = [em.sb(st, "dw%d" % q, [128, 8, 512], BF16) for q in range(2)]
        hT = [em.sb(st, "dhT%d" % q, [128, 8, 512], BF16) for q in range(2)]
        lat = [em.sb(st, "dlat%d" % q, [128, D], F32) for q in range(2)]
        stg = [em.sb(st, "dstg%d" % q, [128, 512], F32) for q in range(3)]
        gg = [em.sb(st, "dgg%d" % q, [128, 2048], BF16) for q in range(4)]
        wab = em.sb(st, "dwab", [128, 8, 32], BF16)
        em.dma("pool", wab[:].rearrange("p c n -> p (c n)"), self.dn_in["ab"][z].ap(), writes=[wab])
        nega = em.sb(st, "dnega", [128, 16], F32)
        dtb = em.sb(st, "ddtb", [128, 16], F32)
        em.dma("sp", nega[:], self.dn_in["alog"][z].ap().partition_broadcast(128), writes=[nega])
        em.dma("sp", dtb[:], self.dn_in["dtb"][z].ap().partition_broadcast(128), writes=[dtb])
        em.op("act", lambda e: e.activation(out=nega[:], in_=nega[:], func=AF.Exp), reads=[nega], writes=[nega])
        em.op("dve", lambda e: e.tensor_scalar(out=nega[:], in0=nega[:], scalar1=-1.0, scalar2=None, op0=ALU.mult), reads=[nega], writes=[nega])
        gb = [em.sb(st, "dgb%d" % q, [128, 48], F32) for q in range(2)]
        tt = [em.sb(st, "dtt%d" % q, [128, 32], F32) for q in range(2)]
        kk = 0
        sk = 0
        for gi, (t0, ng, isctx) in enumerate(self.groups):
            N = ng * 128
            h = hT[gi % 2]
            for ti in range(ng):
                la = lat[kk % 2]
                kk += 1
                em.dma("sp", la[:], LAT.ap()[(t0 + ti) * 128:(t0 + ti + 1) * 128, :], writes=[la])
                self.hT_tile(la, h, ti * 128, modLC, 0, isctx)
            for nb in range(8):
                w = wb[nb % 2]
                self.load_w(w, "dnin", 0, 8, nb * 512, 512)
                for c4 in range(4):
                    c = nb * 4 + c4
                    ps = self.pn()
                    for kc in range(8):
                        self.mm(ps, ps[:, :N], w[:, kc, c4 * 128:(c4 + 1) * 128], h[:, kc, :N], kc == 0, kc == 7, [w, h])
                    s_ = stg[sk % 3]
                    sk += 1
                    if c % 2:
                        em.op("act", lambda e, ps=ps, s_=s_: e.activation(out=s_[:, :N], in_=ps[:, :N], func=AF.Copy), reads=[ps], writes=[s_])
                    else:
                        em.op("dve", lambda e, ps=ps, s_=s_: e.tensor_copy(out=s_[:, :N], in_=ps[:, :N]), reads=[ps], writes=[s_])
                    em.dma("pool", self.dPRE.ap()[c][:, t0 * 128:t0 * 128 + N], s_[:, :N], reads=[s_])
            if z == 0:
                for nb in range(8, 12):
                    w = wb[nb % 2]
                    self.load_w(w, "dnin", 0, 8, nb * 512, 512)
                    for ti in range(ng):
                        ps = self.pn()
                        for kc in range(8):
                            self.mm(ps, ps[:, :], h[:, kc, ti * 128:(ti + 1) * 128], w[:, kc, :], kc == 0, kc == 7, [h, w])
                        dest = gg[ti]
                        c0 = (nb - 8) * 512
                        em.op("act", lambda e, ps=ps, dest=dest, c0=c0: e.activation(out=dest[:, c0:c0 + 512], in_=ps[:, :], func=AF.Copy),
                              reads=[ps], writes=[dest])
                for ti in range(ng):
                    em.dma("pool", self.rG.ap()[t0 + ti], gg[ti][:], reads=[gg[ti]])
            for ti in range(ng):
                ps = self.pn()
                for kc in range(8):
                    self.mm(ps, ps[:, 0:32], h[:, kc, ti * 128:(ti + 1) * 128], wab[:, kc, :], kc == 0, kc == 7, [h, wab])
                g_, t_ = gb[ti % 2], tt[ti % 2]
                em.op("dve", lambda e, ps=ps, t_=t_: e.tensor_tensor(out=t_[:, 0:16], in0=ps[:, 0:16], in1=dtb[:], op=ALU.add), reads=[ps, dtb], writes=[t_])
                em.op("dve", lambda e, ps=ps, t_=t_: e.tensor_scalar(out=t_[:, 16:32], in0=ps[:, 16:32], scalar1=-1.0, scalar2=None, op0=ALU.mult),
                      reads=[ps], writes=[t_])
                em.op("act", lambda e, t_=t_: e.activation(out=t_[:], in_=t_[:], func=AF.Exp), reads=[t_], writes=[t_])
                em.op("act", lambda e, t_=t_, g_=g_: e.activation(out=g_[:, 0:16], in_=t_[:, 0:16], func=AF.Ln, bias=1.0, scale=1.0), reads=[t_], writes=[g_])
                em.op("act", lambda e, t_=t_, g_=g_: e.activation(out=g_[:, 32:48], in_=t_[:, 16:32], func=AF.Ln, bias=1.0, scale=1.0), reads=[t_], writes=[g_])
                em.op("dve", lambda e, g_=g_: e.tensor_tensor(out=g_[:, 0:16], in0=g_[:, 0:16], in1=nega[:], op=ALU.mult), reads=[g_, nega], writes=[g_])
                em.op("dve", lambda e, g_=g_: e.tensor_scalar(out=g_[:, 32:48], in0=g_[:, 32:48], scalar1=-1.0, scalar2=None, op0=ALU.mult), reads=[g_], writes=[g_])
                em.op("dve", lambda e, t_=t_: e.tensor_scalar(out=t_[:, 16:32], in0=t_[:, 16:32], scalar1=1.0, scalar2=None, op0=ALU.add), reads=[t_], writes=[t_])
                em.op("dve", lambda e, t_=t_, g_=g_: e.reciprocal(out=g_[:, 16:32], in_=t_[:, 16:32]), reads=[t_], writes=[g_])
                em.dma("pool", self.dGB[z].ap()[t0 + ti], g_[:], reads=[g_])
        em.barrier()
        em.release(wb + hT + lat + stg + gg + [wab, nega, dtb] + gb + tt)


def dn_phase_a2(self, z):
    em = self.em
    T, NT = self.T, self.NT
    with ExitStack() as st:
        cw = em.sb(st, "cw", [128, 32, 5], F32)
        em.dma("sp", cw[:].rearrange("p c k -> p (c k)"), self.dn_in["conv"][z].ap(), writes=[cw])
        x = [em.sb(st, "cx%d" % q, [128, T], F32) for q in range(2)]
        acc = [em.sb(st, "cacc0", [128, T], F32)] * 2
        yb = em.sb(st, "cyb", [128, T], BF16)
        sq = em.sb(st, "csq", [128, 512], BF16)
        ri = em.sb(st, "cri", [128, 512], F32)
        tp = [em.sb(st, "ctp%d" % q, [128, 8, 128], BF16) for q in range(2)]
        segs = [(0, 256), (256, T)]
        tk = 0
        for c in range(32):
            x_, a_ = x[c % 2], acc[c % 2]
            eng = "dve"
            em.dma("sp", x_[:], self.dPRE.ap()[c], writes=[x_])
            em.op(eng, lambda e, x_=x_, a_=a_, c=c: e.tensor_scalar(out=a_[:], in0=x_[:], scalar1=cw[:, c, 2:3], scalar2=None, op0=ALU.mult),
                  reads=[x_, cw], writes=[a_])
            for k in (0, 1, 3, 4):
                s = k - 2
                for (a, b) in segs:
                    lo, hi = max(a, a - s), min(b, b - s)
                    em.op(eng, lambda e, x_=x_, a_=a_, c=c, k=k, lo=lo, hi=hi, s=s: e.scalar_tensor_tensor(
                        out=a_[:, lo:hi], in0=x_[:, lo + s:hi + s], scalar=cw[:, c, k:k + 1], in1=a_[:, lo:hi], op0=ALU.mult, op1=ALU.add),
                        reads=[x_, a_, cw], writes=[a_])
            em.op("act", lambda e, a_=a_: e.activation(out=a_[:], in_=a_[:], func=AF.Silu), reads=[a_], writes=[a_])
            if c < 16:
                for b0 in range(0, T, 512):
                    n = min(512, T - b0)
                    em.op("dve", lambda e, a_=a_, b0=b0, n=n: e.tensor_tensor(out=sq[:, :n], in0=a_[:, b0:b0 + n], in1=a_[:, b0:b0 + n], op=ALU.mult),
                          reads=[a_], writes=[sq])
                    ps = self.pn()
                    self.mm(ps, ps[:, :n], self.cb(C_ONES), sq[:, :n], True, True, [self.cmb, sq])
                    em.op("act", lambda e, ps=ps, n=n: e.activation(out=ri[:, :n], in_=ps[:, :n], func=AF.Sqrt, bias=1e-6, scale=1.0), reads=[ps], writes=[ri])
                    em.op("dve", lambda e, n=n: e.reciprocal(out=ri[:, :n], in_=ri[:, :n]), reads=[ri], writes=[ri])
                    sc = (128.0 ** -0.5) if c < 8 else 1.0
                    em.op("dve", lambda e, a_=a_, b0=b0, n=n, sc=sc: e.scalar_tensor_tensor(
                        out=yb[:, b0:b0 + n], in0=a_[:, b0:b0 + n], scalar=sc, in1=ri[:, :n], op0=ALU.mult, op1=ALU.mult),
                        reads=[a_, ri], writes=[yb])
                dst = (self.dQT if c < 8 else self.dKT)[z]
                em.dma("pool", dst.ap()[c % 8], yb[:], reads=[yb])
            else:
                em.op("dve", lambda e, a_=a_: e.tensor_copy(out=yb[:], in_=a_[:]), reads=[a_], writes=[yb])
            if c >= 8:
                for t8 in range(0, NT, 8):
                    nt = min(8, NT - t8)
                    ps = self.pn()
                    pb = ps[:].bitcast(BF16)
                    for q in range(nt):
                        em.op("pe", lambda e, pb=pb, q=q, t8=t8: e.transpose(out=pb[:, q * 128:(q + 1) * 128], in_=yb[:, (t8 + q) * 128:(t8 + q + 1) * 128],
                                                                           identity=self.cb(C_ID)), reads=[yb, self.cmb], writes=[ps])
                    t_ = tp[tk % 2]
                    tk += 1
                    em.op("act", lambda e, pb=pb, t_=t_, nt=nt: e.activation(out=t_[:, 0:nt, :].rearrange("p a b -> p (a b)"), in_=pb[:, 0:nt * 128], func=AF.Copy),
                          reads=[ps], writes=[t_])
                    if c < 16:
                        dstap = self.dKM[z].ap()[t8:t8 + nt, :, (c - 8) * 128:(c - 7) * 128].rearrange("t p n -> p t n")
                    else:
                        dstap = self.dV[z].ap()[t8:t8 + nt, :, (c - 16) * 128:(c - 15) * 128].rearrange("t p n -> p t n")
                    em.dma("pool", dstap, t_[:, 0:nt, :], reads=[t_])
        em.barrier()
        em.release([cw, yb, sq, ri] + x + acc[:1] + tp)


MK.dn_alloc = dn_alloc
MK.dn_phase_a1 = dn_phase_a1
MK.dn_phase_a2 = dn_phase_a2


def tri_inverse(self, LT, INV, INVT, Tsb, tmpx, sign, ltf=None):
    em = self.em
    for q, dst in ((0, INV), (1, INVT)):
        em.op("pool", lambda e, dst=dst: e.tensor_copy(out=dst[:], in_=self.cb(C_ID).unsqueeze(1).broadcast_to([128, 4, 128])),
              reads=[self.cmb], writes=[dst])
    for lv in range(6):
        pT = self.pn()
        for h4 in range(4):
            self.mm(pT, pT[:, h4 * 128:(h4 + 1) * 128], LT[:, h4, :] if ltf is None else ltf(h4), INV[:, h4, :], True, True, [LT, INV])
        em.op("act", lambda e, pT=pT: e.activation(out=Tsb[:].rearrange("p a b -> p (a b)"), in_=pT[:, :], func=AF.Copy), reads=[pT], writes=[Tsb])
        pX, pXT = self.pn(), self.pn()
        for h4 in range(4):
            self.mm(pX, pX[:, h4 * 128:(h4 + 1) * 128], INVT[:, h4, :], Tsb[:, h4, :], True, True, [INVT, Tsb])
        for h4 in range(4):
            self.mm(pXT, pXT[:, h4 * 128:(h4 + 1) * 128], Tsb[:, h4, :], INVT[:, h4, :], True, True, [INVT, Tsb])
        for (pp, msk, dst, q) in ((pX, C_LV + lv, INV, 0), (pXT, C_LVT + lv, INVT, 1)):
            tx = tmpx[q]
            em.op("dve", lambda e, pp=pp, msk=msk, tx=tx: e.scalar_tensor_tensor(
                out=tx[:], in0=pp[:, :].rearrange("p (a b) -> p a b", a=4), scalar=-float(sign),
                in1=self.cb(msk).unsqueeze(1).broadcast_to([128, 4, 128]), op0=ALU.mult, op1=ALU.mult),
                reads=[pp, self.cmb], writes=[tx])
            em.op("pool", lambda e, dst=dst, tx=tx: e.tensor_tensor(out=dst[:], in0=dst[:], in1=tx[:], op=ALU.add), reads=[dst, tx], writes=[dst])


MK.tri_inverse = tri_inverse


def dn_phase_s(self, z):
    em = self.em
    T, NT = self.T, self.NT
    O = self.O2[z]
    with ExitStack() as st:
        B = lambda n, sh, dt: em.sb(st, "n" + n, sh, dt)
        qt = B("qt", [128, 8, 128], BF16)
        kt_ = B("ktf", [128, 8, 128], BF16)
        km = B("km", [128, 8, 128], BF16)
        v = B("v", [128, 16, 128], BF16)
        gb = B("gb", [128, 48], F32)
        sm = B("sm", [128, 64], F32)
        vec = B("vec", [128, 6, 16], F32)
        mneg = B("mneg", [128, 2, 4, 128], BF16)
        for q, cidx in ((0, C_MI), (1, C_MS)):
            em.op("dve", lambda e, q=q, cidx=cidx: e.tensor_scalar(
                out=mneg[:, q, :, :], in0=self.cb(cidx).unsqueeze(1).broadcast_to([128, 4, 128]), scalar1=-1.0, scalar2=30000.0,
                op0=ALU.add, op1=ALU.mult), reads=[self.cmb], writes=[mneg])
        Dg = [B("Dg%d" % q, [128, 4, 128], F32) for q in range(2)]
        De = B("De", [128, 4, 128], BF16)
        E = [B("E%d" % q, [128, 4, 128], F32) for q in range(2)]
        LT = B("LT", [128, 4, 128], BF16)
        INV = B("INV", [128, 4, 128], BF16)
        INVT = B("INVT", [128, 4, 128], BF16)
        Tsb = B("Tsb", [128, 4, 128], BF16)
        tmpx = [B("tmpx%d" % q, [128, 4, 128], BF16) for q in range(2)]
        QKd = B("QKd", [128, 16, 128], BF16)
        qgT = B("qgT", [128, 16, 128], BF16)
        WT = B("WT", [128, 16, 128], BF16)
        U = B("U", [128, 16, 128], F32)
        VN = B("VN", [128, 16, 128], BF16)
        ktk = B("ktk", [128, 16, 128], BF16)
        vb = B("vb", [128, 16, 128], BF16)
        kbg = B("kbg", [128, 16, 128], BF16)
        OT = [B("OT%d" % q, [128, 2048], F32) for q in range(2)]
        S = [B("S%d" % q, [128, 8, 128], F32) for q in range(2)]
        Sb = [B("Sb%d" % q, [128, 8, 128], BF16) for q in range(2)]
        stmp = B("stmp", [128, 8, 128], F32)
        for q in range(2):
            em.op("pool", lambda e, q=q: e.memset(S[q][:], 0.0), writes=[S[q]])
            em.op("pool", lambda e, q=q: e.memset(Sb[q][:], 0.0), writes=[Sb[q]])
        bc16 = lambda ap: ap.unsqueeze(2).broadcast_to([128, 16, 128])
        for t in range(NT):
            c0 = t * 128
            em.dma("sp", qt[:], self.dQT[z].ap()[:, :, c0:c0 + 128].rearrange("h p n -> p h n"), writes=[qt])
            em.dma("sp", kt_[:], self.dKT[z].ap()[:, :, c0:c0 + 128].rearrange("h p n -> p h n"), writes=[kt_])
            em.dma("sp", km[:].rearrange("p a b -> p (a b)"), self.dKM[z].ap()[t], writes=[km])
            em.dma("sp", v[:].rearrange("p a b -> p (a b)"), self.dV[z].ap()[t], writes=[v])
            em.dma("sp", gb[:], self.dGB[z].ap()[t], writes=[gb])
            ps = self.pn()
            for q, cidx in enumerate((C_TRI, C_BLK, C_SEL0, C_SEL1)):
                self.mm(ps, ps[:, q * 16:(q + 1) * 16], self.cf(cidx), gb[:, 0:16], True, True, [self.cmf, gb])
            em.op("dve", lambda e, ps=ps: e.tensor_copy(out=sm[:], in_=ps[:, 0:64]), reads=[ps], writes=[sm])
            gc, gl = sm[:, 0:16], sm[:, 16:32]
            em.op("dve", lambda e: e.tensor_tensor(out=vec[:, 0, :], in0=gc, in1=gb[:, 32:48], op=ALU.add), reads=[sm, gb], writes=[vec])
            em.op("act", lambda e: e.activation(out=vec[:, 1, :], in_=gc, func=AF.Exp), reads=[sm], writes=[vec])
            em.op("dve", lambda e: e.tensor_tensor(out=vec[:, 2, :], in0=gl, in1=gc, op=ALU.subtract), reads=[sm], writes=[vec])
            em.op("act", lambda e: e.activation(out=vec[:, 2, :], in_=vec[:, 2, :], func=AF.Exp), reads=[vec], writes=[vec])
            em.op("dve", lambda e: e.tensor_tensor(out=vec[:, 3, :], in0=vec[:, 1, :], in1=gb[:, 16:32], op=ALU.mult), reads=[vec, gb], writes=[vec])
            em.op("act", lambda e: e.activation(out=vec[:, 4:6, :].rearrange("p a b -> p (a b)"), in_=sm[:, 32:64], func=AF.Exp), reads=[sm], writes=[vec])
            em.op("dve", lambda e: e.tensor_tensor(out=vb[:], in0=v[:], in1=bc16(gb[:, 16:32]), op=ALU.mult), reads=[v, gb], writes=[vb])
            for r in range(2):
                kmr = km[:]
                em.op("pool", lambda e, r=r: e.tensor_tensor(
                    out=kbg[:].rearrange("p (a r) n -> p a r n", r=2)[:, :, r, :], in0=km[:],
                    in1=vec[:, 3, :].rearrange("p (a r) -> p a r", r=2)[:, :, r].unsqueeze(2).broadcast_to([128, 8, 128]), op=ALU.mult),
                    reads=[km, vec], writes=[kbg])
                em.op("pool", lambda e, r=r: e.tensor_tensor(
                    out=ktk[:].rearrange("p (a r) n -> p a r n", r=2)[:, :, r, :], in0=km[:],
                    in1=vec[:, 2, :].rearrange("p (a r) -> p a r", r=2)[:, :, r].unsqueeze(2).broadcast_to([128, 8, 128]), op=ALU.mult),
                    reads=[km, vec], writes=[ktk])
            for hg in range(4):
                hs = slice(hg * 4, hg * 4 + 4)
                bc4 = lambda ap: ap.unsqueeze(2).broadcast_to([128, 4, 128])
                idf = self.cf(C_ID).unsqueeze(1).broadcast_to([128, 4, 128])
                em.op("dve", lambda e, hs=hs: e.tensor_tensor(out=Dg[0][:], in0=idf, in1=bc4(sm[:, hs]), op=ALU.mult), reads=[self.cmf, sm], writes=[Dg[0]])
                em.op("dve", lambda e, hs=hs: e.tensor_tensor(out=Dg[1][:], in0=idf, in1=bc4(vec[:, 0, hs]), op=ALU.mult), reads=[self.cmf, vec], writes=[Dg[1]])
                em.op("pool", lambda e, hs=hs: e.tensor_tensor(out=De[:], in0=self.cb(C_ID).unsqueeze(1).broadcast_to([128, 4, 128]), in1=bc4(vec[:, 1, hs]), op=ALU.mult),
                      reads=[self.cmb, vec], writes=[De])
                for q in range(2):
                    p_ = self.pn()
                    self.mm(p_, p_[:, :], self.cf(C_ONES), Dg[q][:].rearrange("p a b -> p (a b)"), True, False, [self.cmf, Dg[q]])
                    self.mm(p_, p_[:, :], self.cb(C_ID), mneg[:, q, :, :].rearrange("p a b -> p (a b)"), False, True, [self.cmb, mneg])
                    em.op("dve", lambda e, p_=p_, q=q, hs=hs: e.tensor_tensor(out=E[q][:], in0=p_[:, :].rearrange("p (a b) -> p a b", a=4),
                                                                           in1=bc4(sm[:, hs]), op=ALU.subtract), reads=[p_, sm], writes=[E[q]])
                    em.op("act", lambda e, q=q: e.activation(out=E[q][:], in_=E[q][:], func=AF.Exp), reads=[E[q]], writes=[E[q]])
                pe_ = self.pn()
                self.mm(pe_, pe_[:, :], self.cb(C_ONES), De[:].rearrange("p a b -> p (a b)"), True, True, [self.cmb, De])
                pk = self.pn()
                for qh in range(2):
                    hq = hg * 2 + qh
                    self.mm(pk, pk[:, qh * 128:(qh + 1) * 128], kt_[:, hq, :], kt_[:, hq, :], True, True, [kt_])
                    self.mm(pk, pk[:, 256 + qh * 128:256 + (qh + 1) * 128], kt_[:, hq, :], qt[:, hq, :], True, True, [kt_, qt])
                rep = lambda ap: ap.rearrange("p (a b) -> p a b", a=2).unsqueeze(2).broadcast_to([128, 2, 2, 128])
                v4 = lambda ap: ap.rearrange("p (a r) b -> p a r b", r=2)
                em.op("dve", lambda e, pk=pk: e.tensor_tensor(out=v4(LT[:]), in0=rep(pk[:, 0:256]), in1=v4(E[1][:]), op=ALU.mult), reads=[pk, E[1]], writes=[LT])
                em.op("dve", lambda e, pk=pk, hs=hs: e.tensor_tensor(out=v4(QKd[:, hs, :]), in0=rep(pk[:, 256:512]), in1=v4(E[0][:]), op=ALU.mult),
                      reads=[pk, E[0]], writes=[QKd])
                em.op("dve", lambda e, pe_=pe_, hs=hs, hg=hg: e.tensor_tensor(
                    out=v4(qgT[:, hs, :]), in0=qt[:, hg * 2:hg * 2 + 2, :].unsqueeze(2).broadcast_to([128, 2, 2, 128]),
                    in1=v4(pe_[:, :].rearrange("p (a b) -> p a b", a=4)), op=ALU.mult), reads=[pe_, qt], writes=[qgT])
                self.tri_inverse(LT, INV, INVT, Tsb, tmpx, 1.0)
                pu, pw = self.pn(), self.pn()
                for h4 in range(4):
                    h = hg * 4 + h4
                    self.mm(pu, pu[:, h4 * 128:(h4 + 1) * 128], INVT[:, h4, :], vb[:, h, :], True, True, [INVT, vb])
                    self.mm(pw, pw[:, h4 * 128:(h4 + 1) * 128], kbg[:, h, :], INVT[:, h4, :], True, True, [INVT, kbg])
                em.op("act", lambda e, pu=pu, hs=hs: e.activation(out=U[:, hs, :].rearrange("p a b -> p (a b)"), in_=pu[:, :], func=AF.Copy), reads=[pu], writes=[U])
                em.op("dve", lambda e, pw=pw, hs=hs: e.tensor_copy(out=WT[:, hs, :].rearrange("p a b -> p (a b)"), in_=pw[:, :]), reads=[pw], writes=[WT])
            o_ = OT[t % 2]
            for c in range(2):
                r0 = c * 64
                rs_ = slice(r0, r0 + 64)
                for hh in range(2):
                    S_, Sb_ = S[hh], Sb[hh]
                    pws = [self.pn(), self.pn()]
                    for h8 in range(8):
                        h = hh * 8 + h8
                        pb_ = pws[h8 // 4]
                        self.mm(pb_, pb_[rs_, (h8 % 4) * 128:(h8 % 4 + 1) * 128], WT[:, h, rs_], Sb_[:, h8, :], True, True, [WT, Sb_])
                    for b2 in range(2):
                        hsl = slice(hh * 8 + b2 * 4, hh * 8 + b2 * 4 + 4)
                        em.op("dve", lambda e, b2=b2, hsl=hsl, pws=pws: e.tensor_tensor(
                            out=VN[rs_, hsl, :].rearrange("p a b -> p (a b)"), in0=U[rs_, hsl, :].rearrange("p a b -> p (a b)"),
                            in1=pws[b2][rs_, :], op=ALU.subtract), reads=[U, pws[b2]], writes=[VN])
                    pos = [self.pn(), self.pn()]
                    pss = [self.pn(), self.pn()]
                    for h8 in range(8):
                        h = hh * 8 + h8
                        po_, ps_ = pos[h8 // 4], pss[h8 // 4]
                        cs_ = slice((h8 % 4) * 128, (h8 % 4 + 1) * 128)
                        self.mm(po_, po_[rs_, cs_], QKd[rs_, h, rs_], VN[rs_, h, :], True, False, [QKd, VN])
                        self.mm(po_, po_[rs_, cs_], qgT[:, h, rs_], Sb_[:, h8, :], False, True, [qgT, Sb_])
                        self.mm(ps_, ps_[:, cs_], ktk[rs_, h, :], VN[rs_, h, :], True, True, [ktk, VN])
                    for b2 in range(2):
                        oc0 = (hh * 8 + b2 * 4) * 128
                        em.op("act", lambda e, b2=b2, oc0=oc0, pos=pos: e.activation(out=o_[rs_, oc0:oc0 + 512], in_=pos[b2][rs_, :], func=AF.Copy),
                              reads=[pos[b2]], writes=[o_])
                    em.op("dve", lambda e, hh=hh, c=c: e.tensor_tensor(
                        out=stmp[:], in0=S_[:], in1=vec[:, 4 + c, hh * 8:hh * 8 + 8].unsqueeze(2).broadcast_to([128, 8, 128]), op=ALU.mult),
                        reads=[S_, vec], writes=[stmp])
                    for b2 in range(2):
                        em.op("dve", lambda e, b2=b2, pss=pss: e.tensor_tensor(
                            out=S_[:, b2 * 4:b2 * 4 + 4, :].rearrange("p a b -> p (a b)"), in0=stmp[:, b2 * 4:b2 * 4 + 4, :].rearrange("p a b -> p (a b)"),
                            in1=pss[b2][:, :], op=ALU.add), reads=[stmp, pss[b2]], writes=[S_])
                    em.op("act", lambda e: e.activation(out=Sb_[:].rearrange("p a b -> p (a b)"), in_=S_[:].rearrange("p a b -> p (a b)"), func=AF.Copy),
                          reads=[S_], writes=[Sb_])
            em.dma("pool", O.ap()[t * 128:(t + 1) * 128, :], o_[:], reads=[o_])
        em.barrier()
        em.release([qt, kt_, km, v, gb, sm, vec, mneg, De, LT, INV, INVT, Tsb, QKd, qgT, WT, U, VN, ktk, vb, kbg, stmp] + Dg + E + tmpx + OT + S + Sb)


MK.dn_phase_s = dn_phase_s


def dn_factory(self):
    em = self.em

    def factory(st):
        ngr = em.sb(st, "zng", [128, 128], F32)
        em.dma("sp", ngr[:], self.dn_in["ng"].ap().partition_broadcast(128), writes=[ngr])
        oo = [em.sb(st, "zoo%d" % q, [128, 4, 128], F32) for q in range(2)]
        p1 = [em.sb(st, "zp1%d" % q, [128, 512], F32) for q in range(2)]
        gt = [em.sb(st, "zgt%d" % q, [128, 512], BF16) for q in range(2)]
        osum = em.sb(st, "zosum", [128, 4, 128], F32)
        sq = em.sb(st, "zsq", [128, 4, 128], F32)
        sg = em.sb(st, "zsg", [128, 4, 128], F32)
        ss = em.sb(st, "zss", [128, 4], F32)
        y = em.sb(st, "zy", [128, 2048], BF16)
        cnt = [0]
        f2 = lambda b: b[:].rearrange("p a b -> p (a b)")

        def make_yT(t, isctx, yT):
            pt = self.ptile(t)
            for blk in range(4):
                k = cnt[0]
                cnt[0] += 1
                o_, a1, g_ = oo[k % 2], p1[k % 2], gt[k % 2]
                cs = slice(blk * 512, (blk + 1) * 512)
                em.dma("sp", f2(o_), self.O2[0].ap()[t * 128:(t + 1) * 128, cs], writes=[o_])
                em.dma("sp", a1[:], self.O2[1].ap()[pt * 128:(pt + 1) * 128, cs], writes=[a1])
                em.dma("sp", g_[:], self.rG.ap()[t][:, cs], writes=[g_])
                ps = self.pn()
                self.mm(ps, ps[:, :], self.cf(C_J0), a1[:], True, True, [self.cmf, a1])
                em.op("dve", lambda e: e.tensor_tensor(out=f2(osum), in0=ps[:, :], in1=f2(o_), op=ALU.add), reads=[ps, o_], writes=[osum])
                em.op("pool", lambda e: e.tensor_tensor(out=sq[:], in0=osum[:], in1=osum[:], op=ALU.mult), reads=[osum], writes=[sq])
                em.op("dve", lambda e: e.tensor_reduce(out=ss[:], in_=sq[:], axis=AX.X, op=ALU.add), reads=[sq], writes=[ss])
                em.op("act", lambda e: e.activation(out=ss[:], in_=ss[:], func=AF.Sqrt, bias=1e-6, scale=1.0 / 128.0), reads=[ss], writes=[ss])
                em.op("dve", lambda e: e.reciprocal(out=ss[:], in_=ss[:]), reads=[ss], writes=[ss])
                em.op("act", lambda e: e.activation(out=f2(sg), in_=g_[:], func=AF.Silu), reads=[g_], writes=[sg])
                em.op("dve", lambda e: e.tensor_tensor(out=osum[:], in0=osum[:], in1=ss[:].unsqueeze(2).broadcast_to([128, 4, 128]), op=ALU.mult),
                      reads=[osum, ss], writes=[osum])
                em.op("pool", lambda e: e.tensor_tensor(out=osum[:], in0=osum[:], in1=ngr[:].unsqueeze(1).broadcast_to([128, 4, 128]), op=ALU.mult),
                      reads=[osum, ngr], writes=[osum])
                em.op("pool", lambda e: e.tensor_tensor(out=y[:, cs], in0=f2(osum), in1=f2(sg), op=ALU.mult), reads=[osum, sg], writes=[y])
            for half in range(2):
                ps = self.pn()
                pb = ps[:].bitcast(BF16)
                for c in range(8):
                    cc = half * 8 + c
                    em.op("pe", lambda e: e.transpose(out=pb[:, c * 128:(c + 1) * 128], in_=y[:, cc * 128:(cc + 1) * 128], identity=self.cb(C_ID)),
                          reads=[y, self.cmb], writes=[ps])
                em.op("act", lambda e: e.activation(out=yT[:, half * 8:(half + 1) * 8, :].rearrange("p c n -> p (c n)"), in_=pb[:, 0:1024], func=AF.Copy),
                      reads=[ps], writes=[yT])
        return make_yT, [ngr, osum, sq, sg, ss, y] + oo + p1 + gt
    return factory


def dn_layer(self, i, modLC, rows):
    if not hasattr(self, "dPRE"):
        self.dn_alloc()
    import os
    dbg = int(os.environ.get("DNDBG", "99"))
    for z in range(2):
        self.dn_phase_a1(modLC, z)
        if dbg >= 2:
            self.dn_phase_a2(z)
        if dbg >= 3:
            self.dn_phase_s(z)
    if dbg >= 4:
        self.phase_f(i, modLC, rows, self.dn_factory(), "dnout", 16)


MK.dn_factory = dn_factory
MK.dn_layer = dn_layer


def rk_alloc(self):
    NT, T = self.NT, self.T
    self.kHT = self.dr("kHT", [8, 128, T], F32)
    fm = lambda n: [self.dr("k%s%d" % (n, z), [8, 128, T], F32) for z in range(2)]
    self.kR, self.kK, self.kA, self.kB = fm("R"), fm("K"), fm("A"), fm("B")
    self.kV = [self.dr("kV%d" % z, [NT, 128, 1024], BF16) for z in range(2)]
    self.kLW = [self.dr("kLW%d" % z, [NT, 128, 1024], F32) for z in range(2)]
    self.kBT = self.dr("kBT", [8, 128, T], BF16)
    self.kGT = self.dr("kGT", [8, 128, T], BF16)
    self.rk_in = dict(
        mix=self.xin("rkmix", [128, 48]), vec=self.xin("rkvec", [128, 32]),
        w0=[self.xin("rkw0_%d" % z, [1, 1024]) for z in range(2)],
        a0=[self.xin("rka0_%d" % z, [128, 8]) for z in range(2)],
        w1=[self.xin("rkw1_%d" % z, [128, 512]) for z in range(2)],
        a1=[self.xin("rka1_%d" % z, [128, 512]) for z in range(2)],
        w2=[self.xin("rkw2_%d" % z, [64, 1024]) for z in range(2)],
        a2=[self.xin("rka2_%d" % z, [64, 1024]) for z in range(2)])


def rk_phase_a1(self, modLC, z):
    em = self.em
    LAT = self.LAT2[z]
    with ExitStack() as st:
        hT = [em.sb(st, "khT%d" % q, [128, 8, 512], F32) for q in range(2)]
        lat = [em.sb(st, "klat%d" % q, [128, D], F32) for q in range(2)]
        kk = 0
        for gi, (t0, ng, isctx) in enumerate(self.groups):
            h = hT[gi % 2]
            for ti in range(ng):
                la = lat[kk % 2]
                kk += 1
                em.dma("sp", la[:], LAT.ap()[(t0 + ti) * 128:(t0 + ti + 1) * 128, :], writes=[la])
                self.hT_tile(la, h, ti * 128, modLC, 0, isctx)
            N = ng * 128
            em.dma("pool", self.kHT.ap()[:, :, t0 * 128:t0 * 128 + N].rearrange("c p n -> p c n"), h[:, :, 0:N], reads=[h])
        em.barrier()
        em.release(hT + lat)


def rk_phase_a2(self, z):
    em = self.em
    T, NT = self.T, self.NT
    I = self.rk_in
    N = 256
    with ExitStack() as st:
        B = lambda n, sh, dt: em.sb(st, "a" + n, sh, dt)
        mixv = B("mixv", [128, 48], F32)
        vecs = B("vecs", [128, 40], F32)
        em.dma("sp", mixv[:], I["mix"].ap(), writes=[mixv])
        em.dma("sp", vecs[:, 0:32], I["vec"].ap(), writes=[vecs])
        em.op("dve", lambda e: e.tensor_scalar(out=vecs[:, 32:40], in0=vecs[:, 8:16], scalar1=-1.0, scalar2=1.0, op0=ALU.mult, op1=ALU.add),
              reads=[vecs], writes=[vecs])
        zo = [z, 1 - z]
        a0 = [B("a0_%d" % q, [128, 8], F32) for q in range(2)]
        w1 = B("w1", [128, 8, 64], BF16)
        a1 = [B("a1_%d" % q, [128, 8, 64], BF16) for q in range(2)]
        w2 = B("w2", [64, 1024], BF16)
        a2 = [B("a2_%d" % q, [64, 1024], BF16) for q in range(2)]
        w0r = B("w0r", [128, 1024], F32)
        em.dma("sp", w0r[:], I["w0"][z].ap().partition_broadcast(128), writes=[w0r])
        em.dma("pool", w1[:].rearrange("p c n -> p (c n)"), I["w1"][z].ap(), writes=[w1])
        em.dma("pool", w2[:], I["w2"][z].ap(), writes=[w2])
        for q in range(2):
            em.dma("sp", a0[q][:], I["a0"][zo[q]].ap(), writes=[a0[q]])
            em.dma("pool", a1[q][:].rearrange("p c n -> p (c n)"), I["a1"][zo[q]].ap(), writes=[a1[q]])
            em.dma("pool", a2[q][:], I["a2"][zo[q]].ap(), writes=[a2[q]])
        Wr, Wk, Wv = [B("W%d" % q, [128, 8, 1024], BF16) for q in range(3)]
        for wb_, nm in ((Wr, "rkr"), (Wk, "rkk"), (Wv, "rkv")):
            self.load_w(wb_, nm, 0, 8, 0, 1024)
        g1 = B("g1", [128, 8, 128], BF16)
        g2 = B("g2", [128, 1, 1024], BF16)
        self.load_w(g1, "rkg1", 0, 8, 0, 128)
        self.load_w(g2, "rkg2", 0, 1, 0, 1024)
        hW = B("hW", [128, 8, N + 2], F32)
        xx = B("xx", [128, 8, N], F32)
        xm = [B("xm%d" % m, [128, 8, N], BF16) for m in range(6)]
        t1T = B("t1T", [64, N], BF16)
        u1T = [B("u1T%d" % q, [64, N], BF16) for q in range(2)]
        gsT = B("gsT", [128, N], BF16)
        ch = {n: B("c" + n, [128, N], F32) for n in ("r", "k", "v", "a", "ao", "kk", "t", "t2", "kd")}
        sqb = B("sqb", [128, N], BF16)
        obf = [B("obf%d" % q, [128, N], BF16) for q in range(2)]
        vtm = [B("vtm%d" % q, [128, 1024], BF16) for q in range(2)]
        lwt = [B("lwt%d" % q, [128, 1024], F32) for q in range(2)]
        groups = [(0, 256, 0, 256)] + [(256 + g * N, N, 256, T) for g in range((T - 256) // N)]
        nob = 0
        for (s0, n_, lo, hi) in groups:
            a_, b_ = max(lo, s0 - 1), min(hi, s0 + n_ + 1)
            if a_ > s0 - 1:
                em.op("pool", lambda e: e.memset(hW[:, :, 0:1], 0.0), writes=[hW])
            if b_ < s0 + n_ + 1:
                em.op("pool", lambda e: e.memset(hW[:, :, N + 1:N + 2], 0.0), writes=[hW])
            em.dma("sp", hW[:, :, a_ - (s0 - 1):b_ - (s0 - 1)], self.kHT.ap()[:, :, a_:b_].rearrange("c p n -> p c n"), writes=[hW])
            em.op("dve", lambda e: e.tensor_tensor(out=xx[:], in0=hW[:, :, 0:N], in1=hW[:, :, 2:N + 2], op=ALU.add), reads=[hW], writes=[xx])
            em.op("dve", lambda e: e.scalar_tensor_tensor(out=xx[:], in0=xx[:], scalar=0.5, in1=hW[:, :, 1:N + 1], op0=ALU.mult, op1=ALU.subtract),
                  reads=[xx, hW], writes=[xx])
            for m in range(6):
                if z == 1 and m == 5:
                    continue
                for c in range(8):
                    em.op("dve", lambda e, m=m, c=c: e.scalar_tensor_tensor(
                        out=xm[m][:, c, :], in0=xx[:, c, :], scalar=mixv[:, m * 8 + c:m * 8 + c + 1], in1=hW[:, c, 1:N + 1],
                        op0=ALU.mult, op1=ALU.add), reads=[xx, hW, mixv], writes=[xm[m]])
            ps = self.pn()
            for kc in range(8):
                self.mm(ps, ps[0:64, 0:N], w1[:, kc, :], xm[3][:, kc, :], kc == 0, kc == 7, [w1, xm[3]])
            em.op("act", lambda e, ps=ps: e.activation(out=t1T[:], in_=ps[0:64, 0:N], func=AF.Tanh), reads=[ps], writes=[t1T])
            for q in range(2 if z == 0 else 1):
                ps = self.pn()
                for kc in range(8):
                    self.mm(ps, ps[0:64, 0:N], a1[q][:, kc, :], xm[4][:, kc, :], kc == 0, kc == 7, [a1[q], xm[4]])
                em.op("act", lambda e, ps=ps, q=q: e.activation(out=u1T[q][:], in_=ps[0:64, 0:N], func=AF.Copy), reads=[ps], writes=[u1T[q]])
            if z == 0:
                ps = self.pn()
                for kc in range(8):
                    self.mm(ps, ps[:, 0:N], g1[:, kc, :], xm[5][:, kc, :], kc == 0, kc == 7, [g1, xm[5]])
                em.op("act", lambda e, ps=ps: e.activation(out=gsT[:], in_=ps[:, 0:N], func=AF.Sigmoid), reads=[ps], writes=[gsT])
            for ti in range(2):
                t = s0 // 128 + ti
                v_, l_ = vtm[ti], lwt[ti]
                for half in range(2):
                    ps = self.pn()
                    for kc in range(8):
                        self.mm(ps, ps[:, :], xm[2][:, kc, ti * 128:(ti + 1) * 128], Wv[:, kc, half * 512:(half + 1) * 512], kc == 0, kc == 7, [xm[2], Wv])
                    em.op("act", lambda e, ps=ps, half=half: e.activation(out=v_[:, half * 512:(half + 1) * 512], in_=ps[:, :], func=AF.Copy), reads=[ps], writes=[v_])
                    ps = self.pn()
                    self.mm(ps, ps[:, :], t1T[:, ti * 128:(ti + 1) * 128], w2[:, half * 512:(half + 1) * 512], True, True, [t1T, w2])
                    em.op("dve", lambda e, ps=ps, half=half: e.tensor_tensor(out=l_[:, half * 512:(half + 1) * 512], in0=ps[:, :], in1=w0r[:, half * 512:(half + 1) * 512], op=ALU.add),
                          reads=[ps, w0r], writes=[l_])
                em.op("act", lambda e: e.activation(out=l_[:], in_=l_[:], func=AF.Sigmoid), reads=[l_], writes=[l_])
                em.op("dve", lambda e: e.tensor_scalar(out=l_[:], in0=l_[:], scalar1=-float(np.exp(-0.5)), scalar2=None, op0=ALU.mult), reads=[l_], writes=[l_])
                em.dma("pool", self.kV[z].ap()[t], v_[:], reads=[v_])
                em.dma("pool", self.kLW[z].ap()[t], l_[:], reads=[l_])
            for c in range(8):
                cs = slice(c * 128, (c + 1) * 128)
                for nm, m, W_ in (("r", 0, Wr), ("k", 1, Wk), ("v", 2, Wv)):
                    ps = self.pn()
                    for kc in range(8):
                        self.mm(ps, ps[:, 0:N], W_[:, kc, cs], xm[m][:, kc, :], kc == 0, kc == 7, [W_, xm[m]])
                    em.op("act", lambda e, ps=ps, nm=nm: e.activation(out=ch[nm][:], in_=ps[:, 0:N], func=AF.Copy), reads=[ps], writes=[ch[nm]])
                for q, nm in ((0, "a"), (1, "ao")):
                    if q == 1 and z == 1:
                        continue
                    ps = self.pn()
                    self.mm(ps, ps[:, 0:N], a2[q][:, cs], u1T[q][:], True, True, [a2[q], u1T[q]])
                    em.op("act", lambda e, ps=ps, nm=nm, q=q: e.activation(out=ch[nm][:], in_=ps[:, 0:N], func=AF.Sigmoid, bias=a0[q][:, c:c + 1], scale=1.0),
                          reads=[ps, a0[q]], writes=[ch[nm]])
                em.op("dve", lambda e: e.tensor_scalar(out=ch["kk"][:], in0=ch["k"][:], scalar1=vecs[:, c:c + 1], scalar2=None, op0=ALU.mult), reads=[ch["k"], vecs], writes=[ch["kk"]])
                em.op("dve", lambda e: e.tensor_tensor(out=sqb[:], in0=ch["kk"][:], in1=ch["kk"][:], op=ALU.mult), reads=[ch["kk"]], writes=[sqb])
                ps = self.pn()
                self.mm(ps, ps[:, 0:N], self.cb(C_B64), sqb[:], True, True, [self.cmb, sqb])
                em.op("act", lambda e, ps=ps: e.activation(out=ch["t"][:], in_=ps[:, 0:N], func=AF.Sqrt, bias=1e-6, scale=1.0), reads=[ps], writes=[ch["t"]])
                em.op("dve", lambda e: e.reciprocal(out=ch["t"][:], in_=ch["t"][:]), reads=[ch["t"]], writes=[ch["t"]])
                em.op("dve", lambda e: e.tensor_tensor(out=ch["kk"][:], in0=ch["kk"][:], in1=ch["t"][:], op=ALU.mult), reads=[ch["kk"], ch["t"]], writes=[ch["kk"]])
                em.op("dve", lambda e: e.tensor_scalar(out=ch["t"][:], in0=ch["a"][:], scalar1=vecs[:, 8 + c:9 + c], scalar2=vecs[:, 32 + c:33 + c], op0=ALU.mult, op1=ALU.add),
                      reads=[ch["a"], vecs], writes=[ch["t"]])
                em.op("dve", lambda e: e.tensor_tensor(out=ch["kd"][:], in0=ch["k"][:], in1=ch["t"][:], op=ALU.mult), reads=[ch["k"], ch["t"]], writes=[ch["kd"]])
                em.op("dve", lambda e: e.tensor_tensor(out=ch["t"][:], in0=ch["kk"][:], in1=ch["a"][:], op=ALU.mult), reads=[ch["kk"], ch["a"]], writes=[ch["t"]])
                em.op("dve", lambda e: e.tensor_scalar(out=ch["kk"][:], in0=ch["kk"][:], scalar1=-1.0, scalar2=None, op0=ALU.mult), reads=[ch["kk"]], writes=[ch["kk"]])
                for dst, nm in ((self.kR, "r"), (self.kK, "kd"), (self.kA, "kk"), (self.kB, "t")):
                    em.dma("pool", dst[z].ap()[c][:, s0:s0 + n_], ch[nm][:, 0:n_], reads=[ch[nm]])
                if z == 0:
                    em.op("dve", lambda e: e.tensor_scalar(out=ch["t2"][:], in0=ch["ao"][:], scalar1=vecs[:, 8 + c:9 + c], scalar2=vecs[:, 32 + c:33 + c], op0=ALU.mult, op1=ALU.add),
                          reads=[ch["ao"], vecs], writes=[ch["t2"]])
                    em.op("dve", lambda e: e.tensor_tensor(out=ch["t2"][:], in0=ch["t2"][:], in1=ch["k"][:], op=ALU.mult), reads=[ch["t2"], ch["k"]], writes=[ch["t2"]])
                    em.op("dve", lambda e: e.tensor_tensor(out=ch["t2"][:], in0=ch["t2"][:], in1=ch["kd"][:], op=ALU.add), reads=[ch["t2"], ch["kd"]], writes=[ch["t2"]])
                    em.op("dve", lambda e: e.scalar_tensor_tensor(out=sqb[:], in0=ch["t2"][:], scalar=vecs[:, 16 + c:17 + c], in1=ch["r"][:], op0=ALU.mult, op1=ALU.mult),
                          reads=[ch["t2"], ch["r"], vecs], writes=[sqb])
                    ps = self.pn()
                    self.mm(ps, ps[:, 0:N], self.cb(C_B64), sqb[:], True, True, [self.cmb, sqb])
                    ob = obf[nob % 2]
                    nob += 1
                    em.op("dve", lambda e, ps=ps, ob=ob: e.tensor_tensor(out=ob[:], in0=ps[:, 0:N], in1=ch["v"][:], op=ALU.mult), reads=[ps, ch["v"]], writes=[ob])
                    em.dma("pool", self.kBT.ap()[c][:, s0:s0 + n_], ob[:, 0:n_], reads=[ob])
                    ps = self.pn()
                    self.mm(ps, ps[:, 0:N], g2[:, 0, cs], gsT[:], True, True, [g2, gsT])
                    ob = obf[nob % 2]
                    nob += 1
                    em.op("act", lambda e, ps=ps, ob=ob: e.activation(out=ob[:], in_=ps[:, 0:N], func=AF.Copy), reads=[ps], writes=[ob])
                    em.dma("pool", self.kGT.ap()[c][:, s0:s0 + n_], ob[:, 0:n_], reads=[ob])
        em.barrier()
        em.release([mixv, vecs, w1, w2, w0r, Wr, Wk, Wv, g1, g2, hW, xx, t1T, gsT, sqb] + a0 + a1 + a2 + xm + u1T + list(ch.values()) + obf + vtm + lwt)


MK.rk_alloc = rk_alloc
MK.rk_phase_a1 = rk_phase_a1
MK.rk_phase_a2 = rk_phase_a2


def rk_phase_s(self, z):
    em = self.em
    T, NT = self.T, self.NT
    O = self.O2[z]
    with ExitStack() as st:
        B = lambda n, sh, dt: em.sb(st, "s" + n, sh, dt)
        ld = {n: B("ld" + n, [128, 8, 128], F32) for n in "rkab"}
        v = B("v", [128, 1024], BF16)
        lw = B("lw", [128, 1024], F32)
        ex = {n: B("ex" + n, [128, 8, 128], F32) for n in ("g", "gx", "ng", "gl")}
        GL = B("GL", [128, 8, 2], F32)
        AR = B("AR", [128, 8, 2, 128], BF16)
        BK = B("BK", [128, 8, 2, 128], BF16)
        BKh = B("BKh", [128, 8, 2, 128], BF16)
        BhT = B("BhT", [128, 8, 128], BF16)
        KhT = B("KhT", [128, 8, 128], BF16)
        MASK4 = B("MASK4", [128, 4, 128], BF16)
        for q, cidx, sgn in ((0, C_MS, -1.0), (1, C_MI, 1.0), (2, C_MS, 1.0), (3, C_MI, 1.0)):
            em.op("dve", lambda e, q=q, cidx=cidx: e.tensor_copy(out=MASK4[:, q, :], in_=self.cb(cidx)), reads=[self.cmb], writes=[MASK4])
        AM = B("AM", [128, 4, 16, 128], BF16)
        INV = B("INV", [128, 4, 128], BF16)
        INVT = B("INVT", [128, 4, 128], BF16)
        IT = B("IT", [128, 16, 128], BF16)
        Tsb = B("Tsb", [128, 4, 128], BF16)
        tmpx = [B("tmpx%d" % q, [128, 4, 128], BF16) for q in range(2)]
        RHSb = B("RHSb", [128, 1024], BF16)
        Ub = B("Ub", [128, 1024], BF16)
        OT = [B("OT%d" % q, [128, 1024], F32) for q in range(2)]
        S = B("S", [128, 8, 2, 64], F32)
        Sb = B("Sb", [128, 8, 2, 64], BF16)
        stmp = B("stmp", [128, 8, 128], F32)
        t2m = B("t2m", [128, 4, 128], F32)
        em.op("pool", lambda e: e.memset(S[:], 0.0), writes=[S])
        em.op("pool", lambda e: e.memset(Sb[:], 0.0), writes=[Sb])
        srcs = dict(r=self.kR[z], k=self.kK[z], a=self.kA[z], b=self.kB[z])
        f2 = lambda ap: ap.rearrange("p a b -> p (a b)")
        for t in range(NT):
            c0 = t * 128
            for n in "rkab":
                em.dma("sp", ld[n][:], srcs[n].ap()[:, :, c0:c0 + 128].rearrange("c p n -> p c n"), writes=[ld[n]])
            em.dma("sp", v[:], self.kV[z].ap()[t], writes=[v])
            em.dma("sp", lw[:], self.kLW[z].ap()[t], writes=[lw])
            for c in range(8):
                ps = self.pn()
                for q, cidx in enumerate((C_TRI, C_TRIS, C_BLK)):
                    self.mm(ps, ps[:, q * 128:(q + 1) * 128], lw[:, c * 128:(c + 1) * 128], self.cf(cidx), True, True, [lw, self.cmf])
                em.op("act", lambda e, ps=ps, c=c: e.activation(out=ex["g"][:, c, :], in_=ps[:, 0:128], func=AF.Exp), reads=[ps], writes=[ex["g"]])
                em.op("act", lambda e, ps=ps, c=c: e.activation(out=ex["gx"][:, c, :], in_=ps[:, 128:256], func=AF.Exp), reads=[ps], writes=[ex["gx"]])
                em.op("act", lambda e, ps=ps, c=c: e.activation(out=ex["ng"][:, c, :], in_=ps[:, 0:128], func=AF.Exp, scale=-1.0), reads=[ps], writes=[ex["ng"]])
                em.op("act", lambda e, ps=ps, c=c: e.activation(out=ex["gl"][:, c, :], in_=ps[:, 256:384], func=AF.Exp), reads=[ps], writes=[ex["gl"]])
            em.op("dve", lambda e: e.tensor_copy(out=GL[:], in_=ex["gl"][:].rearrange("p c (a b) -> p c a b", a=2)[:, :, :, 0]), reads=[ex["gl"]], writes=[GL])
            em.op("dve", lambda e: e.tensor_tensor(out=ex["gl"][:], in0=ex["gl"][:], in1=ex["ng"][:], op=ALU.mult), reads=[ex["gl"], ex["ng"]], writes=[ex["gl"]])
            em.op("dve", lambda e: e.tensor_tensor(out=AR[:, :, 0, :], in0=ld["a"][:], in1=ex["gx"][:], op=ALU.mult), reads=[ld["a"], ex["gx"]], writes=[AR])
            em.op("dve", lambda e: e.tensor_tensor(out=AR[:, :, 1, :], in0=ld["r"][:], in1=ex["g"][:], op=ALU.mult), reads=[ld["r"], ex["g"]], writes=[AR])
            em.op("pool", lambda e: e.tensor_tensor(out=BK[:, :, 0, :], in0=ld["b"][:], in1=ex["ng"][:], op=ALU.mult), reads=[ld["b"], ex["ng"]], writes=[BK])
            em.op("pool", lambda e: e.tensor_tensor(out=BK[:, :, 1, :], in0=ld["k"][:], in1=ex["ng"][:], op=ALU.mult), reads=[ld["k"], ex["ng"]], writes=[BK])
            em.op("pool", lambda e: e.tensor_tensor(out=BKh[:, :, 0, :], in0=ld["b"][:], in1=ex["gl"][:], op=ALU.mult), reads=[ld["b"], ex["gl"]], writes=[BKh])
            em.op("dve", lambda e: e.tensor_tensor(out=BKh[:, :, 1, :], in0=ld["k"][:], in1=ex["gl"][:], op=ALU.mult), reads=[ld["k"], ex["gl"]], writes=[BKh])
            import os
            dS = int(os.environ.get("RKS", "99"))
            if dS <= 1:
                continue
            for q, dstb in ((0, BhT), (1, KhT)):
                ps = self.pn()
                pb = ps[:].bitcast(BF16)
                for c in range(8):
                    em.op("pe", lambda e, pb=pb, c=c, q=q: e.transpose(out=pb[:, c * 128:(c + 1) * 128], in_=BKh[:, c, q, :], identity=self.cb(C_ID)),
                          reads=[BKh, self.cmb], writes=[ps])
                em.op("act", lambda e, pb=pb, dstb=dstb: e.activation(out=f2(dstb[:]), in_=pb[:, 0:1024], func=AF.Copy), reads=[ps], writes=[dstb])
            if dS <= 2:
                continue
            for h in range(16):
                c, base = h // 2, (h % 2) * 64
                ps = self.pn()
                rhs = AR[base:base + 64, c, :, :].rearrange("p a b -> p (a b)")
                self.mm(ps, ps[:, 0:256], BK[base:base + 64, c, 0, :], rhs, True, True, [BK, AR])
                self.mm(ps, ps[:, 256:512], BK[base:base + 64, c, 1, :], rhs, True, True, [BK, AR])
                em.op("dve", lambda e, ps=ps, h=h: e.tensor_tensor(out=AM[:, :, h, :], in0=ps[:, :].rearrange("p (a b) -> p a b", a=4), in1=MASK4[:], op=ALU.mult),
                      reads=[ps, MASK4], writes=[AM])
            if dS <= 3:
                continue
            for hg in range(4):
                self.tri_inverse(AM, INV, INVT, Tsb, tmpx, -1.0, ltf=lambda h4, hg=hg: AM[:, 0, hg * 4 + h4, :])
                em.op("pool", lambda e, hg=hg: e.tensor_copy(out=IT[:, hg * 4:hg * 4 + 4, :], in_=INVT[:]), reads=[INVT], writes=[IT])
            if dS <= 4:
                continue
            o_ = OT[t % 2]
            for cc in range(2):
                r0 = cc * 64
                rs_ = slice(r0, r0 + 64)
                pr = [self.pn(), self.pn()]
                for c in range(8):
                    pb_ = pr[c // 4]
                    q0 = (c % 4) * 128
                    self.mm(pb_, pb_[rs_, q0:q0 + 128], AR[:, c, 0, rs_], Sb[:, c, :, :].rearrange("p a b -> p (a b)"), True, False, [AR, Sb])
                    for e_ in range(2):
                        h = 2 * c + e_
                        self.mm(pb_, pb_[rs_, q0 + e_ * 64:q0 + (e_ + 1) * 64], AM[rs_, 2, h, rs_], v[rs_, h * 64:(h + 1) * 64], False, True, [AM, v])
                for b2 in range(2):
                    em.op("act" if b2 else "dve", lambda e, b2=b2: (e.activation(out=RHSb[rs_, b2 * 512:(b2 + 1) * 512], in_=pr[b2][rs_, :], func=AF.Copy) if b2 else
                                                                   e.tensor_copy(out=RHSb[rs_, 0:512], in_=pr[0][rs_, :])), reads=[pr[b2]], writes=[RHSb])
                pu = [self.pn(), self.pn()]
                for h in range(16):
                    pb_ = pu[h // 8]
                    cs_ = slice((h % 8) * 64, (h % 8 + 1) * 64)
                    self.mm(pb_, pb_[rs_, cs_], IT[rs_, h, rs_], RHSb[rs_, h * 64:(h + 1) * 64], True, True, [IT, RHSb])
                for b2 in range(2):
                    em.op("act" if b2 else "dve", lambda e, b2=b2: (e.activation(out=Ub[rs_, b2 * 512:(b2 + 1) * 512], in_=pu[b2][rs_, :], func=AF.Copy) if b2 else
                                                                   e.tensor_copy(out=Ub[rs_, 0:512], in_=pu[0][rs_, :])), reads=[pu[b2]], writes=[Ub])
                po = [self.pn(), self.pn()]
                pS = [self.pn(), self.pn()]
                for c in range(8):
                    pb_ = po[c // 4]
                    q0 = (c % 4) * 128
                    self.mm(pb_, pb_[rs_, q0:q0 + 128], AR[:, c, 1, rs_], Sb[:, c, :, :].rearrange("p a b -> p (a b)"), True, False, [AR, Sb])
                    for e_ in range(2):
                        h = 2 * c + e_
                        osl = pb_[rs_, q0 + e_ * 64:q0 + (e_ + 1) * 64]
                        self.mm(pb_, osl, AM[rs_, 1, h, rs_], Ub[rs_, h * 64:(h + 1) * 64], False, False, [AM, Ub])
                        self.mm(pb_, osl, AM[rs_, 3, h, rs_], v[rs_, h * 64:(h + 1) * 64], False, True, [AM, v])
                    ps_ = pS[c // 4]
                    self.mm(ps_, ps_[:, q0:q0 + 128], BhT[rs_, c, :], Ub[rs_, c * 128:(c + 1) * 128], True, False, [BhT, Ub])
                    self.mm(ps_, ps_[:, q0:q0 + 128], KhT[rs_, c, :], v[rs_, c * 128:(c + 1) * 128], False, True, [KhT, v])
                for b2 in range(2):
                    em.op("act", lambda e, b2=b2: e.activation(out=o_[rs_, b2 * 512:(b2 + 1) * 512], in_=po[b2][rs_, :], func=AF.Copy), reads=[po[b2]], writes=[o_])
                em.op("dve", lambda e, cc=cc: e.tensor_tensor(out=stmp[:], in0=S[:].rearrange("p c a b -> p c (a b)"),
                                                              in1=GL[:, :, cc].unsqueeze(2).broadcast_to([128, 8, 128]), op=ALU.mult),
                      reads=[S, GL], writes=[stmp])
                for b2 in range(2):
                    em.op("dve", lambda e, b2=b2: e.tensor_tensor(out=t2m[:], in0=pS[b2][:, :].rearrange("p (c n) -> p c n", c=4),
                                                                  in1=self.cb(C_B64).unsqueeze(1).broadcast_to([128, 4, 128]), op=ALU.mult),
                          reads=[pS[b2], self.cmb], writes=[t2m])
                    em.op("pool", lambda e, b2=b2: e.tensor_tensor(out=S[:, b2 * 4:(b2 + 1) * 4, :, :].rearrange("p c a b -> p c (a b)"),
                                                                   in0=stmp[:, b2 * 4:(b2 + 1) * 4, :], in1=t2m[:], op=ALU.add),
                          reads=[stmp, t2m], writes=[S])
                em.op("act", lambda e: e.activation(out=Sb[:].rearrange("p c a b -> p (c a b)"), in_=S[:].rearrange("p c a b -> p (c a b)"), func=AF.Copy),
                      reads=[S], writes=[Sb])
            em.dma("pool", O.ap()[t * 128:(t + 1) * 128, 0:1024], o_[:], reads=[o_])
        em.barrier()
        em.release(list(ld.values()) + list(ex.values()) + [v, lw, GL, AR, BK, BKh, BhT, KhT, MASK4, AM, INV, INVT, IT, Tsb, RHSb, Ub, S, Sb, stmp, t2m] + tmpx + OT)


def rk_factory(self):
    em = self.em

    def factory(st):
        vecs = em.sb(st, "qvecs", [128, 32], F32)
        em.dma("sp", vecs[:], self.rk_in["vec"].ap(), writes=[vecs])
        oo = [em.sb(st, "qoo%d" % q, [128, 8, 64], F32) for q in range(2)]
        p1 = [em.sb(st, "qp1%d" % q, [128, 512], F32) for q in range(2)]
        bt = [em.sb(st, "qbt%d" % q, [128, 8, 128], BF16) for q in range(2)]
        gt = [em.sb(st, "qgt%d" % q, [128, 8, 128], BF16) for q in range(2)]
        osum = em.sb(st, "qosum", [128, 8, 64], F32)
        sq = em.sb(st, "qsq", [128, 8, 64], F32)
        ss = em.sb(st, "qss", [128, 8], F32)
        y = em.sb(st, "qy", [128, 1024], BF16)
        tt = em.sb(st, "qtt", [128, 128], F32)
        cnt = [0]
        f2 = lambda b: b[:].rearrange("p a b -> p (a b)")
        bc8 = lambda ap: ap.unsqueeze(2).broadcast_to([128, 8, 64])

        def make_yT(t, isctx, yT):
            pt = self.ptile(t)
            b_, g_ = bt[t % 2], gt[t % 2]
            em.dma("sp", b_[:], self.kBT.ap()[:, :, t * 128:(t + 1) * 128].rearrange("c p n -> p c n"), writes=[b_])
            em.dma("sp", g_[:], self.kGT.ap()[:, :, t * 128:(t + 1) * 128].rearrange("c p n -> p c n"), writes=[g_])
            for blk in range(2):
                k = cnt[0]
                cnt[0] += 1
                o_, a1 = oo[k % 2], p1[k % 2]
                cs = slice(blk * 512, (blk + 1) * 512)
                em.dma("sp", f2(o_), self.O2[0].ap()[t * 128:(t + 1) * 128, cs], writes=[o_])
                em.dma("sp", a1[:], self.O2[1].ap()[pt * 128:(pt + 1) * 128, cs], writes=[a1])
                ps = self.pn()
                self.mm(ps, ps[:, :], self.cf(C_J0), a1[:], True, True, [self.cmf, a1])
                em.op("dve", lambda e: e.tensor_tensor(out=f2(osum), in0=ps[:, :], in1=f2(o_), op=ALU.add), reads=[ps, o_], writes=[osum])
                em.op("dve", lambda e: e.tensor_reduce(out=ss[:], in_=osum[:], axis=AX.X, op=ALU.add), reads=[osum], writes=[ss])
                em.op("dve", lambda e: e.tensor_scalar(out=ss[:], in0=ss[:], scalar1=-1.0 / 64.0, scalar2=None, op0=ALU.mult), reads=[ss], writes=[ss])
                em.op("dve", lambda e: e.tensor_tensor(out=osum[:], in0=osum[:], in1=bc8(ss[:]), op=ALU.add), reads=[osum, ss], writes=[osum])
                em.op("pool", lambda e: e.tensor_tensor(out=sq[:], in0=osum[:], in1=osum[:], op=ALU.mult), reads=[osum], writes=[sq])
                em.op("dve", lambda e: e.tensor_reduce(out=ss[:], in_=sq[:], axis=AX.X, op=ALU.add), reads=[sq], writes=[ss])
                em.op("act", lambda e: e.activation(out=ss[:], in_=ss[:], func=AF.Sqrt, bias=64e-5, scale=1.0 / 64.0), reads=[ss], writes=[ss])
                em.op("dve", lambda e: e.reciprocal(out=ss[:], in_=ss[:]), reads=[ss], writes=[ss])
                em.op("dve", lambda e: e.tensor_tensor(out=y[:, cs].rearrange("p (a b) -> p a b", a=8), in0=osum[:], in1=bc8(ss[:]), op=ALU.mult),
                      reads=[osum, ss], writes=[y])
            ps = self.pn()
            pb = ps[:].bitcast(BF16)
            for c in range(8):
                em.op("pe", lambda e: e.transpose(out=pb[:, c * 128:(c + 1) * 128], in_=y[:, c * 128:(c + 1) * 128], identity=self.cb(C_ID)),
                      reads=[y, self.cmb], writes=[ps])
            for c in range(8):
                em.op("dve", lambda e: e.scalar_tensor_tensor(out=tt[:], in0=pb[:, c * 128:(c + 1) * 128], scalar=vecs[:, 24 + c:25 + c], in1=b_[:, c, :],
                                                              op0=ALU.mult, op1=ALU.add), reads=[ps, vecs, b_], writes=[tt])
                em.op("pool", lambda e: e.tensor_tensor(out=yT[:, c, :], in0=tt[:], in1=g_[:, c, :], op=ALU.mult), reads=[tt, g_], writes=[yT])
        return make_yT, [vecs, osum, sq, ss, y, tt] + oo + p1 + bt + gt
    return factory


def rk_layer(self, i, modLC, rows):
    if not hasattr(self, "kHT"):
        self.rk_alloc()
    import os
    dbg = int(os.environ.get("RKDBG", "99"))
    for z in range(2):
        self.rk_phase_a1(modLC, z)
        if dbg >= 2:
            self.rk_phase_a2(z)
        if dbg >= 3:
            self.rk_phase_s(z)
    if dbg >= 4:
        self.phase_f(i, modLC, rows, self.rk_factory(), "rkout", 8)


MK.rk_phase_s = rk_phase_s
MK.rk_factory = rk_factory
MK.rk_layer = rk_layer
```

```python
import numpy as np
from contextlib import ExitStack
import concourse.bass as bass
import concourse.mybir as mybir
from concourse.bass_utils import run_bass_kernel_spmd

F32 = mybir.dt.float32
BF16 = mybir.dt.bfloat16
AF = mybir.ActivationFunctionType
ALU = mybir.AluOpType
AX = mybir.AxisListType

D = 1024
NCTX = 256
ALPHA = 8.0 ** 0.25
LN_EPS = 1e-5


class Sem:
    __slots__ = ("h", "total")

    def __init__(self, h):
        self.h = h
        self.total = 0


class Buf:
    __slots__ = ("t", "w", "r", "sem", "name")

    def __init__(self, t, name):
        self.t = t
        self.w = None
        self.r = {}
        self.sem = None
        self.name = name

    def __getitem__(self, k):
        return self.t[k]


class Em:
    def __init__(self, nc):
        self.nc = nc
        self.eng = {"pe": nc.tensor, "act": nc.scalar, "dve": nc.vector, "pool": nc.gpsimd, "sp": nc.sync}
        self.semobj = {k: Sem(nc.alloc_semaphore(name="s_" + k)) for k in self.eng}
        self.waited = {k: {} for k in self.eng}
        self.dma_sems = []
        self.free_dma_sems = []
        self.bufs = []
        self.nins = 0

    def sb(self, stack, name, shape, dt):
        self.uid = getattr(self, "uid", 0) + 1
        t = stack.enter_context(self.nc.sbuf_tensor("sb%d_%s" % (self.uid, name), list(shape), dt))
        b = Buf(t, name)
        self.bufs.append(b)
        return b

    def ps(self, stack, name, shape=(128, 512), dt=F32):
        t = stack.enter_context(self.nc.psum_tensor("pp_" + name, list(shape), dt))
        b = Buf(t, name)
        self.bufs.append(b)
        return b

    def _dma_sem(self):
        if self.free_dma_sems:
            return self.free_dma_sems.pop()
        s = Sem(self.nc.alloc_semaphore(name="d%d" % len(self.dma_sems)))
        self.dma_sems.append(s)
        return s

    def release(self, bufs):
        for b in bufs:
            if b.sem is not None:
                self.free_dma_sems.append(b.sem)
                b.sem = None
            if b in self.bufs:
                self.bufs.remove(b)

    def _wait(self, en, toks):
        E = self.eng[en]
        w = self.waited[en]
        own = self.semobj[en]
        best = {}
        for (s, v) in toks:
            if s is own and en in ("pe", "sp"):
                continue
            if best.get(s, 0) < v:
                best[s] = v
        for s, v in best.items():
            if w.get(s, 0) < v:
                E.wait_ge(s.h, v)
                w[s] = v

    def _deps(self, reads, writes):
        toks = []
        for b in reads:
            if b.w is not None:
                toks.append(b.w)
        for b in writes:
            if b.w is not None:
                toks.append(b.w)
            toks.extend(b.r.items())
        return toks

    @staticmethod
    def _mark(tok, reads, writes):
        s, v = tok
        for b in reads:
            if b.r.get(s, 0) < v:
                b.r[s] = v
        for b in writes:
            b.w = tok
            b.r = {}

    def op(self, en, fn, reads=(), writes=()):
        self._wait(en, self._deps(reads, writes))
        ins = fn(self.eng[en])
        s = self.semobj[en]
        s.total += 1
        ins.then_inc(s.h, 1)
        self._mark((s, s.total), reads, writes)
        self.nins += 1

    def dma(self, qn, out, in_, reads=(), writes=(), sem=None):
        self._wait(qn, self._deps(reads, writes))
        ins = self.eng[qn].dma_start(out=out, in_=in_)
        if sem is None:
            b = writes[0] if writes else reads[0]
            if b.sem is None:
                b.sem = self._dma_sem()
            sem = b.sem
        sem.total += 16
        ins.then_inc(sem.h, 16)
        self._mark((sem, sem.total), reads, writes)
        self.nins += 1

    def barrier(self):
        allsems = list(self.semobj.values()) + self.dma_sems
        for en, E in self.eng.items():
            w = self.waited[en]
            for s in allsems:
                if s.total > 0 and w.get(s, 0) < s.total:
                    E.wait_ge(s.h, s.total)
                    w[s] = s.total
        for b in self.bufs:
            b.w = None
            b.r = {}


def bc(ap_t, offset_elems, dims):
    from concourse.ap import AP
    return AP(ap_t, offset_elems, dims)


C_ID, C_J0, C_J1, C_MRET, C_TRI, C_BLK, C_SEL0, C_SEL1, C_MS, C_MI = range(10)
C_TRIS, C_ONES, C_B64 = 10, 11, 12
C_LVT = 13
C_LV = 19
NCONST = 25
NF32 = 13


def make_consts(z):
    p = np.arange(128)[:, None]
    f = np.arange(128)[None, :]
    blk = (p // 64) == (f // 64)
    cm = np.zeros((NCONST, 128, 128), np.float32)
    cm[C_ID] = np.eye(128)
    J = np.eye(128)[::-1]
    cm[C_J0] = J
    cm[C_MRET] = (p <= f)
    cm[C_TRI] = (p <= f) & blk
    cm[C_TRIS] = (p < f) & blk
    cm[C_BLK] = blk
    cm[C_SEL0] = (p < 64) & (f >= 0)
    cm[C_SEL1] = (p >= 64) & (f >= 0)
    cm[C_MS] = (p < f) & blk
    cm[C_MI] = (p <= f) & blk
    for k in range(6):
        m = ((p >> (k + 1)) == (f >> (k + 1))) & (((p >> k) & 1) == 1) & (((f >> k) & 1) == 0)
        cm[C_LV + k] = m
        cm[C_LVT + k] = m.T
    cm[C_ONES] = 1.0
    cm[C_B64] = blk
    return cm


class MK:
    def __init__(self, L, layers, ncores=4, direct=True, layer_ids=None):
        self.layer_ids = list(range(layers)) if layer_ids is None else list(layer_ids)
        self.ncores = ncores
        self.direct = direct
        self.pairs = [[2 * p, 2 * p + 1] for p in range(max(1, ncores // 2))]
        self.L = L
        self.layers = layers
        self.NLT = L // 128
        self.NT = 2 + self.NLT
        self.T = 256 + L
        self.HT = 256 + L // 2
        self.groups = [(0, 2, True)] + [(2 + 4 * g, 4, False) for g in range(self.NLT // 4)]
        self.half_groups = self.groups
        self.nc = bass.Bass("TRN2", target_bir_lowering=False)
        self.em = Em(self.nc)
        self.ext = {}
        self.units = {}
        self.cc_sem = Sem(self.nc.alloc_semaphore(name="cc"))
        self.em.dma_sems.append(self.cc_sem)
        self.gsem = Sem(self.nc.alloc_semaphore(name="gdma"))
        self.em.dma_sems.append(self.gsem)

    def xin(self, name, shape, dt=F32):
        t = self.nc.dram_tensor(name, list(shape), dt, kind="ExternalInput")
        self.ext[name] = (tuple(shape), dt)
        return t

    def dr(self, name, shape, dt):
        return self.nc.dram_tensor(name, list(shape), dt, kind="Internal")

    def ptile(self, t):
        return 1 - t if t < 2 else 2 + (self.NLT - 1 - (t - 2))

    def unit(self, name, K, N):
        em = self.em
        g = self.dr("wg_" + name, [K, N], BF16)
        self.units[name] = (g, K, N)
        if self.direct:
            full = self.xin("wf_" + name, [K, N])
            em.dma("pool", g.ap(), full.ap(), sem=self.gsem)
            return None
        sh = self.xin("w_" + name, [K // 8, N])
        sb = self.dr("ws_" + name, [K // 8, N], BF16)
        gq = self.dr("wq_" + name, [K // 2, N], BF16)
        em.dma("pool", sb.ap(), sh.ap(), sem=self.gsem)
        return (sb, gq, g)

    def gather_units(self, pend):
        em = self.em
        em.barrier()
        if self.direct:
            return
        for stage in range(2):
            for sb, gq, g in pend:
                rg = [[0, 1, 2, 3], [4, 5, 6, 7]] if stage == 0 else [[0, 4], [1, 5], [2, 6], [3, 7]]
                src, dst = (sb, gq) if stage == 0 else (gq, g)
                ins = self.nc.gpsimd.collective_compute("AllGather", ALU.bypass, replica_groups=rg,
                                                        ins=[src.ap()], outs=[dst.ap()])
                self.cc_sem.total += 1
                ins.then_inc(self.cc_sem.h, 1)
                self.nc.gpsimd.wait_ge(self.cc_sem.h, self.cc_sem.total)
            em.barrier()

    def pair_gather(self, src, dst):
        em = self.em
        em.barrier()
        ins = self.nc.gpsimd.collective_compute("AllGather", ALU.bypass, replica_groups=self.pairs,
                                                ins=[src.ap()], outs=[dst.ap()])
        self.cc_sem.total += 1
        ins.then_inc(self.cc_sem.h, 1)
        em.barrier()

    def wsrc(self, name, kc0, nkc, n0, nw):
        g, K, N = self.units[name]
        return g.ap()[kc0 * 128:(kc0 + nkc) * 128, n0:n0 + nw].rearrange("(c p) n -> p c n", p=128)

    def load_w(self, buf, name, kc0, nkc, n0, nw):
        self.em.dma("sp", buf[:, 0:nkc, 0:nw], self.wsrc(name, kc0, nkc, n0, nw), writes=[buf])

    def setup(self):
        nc, em = self.nc, self.em
        self.pst = ExitStack()
        st = self.pst
        self.cmf = em.sb(st, "cmf", [128, NF32, 128], F32)
        self.cmb = em.sb(st, "cmb", [128, NCONST, 128], BF16)
        cm = self.xin("cm", [128, NCONST, 128])
        em.dma("sp", self.cmf[:], cm.ap()[:, 0:NF32, :], writes=[self.cmf])
        em.dma("pool", self.cmb[:], cm.ap(), writes=[self.cmb])
        self.iota = em.sb(st, "iota", [128, 4], F32)
        em.dma("sp", self.iota[:], self.xin("iota", [128, 4]).ap(), writes=[self.iota])
        self.psb = [em.ps(st, "ps%d" % i) for i in range(8)]
        self.psk = 0
        self.xs = self.xin("xs", [self.T, D])
        self.cvec = self.xin("cvec", [128, 16])
        self.modb = self.xin("modb", [4, 128, 48])
        self.modbf = self.xin("modbf", [4, 6144])
        self.lng = self.xin("lng", [4, 2, D])
        self.lnb = self.xin("lnb", [4, 2, D])
        self.xsf = self.xin("xsf", [self.T, D])
        self.LAT2 = [self.dr("LAT0", [self.T, D], F32), self.dr("LAT1", [self.T, D], F32)]
        self.O2 = [self.dr("O0", [self.T, 2048], F32), self.dr("O1", [self.T, 2048], F32)]
        self.H2T = self.dr("H2T", [len(self.half_groups), 128, 8 * 512], BF16)
        em.dma("pool", self.LAT2[0].ap(), self.xs.ap(), sem=self.gsem)
        em.dma("pool", self.LAT2[1].ap(), self.xsf.ap(), sem=self.gsem)
        self.OUT = self.nc.dram_tensor("out", [self.T, D], F32, kind="ExternalOutput")
        self.flt = [em.sb(st, "flt%d" % q, [128, D], F32) for q in range(2)]
        self.fltk = 0

    def cf(self, i):
        return self.cmf[:, i, :]

    def store_lat(self, t, ou, out=False):
        em = self.em
        em.dma("pool", self.LAT2[0].ap()[t * 128:(t + 1) * 128, :], ou[:], reads=[ou])
        if out:
            em.dma("pool", self.OUT.ap()[t * 128:(t + 1) * 128, :], ou[:], reads=[ou])
        fl = self.flt[self.fltk % 2]
        self.fltk += 1
        for half in range(2):
            ps = self.pn()
            self.mm(ps, ps[:, :], self.cf(C_J0), ou[:, half * 512:(half + 1) * 512], True, True, [self.cmf, ou])
            if half:
                em.op("act", lambda e, ps=ps, fl=fl: e.activation(out=fl[:, 512:1024], in_=ps[:, :], func=AF.Copy), reads=[ps], writes=[fl])
            else:
                em.op("dve", lambda e, ps=ps, fl=fl: e.tensor_copy(out=fl[:, 0:512], in_=ps[:, :]), reads=[ps], writes=[fl])
        pt = self.ptile(t)
        em.dma("pool", self.LAT2[1].ap()[pt * 128:(pt + 1) * 128, :], fl[:], reads=[fl])

    def cb(self, i):
        return self.cmb[:, i, :]

    def pn(self):
        while True:
            b = self.psb[self.psk % 8]
            self.psk += 1
            if b not in getattr(self, "reserved", ()):
                return b

    def mm(self, ps, out, lhsT, rhs, start, stop, reads):
        self.em.op("pe", lambda e: e.matmul(out, lhsT=lhsT, rhs=rhs, start=start, stop=stop), reads=reads, writes=[ps])

    def phase_mod(self, i, st):
        em = self.em
        modLC = em.sb(st, "modLC%d" % i, [128, 48, 2], F32)
        rows = {n: em.sb(st, "%s_%d" % (n, i), [128, D], F32)
                for n in ("GA1L", "GA1C", "GA2L", "GA2C", "LNG0", "LNB0", "LNG1", "LNB1")}
        import os
        for s in range(2 if os.environ.get("MKA", "0") == "0" else 0):
            em.dma("sp", rows["LNG%d" % s][:], self.lng.ap()[i, s:s + 1, :].partition_broadcast(128), writes=[rows["LNG%d" % s]])
            em.dma("sp", rows["LNB%d" % s][:], self.lnb.ap()[i, s:s + 1, :].partition_broadcast(128), writes=[rows["LNB%d" % s]])
        import os
        dbg = int(os.environ.get("MKDBG", "99"))
        if dbg == 0:
            em.barrier()
            return modLC, rows
        with ExitStack() as ts:
            cv = em.sb(ts, "cv", [128, 16], F32)
            em.dma("sp", cv[:], self.cvec.ap(), writes=[cv])
            sT = em.sb(ts, "sT", [128, 8, 2], BF16)
            em.op("act", lambda e: e.activation(out=sT[:].rearrange("p c t -> p (c t)"), in_=cv[:], func=AF.Silu),
                  reads=[cv], writes=[sT])
            sBC = em.sb(ts, "sBC", [128, 8, 2, 128], BF16)
            em.op("dve", lambda e: e.tensor_copy(out=sBC[:], in_=sT[:].unsqueeze(3).broadcast_to([128, 8, 2, 128])),
                  reads=[sT], writes=[sBC])
            mb = em.sb(ts, "mb", [128, 48], F32)
            em.dma("sp", mb[:], self.modb.ap()[i], writes=[mb])
            brow = [em.sb(ts, "brow%d" % q, [128, D], F32) for q in range(2)]
            em.dma("sp", brow[0][:], self.modbf.ap()[i:i + 1, 2048:3072].partition_broadcast(128), writes=[brow[0]])
            em.dma("sp", brow[1][:], self.modbf.ap()[i:i + 1, 5120:6144].partition_broadcast(128), writes=[brow[1]])
            wb = [em.sb(ts, "wm%d" % q, [128, 8, 1536], BF16) for q in range(2)]
            psm = self.pn()
            self.reserved = [psm]
            if dbg == 1:
                em.barrier()
                return modLC, rows
            for piece in range(4):
                w = wb[piece % 2]
                self.load_w(w, "modw%d" % i, 0, 8, piece * 1536, 1536)
                if dbg == 2:
                    continue
                for oc in range(12):
                    g = piece * 12 + oc
                    for kc in range(8):
                        self.mm(psm, psm[:, g * 2:g * 2 + 2], w[:, kc, oc * 128:(oc + 1) * 128], sT[:, kc, :],
                                kc == 0, kc == 7, [w, sT])
                if piece in (1, 3):
                    for t, nm in ((0, "L"), (1, "C")):
                        dest = rows[("GA1" if piece == 1 else "GA2") + nm]
                        br = brow[0 if piece == 1 else 1]
                        for half in range(2):
                            pb = self.pn()
                            for kc in range(8):
                                self.mm(pb, pb[:, :], sBC[:, kc, t, :], w[:, kc, 512 + half * 512:1024 + half * 512],
                                        kc == 0, kc == 7, [w, sBC])
                            em.op("dve", lambda e, pb=pb, dest=dest, br=br, half=half: e.scalar_tensor_tensor(
                                out=dest[:, half * 512:(half + 1) * 512], in0=pb[:, :], scalar=1.0,
                                in1=br[:, half * 512:(half + 1) * 512], op0=ALU.add, op1=ALU.add),
                                reads=[pb, br], writes=[dest])
            em.op("dve", lambda e: e.tensor_tensor(out=modLC[:], in0=psm[:, 0:96].rearrange("p (c t) -> p c t", t=2),
                                                   in1=mb[:].unsqueeze(2).broadcast_to([128, 48, 2]), op=ALU.add),
                  reads=[psm, mb], writes=[modLC])
            self.reserved = []
            for c0 in (8, 32):
                em.op("dve", lambda e, c0=c0: e.tensor_scalar_add(out=modLC[:, c0:c0 + 8, :], in0=modLC[:, c0:c0 + 8, :], scalar1=1.0),
                      reads=[modLC], writes=[modLC])
            em.barrier()
            em.release([cv, sT, sBC, mb] + brow + wb)
        return modLC, rows

    def hT_tile(self, lat_tile, hT, col0, modLC, sub, isctx):
        em = self.em
        t = 1 if isctx else 0
        sh0, sc0 = (0, 8) if sub == 0 else (24, 32)
        for half in range(2):
            ps = self.pn()
            for c4 in range(4):
                c = half * 4 + c4
                em.op("pe", lambda e, ps=ps, c=c, c4=c4: e.transpose(out=ps[:, c4 * 128:(c4 + 1) * 128],
                                                                     in_=lat_tile[:, c * 128:(c + 1) * 128],
                                                                     identity=self.cf(C_ID)),
                      reads=[lat_tile, self.cmf], writes=[ps])
            for c4 in range(4):
                c = half * 4 + c4
                em.op("act", lambda e, ps=ps, c=c, c4=c4: e.activation(
                    out=hT[:, c, col0:col0 + 128], in_=ps[:, c4 * 128:(c4 + 1) * 128], func=AF.Identity,
                    scale=modLC[:, sc0 + c, t:t + 1], bias=modLC[:, sh0 + c, t:t + 1]),
                    reads=[ps, modLC], writes=[hT])

    def tail(self, pso, lat_tile, GA, LNG, LNB, tmp, outt, st6, mv, rs):
        em = self.em
        for half in range(2):
            sl = slice(half * 512, (half + 1) * 512)
            sb_, sap = pso[half] if isinstance(pso[half], tuple) else (pso[half], pso[half][:, :])
            em.op("dve", lambda e, sap=sap, sl=sl: e.tensor_tensor(out=tmp[:, sl], in0=sap, in1=GA[:, sl], op=ALU.mult),
                  reads=[sb_, GA], writes=[tmp])
        em.op("dve", lambda e: e.scalar_tensor_tensor(out=tmp[:], in0=lat_tile[:], scalar=ALPHA, in1=tmp[:], op0=ALU.mult, op1=ALU.add),
              reads=[lat_tile, tmp], writes=[tmp])
        self.layer_norm(tmp, LNG, LNB, outt, st6, mv, rs)

    def layer_norm(self, tmp, LNG, LNB, outt, st6, mv, rs):
        em = self.em
        for half in range(2):
            em.op("dve", lambda e, half=half: e.bn_stats(out=st6[:, half, :], in_=tmp[:, half * 512:(half + 1) * 512]),
                  reads=[tmp], writes=[st6])
        em.op("dve", lambda e: e.bn_aggr(out=mv[:], in_=st6[:]), reads=[st6], writes=[mv])
        em.op("act", lambda e: e.activation(out=rs[:], in_=mv[:, 1:2], func=AF.Sqrt, bias=LN_EPS, scale=1.0), reads=[mv], writes=[rs])
        em.op("dve", lambda e: e.reciprocal(out=rs[:], in_=rs[:]), reads=[rs], writes=[rs])
        em.op("dve", lambda e: e.tensor_scalar(out=tmp[:], in0=tmp[:], scalar1=mv[:, 0:1], scalar2=rs[:, 0:1],
                                               op0=ALU.subtract, op1=ALU.mult), reads=[tmp, mv, rs], writes=[tmp])
        em.op("pool", lambda e: e.tensor_tensor(out=tmp[:], in0=tmp[:], in1=LNG[:], op=ALU.mult), reads=[tmp, LNG], writes=[tmp])
        em.op("pool", lambda e: e.tensor_tensor(out=outt[:], in0=tmp[:], in1=LNB[:], op=ALU.add), reads=[tmp, LNB], writes=[outt])

    def phase_f(self, i, modLC, rows, factory, wout_name, KC):
        em = self.em
        with ExitStack() as st:
            make_yT, mbufs = factory(st)
            wout = em.sb(st, "wout", [128, KC, D], BF16)
            self.load_w(wout, wout_name, 0, KC, 0, D)
            yT = [em.sb(st, "yT%d" % q, [128, KC, 128], BF16) for q in range(2)]
            lat = [em.sb(st, "flat%d" % q, [128, D], F32) for q in range(2)]
            outt = [em.sb(st, "fout%d" % q, [128, D], F32) for q in range(2)]
            tmp = em.sb(st, "ftmp", [128, D], F32)
            st6 = em.sb(st, "fst6", [128, 2, 6], F32)
            mv = em.sb(st, "fmv", [128, 2], F32)
            rs = em.sb(st, "frs", [128, 1], F32)
            h2g = [em.sb(st, "h2g%d" % q, [128, 8, 512], BF16) for q in range(2)]
            k = 0
            for gi, (t0, ng, isctx) in enumerate(self.groups):
                own = gi < len(self.half_groups)
                sfx = "C" if isctx else "L"
                for ti in range(ng):
                    t = t0 + ti
                    y = yT[k % 2]
                    la = lat[k % 2]
                    ou = outt[k % 2]
                    k += 1
                    make_yT(t, isctx, y)
                    em.dma("sp", la[:], self.LAT2[0].ap()[t * 128:(t + 1) * 128, :], writes=[la])
                    pso = [self.pn(), self.pn()]
                    for half in range(2):
                        for kc in range(KC):
                            self.mm(pso[half], pso[half][:, :], y[:, kc, :], wout[:, kc, half * 512:(half + 1) * 512],
                                    kc == 0, kc == KC - 1, [y, wout])
                    self.tail(pso, la, rows["GA1" + sfx], rows["LNG0"], rows["LNB0"], tmp, ou, st6, mv, rs)
                    self.store_lat(t, ou)
                    if own:
                        self.hT_tile(ou, h2g[gi % 2], ti * 128, modLC, 1, isctx)
                if own:
                    em.dma("pool", self.H2T.ap()[gi].rearrange("p (c n) -> p c n", c=8), h2g[gi % 2][:], reads=[h2g[gi % 2]])
            em.barrier()
            em.release([wout, tmp, st6, mv, rs] + yT + lat + outt + h2g + mbufs)

    def ffn_tail_bufs(self, st):
        em = self.em
        d = dict(lat=[em.sb(st, "glat%d" % q, [128, D], F32) for q in range(2)],
                 outt=[em.sb(st, "gout%d" % q, [128, D], F32) for q in range(2)],
                 tmp=em.sb(st, "gtmp", [128, D], F32), st6=em.sb(st, "gst6", [128, 2, 6], F32),
                 mv=em.sb(st, "gmv", [128, 2], F32), rs=em.sb(st, "grs", [128, 1], F32))
        return d

    def ffn_store(self, t, ou):
        self.store_lat(t, ou, out=True)

    def ffn_dense(self, li, modLC, rows):
        em = self.em
        with ExitStack() as st:
            wd = em.sb(st, "wd", [128, 22, D], BF16)
            self.load_w(wd, "ffndn%d" % li, 0, 22, 0, D)
            wg = [em.sb(st, "wg%d" % q, [128, 8, 256], BF16) for q in range(2)]
            wu = [em.sb(st, "wu%d" % q, [128, 8, 256], BF16) for q in range(2)]
            h2 = [em.sb(st, "h2_%d" % q, [128, 8, 512], BF16) for q in range(2)]
            act = em.sb(st, "act", [128, 22, 512], BF16)
            sg = [em.sb(st, "sg%d" % q, [128, 512], F32) for q in range(2)]
            B = self.ffn_tail_bufs(st)
            k = 0
            for gi, (t0, ng, isctx) in enumerate(self.half_groups):
                N = ng * 128
                h = h2[gi % 2]
                em.dma("sp", h[:], self.H2T.ap()[gi].rearrange("p (c n) -> p c n", c=8), writes=[h])
                for s in range(11):
                    g_, u_ = wg[s % 2], wu[s % 2]
                    self.load_w(g_, "ffngu%d" % li, 0, 8, s * 256, 256)
                    self.load_w(u_, "ffngu%d" % li, 0, 8, 2816 + s * 256, 256)
                    for fc in range(2):
                        f = s * 2 + fc
                        pg, pu = self.pn(), self.pn()
                        for kc in range(8):
                            self.mm(pg, pg[:, :N], g_[:, kc, fc * 128:(fc + 1) * 128], h[:, kc, :N], kc == 0, kc == 7, [g_, h])
                        for kc in range(8):
                            self.mm(pu, pu[:, :N], u_[:, kc, fc * 128:(fc + 1) * 128], h[:, kc, :N], kc == 0, kc == 7, [u_, h])
                        s_ = sg[f % 2]
                        em.op("act", lambda e, pg=pg, s_=s_: e.activation(out=s_[:, :N], in_=pg[:, :N], func=AF.Silu), reads=[pg], writes=[s_])
                        em.op("dve", lambda e, pu=pu, s_=s_, f=f: e.tensor_tensor(out=act[:, f, :N], in0=s_[:, :N], in1=pu[:, :N], op=ALU.mult),
                              reads=[pu, s_], writes=[act])
                sfx = "C" if isctx else "L"
                for ti in range(ng):
                    t = t0 + ti
                    la, ou = B["lat"][k % 2], B["outt"][k % 2]
                    k += 1
                    em.dma("sp", la[:], self.LAT2[0].ap()[t * 128:(t + 1) * 128, :], writes=[la])
                    pso = [self.pn(), self.pn()]
                    for half in range(2):
                        for f in range(22):
                            self.mm(pso[half], pso[half][:, :], act[:, f, ti * 128:(ti + 1) * 128], wd[:, f, half * 512:(half + 1) * 512],
                                    f == 0, f == 21, [act, wd])
                    self.tail(pso, la, rows["GA2" + sfx], rows["LNG1"], rows["LNB1"], B["tmp"], ou, B["st6"], B["mv"], B["rs"])
                    self.ffn_store(t, ou)
            em.barrier()
            em.release([wd, act] + wg + wu + h2 + sg + B["lat"] + B["outt"] + [B["tmp"], B["st6"], B["mv"], B["rs"]])

    def phase_h(self):
        em = self.em
        self.pair_gather(self.FX, self.FXG)
        with ExitStack() as st:
            a = [[em.sb(st, "ha%d_%d" % (s, q), [128, D], F32) for s in range(2)] for q in range(2)]
            ou = [em.sb(st, "ho%d" % q, [128, D], F32) for q in range(2)]
            k = 0
            for t in range(2 + self.NLT // 2, self.NT):
                pt = self.ptile(t)
                aa, o_ = a[k % 2], ou[k % 2]
                k += 1
                for s in range(2):
                    em.dma("sp", aa[s][:], self.FXG.ap()[s * self.HT + pt * 128:s * self.HT + (pt + 1) * 128, :], writes=[aa[s]])
                for half in range(2):
                    ps = self.pn()
                    for s in range(2):
                        self.mm(ps, ps[:, :], self.cf(C_J0 + s), aa[s][:, half * 512:(half + 1) * 512], s == 0, s == 1, [self.cmf, aa[s]])
                    em.op("act" if half else "dve", lambda e, ps=ps, half=half, o_=o_: (
                        e.activation(out=o_[:, half * 512:(half + 1) * 512], in_=ps[:, :], func=AF.Copy) if half else
                        e.tensor_copy(out=o_[:, half * 512:(half + 1) * 512], in_=ps[:, :])), reads=[ps], writes=[o_])
                em.dma("pool", self.LAT.ap()[t * 128:(t + 1) * 128, :], o_[:], reads=[o_])
            em.barrier()
            em.release(a[0] + a[1] + ou)

    def ret_alloc(self):
        NT = self.NT
        self.rQT2 = [self.dr("rQT%d" % z, [NT, 128, 1024], BF16) for z in range(2)]
        self.rKT2 = [self.dr("rKT%d" % z, [NT, 128, 1024], BF16) for z in range(2)]
        self.rKTM2 = [self.dr("rKTM%d" % z, [NT, 128, 1024], BF16) for z in range(2)]
        self.rV2 = [self.dr("rV%d" % z, [NT, 128, 2048], BF16) for z in range(2)]
        self.rG = self.dr("rG", [NT, 128, 2048], BF16)
        self.rope2 = [self.xin("rope%d" % z, [NT, 128, 256]) for z in range(2)]

    def ret_phase_a(self, j, modLC, z):
        em = self.em
        self.LAT, self.rope = self.LAT2[z], self.rope2[z]
        self.rQT, self.rKT, self.rKTM, self.rV = self.rQT2[z], self.rKT2[z], self.rKTM2[z], self.rV2[z]
        with ExitStack() as st:
            wb = [em.sb(st, "rw%d" % q, [128, 8, 512], BF16) for q in range(2)]
            hT = [em.sb(st, "rhT%d" % q, [128, 8, 512], BF16) for q in range(2)]
            lat = [em.sb(st, "rlat%d" % q, [128, D], F32) for q in range(2)]
            qr = [em.sb(st, "rqr%d" % q, [128, 1024], BF16) for q in range(4)]
            kr = [em.sb(st, "rkr%d" % q, [128, 1024], BF16) for q in range(4)]
            vv = [em.sb(st, "rvv%d" % q, [128, 2048], BF16) for q in range(4)]
            gg = [em.sb(st, "rgg%d" % q, [128, 2048], BF16) for q in range(4)]
            rp = [em.sb(st, "rrp%d" % q, [128, 256], F32) for q in range(4)]
            cos4 = [em.sb(st, "rcos%d" % q, [128, 4, 64], F32) for q in range(4)]
            sin4 = [em.sb(st, "rsin%d" % q, [128, 4, 64], F32) for q in range(4)]
            xf = [em.sb(st, "rxf%d" % q, [128, 512], F32) for q in range(2)]
            t1 = [em.sb(st, "rt1%d" % q, [128, 512], F32) for q in range(2)]
            t2 = [em.sb(st, "rt2%d" % q, [128, 4, 64], F32) for q in range(2)]
            t3 = [em.sb(st, "rt3%d" % q, [128, 4, 64], F32) for q in range(2)]
            qT = [em.sb(st, "rqT%d" % q, [128, 1024], BF16) for q in range(2)]
            kk = 0
            rk = 0
            for gi, (t0, ng, isctx) in enumerate(self.groups):
                h = hT[gi % 2]
                for ti in range(ng):
                    la = lat[kk % 2]
                    kk += 1
                    em.dma("sp", la[:], self.LAT.ap()[(t0 + ti) * 128:(t0 + ti + 1) * 128, :], writes=[la])
                    self.hT_tile(la, h, ti * 128, modLC, 0, isctx)
                    em.dma("sp", rp[ti][:], self.rope.ap()[t0 + ti], writes=[rp[ti]])
                    em.op("pool", lambda e, ti=ti: e.tensor_copy(out=cos4[ti][:].rearrange("p (a b) f -> p a (b f)", a=2),
                                                                 in_=rp[ti][:, 0:128].unsqueeze(1).broadcast_to([128, 2, 128])),
                          reads=[rp[ti]], writes=[cos4[ti]])
                    em.op("pool", lambda e, ti=ti: e.tensor_copy(out=sin4[ti][:].rearrange("p (a b) f -> p a (b f)", a=2),
                                                                 in_=rp[ti][:, 128:256].unsqueeze(1).broadcast_to([128, 2, 128])),
                          reads=[rp[ti]], writes=[sin4[ti]])
                for nb in range(12 if z == 0 else 8):
                    w = wb[nb % 2]
                    self.load_w(w, "retin%d" % j, 0, 8, nb * 512, 512)
                    for ti in range(ng):
                        ps = self.pn()
                        for kc in range(8):
                            self.mm(ps, ps[:, :], h[:, kc, ti * 128:(ti + 1) * 128], w[:, kc, :], kc == 0, kc == 7, [h, w])
                        if nb < 4:
                            dest = (qr if nb < 2 else kr)[ti]
                            dsl = dest[:, (nb % 2) * 512:(nb % 2 + 1) * 512].rearrange("p (a s f) -> p a s f", a=4, s=2)
                            x_, t1_, t2_, t3_ = xf[rk % 2], t1[rk % 2], t2[rk % 2], t3[rk % 2]
                            rk += 1
                            sc = 1.0 if nb < 2 else 1.0 / 16.0
                            em.op("act", lambda e, ps=ps, x_=x_, sc=sc: e.activation(out=x_[:], in_=ps[:, :], func=AF.Copy, scale=sc),
                                  reads=[ps], writes=[x_])
                            X = x_[:].rearrange("p (a s f) -> p a s f", a=4, s=2)
                            T1 = t1_[:].rearrange("p (a s f) -> p a s f", a=4, s=2)
                            em.op("dve", lambda e, X=X, T1=T1, ti=ti: e.tensor_tensor(
                                out=T1, in0=X, in1=cos4[ti][:].unsqueeze(2).broadcast_to([128, 4, 2, 64]), op=ALU.mult),
                                reads=[x_, cos4[ti]], writes=[t1_])
                            em.op("pool", lambda e, X=X, t2_=t2_, ti=ti: e.tensor_tensor(out=t2_[:], in0=X[:, :, 1, :], in1=sin4[ti][:], op=ALU.mult),
                                  reads=[x_, sin4[ti]], writes=[t2_])
                            em.op("pool", lambda e, X=X, t3_=t3_, ti=ti: e.tensor_tensor(out=t3_[:], in0=X[:, :, 0, :], in1=sin4[ti][:], op=ALU.mult),
                                  reads=[x_, sin4[ti]], writes=[t3_])
                            em.op("dve", lambda e, dsl=dsl, T1=T1, t2_=t2_: e.tensor_tensor(out=dsl[:, :, 0, :], in0=T1[:, :, 0, :], in1=t2_[:], op=ALU.subtract),
                                  reads=[t1_, t2_], writes=[dest])
                            em.op("dve", lambda e, dsl=dsl, T1=T1, t3_=t3_: e.tensor_tensor(out=dsl[:, :, 1, :], in0=T1[:, :, 1, :], in1=t3_[:], op=ALU.add),
                                  reads=[t1_, t3_], writes=[dest])
                        else:
                            dest = (vv if nb < 8 else gg)[ti]
                            c0 = (nb % 4) * 512
                            if (nb + ti) % 2:
                                em.op("act", lambda e, ps=ps, dest=dest, c0=c0: e.activation(out=dest[:, c0:c0 + 512], in_=ps[:, :], func=AF.Copy),
                                      reads=[ps], writes=[dest])
                            else:
                                em.op("dve", lambda e, ps=ps, dest=dest, c0=c0: e.tensor_copy(out=dest[:, c0:c0 + 512], in_=ps[:, :]),
                                      reads=[ps], writes=[dest])
                for ti in range(ng):
                    t = t0 + ti
                    em.dma("pool", self.rKTM.ap()[t], kr[ti][:], reads=[kr[ti]])
                    em.dma("pool", self.rV.ap()[t], vv[ti][:], reads=[vv[ti]])
                    if z == 0:
                        em.dma("pool", self.rG.ap()[t], gg[ti][:], reads=[gg[ti]])
                    for src, dstT in ((qr[ti], self.rQT), (kr[ti], self.rKT)):
                        ps = self.pn()
                        pb = ps[:].bitcast(BF16)
                        for c in range(8):
                            em.op("pe", lambda e, pb=pb, src=src, c=c: e.transpose(out=pb[:, c * 128:(c + 1) * 128], in_=src[:, c * 128:(c + 1) * 128],
                                                                                 identity=self.cb(C_ID)), reads=[src, self.cmb], writes=[ps])
                        q_ = qT[kk % 2]
                        kk += 1
                        em.op("act", lambda e, pb=pb, q_=q_: e.activation(out=q_[:], in_=pb[:, 0:1024], func=AF.Copy), reads=[ps], writes=[q_])
                        em.dma("pool", dstT.ap()[t], q_[:], reads=[q_])
            em.barrier()
            em.release(wb + hT + lat + qr + kr + vv + gg + rp + cos4 + sin4 + xf + t1 + t2 + t3 + qT)

    def ret_phase_s(self, j, z):
        em = self.em
        dec_in = self.xin("retdec%d_%d" % (j, z), [1, 4])
        self.rQT, self.rKT, self.rKTM, self.rV, self.O = self.rQT2[z], self.rKT2[z], self.rKTM2[z], self.rV2[z], self.O2[z]
        with ExitStack() as st:
            dec = em.sb(st, "sdec", [128, 4], F32)
            em.dma("sp", dec[:], dec_in.ap().partition_broadcast(128), writes=[dec])
            lsp = em.sb(st, "slsp", [128, 4], F32)
            em.op("act", lambda e: e.activation(out=lsp[:], in_=dec[:], func=AF.Exp, scale=-1.0), reads=[dec], writes=[lsp])
            em.op("act", lambda e: e.activation(out=lsp[:], in_=lsp[:], func=AF.Ln, bias=1.0, scale=1.0), reads=[lsp], writes=[lsp])
            outsc = em.sb(st, "soutsc", [128, 4], F32)
            scsc = em.sb(st, "sscsc", [128, 4], F32)
            kdec = em.sb(st, "skdec", [128, 4], F32)
            gC = em.sb(st, "sgC", [128, 4], F32)
            io = self.iota
            em.op("act", lambda e: e.activation(out=outsc[:], in_=lsp[:], func=AF.Exp, scale=io[:, 1:2]), reads=[lsp, io], writes=[outsc])
            em.op("act", lambda e: e.activation(out=scsc[:], in_=lsp[:], func=AF.Exp, scale=io[:, 0:1]), reads=[lsp, io], writes=[scsc])
            em.op("act", lambda e: e.activation(out=kdec[:], in_=lsp[:], func=AF.Exp, scale=io[:, 3:4]), reads=[lsp, io], writes=[kdec])
            em.op("act", lambda e: e.activation(out=gC[:], in_=lsp[:], func=AF.Exp, scale=-128.0), reads=[lsp], writes=[gC])
            S = [[em.sb(st, "sS%d_%d" % (h, dc), [128, 512], F32) for dc in range(2)] for h in range(4)]
            Sb = [[em.sb(st, "sSb%d_%d" % (h, dc), [128, 512], BF16) for dc in range(2)] for h in range(4)]
            for h in range(4):
                for dc in range(2):
                    em.op("pool", lambda e, h=h, dc=dc: e.memset(S[h][dc][:], 0.0), writes=[S[h][dc]])
                    em.op("pool", lambda e, h=h, dc=dc: e.memset(Sb[h][dc][:], 0.0), writes=[Sb[h][dc]])
            qt = [em.sb(st, "sqt%d" % q, [128, 8, 128], BF16) for q in range(2)]
            kt = [em.sb(st, "skt%d" % q, [128, 8, 128], BF16) for q in range(2)]
            ktm = [em.sb(st, "sktm%d" % q, [128, 1024], BF16) for q in range(2)]
            v = [em.sb(st, "sv%d" % q, [128, 2048], BF16) for q in range(2)]
            ot = [em.sb(st, "sot%d" % q, [128, 2048], F32) for q in range(2)]
            scb = [em.sb(st, "sscb%d" % q, [128, 128], BF16) for q in range(2)]
            kd = [em.sb(st, "skd%d" % q, [128, 256], BF16) for q in range(2)]
            n = 0
            for t in range(self.NT):
                q_, k_, km_, v_, o_ = qt[t % 2], kt[t % 2], ktm[t % 2], v[t % 2], ot[t % 2]
                em.dma("sp", q_[:], self.rQT.ap()[t].rearrange("p (c n) -> p c n", c=8), writes=[q_])
                em.dma("sp", k_[:], self.rKT.ap()[t].rearrange("p (c n) -> p c n", c=8), writes=[k_])
                em.dma("sp", km_[:], self.rKTM.ap()[t], writes=[km_])
                em.dma("sp", v_[:], self.rV.ap()[t], writes=[v_])
                for h in range(4):
                    sc_, kd_ = scb[n % 2], kd[n % 2]
                    n += 1
                    ps = self.pn()
                    for dc in range(2):
                        self.mm(ps, ps[:, 0:128], k_[:, 2 * h + dc, :], q_[:, 2 * h + dc, :], dc == 0, dc == 1, [k_, q_])
                    em.op("dve", lambda e, ps=ps, sc_=sc_, h=h: e.scalar_tensor_tensor(
                        out=sc_[:], in0=ps[:, 0:128], scalar=scsc[:, h:h + 1], in1=self.cf(C_MRET), op0=ALU.mult, op1=ALU.mult),
                        reads=[ps, scsc, self.cmf], writes=[sc_])
                    po = self.pn()
                    vh = v_[:, h * 512:(h + 1) * 512]
                    self.mm(po, po[:, :], sc_[:], vh, True, False, [sc_, v_])
                    for dc in range(2):
                        self.mm(po, po[:, :], q_[:, 2 * h + dc, :], Sb[h][dc][:], False, dc == 1, [q_, Sb[h][dc]])
                    em.op("act", lambda e, po=po, o_=o_, h=h: e.activation(out=o_[:, h * 512:(h + 1) * 512], in_=po[:, :], func=AF.Identity,
                                                                        scale=outsc[:, h:h + 1]), reads=[po, outsc], writes=[o_])
                    em.op("pool", lambda e, kd_=kd_, km_=km_, h=h: e.tensor_scalar(out=kd_[:], in0=km_[:, h * 256:(h + 1) * 256],
                                                                               scalar1=kdec[:, h:h + 1], scalar2=None, op0=ALU.mult),
                          reads=[km_, kdec], writes=[kd_])
                    for dc in range(2):
                        pu = self.pn()
                        self.mm(pu, pu[:, :], kd_[:, dc * 128:(dc + 1) * 128], vh, True, True, [kd_, v_])
                        S_, Sb_ = S[h][dc], Sb[h][dc]
                        em.op("dve", lambda e, pu=pu, S_=S_, h=h: e.scalar_tensor_tensor(
                            out=S_[:], in0=S_[:], scalar=gC[:, h:h + 1], in1=pu[:, :], op0=ALU.mult, op1=ALU.add),
                            reads=[pu, S_, gC], writes=[S_])
                        em.op("act", lambda e, S_=S_, Sb_=Sb_: e.activation(out=Sb_[:], in_=S_[:], func=AF.Copy), reads=[S_], writes=[Sb_])
                em.dma("pool", self.O.ap()[t * 128:(t + 1) * 128, :], o_[:], reads=[o_])
            em.barrier()
            em.release([dec, lsp, outsc, scsc, kdec, gC] + sum(S, []) + sum(Sb, []) + qt + kt + ktm + v + ot + scb + kd)

    def ret_factory(self, j):
        em = self.em
        gn_in = self.xin("retgn%d" % j, [1, 2048])

        def factory(st):
            gng = em.sb(st, "ygng", [128, 2048], F32)
            em.dma("sp", gng[:], gn_in.ap().partition_broadcast(128), writes=[gng])
            oo = [em.sb(st, "yoo%d" % q, [128, 512], F32) for q in range(2)]
            p0 = [em.sb(st, "yp0%d" % q, [128, 512], F32) for q in range(2)]
            p1 = [em.sb(st, "yp1%d" % q, [128, 512], F32) for q in range(2)]
            gt = [em.sb(st, "ygt%d" % q, [128, 512], BF16) for q in range(2)]
            osum = em.sb(st, "yosum", [128, 512], F32)
            sg = em.sb(st, "ysg", [128, 512], F32)
            y = em.sb(st, "yy", [128, 2048], BF16)
            st6 = em.sb(st, "yst6", [128, 6], F32)
            mv = em.sb(st, "ymv", [128, 2], F32)
            rs = em.sb(st, "yrs", [128, 1], F32)
            cnt = [0]

            def make_yT(t, isctx, yT):
                pt = self.ptile(t)
                for h in range(4):
                    k = cnt[0]
                    cnt[0] += 1
                    o_, a0, a1, g_ = oo[k % 2], p0[k % 2], p1[k % 2], gt[k % 2]
                    cs = slice(h * 512, (h + 1) * 512)
                    em.dma("sp", o_[:], self.O2[0].ap()[t * 128:(t + 1) * 128, cs], writes=[o_])
                    em.dma("sp", a1[:], self.O2[1].ap()[pt * 128:(pt + 1) * 128, cs], writes=[a1])
                    em.dma("sp", g_[:], self.rG.ap()[t][:, cs], writes=[g_])
                    ps = self.pn()
                    self.mm(ps, ps[:, :], self.cf(C_J0), a1[:], True, True, [self.cmf, a1])
                    em.op("dve", lambda e, ps=ps, o_=o_: e.tensor_tensor(out=osum[:], in0=ps[:, :], in1=o_[:], op=ALU.add),
                          reads=[ps, o_], writes=[osum])
                    em.op("dve", lambda e: e.bn_stats(out=st6[:], in_=osum[:]), reads=[osum], writes=[st6])
                    em.op("dve", lambda e: e.bn_aggr(out=mv[:], in_=st6[:]), reads=[st6], writes=[mv])
                    em.op("act", lambda e: e.activation(out=rs[:], in_=mv[:, 1:2], func=AF.Sqrt, bias=1e-5, scale=1.0), reads=[mv], writes=[rs])
                    em.op("dve", lambda e: e.reciprocal(out=rs[:], in_=rs[:]), reads=[rs], writes=[rs])
                    em.op("dve", lambda e: e.tensor_scalar(out=osum[:], in0=osum[:], scalar1=mv[:, 0:1], scalar2=rs[:, 0:1],
                                                           op0=ALU.subtract, op1=ALU.mult), reads=[osum, mv, rs], writes=[osum])
                    em.op("act", lambda e, g_=g_: e.activation(out=sg[:], in_=g_[:], func=AF.Silu), reads=[g_], writes=[sg])
                    em.op("pool", lambda e, cs=cs: e.tensor_tensor(out=osum[:], in0=osum[:], in1=gng[:, cs], op=ALU.mult),
                          reads=[osum, gng], writes=[osum])
                    em.op("pool", lambda e, cs=cs: e.tensor_tensor(out=y[:, cs], in0=osum[:], in1=sg[:], op=ALU.mult),
                          reads=[osum, sg], writes=[y])
                for half in range(2):
                    ps = self.pn()
                    pb = ps[:].bitcast(BF16)
                    for c in range(8):
                        cc = half * 8 + c
                        em.op("pe", lambda e, pb=pb, c=c, cc=cc: e.transpose(out=pb[:, c * 128:(c + 1) * 128], in_=y[:, cc * 128:(cc + 1) * 128],
                                                                           identity=self.cb(C_ID)), reads=[y, self.cmb], writes=[ps])
                    em.op("act", lambda e, pb=pb, half=half: e.activation(
                        out=yT[:, half * 8:(half + 1) * 8, :].rearrange("p c n -> p (c n)"), in_=pb[:, 0:1024], func=AF.Copy),
                        reads=[ps], writes=[yT])
            return make_yT, [gng, osum, sg, y, st6, mv, rs] + oo + p0 + p1 + gt
        return factory

    def build(self):
        em = self.em
        self.setup()
        pend = []
        stop = getattr(self, "stop", 99)
        for i in self.layer_ids:
            pend.append(self.unit("modw%d" % i, 1024, 6144))
            kind, j = i % 3, i // 3
            if kind == 0:
                pend += [self.unit("retin%d" % j, 1024, 6144), self.unit("retout%d" % j, 2048, 1024)]
            elif kind == 1:
                pend += [self.unit("dnin", 1024, 6144), self.unit("dnout", 2048, 1024)]
            else:
                pend += [self.unit(n, 1024, 1024) for n in ("rkr", "rkk", "rkv", "rkout")]
                pend += [self.unit("rkg1", 1024, 128), self.unit("rkg2", 128, 1024)]
            if stop == 5 and i == self.layer_ids[-1]:
                pass
            elif i % 2 == 0 and not getattr(self, "force_moe", False):
                pend += [self.unit("ffngu%d" % (i // 2), 1024, 5632), self.unit("ffndn%d" % (i // 2), 2816, 1024)]
            else:
                for e_ in range(8):
                    pend += [self.unit("moegu%d_%d" % (i // 2, e_), 1024, 7168), self.unit("moedn%d_%d" % (i // 2, e_), 3584, 1024)]
        self.gather_units(pend)
        stop = getattr(self, "stop", 99)
        if self.layers >= 1:
            self.ret_alloc()
        if stop == 0:
            em.barrier()
            return self.nc
        for i in self.layer_ids:
            kind, j = i % 3, i // 3
            with ExitStack() as lst:
                modLC, rows = self.phase_mod(i, lst)
                if stop == 1:
                    return self.nc
                if kind == 0:
                    for z in range(2):
                        self.ret_phase_a(j, modLC, z)
                        self.ret_phase_s(j, z)
                    if stop == 3:
                        import os
                        which = os.environ.get("MKDUMP", "O0")
                        dbg = self.nc.dram_tensor("dbg", [self.T, 2048], F32, kind="ExternalOutput")
                        srcs = {"O0": self.O2[0].ap(), "O1": self.O2[1].ap(),
                                "V0": self.rV2[0].ap().rearrange("t p n -> (t p) n"),
                                "G": self.rG.ap().rearrange("t p n -> (t p) n")}
                        if which in srcs:
                            em.dma("pool", dbg.ap(), srcs[which], sem=self.gsem)
                        elif which == "K0":
                            em.dma("pool", dbg.ap()[:, 0:1024], self.rKTM2[0].ap().rearrange("t p n -> (t p) n"), sem=self.gsem)
                        elif which == "QT0":
                            em.dma("pool", dbg.ap()[:, 0:1024], self.rQT2[0].ap().rearrange("t p n -> (t p) n"), sem=self.gsem)
                        em.barrier()
                        return self.nc
                    self.phase_f(i, modLC, rows, self.ret_factory(j), "retout%d" % j, 16)
                elif kind == 1:
                    self.dn_layer(i, modLC, rows)
                else:
                    self.rk_layer(i, modLC, rows)
                if stop == 5 and i == self.layer_ids[-1]:
                    em.dma("pool", self.OUT.ap(), self.LAT2[0].ap(), sem=self.gsem)
                    em.barrier()
                    return self.nc
                if i % 2 == 0 and not getattr(self, "force_moe", False):
                    self.ffn_dense(i // 2, modLC, rows)
                else:
                    self.ffn_moe(i // 2, modLC, rows)
                em.barrier()
                em.release([modLC] + list(rows.values()))
        em.barrier()
        return self.nc


def rope_tables(L, z):
    NT = 2 + L // 128
    tab = np.zeros((NT, 128, 256), np.float32)
    tab[:2, :, 0:128] = 1.0
    inv = (10000.0 ** (-np.arange(64, dtype=np.float32) / 64)).astype(np.float32)
    n = np.arange(L)
    if z:
        n = n[::-1]
    row = (n // 64).astype(np.float32)[:, None] * inv[None, :]
    col = (n % 64).astype(np.float32)[:, None] * inv[None, :]
    full = np.concatenate([np.cos(row), np.cos(col), np.sin(row), np.sin(col)], 1).astype(np.float32)
    tab[2:] = full.reshape(L // 128, 128, 256)
    return tab


def host_inputs(mk, inp, core):
    b, z = core, 0
    L = mk.L
    f = lambda a: np.ascontiguousarray(a, dtype=np.float32)
    x = inp["x"][b][:L]
    cx = inp["ctx"][b]
    m = {}
    m["xs"] = f(np.concatenate([cx, x], 0))
    m["xsf"] = f(np.concatenate([cx[::-1], x[::-1]], 0))
    cv = np.stack([inp["c"][b], inp["c_ctx"]], -1).reshape(8, 128, 2).transpose(1, 0, 2).reshape(128, 16)
    m["cvec"] = f(cv)
    m["modb"] = f(inp["mod_b"].reshape(4, 48, 128).transpose(0, 2, 1))
    m["modbf"] = f(inp["mod_b"])
    m["lng"] = f(inp["ln_g"])
    m["lnb"] = f(inp["ln_b"])
    m["cm"] = f(make_consts(z).transpose(1, 0, 2))
    p = np.arange(128, dtype=np.float32)
    m["iota"] = f(np.stack([p + 1, -(p + 1), 0 * p, p - 127], 1))
    m["rope0"] = rope_tables(L, 0)
    m["rope1"] = rope_tables(L, 1)
    W = {}
    for i in range(4):
        W["modw%d" % i] = inp["mod_w"][i]
    for j in range(2):
        W["retin%d" % j] = inp["ret_w_in"][j]
        W["retout%d" % j] = inp["ret_w_out"][j]
        for zz in range(2):
            m["retdec%d_%d" % (j, zz)] = f(inp["ret_decay"][j][zz][None, :])
        m["retgn%d" % j] = f(inp["ret_gn_g"][j][None, :])
        W["ffngu%d" % j] = inp["ffn_w_gu"][j]
        W["ffndn%d" % j] = inp["ffn_w_down"][j]
        for e_ in range(8):
            W["moegu%d_%d" % (j, e_)] = inp["moe_w_gu"][j][e_]
            W["moedn%d_%d" % (j, e_)] = inp["moe_w_down"][j][e_]
    for j in range(2):
        m["moer%d" % j] = f(inp["moe_router"][j].reshape(8, 128, 8).transpose(1, 0, 2).reshape(128, 64))
    W["dnin"] = inp["dn_w_in"][0][:, :6144]
    W["dnout"] = inp["dn_w_out"][0]
    for q, nme in enumerate(("rkr", "rkk", "rkv")):
        W[nme] = inp["rk_w_rkv"][0][q]
    W["rkout"] = inp["rk_w_out"][0]
    W["rkg1"] = inp["rk_g1"][0]
    W["rkg2"] = inp["rk_g2"][0]
    for name in mk.ext:
        if name.startswith("w_"):
            w = W[name[2:]]
            k8 = w.shape[0] // 8
            m[name] = f(w[core * k8:(core + 1) * k8])
        elif name.startswith("wf_"):
            m[name] = f(W[name[3:]])
    host_extra(mk, inp, core, m)
    out = {}
    for name, (shape, dt) in mk.ext.items():
        a = m[name]
        assert tuple(a.shape) == tuple(shape), (name, a.shape, shape)
        out[name] = a
    return out


def host_extra(mk, inp, core, m):
    f = lambda a: np.ascontiguousarray(a, dtype=np.float32)
    wab = inp["dn_w_in"][0][:, 6144:]
    cw = inp["dn_conv_w"][0]
    for z in range(2):
        a = wab[:, z * 32:(z + 1) * 32]
        m["dnab%d" % z] = f(a.reshape(8, 128, 32).transpose(1, 0, 2).reshape(128, 256))
        m["dnalog%d" % z] = f(inp["dn_a_log"][0][z][None, :])
        m["dndtb%d" % z] = f(inp["dn_dt_bias"][0][z][None, :])
        c = cw if z == 0 else cw[::-1]
        m["dnconv%d" % z] = f(c.T.reshape(32, 128, 5).transpose(1, 0, 2).reshape(128, 160))
    m["dnng"] = f(inp["dn_norm_g"][0][None, :])
    fm8 = lambda v_: v_.reshape(8, 128).T
    m["rkmix"] = f(np.concatenate([fm8(inp["rk_mix"][0][q]) for q in range(6)], 1))
    m["rkvec"] = f(np.concatenate([fm8(inp["rk_k_k"][0]), fm8(inp["rk_k_a"][0]), fm8(inp["rk_r_k"][0].reshape(-1)), fm8(inp["rk_lnx_g"][0])], 1))
    lo = lambda w_: w_.reshape(8, 128, 64).transpose(1, 0, 2).reshape(128, 512)
    for z in range(2):
        m["rkw0_%d" % z] = f(inp["rk_w0"][0][z][None, :])
        m["rka0_%d" % z] = f(fm8(inp["rk_a0"][0][z]))
        m["rkw1_%d" % z] = f(lo(inp["rk_w1"][0][z]))
        m["rka1_%d" % z] = f(lo(inp["rk_a1"][0][z]))
        m["rkw2_%d" % z] = f(inp["rk_w2"][0][z])
        m["rka2_%d" % z] = f(inp["rk_a2"][0][z])


_CACHE = {}


def run_model(inp, L=8192, layers=4):
    key = (L, layers)
    if key not in _CACHE:
        mk = MK(L, layers)
        mk.build()
        _CACHE[key] = mk
    mk = _CACHE[key]
    nco = mk.ncores
    in_maps = [host_inputs(mk, inp, c) for c in range(nco)]
    res = run_bass_kernel_spmd(mk.nc, in_maps, core_ids=list(range(nco)))
    lat = np.zeros((4, L, D), np.float32)
    cx = np.zeros((4, NCTX, D), np.float32)
    for c in range(nco):
        o = res.results[c]["out"]
        lat[c] = o[256:]
        cx[c] = o[:256]
    return lat, cx


def kernel(**inputs):
    inp = {k: np.asarray(v) for k, v in inputs.items()}
    lat, _ = run_model(inp, 8192, 4)
    return lat


def ffn_moe(self, li, modLC, rows):
    em = self.em
    rin = self.xin("moer%d" % li, [128, 64])
    with ExitStack() as st:
        wr = em.sb(st, "mwr", [128, 8, 8], BF16)
        em.dma("pool", wr[:].rearrange("p c e -> p (c e)"), rin.ap(), writes=[wr])
        h2 = em.sb(st, "mh2", [128, 8, 512], BF16)
        wg = [em.sb(st, "mwg%d" % q, [128, 8, 256], BF16) for q in range(2)]
        wu = [em.sb(st, "mwu%d" % q, [128, 8, 256], BF16) for q in range(2)]
        wd = [em.sb(st, "mwd%d" % q, [128, 28, 512], BF16) for q in range(2)]
        act = em.sb(st, "mact", [128, 28, 512], BF16)
        acc = em.sb(st, "macc", [128, 4, D], F32)
        sg = [em.sb(st, "msg%d" % q, [128, 512], F32) for q in range(2)]
        lg = em.sb(st, "mlg", [128, 8], F32)
        eq = em.sb(st, "meq", [128, 8], F32)
        l2 = em.sb(st, "ml2", [128, 8], F32)
        ex = em.sb(st, "mex", [128, 8], F32)
        m1 = em.sb(st, "mm1", [128, 4], F32)
        gate = em.sb(st, "mgate", [128, 4, 8], F32)
        la = em.sb(st, "mlat", [128, D], F32)
        ou = em.sb(st, "mout", [128, D], F32)
        tmp = em.sb(st, "mtmp", [128, D], F32)
        st6 = em.sb(st, "mst6", [128, 2, 6], F32)
        mv = em.sb(st, "mmv", [128, 2], F32)
        rs = em.sb(st, "mrs", [128, 1], F32)
        nf = 0
        for gi, (t0, ng, isctx) in enumerate(self.groups):
            N = ng * 128
            em.dma("sp", h2[:], self.H2T.ap()[gi].rearrange("p (c n) -> p c n", c=8), writes=[h2])
            for ti in range(ng):
                ps = self.pn()
                for kc in range(8):
                    self.mm(ps, ps[:, 0:8], h2[:, kc, ti * 128:(ti + 1) * 128], wr[:, kc, :], kc == 0, kc == 7, [h2, wr])
                em.op("dve", lambda e, ps=ps: e.tensor_copy(out=lg[:], in_=ps[:, 0:8]), reads=[ps], writes=[lg])
                em.op("dve", lambda e: e.tensor_reduce(out=m1[:, 0:1], in_=lg[:], axis=AX.X, op=ALU.max), reads=[lg], writes=[m1])
                em.op("dve", lambda e: e.tensor_scalar(out=eq[:], in0=lg[:], scalar1=m1[:, 0:1], scalar2=None, op0=ALU.is_equal),
                      reads=[lg, m1], writes=[eq])
                em.op("dve", lambda e: e.scalar_tensor_tensor(out=l2[:], in0=eq[:], scalar=-1e30, in1=lg[:], op0=ALU.mult, op1=ALU.add),
                      reads=[eq, lg], writes=[l2])
                em.op("dve", lambda e: e.tensor_reduce(out=m1[:, 1:2], in_=l2[:], axis=AX.X, op=ALU.max), reads=[l2], writes=[m1])
                em.op("dve", lambda e: e.tensor_scalar(out=eq[:], in0=lg[:], scalar1=m1[:, 1:2], scalar2=None, op0=ALU.is_ge),
                      reads=[lg, m1], writes=[eq])
                em.op("dve", lambda e: e.tensor_scalar(out=m1[:, 2:3], in0=m1[:, 0:1], scalar1=-1.0, scalar2=None, op0=ALU.mult),
                      reads=[m1], writes=[m1])
                em.op("act", lambda e: e.activation(out=ex[:], in_=lg[:], func=AF.Exp, bias=m1[:, 2:3], scale=1.0), reads=[lg, m1], writes=[ex])
                em.op("dve", lambda e: e.tensor_tensor(out=ex[:], in0=ex[:], in1=eq[:], op=ALU.mult), reads=[ex, eq], writes=[ex])
                em.op("dve", lambda e: e.tensor_reduce(out=m1[:, 3:4], in_=ex[:], axis=AX.X, op=ALU.add), reads=[ex], writes=[m1])
                em.op("dve", lambda e: e.reciprocal(out=m1[:, 3:4], in_=m1[:, 3:4]), reads=[m1], writes=[m1])
                em.op("dve", lambda e, ti=ti: e.tensor_scalar(out=gate[:, ti, :], in0=ex[:], scalar1=m1[:, 3:4], scalar2=None, op0=ALU.mult),
                      reads=[ex, m1], writes=[gate])
            for e_ in range(8):
                for half in range(2):
                    self.load_w(wd[half], "moedn%d_%d" % (li, e_), 0, 28, half * 512, 512)
                for s in range(14):
                    g_, u_ = wg[s % 2], wu[s % 2]
                    self.load_w(g_, "moegu%d_%d" % (li, e_), 0, 8, s * 256, 256)
                    self.load_w(u_, "moegu%d_%d" % (li, e_), 0, 8, 3584 + s * 256, 256)
                    for fc in range(2):
                        f = s * 2 + fc
                        pg, pu = self.pn(), self.pn()
                        for kc in range(8):
                            self.mm(pg, pg[:, :N], g_[:, kc, fc * 128:(fc + 1) * 128], h2[:, kc, :N], kc == 0, kc == 7, [g_, h2])
                        for kc in range(8):
                            self.mm(pu, pu[:, :N], u_[:, kc, fc * 128:(fc + 1) * 128], h2[:, kc, :N], kc == 0, kc == 7, [u_, h2])
                        s_ = sg[nf % 2]
                        nf += 1
                        em.op("act", lambda e, pg=pg, s_=s_: e.activation(out=s_[:, :N], in_=pg[:, :N], func=AF.Silu), reads=[pg], writes=[s_])
                        em.op("dve", lambda e, pu=pu, s_=s_, f=f: e.tensor_tensor(out=act[:, f, :N], in0=s_[:, :N], in1=pu[:, :N], op=ALU.mult),
                              reads=[pu, s_], writes=[act])
                for ti in range(ng):
                    for half in range(2):
                        ps = self.pn()
                        for f in range(28):
                            self.mm(ps, ps[:, :], act[:, f, ti * 128:(ti + 1) * 128], wd[half][:, f, :], f == 0, f == 27, [act, wd[half]])
                        asl = acc[:, ti, half * 512:(half + 1) * 512]
                        if e_ == 0:
                            em.op("dve", lambda e, ps=ps, asl=asl, ti=ti, e_=e_: e.tensor_scalar(
                                out=asl, in0=ps[:, :], scalar1=gate[:, ti, e_:e_ + 1], scalar2=None, op0=ALU.mult),
                                reads=[ps, gate], writes=[acc])
                        else:
                            em.op("dve", lambda e, ps=ps, asl=asl, ti=ti, e_=e_: e.scalar_tensor_tensor(
                                out=asl, in0=ps[:, :], scalar=gate[:, ti, e_:e_ + 1], in1=asl, op0=ALU.mult, op1=ALU.add),
                                reads=[ps, gate, acc], writes=[acc])
            sfx = "C" if isctx else "L"
            for ti in range(ng):
                t = t0 + ti
                em.dma("sp", la[:], self.LAT2[0].ap()[t * 128:(t + 1) * 128, :], writes=[la])
                srcs = [(acc, acc[:, ti, 0:512]), (acc, acc[:, ti, 512:1024])]
                self.tail(srcs, la, rows["GA2" + sfx], rows["LNG1"], rows["LNB1"], tmp, ou, st6, mv, rs)
                self.ffn_store(t, ou)
        em.barrier()
        em.release([wr, h2, act, acc, lg, eq, l2, ex, m1, gate, la, ou, tmp, st6, mv, rs] + wg + wu + wd + sg)


MK.ffn_moe = ffn_moe


def dn_alloc(self):
    NT, T = self.NT, self.T
    self.dPRE = self.dr("dPRE", [32, 128, T], F32)
    self.dQT = [self.dr("dQT%d" % z, [8, 128, T], BF16) for z in range(2)]
    self.dKT = [self.dr("dKT%d" % z, [8, 128, T], BF16) for z in range(2)]
    self.dKM = [self.dr("dKM%d" % z, [NT, 128, 1024], BF16) for z in range(2)]
    self.dV = [self.dr("dV%d" % z, [NT, 128, 2048], BF16) for z in range(2)]
    self.dGB = [self.dr("dGB%d" % z, [NT, 128, 48], F32) for z in range(2)]
    self.dn_in = dict(
        ab=[self.xin("dnab%d" % z, [128, 8 * 32]) for z in range(2)],
        alog=[self.xin("dnalog%d" % z, [1, 16]) for z in range(2)],
        dtb=[self.xin("dndtb%d" % z, [1, 16]) for z in range(2)],
        conv=[self.xin("dnconv%d" % z, [128, 32 * 5]) for z in range(2)],
        ng=self.xin("dnng", [1, 128]))


def dn_phase_a1(self, modLC, z):
    em = self.em
    LAT = self.LAT2[z]
    with ExitStack() as st:
        wb = [em.sb(st, "dw%d" % q, [128, 8, 512], BF16) for q in range(2)]
        hT = [em.sb(st, "dhT%d" % q, [128, 8, 512], BF16) for q in range(2)]
        lat = [em.sb(st, "dlat%d" % q, [128, D], F32) for q in range(2)]
        stg = [em.sb(st, "dstg%d" % q, [128, 512], F32) for q in range(3)]
        gg = [em.sb(st, "dgg%d" % q, [128, 2048], BF16) for q in range(4)]
        wab = em.sb(st, "dwab", [128, 8, 32], BF16)
        em.dma("pool", wab[:].rearrange("p c n -> p (c n)"), self.dn_in["ab"][z].ap(), writes=[wab])
        nega = em.sb(st, "dnega", [128, 16], F32)
        dtb = em.sb(st, "ddtb", [128, 16], F32)
        em.dma("sp", nega[:], self.dn_in["alog"][z].ap().partition_broadcast(128), writes=[nega])
        em.dma("sp", dtb[:], self.dn_in["dtb"][z].ap().partition_broadcast(128), writes=[dtb])
        em.op("act", lambda e: e.activation(out=nega[:], in_=nega[:], func=AF.Exp), reads=[nega], writes=[nega])
        em.op("dve", lambda e: e.tensor_scalar(out=nega[:], in0=nega[:], scalar1=-1.0, scalar2=None, op0=ALU.mult), reads=[nega], writes=[nega])
        gb = [em.sb(st, "dgb%d" % q, [128, 48], F32) for q in range(2)]
        tt = [em.sb(st, "dtt%d" % q, [128, 32], F32) for q in range(2)]
        kk = 0
        sk = 0
        for gi, (t0, ng, isctx) in enumerate(self.groups):
            N = ng * 128
            h = hT[gi % 2]
            for ti in range(ng):
                la = lat[kk % 2]
                kk += 1
                em.dma("sp", la[:], LAT.ap()[(t0 + ti) * 128:(t0 + ti + 1) * 128, :], writes=[la])
                self.hT_tile(la, h, ti * 128, modLC, 0, isctx)
            for nb in range(8):
                w = wb[nb % 2]
                self.load_w(w, "dnin", 0, 8, nb * 512, 512)
                for c4 in range(4):
                    c = nb * 4 + c4
                    ps = self.pn()
                    for kc in range(8):
                        self.mm(ps, ps[:, :N], w[:, kc, c4 * 128:(c4 + 1) * 128], h[:, kc, :N], kc == 0, kc == 7, [w, h])
                    s_ = stg[sk % 3]
                    sk += 1
                    if c % 2:
                        em.op("act", lambda e, ps=ps, s_=s_: e.activation(out=s_[:, :N], in_=ps[:, :N], func=AF.Copy), reads=[ps], writes=[s_])
                    else:
                        em.op("dve", lambda e, ps=ps, s_=s_: e.tensor_copy(out=s_[:, :N], in_=ps[:, :N]), reads=[ps], writes=[s_])
                    em.dma("pool", self.dPRE.ap()[c][:, t0 * 128:t0 * 128 + N], s_[:, :N], reads=[s_])
            if z == 0:
                for nb in range(8, 12):
                    w = wb[nb % 2]
                    self.load_w(w, "dnin", 0, 8, nb * 512, 512)
                    for ti in range(ng):
                        ps = self.pn()
                        for kc in range(8):
                            self.mm(ps, ps[:, :], h[:, kc, ti * 128:(ti + 1) * 128], w[:, kc, :], kc == 0, kc == 7, [h, w])
                        dest = gg[ti]
                        c0 = (nb - 8) * 512
                        em.op("act", lambda e, ps=ps, dest=dest, c0=c0: e.activation(out=dest[:, c0:c0 + 512], in_=ps[:, :], func=AF.Copy),
                              reads=[ps], writes=[dest])
                for ti in range(ng):
                    em.dma("pool", self.rG.ap()[t0 + ti], gg[ti][:], reads=[gg[ti]])
            for ti in range(ng):
                ps = self.pn()
                for kc in range(8):
                    self.mm(ps, ps[:, 0:32], h[:, kc, ti * 128:(ti + 1) * 128], wab[:, kc, :], kc == 0, kc == 7, [h, wab])
                g_, t_ = gb[ti % 2], tt[ti % 2]
                em.op("dve", lambda e, ps=ps, t_=t_: e.tensor_tensor(out=t_[:, 0:16], in0=ps[:, 0:16], in1=dtb[:], op=ALU.add), reads=[ps, dtb], writes=[t_])
                em.op("dve", lambda e, ps=ps, t_=t_: e.tensor_scalar(out=t_[:, 16:32], in0=ps[:, 16:32], scalar1=-1.0, scalar2=None, op0=ALU.mult),
                      reads=[ps], writes=[t_])
                em.op("act", lambda e, t_=t_: e.activation(out=t_[:], in_=t_[:], func=AF.Exp), reads=[t_], writes=[t_])
                em.op("act", lambda e, t_=t_, g_=g_: e.activation(out=g_[:, 0:16], in_=t_[:, 0:16], func=AF.Ln, bias=1.0, scale=1.0), reads=[t_], writes=[g_])
                em.op("act", lambda e, t_=t_, g_=g_: e.activation(out=g_[:, 32:48], in_=t_[:, 16:32], func=AF.Ln, bias=1.0, scale=1.0), reads=[t_], writes=[g_])
                em.op("dve", lambda e, g_=g_: e.tensor_tensor(out=g_[:, 0:16], in0=g_[:, 0:16], in1=nega[:], op=ALU.mult), reads=[g_, nega], writes=[g_])
                em.op("dve", lambda e, g_=g_: e.tensor_scalar(out=g_[:, 32:48], in0=g_[:, 32:48], scalar1=-1.0, scalar2=None, op0=ALU.mult), reads=[g_], writes=[g_])
                em.op("dve", lambda e, t_=t_: e.tensor_scalar(out=t_[:, 16:32], in0=t_[:, 16:32], scalar1=1.0, scalar2=None, op0=ALU.add), reads=[t_], writes=[t_])
                em.op("dve", lambda e, t_=t_, g_=g_: e.reciprocal(out=g_[:, 16:32], in_=t_[:, 16:32]), reads=[t_], writes=[g_])
                em.dma("pool", self.dGB[z].ap()[t0 + ti], g_[:], reads=[g_])
        em.barrier()
        em.release(wb + hT + lat + stg + gg + [wab, nega, dtb] + gb + tt)


def dn_phase_a2(self, z):
    em = self.em
    T, NT = self.T, self.NT
    with ExitStack() as st:
        cw = em.sb(st, "cw", [128, 32, 5], F32)
        em.dma("sp", cw[:].rearrange("p c k -> p (c k)"), self.dn_in["conv"][z].ap(), writes=[cw])
        x = [em.sb(st, "cx%d" % q, [128, T], F32) for q in range(2)]
        acc = [em.sb(st, "cacc0", [128, T], F32)] * 2
        yb = em.sb(st, "cyb", [128, T], BF16)
        sq = em.sb(st, "csq", [128, 512], BF16)
        ri = em.sb(st, "cri", [128, 512], F32)
        tp = [em.sb(st, "ctp%d" % q, [128, 8, 128], BF16) for q in range(2)]
        segs = [(0, 256), (256, T)]
        tk = 0
        for c in range(32):
            x_, a_ = x[c % 2], acc[c % 2]
            eng = "dve"
            em.dma("sp", x_[:], self.dPRE.ap()[c], writes=[x_])
            em.op(eng, lambda e, x_=x_, a_=a_, c=c: e.tensor_scalar(out=a_[:], in0=x_[:], scalar1=cw[:, c, 2:3], scalar2=None, op0=ALU.mult),
                  reads=[x_, cw], writes=[a_])
            for k in (0, 1, 3, 4):
                s = k - 2
                for (a, b) in segs:
                    lo, hi = max(a, a - s), min(b, b - s)
                    em.op(eng, lambda e, x_=x_, a_=a_, c=c, k=k, lo=lo, hi=hi, s=s: e.scalar_tensor_tensor(
                        out=a_[:, lo:hi], in0=x_[:, lo + s:hi + s], scalar=cw[:, c, k:k + 1], in1=a_[:, lo:hi], op0=ALU.mult, op1=ALU.add),
                        reads=[x_, a_, cw], writes=[a_])
            em.op("act", lambda e, a_=a_: e.activation(out=a_[:], in_=a_[:], func=AF.Silu), reads=[a_], writes=[a_])
            if c < 16:
                for b0 in range(0, T, 512):
                    n = min(512, T - b0)
                    em.op("dve", lambda e, a_=a_, b0=b0, n=n: e.tensor_tensor(out=sq[:, :n], in0=a_[:, b0:b0 + n], in1=a_[:, b0:b0 + n], op=ALU.mult),
                          reads=[a_], writes=[sq])
                    ps = self.pn()
                    self.mm(ps, ps[:, :n], self.cb(C_ONES), sq[:, :n], True, True, [self.cmb, sq])
                    em.op("act", lambda e, ps=ps, n=n: e.activation(out=ri[:, :n], in_=ps[:, :n], func=AF.Sqrt, bias=1e-6, scale=1.0), reads=[ps], writes=[ri])
                    em.op("dve", lambda e, n=n: e.reciprocal(out=ri[:, :n], in_=ri[:, :n]), reads=[ri], writes=[ri])
                    sc = (128.0 ** -0.5) if c < 8 else 1.0
                    em.op("dve", lambda e, a_=a_, b0=b0, n=n, sc=sc: e.scalar_tensor_tensor(
                        out=yb[:, b0:b0 + n], in0=a_[:, b0:b0 + n], scalar=sc, in1=ri[:, :n], op0=ALU.mult, op1=ALU.mult),
                        reads=[a_, ri], writes=[yb])
                dst = (self.dQT if c < 8 else self.dKT)[z]
                em.dma("pool", dst.ap()[c % 8], yb[:], reads=[yb])
            else:
                em.op("dve", lambda e, a_=a_: e.tensor_copy(out=yb[:], in_=a_[:]), reads=[a_], writes=[yb])
            if c >= 8:
                for t8 in range(0, NT, 8):
                    nt = min(8, NT - t8)
                    ps = self.pn()
                    pb = ps[:].bitcast(BF16)
                    for q in range(nt):
                        em.op("pe", lambda e, pb=pb, q=q, t8=t8: e.transpose(out=pb[:, q * 128:(q + 1) * 128], in_=yb[:, (t8 + q) * 128:(t8 + q + 1) * 128],
                                                                           identity=self.cb(C_ID)), reads=[yb, self.cmb], writes=[ps])
                    t_ = tp[tk % 2]
                    tk += 1
                    em.op("act", lambda e, pb=pb, t_=t_, nt=nt: e.activation(out=t_[:, 0:nt, :].rearrange("p a b -> p (a b)"), in_=pb[:, 0:nt * 128], func=AF.Copy),
                          reads=[ps], writes=[t_])
                    if c < 16:
                        dstap = self.dKM[z].ap()[t8:t8 + nt, :, (c - 8) * 128:(c - 7) * 128].rearrange("t p n -> p t n")
                    else:
                        dstap = self.dV[z].ap()[t8:t8 + nt, :, (c - 16) * 128:(c - 15) * 128].rearrange("t p n -> p t n")
                    em.dma("pool", dstap, t_[:, 0:nt, :], reads=[t_])
        em.barrier()
        em.release([cw, yb, sq, ri] + x + acc[:1] + tp)


MK.dn_alloc = dn_alloc
MK.dn_phase_a1 = dn_phase_a1
MK.dn_phase_a2 = dn_phase_a2


def tri_inverse(self, LT, INV, INVT, Tsb, tmpx, sign, ltf=None):
    em = self.em
    for q, dst in ((0, INV), (1, INVT)):
        em.op("pool", lambda e, dst=dst: e.tensor_copy(out=dst[:], in_=self.cb(C_ID).unsqueeze(1).broadcast_to([128, 4, 128])),
              reads=[self.cmb], writes=[dst])
    for lv in range(6):
        pT = self.pn()
        for h4 in range(4):
            self.mm(pT, pT[:, h4 * 128:(h4 + 1) * 128], LT[:, h4, :] if ltf is None else ltf(h4), INV[:, h4, :], True, True, [LT, INV])
        em.op("act", lambda e, pT=pT: e.activation(out=Tsb[:].rearrange("p a b -> p (a b)"), in_=pT[:, :], func=AF.Copy), reads=[pT], writes=[Tsb])
        pX, pXT = self.pn(), self.pn()
        for h4 in range(4):
            self.mm(pX, pX[:, h4 * 128:(h4 + 1) * 128], INVT[:, h4, :], Tsb[:, h4, :], True, True, [INVT, Tsb])
        for h4 in range(4):
            self.mm(pXT, pXT[:, h4 * 128:(h4 + 1) * 128], Tsb[:, h4, :], INVT[:, h4, :], True, True, [INVT, Tsb])
        for (pp, msk, dst, q) in ((pX, C_LV + lv, INV, 0), (pXT, C_LVT + lv, INVT, 1)):
            tx = tmpx[q]
            em.op("dve", lambda e, pp=pp, msk=msk, tx=tx: e.scalar_tensor_tensor(
                out=tx[:], in0=pp[:, :].rearrange("p (a b) -> p a b", a=4), scalar=-float(sign),
                in1=self.cb(msk).unsqueeze(1).broadcast_to([128, 4, 128]), op0=ALU.mult, op1=ALU.mult),
                reads=[pp, self.cmb], writes=[tx])
            em.op("pool", lambda e, dst=dst, tx=tx: e.tensor_tensor(out=dst[:], in0=dst[:], in1=tx[:], op=ALU.add), reads=[dst, tx], writes=[dst])


MK.tri_inverse = tri_inverse


def tri_inverse16(self, ltf, ltbufs, INV, INVT, Tsb, tmpx, sign):
    em = self.em
    for g in range(4):
        for dst in (INV[g], INVT[g]):
            em.op("pool", lambda e, dst=dst: e.tensor_copy(out=dst[:], in_=self.cb(C_ID).unsqueeze(1).broadcast_to([128, 4, 128])),
                  reads=[self.cmb], writes=[dst])
    f2 = lambda b: b[:].rearrange("p a b -> p (a b)")
    for lv in range(6):
        pT = [self.pn() for _ in range(4)]
        for g in range(4):
            for h4 in range(4):
                self.mm(pT[g], pT[g][:, h4 * 128:(h4 + 1) * 128], ltf(g * 4 + h4), INV[g][:, h4, :], True, True, [ltbufs[g], INV[g]])
        for g in range(4):
            if g % 2:
                em.op("dve", lambda e, g=g: e.tensor_copy(out=f2(Tsb[g]), in_=pT[g][:, :]), reads=[pT[g]], writes=[Tsb[g]])
            else:
                em.op("act", lambda e, g=g: e.activation(out=f2(Tsb[g]), in_=pT[g][:, :], func=AF.Copy), reads=[pT[g]], writes=[Tsb[g]])
        pX = [self.pn() for _ in range(4)]
        for g in range(4):
            for h4 in range(4):
                self.mm(pX[g], pX[g][:, h4 * 128:(h4 + 1) * 128], INVT[g][:, h4, :], Tsb[g][:, h4, :], True, True, [INVT[g], Tsb[g]])
        pXT = [self.pn() for _ in range(4)]
        for g in range(4):
            for h4 in range(4):
                self.mm(pXT[g], pXT[g][:, h4 * 128:(h4 + 1) * 128], Tsb[g][:, h4, :], INVT[g][:, h4, :], True, True, [INVT[g], Tsb[g]])
        for (pp, msk, dstl, q) in ((pX, C_LV + lv, INV, 0), (pXT, C_LVT + lv, INVT, 1)):
            for g in range(4):
                tx = tmpx[q][g]
                em.op("dve", lambda e, pp=pp, msk=msk, tx=tx, g=g: e.scalar_tensor_tensor(
                    out=tx[:], in0=pp[g][:, :].rearrange("p (a b) -> p a b", a=4), scalar=-float(sign),
                    in1=self.cb(msk).unsqueeze(1).broadcast_to([128, 4, 128]), op0=ALU.mult, op1=ALU.mult),
                    reads=[pp[g], self.cmb], writes=[tx])
                em.op("pool", lambda e, dstl=dstl, tx=tx, g=g: e.tensor_tensor(out=dstl[g][:], in0=dstl[g][:], in1=tx[:], op=ALU.add),
                      reads=[dstl[g], tx], writes=[dstl[g]])


MK.tri_inverse16 = tri_inverse16


def dn_phase_s(self, z):
    em = self.em
    T, NT = self.T, self.NT
    O = self.O2[z]
    with ExitStack() as st:
        B = lambda n, sh, dt: em.sb(st, "n" + n, sh, dt)
        qt = B("qt", [128, 8, 128], BF16)
        kt_ = B("ktf", [128, 8, 128], BF16)
        km = B("km", [128, 8, 128], BF16)
        v = B("v", [128, 16, 128], BF16)
        gb = B("gb", [128, 48], F32)
        sm = B("sm", [128, 64], F32)
        vec = B("vec", [128, 6, 16], F32)
        mneg = B("mneg", [128, 2, 4, 128], BF16)
        for q, cidx in ((0, C_MI), (1, C_MS)):
            em.op("dve", lambda e, q=q, cidx=cidx: e.tensor_scalar(
                out=mneg[:, q, :, :], in0=self.cb(cidx).unsqueeze(1).broadcast_to([128, 4, 128]), scalar1=-1.0, scalar2=30000.0,
                op0=ALU.add, op1=ALU.mult), reads=[self.cmb], writes=[mneg])
        DgA = [[B("Dg%d_%d" % (q, r), [128, 4, 128], F32) for q in range(2)] for r in range(2)]
        DeA = [B("De%d" % r, [128, 4, 128], BF16) for r in range(2)]
        EA = [[B("E%d_%d" % (q, r), [128, 4, 128], F32) for q in range(2)] for r in range(2)]
        LTg = [B("LT%d" % g, [128, 4, 128], BF16) for g in range(4)]
        INVg = [B("INV%d" % g, [128, 4, 128], BF16) for g in range(4)]
        INVTg = [B("INVT%d" % g, [128, 4, 128], BF16) for g in range(4)]
        Tsbg = [B("Tsb%d" % g, [128, 4, 128], BF16) for g in range(4)]
        tmpxg = [[B("tmpx%d_%d" % (q, g), [128, 4, 128], BF16) for g in range(4)] for q in range(2)]
        QKd = B("QKd", [128, 16, 128], BF16)
        qgT = B("qgT", [128, 16, 128], BF16)
        WT = B("WT", [128, 16, 128], BF16)
        U = B("U", [128, 16, 128], F32)
        VN = [B("VN%d" % q, [128, 8, 128], BF16) for q in range(2)]
        stmp2 = [B("stmp2_%d" % q, [128, 8, 128], F32) for q in range(2)]
        ktk = B("ktk", [128, 16, 128], BF16)
        vb = B("vb", [128, 16, 128], BF16)
        kbg = B("kbg", [128, 16, 128], BF16)
        OT = [B("OT%d" % q, [128, 2048], F32) for q in range(2)]
        S = [B("S%d" % q, [128, 8, 128], F32) for q in range(2)]
        Sb = [B("Sb%d" % q, [128, 8, 128], BF16) for q in range(2)]
        stmp = B("stmp", [128, 8, 128], F32)
        for q in range(2):
            em.op("pool", lambda e, q=q: e.memset(S[q][:], 0.0), writes=[S[q]])
            em.op("pool", lambda e, q=q: e.memset(Sb[q][:], 0.0), writes=[Sb[q]])
        bc16 = lambda ap: ap.unsqueeze(2).broadcast_to([128, 16, 128])
        for t in range(NT):
            c0 = t * 128
            em.dma("sp", qt[:], self.dQT[z].ap()[:, :, c0:c0 + 128].rearrange("h p n -> p h n"), writes=[qt])
            em.dma("sp", kt_[:], self.dKT[z].ap()[:, :, c0:c0 + 128].rearrange("h p n -> p h n"), writes=[kt_])
            em.dma("sp", km[:].rearrange("p a b -> p (a b)"), self.dKM[z].ap()[t], writes=[km])
            em.dma("sp", v[:].rearrange("p a b -> p (a b)"), self.dV[z].ap()[t], writes=[v])
            em.dma("sp", gb[:], self.dGB[z].ap()[t], writes=[gb])
            ps = self.pn()
            for q, cidx in enumerate((C_TRI, C_BLK, C_SEL0, C_SEL1)):
                self.mm(ps, ps[:, q * 16:(q + 1) * 16], self.cf(cidx), gb[:, 0:16], True, True, [self.cmf, gb])
            em.op("dve", lambda e, ps=ps: e.tensor_copy(out=sm[:], in_=ps[:, 0:64]), reads=[ps], writes=[sm])
            gc, gl = sm[:, 0:16], sm[:, 16:32]
            em.op("dve", lambda e: e.tensor_tensor(out=vec[:, 0, :], in0=gc, in1=gb[:, 32:48], op=ALU.add), reads=[sm, gb], writes=[vec])
            em.op("act", lambda e: e.activation(out=vec[:, 1, :], in_=gc, func=AF.Exp), reads=[sm], writes=[vec])
            em.op("dve", lambda e: e.tensor_tensor(out=vec[:, 2, :], in0=gl, in1=gc, op=ALU.subtract), reads=[sm], writes=[vec])
            em.op("act", lambda e: e.activation(out=vec[:, 2, :], in_=vec[:, 2, :], func=AF.Exp), reads=[vec], writes=[vec])
            em.op("dve", lambda e: e.tensor_tensor(out=vec[:, 3, :], in0=vec[:, 1, :], in1=gb[:, 16:32], op=ALU.mult), reads=[vec, gb], writes=[vec])
            em.op("act", lambda e: e.activation(out=vec[:, 4:6, :].rearrange("p a b -> p (a b)"), in_=sm[:, 32:64], func=AF.Exp), reads=[sm], writes=[vec])
            em.op("dve", lambda e: e.tensor_tensor(out=vb[:], in0=v[:], in1=bc16(gb[:, 16:32]), op=ALU.mult), reads=[v, gb], writes=[vb])
            for r in range(2):
                kmr = km[:]
                em.op("pool", lambda e, r=r: e.tensor_tensor(
                    out=kbg[:].rearrange("p (a r) n -> p a r n", r=2)[:, :, r, :], in0=km[:],
                    in1=vec[:, 3, :].rearrange("p (a r) -> p a r", r=2)[:, :, r].unsqueeze(2).broadcast_to([128, 8, 128]), op=ALU.mult),
                    reads=[km, vec], writes=[kbg])
                em.op("pool", lambda e, r=r: e.tensor_tensor(
                    out=ktk[:].rearrange("p (a r) n -> p a r n", r=2)[:, :, r, :], in0=km[:],
                    in1=vec[:, 2, :].rearrange("p (a r) -> p a r", r=2)[:, :, r].unsqueeze(2).broadcast_to([128, 8, 128]), op=ALU.mult),
                    reads=[km, vec], writes=[ktk])
            for hg in range(4):
                hs = slice(hg * 4, hg * 4 + 4)
                Dg, De, E, LT = DgA[hg % 2], DeA[hg % 2], EA[hg % 2], LTg[hg]
                bc4 = lambda ap: ap.unsqueeze(2).broadcast_to([128, 4, 128])
                idf = self.cf(C_ID).unsqueeze(1).broadcast_to([128, 4, 128])
                em.op("dve", lambda e, hs=hs: e.tensor_tensor(out=Dg[0][:], in0=idf, in1=bc4(sm[:, hs]), op=ALU.mult), reads=[self.cmf, sm], writes=[Dg[0]])
                em.op("dve", lambda e, hs=hs: e.tensor_tensor(out=Dg[1][:], in0=idf, in1=bc4(vec[:, 0, hs]), op=ALU.mult), reads=[self.cmf, vec], writes=[Dg[1]])
                em.op("pool", lambda e, hs=hs: e.tensor_tensor(out=De[:], in0=self.cb(C_ID).unsqueeze(1).broadcast_to([128, 4, 128]), in1=bc4(vec[:, 1, hs]), op=ALU.mult),
                      reads=[self.cmb, vec], writes=[De])
                for q in range(2):
                    p_ = self.pn()
                    self.mm(p_, p_[:, :], self.cf(C_ONES), Dg[q][:].rearrange("p a b -> p (a b)"), True, False, [self.cmf, Dg[q]])
                    self.mm(p_, p_[:, :], self.cb(C_ID), mneg[:, q, :, :].rearrange("p a b -> p (a b)"), False, True, [self.cmb, mneg])
                    em.op("dve", lambda e, p_=p_, q=q, hs=hs: e.tensor_tensor(out=E[q][:], in0=p_[:, :].rearrange("p (a b) -> p a b", a=4),
                                                                           in1=bc4(sm[:, hs]), op=ALU.subtract), reads=[p_, sm], writes=[E[q]])
                    em.op("act", lambda e, q=q: e.activation(out=E[q][:], in_=E[q][:], func=AF.Exp), reads=[E[q]], writes=[E[q]])
                pe_ = self.pn()
                self.mm(pe_, pe_[:, :], self.cb(C_ONES), De[:].rearrange("p a b -> p (a b)"), True, True, [self.cmb, De])
                pk = self.pn()
                for qh in range(2):
                    hq = hg * 2 + qh
                    self.mm(pk, pk[:, qh * 128:(qh + 1) * 128], kt_[:, hq, :], kt_[:, hq, :], True, True, [kt_])
                    self.mm(pk, pk[:, 256 + qh * 128:256 + (qh + 1) * 128], kt_[:, hq, :], qt[:, hq, :], True, True, [kt_, qt])
                rep = lambda ap: ap.rearrange("p (a b) -> p a b", a=2).unsqueeze(2).broadcast_to([128, 2, 2, 128])
                v4 = lambda ap: ap.rearrange("p (a r) b -> p a r b", r=2)
                em.op("dve", lambda e, pk=pk: e.tensor_tensor(out=v4(LT[:]), in0=rep(pk[:, 0:256]), in1=v4(E[1][:]), op=ALU.mult), reads=[pk, E[1]], writes=[LT])
                em.op("dve", lambda e, pk=pk, hs=hs: e.tensor_tensor(out=v4(QKd[:, hs, :]), in0=rep(pk[:, 256:512]), in1=v4(E[0][:]), op=ALU.mult),
                      reads=[pk, E[0]], writes=[QKd])
                em.op("dve", lambda e, pe_=pe_, hs=hs, hg=hg: e.tensor_tensor(
                    out=v4(qgT[:, hs, :]), in0=qt[:, hg * 2:hg * 2 + 2, :].unsqueeze(2).broadcast_to([128, 2, 2, 128]),
                    in1=v4(pe_[:, :].rearrange("p (a b) -> p a b", a=4)), op=ALU.mult), reads=[pe_, qt], writes=[qgT])
            self.tri_inverse16(lambda h: LTg[h // 4][:, h % 4, :], LTg, INVg, INVTg, Tsbg, tmpxg, 1.0)
            for hg in range(4):
                hs = slice(hg * 4, hg * 4 + 4)
                INVT = INVTg[hg]
                pu, pw = self.pn(), self.pn()
                for h4 in range(4):
                    h = hg * 4 + h4
                    self.mm(pu, pu[:, h4 * 128:(h4 + 1) * 128], INVT[:, h4, :], vb[:, h, :], True, True, [INVT, vb])
                    self.mm(pw, pw[:, h4 * 128:(h4 + 1) * 128], kbg[:, h, :], INVT[:, h4, :], True, True, [INVT, kbg])
                em.op("act", lambda e, pu=pu, hs=hs: e.activation(out=U[:, hs, :].rearrange("p a b -> p (a b)"), in_=pu[:, :], func=AF.Copy), reads=[pu], writes=[U])
                em.op("dve", lambda e, pw=pw, hs=hs: e.tensor_copy(out=WT[:, hs, :].rearrange("p a b -> p (a b)"), in_=pw[:, :]), reads=[pw], writes=[WT])
            o_ = OT[t % 2]
            for c in range(2):
                r0 = c * 64
                rs_ = slice(r0, r0 + 64)
                pws = [[self.pn(), self.pn()] for hh in range(2)]
                for hh in range(2):
                    for h8 in range(8):
                        h = hh * 8 + h8
                        pb_ = pws[hh][h8 // 4]
                        self.mm(pb_, pb_[rs_, (h8 % 4) * 128:(h8 % 4 + 1) * 128], WT[:, h, rs_], Sb[hh][:, h8, :], True, True, [WT, Sb[hh]])
                for hh in range(2):
                    for b2 in range(2):
                        hsl = slice(hh * 8 + b2 * 4, hh * 8 + b2 * 4 + 4)
                        em.op("dve", lambda e, b2=b2, hsl=hsl, hh=hh: e.tensor_tensor(
                            out=VN[hh][rs_, b2 * 4:b2 * 4 + 4, :].rearrange("p a b -> p (a b)"), in0=U[rs_, hsl, :].rearrange("p a b -> p (a b)"),
                            in1=pws[hh][b2][rs_, :], op=ALU.subtract), reads=[U, pws[hh][b2]], writes=[VN[hh]])
                for hh in range(2):
                    S_, Sb_ = S[hh], Sb[hh]
                    pos = [self.pn(), self.pn()]
                    pss = [self.pn(), self.pn()]
                    for h8 in range(8):
                        h = hh * 8 + h8
                        po_, ps_ = pos[h8 // 4], pss[h8 // 4]
                        cs_ = slice((h8 % 4) * 128, (h8 % 4 + 1) * 128)
                        self.mm(po_, po_[rs_, cs_], QKd[rs_, h, rs_], VN[hh][rs_, h8, :], True, False, [QKd, VN[hh]])
                        self.mm(po_, po_[rs_, cs_], qgT[:, h, rs_], Sb_[:, h8, :], False, True, [qgT, Sb_])
                        self.mm(ps_, ps_[:, cs_], ktk[rs_, h, :], VN[hh][rs_, h8, :], True, True, [ktk, VN[hh]])
                    for b2 in range(2):
                        oc0 = (hh * 8 + b2 * 4) * 128
                        em.op("act", lambda e, b2=b2, oc0=oc0, pos=pos: e.activation(out=o_[rs_, oc0:oc0 + 512], in_=pos[b2][rs_, :], func=AF.Copy),
                              reads=[pos[b2]], writes=[o_])
                    st_ = stmp2[hh]
                    em.op("dve", lambda e, hh=hh, c=c, st_=st_, S_=S_: e.tensor_tensor(
                        out=st_[:], in0=S_[:], in1=vec[:, 4 + c, hh * 8:hh * 8 + 8].unsqueeze(2).broadcast_to([128, 8, 128]), op=ALU.mult),
                        reads=[S_, vec], writes=[st_])
                    for b2 in range(2):
                        em.op("dve", lambda e, b2=b2, pss=pss, st_=st_, S_=S_: e.tensor_tensor(
                            out=S_[:, b2 * 4:b2 * 4 + 4, :].rearrange("p a b -> p (a b)"), in0=st_[:, b2 * 4:b2 * 4 + 4, :].rearrange("p a b -> p (a b)"),
                            in1=pss[b2][:, :], op=ALU.add), reads=[st_, pss[b2]], writes=[S_])
                    em.op("act", lambda e, S_=S_, Sb_=Sb_: e.activation(out=Sb_[:].rearrange("p a b -> p (a b)"), in_=S_[:].rearrange("p a b -> p (a b)"), func=AF.Copy),
                          reads=[S_], writes=[Sb_])
            em.dma("pool", O.ap()[t * 128:(t + 1) * 128, :], o_[:], reads=[o_])
        em.barrier()
        em.release([qt, kt_, km, v, gb, sm, vec, mneg, QKd, qgT, WT, U, ktk, vb, kbg, stmp] + VN + stmp2 + DgA[0] + DgA[1] + DeA + EA[0] + EA[1] + LTg + INVg + INVTg + Tsbg + tmpxg[0] + tmpxg[1] + OT + S + Sb)


MK.dn_phase_s = dn_phase_s


def dn_factory(self):
    em = self.em

    def factory(st):
        ngr = em.sb(st, "zng", [128, 128], F32)
        em.dma("sp", ngr[:], self.dn_in["ng"].ap().partition_broadcast(128), writes=[ngr])
        oo = [em.sb(st, "zoo%d" % q, [128, 4, 128], F32) for q in range(2)]
        p1 = [em.sb(st, "zp1%d" % q, [128, 512], F32) for q in range(2)]
        gt = [em.sb(st, "zgt%d" % q, [128, 512], BF16) for q in range(2)]
        osum = em.sb(st, "zosum", [128, 4, 128], F32)
        sq = em.sb(st, "zsq", [128, 4, 128], F32)
        sg = em.sb(st, "zsg", [128, 4, 128], F32)
        ss = em.sb(st, "zss", [128, 4], F32)
        y = em.sb(st, "zy", [128, 2048], BF16)
        cnt = [0]
        f2 = lambda b: b[:].rearrange("p a b -> p (a b)")

        def make_yT(t, isctx, yT):
            pt = self.ptile(t)
            for blk in range(4):
                k = cnt[0]
                cnt[0] += 1
                o_, a1, g_ = oo[k % 2], p1[k % 2], gt[k % 2]
                cs = slice(blk * 512, (blk + 1) * 512)
                em.dma("sp", f2(o_), self.O2[0].ap()[t * 128:(t + 1) * 128, cs], writes=[o_])
                em.dma("sp", a1[:], self.O2[1].ap()[pt * 128:(pt + 1) * 128, cs], writes=[a1])
                em.dma("sp", g_[:], self.rG.ap()[t][:, cs], writes=[g_])
                ps = self.pn()
                self.mm(ps, ps[:, :], self.cf(C_J0), a1[:], True, True, [self.cmf, a1])
                em.op("dve", lambda e: e.tensor_tensor(out=f2(osum), in0=ps[:, :], in1=f2(o_), op=ALU.add), reads=[ps, o_], writes=[osum])
                em.op("pool", lambda e: e.tensor_tensor(out=sq[:], in0=osum[:], in1=osum[:], op=ALU.mult), reads=[osum], writes=[sq])
                em.op("dve", lambda e: e.tensor_reduce(out=ss[:], in_=sq[:], axis=AX.X, op=ALU.add), reads=[sq], writes=[ss])
                em.op("act", lambda e: e.activation(out=ss[:], in_=ss[:], func=AF.Sqrt, bias=1e-6, scale=1.0 / 128.0), reads=[ss], writes=[ss])
                em.op("dve", lambda e: e.reciprocal(out=ss[:], in_=ss[:]), reads=[ss], writes=[ss])
                em.op("act", lambda e: e.activation(out=f2(sg), in_=g_[:], func=AF.Silu), reads=[g_], writes=[sg])
                em.op("dve", lambda e: e.tensor_tensor(out=osum[:], in0=osum[:], in1=ss[:].unsqueeze(2).broadcast_to([128, 4, 128]), op=ALU.mult),
                      reads=[osum, ss], writes=[osum])
                em.op("pool", lambda e: e.tensor_tensor(out=osum[:], in0=osum[:], in1=ngr[:].unsqueeze(1).broadcast_to([128, 4, 128]), op=ALU.mult),
                      reads=[osum, ngr], writes=[osum])
                em.op("pool", lambda e: e.tensor_tensor(out=y[:, cs], in0=f2(osum), in1=f2(sg), op=ALU.mult), reads=[osum, sg], writes=[y])
            for half in range(2):
                ps = self.pn()
                pb = ps[:].bitcast(BF16)
                for c in range(8):
                    cc = half * 8 + c
                    em.op("pe", lambda e: e.transpose(out=pb[:, c * 128:(c + 1) * 128], in_=y[:, cc * 128:(cc + 1) * 128], identity=self.cb(C_ID)),
                          reads=[y, self.cmb], writes=[ps])
                em.op("act", lambda e: e.activation(out=yT[:, half * 8:(half + 1) * 8, :].rearrange("p c n -> p (c n)"), in_=pb[:, 0:1024], func=AF.Copy),
                      reads=[ps], writes=[yT])
        return make_yT, [ngr, osum, sq, sg, ss, y] + oo + p1 + gt
    return factory


def dn_layer(self, i, modLC, rows):
    if not hasattr(self, "dPRE"):
        self.dn_alloc()
    import os
    dbg = int(os.environ.get("DNDBG", "99"))
    for z in range(2):
        self.dn_phase_a1(modLC, z)
        if dbg >= 2:
            self.dn_phase_a2(z)
        if dbg >= 3:
            self.dn_phase_s(z)
    if dbg >= 4:
        self.phase_f(i, modLC, rows, self.dn_factory(), "dnout", 16)


MK.dn_factory = dn_factory
MK.dn_layer = dn_layer


def rk_alloc(self):
    NT, T = self.NT, self.T
    self.kHT = self.dr("kHT", [8, 128, T], F32)
    fm = lambda n: [self.dr("k%s%d" % (n, z), [8, 128, T], F32) for z in range(2)]
    self.kR, self.kK, self.kA, self.kB = fm("R"), fm("K"), fm("A"), fm("B")
    self.kV = [self.dr("kV%d" % z, [NT, 128, 1024], BF16) for z in range(2)]
    self.kLW = [self.dr("kLW%d" % z, [NT, 128, 1024], F32) for z in range(2)]
    self.kBT = self.dr("kBT", [8, 128, T], BF16)
    self.kGT = self.dr("kGT", [8, 128, T], BF16)
    self.rk_in = dict(
        mix=self.xin("rkmix", [128, 48]), vec=self.xin("rkvec", [128, 32]),
        w0=[self.xin("rkw0_%d" % z, [1, 1024]) for z in range(2)],
        a0=[self.xin("rka0_%d" % z, [128, 8]) for z in range(2)],
        w1=[self.xin("rkw1_%d" % z, [128, 512]) for z in range(2)],
        a1=[self.xin("rka1_%d" % z, [128, 512]) for z in range(2)],
        w2=[self.xin("rkw2_%d" % z, [64, 1024]) for z in range(2)],
        a2=[self.xin("rka2_%d" % z, [64, 1024]) for z in range(2)])


def rk_phase_a1(self, modLC, z):
    em = self.em
    LAT = self.LAT2[z]
    with ExitStack() as st:
        hT = [em.sb(st, "khT%d" % q, [128, 8, 512], F32) for q in range(2)]
        lat = [em.sb(st, "klat%d" % q, [128, D], F32) for q in range(2)]
        kk = 0
        for gi, (t0, ng, isctx) in enumerate(self.groups):
            h = hT[gi % 2]
            for ti in range(ng):
                la = lat[kk % 2]
                kk += 1
                em.dma("sp", la[:], LAT.ap()[(t0 + ti) * 128:(t0 + ti + 1) * 128, :], writes=[la])
                self.hT_tile(la, h, ti * 128, modLC, 0, isctx)
            N = ng * 128
            em.dma("pool", self.kHT.ap()[:, :, t0 * 128:t0 * 128 + N].rearrange("c p n -> p c n"), h[:, :, 0:N], reads=[h])
        em.barrier()
        em.release(hT + lat)


def rk_phase_a2(self, z):
    em = self.em
    T, NT = self.T, self.NT
    I = self.rk_in
    N = 256
    with ExitStack() as st:
        B = lambda n, sh, dt: em.sb(st, "a" + n, sh, dt)
        mixv = B("mixv", [128, 48], F32)
        vecs = B("vecs", [128, 40], F32)
        em.dma("sp", mixv[:], I["mix"].ap(), writes=[mixv])
        em.dma("sp", vecs[:, 0:32], I["vec"].ap(), writes=[vecs])
        em.op("dve", lambda e: e.tensor_scalar(out=vecs[:, 32:40], in0=vecs[:, 8:16], scalar1=-1.0, scalar2=1.0, op0=ALU.mult, op1=ALU.add),
              reads=[vecs], writes=[vecs])
        zo = [z, 1 - z]
        a0 = [B("a0_%d" % q, [128, 8], F32) for q in range(2)]
        w1 = B("w1", [128, 8, 64], BF16)
        a1 = [B("a1_%d" % q, [128, 8, 64], BF16) for q in range(2)]
        w2 = B("w2", [64, 1024], BF16)
        a2 = [B("a2_%d" % q, [64, 1024], BF16) for q in range(2)]
        w0r = B("w0r", [128, 1024], F32)
        em.dma("sp", w0r[:], I["w0"][z].ap().partition_broadcast(128), writes=[w0r])
        em.dma("pool", w1[:].rearrange("p c n -> p (c n)"), I["w1"][z].ap(), writes=[w1])
        em.dma("pool", w2[:], I["w2"][z].ap(), writes=[w2])
        for q in range(2):
            em.dma("sp", a0[q][:], I["a0"][zo[q]].ap(), writes=[a0[q]])
            em.dma("pool", a1[q][:].rearrange("p c n -> p (c n)"), I["a1"][zo[q]].ap(), writes=[a1[q]])
            em.dma("pool", a2[q][:], I["a2"][zo[q]].ap(), writes=[a2[q]])
        Wr, Wk, Wv = [B("W%d" % q, [128, 8, 1024], BF16) for q in range(3)]
        for wb_, nm in ((Wr, "rkr"), (Wk, "rkk"), (Wv, "rkv")):
            self.load_w(wb_, nm, 0, 8, 0, 1024)
        g1 = B("g1", [128, 8, 128], BF16)
        g2 = B("g2", [128, 1, 1024], BF16)
        self.load_w(g1, "rkg1", 0, 8, 0, 128)
        self.load_w(g2, "rkg2", 0, 1, 0, 1024)
        hW = B("hW", [128, 8, N + 2], F32)
        xx = B("xx", [128, 8, N], F32)
        xm = [B("xm%d" % m, [128, 8, N], BF16) for m in range(6)]
        t1T = B("t1T", [64, N], BF16)
        u1T = [B("u1T%d" % q, [64, N], BF16) for q in range(2)]
        gsT = B("gsT", [128, N], BF16)
        ch = {n: B("c" + n, [128, N], F32) for n in ("r", "k", "v", "a", "ao", "kk", "t", "t2", "kd")}
        sqb = B("sqb", [128, N], BF16)
        obf = [B("obf%d" % q, [128, N], BF16) for q in range(2)]
        vtm = [B("vtm%d" % q, [128, 1024], BF16) for q in range(2)]
        lwt = [B("lwt%d" % q, [128, 1024], F32) for q in range(2)]
        groups = [(0, 256, 0, 256)] + [(256 + g * N, N, 256, T) for g in range((T - 256) // N)]
        nob = 0
        for (s0, n_, lo, hi) in groups:
            a_, b_ = max(lo, s0 - 1), min(hi, s0 + n_ + 1)
            if a_ > s0 - 1:
                em.op("pool", lambda e: e.memset(hW[:, :, 0:1], 0.0), writes=[hW])
            if b_ < s0 + n_ + 1:
                em.op("pool", lambda e: e.memset(hW[:, :, N + 1:N + 2], 0.0), writes=[hW])
            em.dma("sp", hW[:, :, a_ - (s0 - 1):b_ - (s0 - 1)], self.kHT.ap()[:, :, a_:b_].rearrange("c p n -> p c n"), writes=[hW])
            em.op("dve", lambda e: e.tensor_tensor(out=xx[:], in0=hW[:, :, 0:N], in1=hW[:, :, 2:N + 2], op=ALU.add), reads=[hW], writes=[xx])
            em.op("dve", lambda e: e.scalar_tensor_tensor(out=xx[:], in0=xx[:], scalar=0.5, in1=hW[:, :, 1:N + 1], op0=ALU.mult, op1=ALU.subtract),
                  reads=[xx, hW], writes=[xx])
            for m in range(6):
                if z == 1 and m == 5:
                    continue
                for c in range(8):
                    em.op("dve", lambda e, m=m, c=c: e.scalar_tensor_tensor(
                        out=xm[m][:, c, :], in0=xx[:, c, :], scalar=mixv[:, m * 8 + c:m * 8 + c + 1], in1=hW[:, c, 1:N + 1],
                        op0=ALU.mult, op1=ALU.add), reads=[xx, hW, mixv], writes=[xm[m]])
            ps = self.pn()
            for kc in range(8):
                self.mm(ps, ps[0:64, 0:N], w1[:, kc, :], xm[3][:, kc, :], kc == 0, kc == 7, [w1, xm[3]])
            em.op("act", lambda e, ps=ps: e.activation(out=t1T[:], in_=ps[0:64, 0:N], func=AF.Tanh), reads=[ps], writes=[t1T])
            for q in range(2 if z == 0 else 1):
                ps = self.pn()
                for kc in range(8):
                    self.mm(ps, ps[0:64, 0:N], a1[q][:, kc, :], xm[4][:, kc, :], kc == 0, kc == 7, [a1[q], xm[4]])
                em.op("act", lambda e, ps=ps, q=q: e.activation(out=u1T[q][:], in_=ps[0:64, 0:N], func=AF.Copy), reads=[ps], writes=[u1T[q]])
            if z == 0:
                ps = self.pn()
                for kc in range(8):
                    self.mm(ps, ps[:, 0:N], g1[:, kc, :], xm[5][:, kc, :], kc == 0, kc == 7, [g1, xm[5]])
                em.op("act", lambda e, ps=ps: e.activation(out=gsT[:], in_=ps[:, 0:N], func=AF.Sigmoid), reads=[ps], writes=[gsT])
            for ti in range(2):
                t = s0 // 128 + ti
                v_, l_ = vtm[ti], lwt[ti]
                for half in range(2):
                    ps = self.pn()
                    for kc in range(8):
                        self.mm(ps, ps[:, :], xm[2][:, kc, ti * 128:(ti + 1) * 128], Wv[:, kc, half * 512:(half + 1) * 512], kc == 0, kc == 7, [xm[2], Wv])
                    em.op("act", lambda e, ps=ps, half=half: e.activation(out=v_[:, half * 512:(half + 1) * 512], in_=ps[:, :], func=AF.Copy), reads=[ps], writes=[v_])
                    ps = self.pn()
                    self.mm(ps, ps[:, :], t1T[:, ti * 128:(ti + 1) * 128], w2[:, half * 512:(half + 1) * 512], True, True, [t1T, w2])
                    em.op("dve", lambda e, ps=ps, half=half: e.tensor_tensor(out=l_[:, half * 512:(half + 1) * 512], in0=ps[:, :], in1=w0r[:, half * 512:(half + 1) * 512], op=ALU.add),
                          reads=[ps, w0r], writes=[l_])
                em.op("act", lambda e: e.activation(out=l_[:], in_=l_[:], func=AF.Sigmoid), reads=[l_], writes=[l_])
                em.op("dve", lambda e: e.tensor_scalar(out=l_[:], in0=l_[:], scalar1=-float(np.exp(-0.5)), scalar2=None, op0=ALU.mult), reads=[l_], writes=[l_])
                em.dma("pool", self.kV[z].ap()[t], v_[:], reads=[v_])
                em.dma("pool", self.kLW[z].ap()[t], l_[:], reads=[l_])
            for c in range(8):
                cs = slice(c * 128, (c + 1) * 128)
                for nm, m, W_ in (("r", 0, Wr), ("k", 1, Wk), ("v", 2, Wv)):
                    ps = self.pn()
                    for kc in range(8):
                        self.mm(ps, ps[:, 0:N], W_[:, kc, cs], xm[m][:, kc, :], kc == 0, kc == 7, [W_, xm[m]])
                    em.op("act", lambda e, ps=ps, nm=nm: e.activation(out=ch[nm][:], in_=ps[:, 0:N], func=AF.Copy), reads=[ps], writes=[ch[nm]])
                for q, nm in ((0, "a"), (1, "ao")):
                    if q == 1 and z == 1:
                        continue
                    ps = self.pn()
                    self.mm(ps, ps[:, 0:N], a2[q][:, cs], u1T[q][:], True, True, [a2[q], u1T[q]])
                    em.op("act", lambda e, ps=ps, nm=nm, q=q: e.activation(out=ch[nm][:], in_=ps[:, 0:N], func=AF.Sigmoid, bias=a0[q][:, c:c + 1], scale=1.0),
                          reads=[ps, a0[q]], writes=[ch[nm]])
                em.op("dve", lambda e: e.tensor_scalar(out=ch["kk"][:], in0=ch["k"][:], scalar1=vecs[:, c:c + 1], scalar2=None, op0=ALU.mult), reads=[ch["k"], vecs], writes=[ch["kk"]])
                em.op("dve", lambda e: e.tensor_tensor(out=sqb[:], in0=ch["kk"][:], in1=ch["kk"][:], op=ALU.mult), reads=[ch["kk"]], writes=[sqb])
                ps = self.pn()
                self.mm(ps, ps[:, 0:N], self.cb(C_B64), sqb[:], True, True, [self.cmb, sqb])
                em.op("act", lambda e, ps=ps: e.activation(out=ch["t"][:], in_=ps[:, 0:N], func=AF.Sqrt, bias=1e-6, scale=1.0), reads=[ps], writes=[ch["t"]])
                em.op("dve", lambda e: e.reciprocal(out=ch["t"][:], in_=ch["t"][:]), reads=[ch["t"]], writes=[ch["t"]])
                em.op("dve", lambda e: e.tensor_tensor(out=ch["kk"][:], in0=ch["kk"][:], in1=ch["t"][:], op=ALU.mult), reads=[ch["kk"], ch["t"]], writes=[ch["kk"]])
                em.op("dve", lambda e: e.tensor_scalar(out=ch["t"][:], in0=ch["a"][:], scalar1=vecs[:, 8 + c:9 + c], scalar2=vecs[:, 32 + c:33 + c], op0=ALU.mult, op1=ALU.add),
                      reads=[ch["a"], vecs], writes=[ch["t"]])
                em.op("dve", lambda e: e.tensor_tensor(out=ch["kd"][:], in0=ch["k"][:], in1=ch["t"][:], op=ALU.mult), reads=[ch["k"], ch["t"]], writes=[ch["kd"]])
                em.op("dve", lambda e: e.tensor_tensor(out=ch["t"][:], in0=ch["kk"][:], in1=ch["a"][:], op=ALU.mult), reads=[ch["kk"], ch["a"]], writes=[ch["t"]])
                em.op("dve", lambda e: e.tensor_scalar(out=ch["kk"][:], in0=ch["kk"][:], scalar1=-1.0, scalar2=None, op0=ALU.mult), reads=[ch["kk"]], writes=[ch["kk"]])
                for dst, nm in ((self.kR, "r"), (self.kK, "kd"), (self.kA, "kk"), (self.kB, "t")):
                    em.dma("pool", dst[z].ap()[c][:, s0:s0 + n_], ch[nm][:, 0:n_], reads=[ch[nm]])
                if z == 0:
                    em.op("dve", lambda e: e.tensor_scalar(out=ch["t2"][:], in0=ch["ao"][:], scalar1=vecs[:, 8 + c:9 + c], scalar2=vecs[:, 32 + c:33 + c], op0=ALU.mult, op1=ALU.add),
                          reads=[ch["ao"], vecs], writes=[ch["t2"]])
                    em.op("dve", lambda e: e.tensor_tensor(out=ch["t2"][:], in0=ch["t2"][:], in1=ch["k"][:], op=ALU.mult), reads=[ch["t2"], ch["k"]], writes=[ch["t2"]])
                    em.op("dve", lambda e: e.tensor_tensor(out=ch["t2"][:], in0=ch["t2"][:], in1=ch["kd"][:], op=ALU.add), reads=[ch["t2"], ch["kd"]], writes=[ch["t2"]])
                    em.op("dve", lambda e: e.scalar_tensor_tensor(out=sqb[:], in0=ch["t2"][:], scalar=vecs[:, 16 + c:17 + c], in1=ch["r"][:], op0=ALU.mult, op1=ALU.mult),
                          reads=[ch["t2"], ch["r"], vecs], writes=[sqb])
                    ps = self.pn()
                    self.mm(ps, ps[:, 0:N], self.cb(C_B64), sqb[:], True, True, [self.cmb, sqb])
                    ob = obf[nob % 2]
                    nob += 1
                    em.op("dve", lambda e, ps=ps, ob=ob: e.tensor_tensor(out=ob[:], in0=ps[:, 0:N], in1=ch["v"][:], op=ALU.mult), reads=[ps, ch["v"]], writes=[ob])
                    em.dma("pool", self.kBT.ap()[c][:, s0:s0 + n_], ob[:, 0:n_], reads=[ob])
                    ps = self.pn()
                    self.mm(ps, ps[:, 0:N], g2[:, 0, cs], gsT[:], True, True, [g2, gsT])
                    ob = obf[nob % 2]
                    nob += 1
                    em.op("act", lambda e, ps=ps, ob=ob: e.activation(out=ob[:], in_=ps[:, 0:N], func=AF.Copy), reads=[ps], writes=[ob])
                    em.dma("pool", self.kGT.ap()[c][:, s0:s0 + n_], ob[:, 0:n_], reads=[ob])
        em.barrier()
        em.release([mixv, vecs, w1, w2, w0r, Wr, Wk, Wv, g1, g2, hW, xx, t1T, gsT, sqb] + a0 + a1 + a2 + xm + u1T + list(ch.values()) + obf + vtm + lwt)


MK.rk_alloc = rk_alloc
MK.rk_phase_a1 = rk_phase_a1
MK.rk_phase_a2 = rk_phase_a2


def rk_phase_s(self, z):
    em = self.em
    T, NT = self.T, self.NT
    O = self.O2[z]
    with ExitStack() as st:
        B = lambda n, sh, dt: em.sb(st, "s" + n, sh, dt)
        ld = {n: B("ld" + n, [128, 8, 128], F32) for n in "rkab"}
        v = B("v", [128, 1024], BF16)
        lw = B("lw", [128, 1024], F32)
        ex = {n: B("ex" + n, [128, 8, 128], F32) for n in ("g", "gx", "ng", "gl")}
        GL = B("GL", [128, 8, 2], F32)
        AR = B("AR", [128, 8, 2, 128], BF16)
        BK = B("BK", [128, 8, 2, 128], BF16)
        BKh = B("BKh", [128, 8, 2, 128], BF16)
        BhT = B("BhT", [128, 8, 128], BF16)
        KhT = B("KhT", [128, 8, 128], BF16)
        MASK4 = B("MASK4", [128, 4, 128], BF16)
        for q, cidx, sgn in ((0, C_MS, -1.0), (1, C_MI, 1.0), (2, C_MS, 1.0), (3, C_MI, 1.0)):
            em.op("dve", lambda e, q=q, cidx=cidx: e.tensor_copy(out=MASK4[:, q, :], in_=self.cb(cidx)), reads=[self.cmb], writes=[MASK4])
        AM = B("AM", [128, 4, 16, 128], BF16)
        INVg = [B("INV%d" % g, [128, 4, 128], BF16) for g in range(4)]
        INVTg = [B("INVT%d" % g, [128, 4, 128], BF16) for g in range(4)]
        Tsbg = [B("Tsb%d" % g, [128, 4, 128], BF16) for g in range(4)]
        tmpxg = [[B("tmpx%d_%d" % (q, g), [128, 4, 128], BF16) for g in range(4)] for q in range(2)]
        RHSb = B("RHSb", [128, 1024], BF16)
        Ub = B("Ub", [128, 1024], BF16)
        OT = [B("OT%d" % q, [128, 1024], F32) for q in range(2)]
        S = B("S", [128, 8, 2, 64], F32)
        Sb = B("Sb", [128, 8, 2, 64], BF16)
        stmp = B("stmp", [128, 8, 128], F32)
        t2m = B("t2m", [128, 4, 128], F32)
        em.op("pool", lambda e: e.memset(S[:], 0.0), writes=[S])
        em.op("pool", lambda e: e.memset(Sb[:], 0.0), writes=[Sb])
        srcs = dict(r=self.kR[z], k=self.kK[z], a=self.kA[z], b=self.kB[z])
        f2 = lambda ap: ap.rearrange("p a b -> p (a b)")
        for t in range(NT):
            c0 = t * 128
            for n in "rkab":
                em.dma("sp", ld[n][:], srcs[n].ap()[:, :, c0:c0 + 128].rearrange("c p n -> p c n"), writes=[ld[n]])
            em.dma("sp", v[:], self.kV[z].ap()[t], writes=[v])
            em.dma("sp", lw[:], self.kLW[z].ap()[t], writes=[lw])
            for c in range(8):
                ps = self.pn()
                for q, cidx in enumerate((C_TRI, C_TRIS, C_BLK)):
                    self.mm(ps, ps[:, q * 128:(q + 1) * 128], lw[:, c * 128:(c + 1) * 128], self.cf(cidx), True, True, [lw, self.cmf])
                em.op("act", lambda e, ps=ps, c=c: e.activation(out=ex["g"][:, c, :], in_=ps[:, 0:128], func=AF.Exp), reads=[ps], writes=[ex["g"]])
                em.op("act", lambda e, ps=ps, c=c: e.activation(out=ex["gx"][:, c, :], in_=ps[:, 128:256], func=AF.Exp), reads=[ps], writes=[ex["gx"]])
                em.op("act", lambda e, ps=ps, c=c: e.activation(out=ex["ng"][:, c, :], in_=ps[:, 0:128], func=AF.Exp, scale=-1.0), reads=[ps], writes=[ex["ng"]])
                em.op("act", lambda e, ps=ps, c=c: e.activation(out=ex["gl"][:, c, :], in_=ps[:, 256:384], func=AF.Exp), reads=[ps], writes=[ex["gl"]])
            em.op("dve", lambda e: e.tensor_copy(out=GL[:], in_=ex["gl"][:].rearrange("p c (a b) -> p c a b", a=2)[:, :, :, 0]), reads=[ex["gl"]], writes=[GL])
            em.op("dve", lambda e: e.tensor_tensor(out=ex["gl"][:], in0=ex["gl"][:], in1=ex["ng"][:], op=ALU.mult), reads=[ex["gl"], ex["ng"]], writes=[ex["gl"]])
            em.op("dve", lambda e: e.tensor_tensor(out=AR[:, :, 0, :], in0=ld["a"][:], in1=ex["gx"][:], op=ALU.mult), reads=[ld["a"], ex["gx"]], writes=[AR])
            em.op("dve", lambda e: e.tensor_tensor(out=AR[:, :, 1, :], in0=ld["r"][:], in1=ex["g"][:], op=ALU.mult), reads=[ld["r"], ex["g"]], writes=[AR])
            em.op("pool", lambda e: e.tensor_tensor(out=BK[:, :, 0, :], in0=ld["b"][:], in1=ex["ng"][:], op=ALU.mult), reads=[ld["b"], ex["ng"]], writes=[BK])
            em.op("pool", lambda e: e.tensor_tensor(out=BK[:, :, 1, :], in0=ld["k"][:], in1=ex["ng"][:], op=ALU.mult), reads=[ld["k"], ex["ng"]], writes=[BK])
            em.op("pool", lambda e: e.tensor_tensor(out=BKh[:, :, 0, :], in0=ld["b"][:], in1=ex["gl"][:], op=ALU.mult), reads=[ld["b"], ex["gl"]], writes=[BKh])
            em.op("dve", lambda e: e.tensor_tensor(out=BKh[:, :, 1, :], in0=ld["k"][:], in1=ex["gl"][:], op=ALU.mult), reads=[ld["k"], ex["gl"]], writes=[BKh])
            import os
            dS = int(os.environ.get("RKS", "99"))
            if dS <= 1:
                continue
            for q, dstb in ((0, BhT), (1, KhT)):
                ps = self.pn()
                pb = ps[:].bitcast(BF16)
                for c in range(8):
                    em.op("pe", lambda e, pb=pb, c=c, q=q: e.transpose(out=pb[:, c * 128:(c + 1) * 128], in_=BKh[:, c, q, :], identity=self.cb(C_ID)),
                          reads=[BKh, self.cmb], writes=[ps])
                em.op("act", lambda e, pb=pb, dstb=dstb: e.activation(out=f2(dstb[:]), in_=pb[:, 0:1024], func=AF.Copy), reads=[ps], writes=[dstb])
            if dS <= 2:
                continue
            for h in range(16):
                c, base = h // 2, (h % 2) * 64
                ps = self.pn()
                rhs = AR[base:base + 64, c, :, :].rearrange("p a b -> p (a b)")
                self.mm(ps, ps[:, 0:256], BK[base:base + 64, c, 0, :], rhs, True, True, [BK, AR])
                self.mm(ps, ps[:, 256:512], BK[base:base + 64, c, 1, :], rhs, True, True, [BK, AR])
                em.op("dve", lambda e, ps=ps, h=h: e.tensor_tensor(out=AM[:, :, h, :], in0=ps[:, :].rearrange("p (a b) -> p a b", a=4), in1=MASK4[:], op=ALU.mult),
                      reads=[ps, MASK4], writes=[AM])
            if dS <= 3:
                continue
            self.tri_inverse16(lambda h: AM[:, 0, h, :], [AM] * 4, INVg, INVTg, Tsbg, tmpxg, -1.0)
            if dS <= 4:
                continue
            o_ = OT[t % 2]
            for cc in range(2):
                r0 = cc * 64
                rs_ = slice(r0, r0 + 64)
                pr = [self.pn(), self.pn()]
                for c in range(8):
                    pb_ = pr[c // 4]
                    q0 = (c % 4) * 128
                    self.mm(pb_, pb_[rs_, q0:q0 + 128], AR[:, c, 0, rs_], Sb[:, c, :, :].rearrange("p a b -> p (a b)"), True, False, [AR, Sb])
                    for e_ in range(2):
                        h = 2 * c + e_
                        self.mm(pb_, pb_[rs_, q0 + e_ * 64:q0 + (e_ + 1) * 64], AM[rs_, 2, h, rs_], v[rs_, h * 64:(h + 1) * 64], False, True, [AM, v])
                for b2 in range(2):
                    em.op("act" if b2 else "dve", lambda e, b2=b2: (e.activation(out=RHSb[rs_, b2 * 512:(b2 + 1) * 512], in_=pr[b2][rs_, :], func=AF.Copy) if b2 else
                                                                   e.tensor_copy(out=RHSb[rs_, 0:512], in_=pr[0][rs_, :])), reads=[pr[b2]], writes=[RHSb])
                pu = [self.pn(), self.pn()]
                for h in range(16):
                    pb_ = pu[h // 8]
                    cs_ = slice((h % 8) * 64, (h % 8 + 1) * 64)
                    self.mm(pb_, pb_[rs_, cs_], INVTg[h // 4][rs_, h % 4, rs_], RHSb[rs_, h * 64:(h + 1) * 64], True, True, [INVTg[h // 4], RHSb])
                for b2 in range(2):
                    em.op("act" if b2 else "dve", lambda e, b2=b2: (e.activation(out=Ub[rs_, b2 * 512:(b2 + 1) * 512], in_=pu[b2][rs_, :], func=AF.Copy) if b2 else
                                                                   e.tensor_copy(out=Ub[rs_, 0:512], in_=pu[0][rs_, :])), reads=[pu[b2]], writes=[Ub])
                po = [self.pn(), self.pn()]
                pS = [self.pn(), self.pn()]
                for c in range(8):
                    pb_ = po[c // 4]
                    q0 = (c % 4) * 128
                    self.mm(pb_, pb_[rs_, q0:q0 + 128], AR[:, c, 1, rs_], Sb[:, c, :, :].rearrange("p a b -> p (a b)"), True, False, [AR, Sb])
                    for e_ in range(2):
                        h = 2 * c + e_
                        osl = pb_[rs_, q0 + e_ * 64:q0 + (e_ + 1) * 64]
                        self.mm(pb_, osl, AM[rs_, 1, h, rs_], Ub[rs_, h * 64:(h + 1) * 64], False, False, [AM, Ub])
                        self.mm(pb_, osl, AM[rs_, 3, h, rs_], v[rs_, h * 64:(h + 1) * 64], False, True, [AM, v])
                    ps_ = pS[c // 4]
                    self.mm(ps_, ps_[:, q0:q0 + 128], BhT[rs_, c, :], Ub[rs_, c * 128:(c + 1) * 128], True, False, [BhT, Ub])
                    self.mm(ps_, ps_[:, q0:q0 + 128], KhT[rs_, c, :], v[rs_, c * 128:(c + 1) * 128], False, True, [KhT, v])
                for b2 in range(2):
                    em.op("act", lambda e, b2=b2: e.activation(out=o_[rs_, b2 * 512:(b2 + 1) * 512], in_=po[b2][rs_, :], func=AF.Copy), reads=[po[b2]], writes=[o_])
                em.op("dve", lambda e, cc=cc: e.tensor_tensor(out=stmp[:], in0=S[:].rearrange("p c a b -> p c (a b)"),
                                                              in1=GL[:, :, cc].unsqueeze(2).broadcast_to([128, 8, 128]), op=ALU.mult),
                      reads=[S, GL], writes=[stmp])
                for b2 in range(2):
                    em.op("dve", lambda e, b2=b2: e.tensor_tensor(out=t2m[:], in0=pS[b2][:, :].rearrange("p (c n) -> p c n", c=4),
                                                                  in1=self.cb(C_B64).unsqueeze(1).broadcast_to([128, 4, 128]), op=ALU.mult),
                          reads=[pS[b2], self.cmb], writes=[t2m])
                    em.op("pool", lambda e, b2=b2: e.tensor_tensor(out=S[:, b2 * 4:(b2 + 1) * 4, :, :].rearrange("p c a b -> p c (a b)"),
                                                                   in0=stmp[:, b2 * 4:(b2 + 1) * 4, :], in1=t2m[:], op=ALU.add),
                          reads=[stmp, t2m], writes=[S])
                em.op("act", lambda e: e.activation(out=Sb[:].rearrange("p c a b -> p (c a b)"), in_=S[:].rearrange("p c a b -> p (c a b)"), func=AF.Copy),
                      reads=[S], writes=[Sb])
            em.dma("pool", O.ap()[t * 128:(t + 1) * 128, 0:1024], o_[:], reads=[o_])
        em.barrier()
        em.release(list(ld.values()) + list(ex.values()) + [v, lw, GL, AR, BK, BKh, BhT, KhT, MASK4, AM, RHSb, Ub, S, Sb, stmp, t2m] + INVg + INVTg + Tsbg + tmpxg[0] + tmpxg[1] + OT)


def rk_factory(self):
    em = self.em

    def factory(st):
        vecs = em.sb(st, "qvecs", [128, 32], F32)
        em.dma("sp", vecs[:], self.rk_in["vec"].ap(), writes=[vecs])
        oo = [em.sb(st, "qoo%d" % q, [128, 8, 64], F32) for q in range(2)]
        p1 = [em.sb(st, "qp1%d" % q, [128, 512], F32) for q in range(2)]
        bt = [em.sb(st, "qbt%d" % q, [128, 8, 128], BF16) for q in range(2)]
        gt = [em.sb(st, "qgt%d" % q, [128, 8, 128], BF16) for q in range(2)]
        osum = em.sb(st, "qosum", [128, 8, 64], F32)
        sq = em.sb(st, "qsq", [128, 8, 64], F32)
        ss = em.sb(st, "qss", [128, 8], F32)
        y = em.sb(st, "qy", [128, 1024], BF16)
        tt = em.sb(st, "qtt", [128, 128], F32)
        cnt = [0]
        f2 = lambda b: b[:].rearrange("p a b -> p (a b)")
        bc8 = lambda ap: ap.unsqueeze(2).broadcast_to([128, 8, 64])

        def make_yT(t, isctx, yT):
            pt = self.ptile(t)
            b_, g_ = bt[t % 2], gt[t % 2]
            em.dma("sp", b_[:], self.kBT.ap()[:, :, t * 128:(t + 1) * 128].rearrange("c p n -> p c n"), writes=[b_])
            em.dma("sp", g_[:], self.kGT.ap()[:, :, t * 128:(t + 1) * 128].rearrange("c p n -> p c n"), writes=[g_])
            for blk in range(2):
                k = cnt[0]
                cnt[0] += 1
                o_, a1 = oo[k % 2], p1[k % 2]
                cs = slice(blk * 512, (blk + 1) * 512)
                em.dma("sp", f2(o_), self.O2[0].ap()[t * 128:(t + 1) * 128, cs], writes=[o_])
                em.dma("sp", a1[:], self.O2[1].ap()[pt * 128:(pt + 1) * 128, cs], writes=[a1])
                ps = self.pn()
                self.mm(ps, ps[:, :], self.cf(C_J0), a1[:], True, True, [self.cmf, a1])
                em.op("dve", lambda e: e.tensor_tensor(out=f2(osum), in0=ps[:, :], in1=f2(o_), op=ALU.add), reads=[ps, o_], writes=[osum])
                em.op("dve", lambda e: e.tensor_reduce(out=ss[:], in_=osum[:], axis=AX.X, op=ALU.add), reads=[osum], writes=[ss])
                em.op("dve", lambda e: e.tensor_scalar(out=ss[:], in0=ss[:], scalar1=-1.0 / 64.0, scalar2=None, op0=ALU.mult), reads=[ss], writes=[ss])
                em.op("dve", lambda e: e.tensor_tensor(out=osum[:], in0=osum[:], in1=bc8(ss[:]), op=ALU.add), reads=[osum, ss], writes=[osum])
                em.op("pool", lambda e: e.tensor_tensor(out=sq[:], in0=osum[:], in1=osum[:], op=ALU.mult), reads=[osum], writes=[sq])
                em.op("dve", lambda e: e.tensor_reduce(out=ss[:], in_=sq[:], axis=AX.X, op=ALU.add), reads=[sq], writes=[ss])
                em.op("act", lambda e: e.activation(out=ss[:], in_=ss[:], func=AF.Sqrt, bias=64e-5, scale=1.0 / 64.0), reads=[ss], writes=[ss])
                em.op("dve", lambda e: e.reciprocal(out=ss[:], in_=ss[:]), reads=[ss], writes=[ss])
                em.op("dve", lambda e: e.tensor_tensor(out=y[:, cs].rearrange("p (a b) -> p a b", a=8), in0=osum[:], in1=bc8(ss[:]), op=ALU.mult),
                      reads=[osum, ss], writes=[y])
            ps = self.pn()
            pb = ps[:].bitcast(BF16)
            for c in range(8):
                em.op("pe", lambda e: e.transpose(out=pb[:, c * 128:(c + 1) * 128], in_=y[:, c * 128:(c + 1) * 128], identity=self.cb(C_ID)),
                      reads=[y, self.cmb], writes=[ps])
            for c in range(8):
                em.op("dve", lambda e: e.scalar_tensor_tensor(out=tt[:], in0=pb[:, c * 128:(c + 1) * 128], scalar=vecs[:, 24 + c:25 + c], in1=b_[:, c, :],
                                                              op0=ALU.mult, op1=ALU.add), reads=[ps, vecs, b_], writes=[tt])
                em.op("pool", lambda e: e.tensor_tensor(out=yT[:, c, :], in0=tt[:], in1=g_[:, c, :], op=ALU.mult), reads=[tt, g_], writes=[yT])
        return make_yT, [vecs, osum, sq, ss, y, tt] + oo + p1 + bt + gt
    return factory


def rk_layer(self, i, modLC, rows):
    if not hasattr(self, "kHT"):
        self.rk_alloc()
    import os
    dbg = int(os.environ.get("RKDBG", "99"))
    for z in range(2):
        self.rk_phase_a1(modLC, z)
        if dbg >= 2:
            self.rk_phase_a2(z)
        if dbg >= 3:
            self.rk_phase_s(z)
    if dbg >= 4:
        self.phase_f(i, modLC, rows, self.rk_factory(), "rkout", 8)


MK.rk_phase_s = rk_phase_s
MK.rk_factory = rk_factory
MK.rk_layer = rk_layer
```

```python
import numpy as np
from contextlib import ExitStack
import concourse.bass as bass
import concourse.mybir as mybir
from concourse.bass_utils import run_bass_kernel_spmd

F32 = mybir.dt.float32
BF16 = mybir.dt.bfloat16
AF = mybir.ActivationFunctionType
ALU = mybir.AluOpType
AX = mybir.AxisListType

D = 1024
NCTX = 256
ALPHA = 8.0 ** 0.25
LN_EPS = 1e-5


class Sem:
    __slots__ = ("h", "total")

    def __init__(self, h):
        self.h = h
        self.total = 0


class Buf:
    __slots__ = ("t", "w", "r", "sem", "name")

    def __init__(self, t, name):
        self.t = t
        self.w = None
        self.r = {}
        self.sem = None
        self.name = name

    def __getitem__(self, k):
        return self.t[k]


class Em:
    def __init__(self, nc):
        self.nc = nc
        self.eng = {"pe": nc.tensor, "act": nc.scalar, "dve": nc.vector, "pool": nc.gpsimd, "sp": nc.sync}
        self.semobj = {k: Sem(nc.alloc_semaphore(name="s_" + k)) for k in self.eng}
        self.waited = {k: {} for k in self.eng}
        self.dma_sems = []
        self.free_dma_sems = []
        self.bufs = []
        self.nins = 0

    def sb(self, stack, name, shape, dt):
        self.uid = getattr(self, "uid", 0) + 1
        t = stack.enter_context(self.nc.sbuf_tensor("sb%d_%s" % (self.uid, name), list(shape), dt))
        b = Buf(t, name)
        self.bufs.append(b)
        return b

    def ps(self, stack, name, shape=(128, 512), dt=F32):
        t = stack.enter_context(self.nc.psum_tensor("pp_" + name, list(shape), dt))
        b = Buf(t, name)
        self.bufs.append(b)
        return b

    def _dma_sem(self):
        if self.free_dma_sems:
            return self.free_dma_sems.pop()
        s = Sem(self.nc.alloc_semaphore(name="d%d" % len(self.dma_sems)))
        self.dma_sems.append(s)
        return s

    def release(self, bufs):
        for b in bufs:
            if b.sem is not None:
                self.free_dma_sems.append(b.sem)
                b.sem = None
            if b in self.bufs:
                self.bufs.remove(b)

    def _wait(self, en, toks):
        E = self.eng[en]
        w = self.waited[en]
        own = self.semobj[en]
        best = {}
        for (s, v) in toks:
            if s is own and en in ("pe", "sp"):
                continue
            if best.get(s, 0) < v:
                best[s] = v
        for s, v in best.items():
            if w.get(s, 0) < v:
                E.wait_ge(s.h, v)
                w[s] = v

    def _deps(self, reads, writes):
        toks = []
        for b in reads:
            if b.w is not None:
                toks.append(b.w)
        for b in writes:
            if b.w is not None:
                toks.append(b.w)
            toks.extend(b.r.items())
        return toks

    @staticmethod
    def _mark(tok, reads, writes):
        s, v = tok
        for b in reads:
            if b.r.get(s, 0) < v:
                b.r[s] = v
        for b in writes:
            b.w = tok
            b.r = {}

    def op(self, en, fn, reads=(), writes=()):
        self._wait(en, self._deps(reads, writes))
        ins = fn(self.eng[en])
        s = self.semobj[en]
        s.total += 1
        ins.then_inc(s.h, 1)
        self._mark((s, s.total), reads, writes)
        self.nins += 1

    def dma(self, qn, out, in_, reads=(), writes=(), sem=None):
        self._wait(qn, self._deps(reads, writes))
        ins = self.eng[qn].dma_start(out=out, in_=in_)
        if sem is None:
            b = writes[0] if writes else reads[0]
            if b.sem is None:
                b.sem = self._dma_sem()
            sem = b.sem
        sem.total += 16
        ins.then_inc(sem.h, 16)
        self._mark((sem, sem.total), reads, writes)
        self.nins += 1

    def barrier(self):
        allsems = list(self.semobj.values()) + self.dma_sems
        for en, E in self.eng.items():
            w = self.waited[en]
            for s in allsems:
                if s.total > 0 and w.get(s, 0) < s.total:
                    E.wait_ge(s.h, s.total)
                    w[s] = s.total
        for b in self.bufs:
            b.w = None
            b.r = {}


def bc(ap_t, offset_elems, dims):
    from concourse.ap import AP
    return AP(ap_t, offset_elems, dims)


C_ID, C_J0, C_J1, C_MRET, C_TRI, C_BLK, C_SEL0, C_SEL1, C_MS, C_MI = range(10)
C_TRIS, C_ONES, C_B64 = 10, 11, 12
C_LVT = 13
C_LV = 19
NCONST = 25
NF32 = 13


def make_consts(z):
    p = np.arange(128)[:, None]
    f = np.arange(128)[None, :]
    blk = (p // 64) == (f // 64)
    cm = np.zeros((NCONST, 128, 128), np.float32)
    cm[C_ID] = np.eye(128)
    J = np.eye(128)[::-1]
    cm[C_J0] = J
    cm[C_MRET] = (p <= f)
    cm[C_TRI] = (p <= f) & blk
    cm[C_TRIS] = (p < f) & blk
    cm[C_BLK] = blk
    cm[C_SEL0] = (p < 64) & (f >= 0)
    cm[C_SEL1] = (p >= 64) & (f >= 0)
    cm[C_MS] = (p < f) & blk
    cm[C_MI] = (p <= f) & blk
    for k in range(6):
        m = ((p >> (k + 1)) == (f >> (k + 1))) & (((p >> k) & 1) == 1) & (((f >> k) & 1) == 0)
        cm[C_LV + k] = m
        cm[C_LVT + k] = m.T
    cm[C_ONES] = 1.0
    cm[C_B64] = blk
    return cm


class MK:
    def __init__(self, L, layers, ncores=4, direct=True, layer_ids=None):
        self.layer_ids = list(range(layers)) if layer_ids is None else list(layer_ids)
        self.ncores = ncores
        self.direct = direct
        self.pairs = [[2 * p, 2 * p + 1] for p in range(max(1, ncores // 2))]
        self.L = L
        self.layers = layers
        self.NLT = L // 128
        self.NT = 2 + self.NLT
        self.T = 256 + L
        self.HT = 256 + L // 2
        self.groups = [(0, 2, True)] + [(2 + 4 * g, 4, False) for g in range(self.NLT // 4)]
        self.half_groups = self.groups
        self.nc = bass.Bass("TRN2", target_bir_lowering=False)
        self.em = Em(self.nc)
        self.ext = {}
        self.units = {}
        self.cc_sem = Sem(self.nc.alloc_semaphore(name="cc"))
        self.em.dma_sems.append(self.cc_sem)
        self.gsem = Sem(self.nc.alloc_semaphore(name="gdma"))
        self.em.dma_sems.append(self.gsem)

    def xin(self, name, shape, dt=F32):
        t = self.nc.dram_tensor(name, list(shape), dt, kind="ExternalInput")
        self.ext[name] = (tuple(shape), dt)
        return t

    def dr(self, name, shape, dt):
        return self.nc.dram_tensor(name, list(shape), dt, kind="Internal")

    def ptile(self, t):
        return 1 - t if t < 2 else 2 + (self.NLT - 1 - (t - 2))

    def unit(self, name, K, N):
        em = self.em
        g = self.dr("wg_" + name, [K, N], BF16)
        self.units[name] = (g, K, N)
        if self.direct:
            full = self.xin("wf_" + name, [K, N])
            em.dma("pool", g.ap(), full.ap(), sem=self.gsem)
            return None
        sh = self.xin("w_" + name, [K // 8, N])
        sb = self.dr("ws_" + name, [K // 8, N], BF16)
        gq = self.dr("wq_" + name, [K // 2, N], BF16)
        em.dma("pool", sb.ap(), sh.ap(), sem=self.gsem)
        return (sb, gq, g)

    def gather_units(self, pend):
        em = self.em
        em.barrier()
        if self.direct:
            return
        for stage in range(2):
            for sb, gq, g in pend:
                rg = [[0, 1, 2, 3], [4, 5, 6, 7]] if stage == 0 else [[0, 4], [1, 5], [2, 6], [3, 7]]
                src, dst = (sb, gq) if stage == 0 else (gq, g)
                ins = self.nc.gpsimd.collective_compute("AllGather", ALU.bypass, replica_groups=rg,
                                                        ins=[src.ap()], outs=[dst.ap()])
                self.cc_sem.total += 1
                ins.then_inc(self.cc_sem.h, 1)
                self.nc.gpsimd.wait_ge(self.cc_sem.h, self.cc_sem.total)
            em.barrier()

    def pair_gather(self, src, dst):
        em = self.em
        em.barrier()
        ins = self.nc.gpsimd.collective_compute("AllGather", ALU.bypass, replica_groups=self.pairs,
                                                ins=[src.ap()], outs=[dst.ap()])
        self.cc_sem.total += 1
        ins.then_inc(self.cc_sem.h, 1)
        em.barrier()

    def wsrc(self, name, kc0, nkc, n0, nw):
        g, K, N = self.units[name]
        return g.ap()[kc0 * 128:(kc0 + nkc) * 128, n0:n0 + nw].rearrange("(c p) n -> p c n", p=128)

    def load_w(self, buf, name, kc0, nkc, n0, nw):
        self.em.dma("sp", buf[:, 0:nkc, 0:nw], self.wsrc(name, kc0, nkc, n0, nw), writes=[buf])

    def setup(self):
        nc, em = self.nc, self.em
        self.pst = ExitStack()
        st = self.pst
        self.cmf = em.sb(st, "cmf", [128, NF32, 128], F32)
        self.cmb = em.sb(st, "cmb", [128, NCONST, 128], BF16)
        cm = self.xin("cm", [128, NCONST, 128])
        em.dma("sp", self.cmf[:], cm.ap()[:, 0:NF32, :], writes=[self.cmf])
        em.dma("pool", self.cmb[:], cm.ap(), writes=[self.cmb])
        self.iota = em.sb(st, "iota", [128, 4], F32)
        em.dma("sp", self.iota[:], self.xin("iota", [128, 4]).ap(), writes=[self.iota])
        self.psb = [em.ps(st, "ps%d" % i) for i in range(8)]
        self.psk = 0
        self.xs = self.xin("xs", [self.T, D])
        self.cvec = self.xin("cvec", [128, 16])
        self.modb = self.xin("modb", [4, 128, 48])
        self.modbf = self.xin("modbf", [4, 6144])
        self.lng = self.xin("lng", [4, 2, D])
        self.lnb = self.xin("lnb", [4, 2, D])
        self.xsf = self.xin("xsf", [self.T, D])
        self.LAT2 = [self.dr("LAT0", [self.T, D], F32), self.dr("LAT1", [self.T, D], F32)]
        self.O2 = [self.dr("O0", [self.T, 2048], F32), self.dr("O1", [self.T, 2048], F32)]
        self.H2T = self.dr("H2T", [len(self.half_groups), 128, 8 * 512], BF16)
        em.dma("pool", self.LAT2[0].ap(), self.xs.ap(), sem=self.gsem)
        em.dma("pool", self.LAT2[1].ap(), self.xsf.ap(), sem=self.gsem)
        self.OUT = self.nc.dram_tensor("out", [self.T, D], F32, kind="ExternalOutput")
        self.flt = [em.sb(st, "flt%d" % q, [128, D], F32) for q in range(2)]
        self.fltk = 0

    def cf(self, i):
        return self.cmf[:, i, :]

    def store_lat(self, t, ou, out=False):
        em = self.em
        em.dma("pool", self.LAT2[0].ap()[t * 128:(t + 1) * 128, :], ou[:], reads=[ou])
        if out:
            em.dma("pool", self.OUT.ap()[t * 128:(t + 1) * 128, :], ou[:], reads=[ou])
        fl = self.flt[self.fltk % 2]
        self.fltk += 1
        for half in range(2):
            ps = self.pn()
            self.mm(ps, ps[:, :], self.cf(C_J0), ou[:, half * 512:(half + 1) * 512], True, True, [self.cmf, ou])
            if half:
                em.op("act", lambda e, ps=ps, fl=fl: e.activation(out=fl[:, 512:1024], in_=ps[:, :], func=AF.Copy), reads=[ps], writes=[fl])
            else:
                em.op("dve", lambda e, ps=ps, fl=fl: e.tensor_copy(out=fl[:, 0:512], in_=ps[:, :]), reads=[ps], writes=[fl])
        pt = self.ptile(t)
        em.dma("pool", self.LAT2[1].ap()[pt * 128:(pt + 1) * 128, :], fl[:], reads=[fl])

    def cb(self, i):
        return self.cmb[:, i, :]

    def pn(self):
        while True:
            b = self.psb[self.psk % 8]
            self.psk += 1
            if b not in getattr(self, "reserved", ()):
                return b

    def mm(self, ps, out, lhsT, rhs, start, stop, reads):
        self.em.op("pe", lambda e: e.matmul(out, lhsT=lhsT, rhs=rhs, start=start, stop=stop), reads=reads, writes=[ps])

    def phase_mod(self, i, st):
        em = self.em
        modLC = em.sb(st, "modLC%d" % i, [128, 48, 2], F32)
        rows = {n: em.sb(st, "%s_%d" % (n, i), [128, D], F32)
                for n in ("GA1L", "GA1C", "GA2L", "GA2C", "LNG0", "LNB0", "LNG1", "LNB1")}
        import os
        for s in range(2 if os.environ.get("MKA", "0") == "0" else 0):
            em.dma("sp", rows["LNG%d" % s][:], self.lng.ap()[i, s:s + 1, :].partition_broadcast(128), writes=[rows["LNG%d" % s]])
            em.dma("sp", rows["LNB%d" % s][:], self.lnb.ap()[i, s:s + 1, :].partition_broadcast(128), writes=[rows["LNB%d" % s]])
        import os
        dbg = int(os.environ.get("MKDBG", "99"))
        if dbg == 0:
            em.barrier()
            return modLC, rows
        with ExitStack() as ts:
            cv = em.sb(ts, "cv", [128, 16], F32)
            em.dma("sp", cv[:], self.cvec.ap(), writes=[cv])
            sT = em.sb(ts, "sT", [128, 8, 2], BF16)
            em.op("act", lambda e: e.activation(out=sT[:].rearrange("p c t -> p (c t)"), in_=cv[:], func=AF.Silu),
                  reads=[cv], writes=[sT])
            sBC = em.sb(ts, "sBC", [128, 8, 2, 128], BF16)
            em.op("dve", lambda e: e.tensor_copy(out=sBC[:], in_=sT[:].unsqueeze(3).broadcast_to([128, 8, 2, 128])),
                  reads=[sT], writes=[sBC])
            mb = em.sb(ts, "mb", [128, 48], F32)
            em.dma("sp", mb[:], self.modb.ap()[i], writes=[mb])
            brow = [em.sb(ts, "brow%d" % q, [128, D], F32) for q in range(2)]
            em.dma("sp", brow[0][:], self.modbf.ap()[i:i + 1, 2048:3072].partition_broadcast(128), writes=[brow[0]])
            em.dma("sp", brow[1][:], self.modbf.ap()[i:i + 1, 5120:6144].partition_broadcast(128), writes=[brow[1]])
            wb = [em.sb(ts, "wm%d" % q, [128, 8, 1536], BF16) for q in range(2)]
            psm = self.pn()
            self.reserved = [psm]
            if dbg == 1:
                em.barrier()
                return modLC, rows
            for piece in range(4):
                w = wb[piece % 2]
                self.load_w(w, "modw%d" % i, 0, 8, piece * 1536, 1536)
                if dbg == 2:
                    continue
                for oc in range(12):
                    g = piece * 12 + oc
                    for kc in range(8):
                        self.mm(psm, psm[:, g * 2:g * 2 + 2], w[:, kc, oc * 128:(oc + 1) * 128], sT[:, kc, :],
                                kc == 0, kc == 7, [w, sT])
                if piece in (1, 3):
                    for t, nm in ((0, "L"), (1, "C")):
                        dest = rows[("GA1" if piece == 1 else "GA2") + nm]
                        br = brow[0 if piece == 1 else 1]
                        for half in range(2):
                            pb = self.pn()
                            for kc in range(8):
                                self.mm(pb, pb[:, :], sBC[:, kc, t, :], w[:, kc, 512 + half * 512:1024 + half * 512],
                                        kc == 0, kc == 7, [w, sBC])
                            em.op("dve", lambda e, pb=pb, dest=dest, br=br, half=half: e.scalar_tensor_tensor(
                                out=dest[:, half * 512:(half + 1) * 512], in0=pb[:, :], scalar=1.0,
                                in1=br[:, half * 512:(half + 1) * 512], op0=ALU.add, op1=ALU.add),
                                reads=[pb, br], writes=[dest])
            em.op("dve", lambda e: e.tensor_tensor(out=modLC[:], in0=psm[:, 0:96].rearrange("p (c t) -> p c t", t=2),
                                                   in1=mb[:].unsqueeze(2).broadcast_to([128, 48, 2]), op=ALU.add),
                  reads=[psm, mb], writes=[modLC])
            self.reserved = []
            for c0 in (8, 32):
                em.op("dve", lambda e, c0=c0: e.tensor_scalar_add(out=modLC[:, c0:c0 + 8, :], in0=modLC[:, c0:c0 + 8, :], scalar1=1.0),
                      reads=[modLC], writes=[modLC])
            em.barrier()
            em.release([cv, sT, sBC, mb] + brow + wb)
        return modLC, rows

    def hT_tile(self, lat_tile, hT, col0, modLC, sub, isctx):
        em = self.em
        t = 1 if isctx else 0
        sh0, sc0 = (0, 8) if sub == 0 else (24, 32)
        for half in range(2):
            ps = self.pn()
            for c4 in range(4):
                c = half * 4 + c4
                em.op("pe", lambda e, ps=ps, c=c, c4=c4: e.transpose(out=ps[:, c4 * 128:(c4 + 1) * 128],
                                                                     in_=lat_tile[:, c * 128:(c + 1) * 128],
                                                                     identity=self.cf(C_ID)),
                      reads=[lat_tile, self.cmf], writes=[ps])
            for c4 in range(4):
                c = half * 4 + c4
                em.op("act", lambda e, ps=ps, c=c, c4=c4: e.activation(
                    out=hT[:, c, col0:col0 + 128], in_=ps[:, c4 * 128:(c4 + 1) * 128], func=AF.Identity,
                    scale=modLC[:, sc0 + c, t:t + 1], bias=modLC[:, sh0 + c, t:t + 1]),
                    reads=[ps, modLC], writes=[hT])

    def tail(self, pso, lat_tile, GA, LNG, LNB, tmp, outt, st6, mv, rs):
        em = self.em
        for half in range(2):
            sl = slice(half * 512, (half + 1) * 512)
            sb_, sap = pso[half] if isinstance(pso[half], tuple) else (pso[half], pso[half][:, :])
            em.op("dve", lambda e, sap=sap, sl=sl: e.tensor_tensor(out=tmp[:, sl], in0=sap, in1=GA[:, sl], op=ALU.mult),
                  reads=[sb_, GA], writes=[tmp])
        em.op("dve", lambda e: e.scalar_tensor_tensor(out=tmp[:], in0=lat_tile[:], scalar=ALPHA, in1=tmp[:], op0=ALU.mult, op1=ALU.add),
              reads=[lat_tile, tmp], writes=[tmp])
        self.layer_norm(tmp, LNG, LNB, outt, st6, mv, rs)

    def layer_norm(self, tmp, LNG, LNB, outt, st6, mv, rs):
        em = self.em
        for half in range(2):
            em.op("dve", lambda e, half=half: e.bn_stats(out=st6[:, half, :], in_=tmp[:, half * 512:(half + 1) * 512]),
                  reads=[tmp], writes=[st6])
        em.op("dve", lambda e: e.bn_aggr(out=mv[:], in_=st6[:]), reads=[st6], writes=[mv])
        em.op("act", lambda e: e.activation(out=rs[:], in_=mv[:, 1:2], func=AF.Sqrt, bias=LN_EPS, scale=1.0), reads=[mv], writes=[rs])
        em.op("dve", lambda e: e.reciprocal(out=rs[:], in_=rs[:]), reads=[rs], writes=[rs])
        em.op("dve", lambda e: e.tensor_scalar(out=tmp[:], in0=tmp[:], scalar1=mv[:, 0:1], scalar2=rs[:, 0:1],
                                               op0=ALU.subtract, op1=ALU.mult), reads=[tmp, mv, rs], writes=[tmp])
        em.op("pool", lambda e: e.tensor_tensor(out=tmp[:], in0=tmp[:], in1=LNG[:], op=ALU.mult), reads=[tmp, LNG], writes=[tmp])
        em.op("pool", lambda e: e.tensor_tensor(out=outt[:], in0=tmp[:], in1=LNB[:], op=ALU.add), reads=[tmp, LNB], writes=[outt])

    def phase_f(self, i, modLC, rows, factory, wout_name, KC):
        em = self.em
        with ExitStack() as st:
            make_yT, mbufs = factory(st)
            wout = em.sb(st, "wout", [128, KC, D], BF16)
            self.load_w(wout, wout_name, 0, KC, 0, D)
            yT = [em.sb(st, "yT%d" % q, [128, KC, 128], BF16) for q in range(2)]
            lat = [em.sb(st, "flat%d" % q, [128, D], F32) for q in range(2)]
            outt = [em.sb(st, "fout%d" % q, [128, D], F32) for q in range(2)]
            tmp = em.sb(st, "ftmp", [128, D], F32)
            st6 = em.sb(st, "fst6", [128, 2, 6], F32)
            mv = em.sb(st, "fmv", [128, 2], F32)
            rs = em.sb(st, "frs", [128, 1], F32)
            h2g = [em.sb(st, "h2g%d" % q, [128, 8, 512], BF16) for q in range(2)]
            k = 0
            for gi, (t0, ng, isctx) in enumerate(self.groups):
                own = gi < len(self.half_groups)
                sfx = "C" if isctx else "L"
                for ti in range(ng):
                    t = t0 + ti
                    y = yT[k % 2]
                    la = lat[k % 2]
                    ou = outt[k % 2]
                    k += 1
                    make_yT(t, isctx, y)
                    em.dma("sp", la[:], self.LAT2[0].ap()[t * 128:(t + 1) * 128, :], writes=[la])
                    pso = [self.pn(), self.pn()]
                    for half in range(2):
                        for kc in range(KC):
                            self.mm(pso[half], pso[half][:, :], y[:, kc, :], wout[:, kc, half * 512:(half + 1) * 512],
                                    kc == 0, kc == KC - 1, [y, wout])
                    self.tail(pso, la, rows["GA1" + sfx], rows["LNG0"], rows["LNB0"], tmp, ou, st6, mv, rs)
                    self.store_lat(t, ou)
                    if own:
                        self.hT_tile(ou, h2g[gi % 2], ti * 128, modLC, 1, isctx)
                if own:
                    em.dma("pool", self.H2T.ap()[gi].rearrange("p (c n) -> p c n", c=8), h2g[gi % 2][:], reads=[h2g[gi % 2]])
            em.barrier()
            em.release([wout, tmp, st6, mv, rs] + yT + lat + outt + h2g + mbufs)

    def ffn_tail_bufs(self, st):
        em = self.em
        d = dict(lat=[em.sb(st, "glat%d" % q, [128, D], F32) for q in range(2)],
                 outt=[em.sb(st, "gout%d" % q, [128, D], F32) for q in range(2)],
                 tmp=em.sb(st, "gtmp", [128, D], F32), st6=em.sb(st, "gst6", [128, 2, 6], F32),
                 mv=em.sb(st, "gmv", [128, 2], F32), rs=em.sb(st, "grs", [128, 1], F32))
        return d

    def ffn_store(self, t, ou):
        self.store_lat(t, ou, out=True)

    def ffn_dense(self, li, modLC, rows):
        em = self.em
        with ExitStack() as st:
            wd = em.sb(st, "wd", [128, 22, D], BF16)
            self.load_w(wd, "ffndn%d" % li, 0, 22, 0, D)
            wg = [em.sb(st, "wg%d" % q, [128, 8, 256], BF16) for q in range(2)]
            wu = [em.sb(st, "wu%d" % q, [128, 8, 256], BF16) for q in range(2)]
            h2 = [em.sb(st, "h2_%d" % q, [128, 8, 512], BF16) for q in range(2)]
            act = em.sb(st, "act", [128, 22, 512], BF16)
            sg = [em.sb(st, "sg%d" % q, [128, 512], F32) for q in range(2)]
            B = self.ffn_tail_bufs(st)
            k = 0
            for gi, (t0, ng, isctx) in enumerate(self.half_groups):
                N = ng * 128
                h = h2[gi % 2]
                em.dma("sp", h[:], self.H2T.ap()[gi].rearrange("p (c n) -> p c n", c=8), writes=[h])
                for s in range(11):
                    g_, u_ = wg[s % 2], wu[s % 2]
                    self.load_w(g_, "ffngu%d" % li, 0, 8, s * 256, 256)
                    self.load_w(u_, "ffngu%d" % li, 0, 8, 2816 + s * 256, 256)
                    for fc in range(2):
                        f = s * 2 + fc
                        pg, pu = self.pn(), self.pn()
                        for kc in range(8):
                            self.mm(pg, pg[:, :N], g_[:, kc, fc * 128:(fc + 1) * 128], h[:, kc, :N], kc == 0, kc == 7, [g_, h])
                        for kc in range(8):
                            self.mm(pu, pu[:, :N], u_[:, kc, fc * 128:(fc + 1) * 128], h[:, kc, :N], kc == 0, kc == 7, [u_, h])
                        s_ = sg[f % 2]
                        em.op("act", lambda e, pg=pg, s_=s_: e.activation(out=s_[:, :N], in_=pg[:, :N], func=AF.Silu), reads=[pg], writes=[s_])
                        em.op("dve", lambda e, pu=pu, s_=s_, f=f: e.tensor_tensor(out=act[:, f, :N], in0=s_[:, :N], in1=pu[:, :N], op=ALU.mult),
                              reads=[pu, s_], writes=[act])
                sfx = "C" if isctx else "L"
                for ti in range(ng):
                    t = t0 + ti
                    la, ou = B["lat"][k % 2], B["outt"][k % 2]
                    k += 1
                    em.dma("sp", la[:], self.LAT2[0].ap()[t * 128:(t + 1) * 128, :], writes=[la])
                    pso = [self.pn(), self.pn()]
                    for half in range(2):
                        for f in range(22):
                            self.mm(pso[half], pso[half][:, :], act[:, f, ti * 128:(ti + 1) * 128], wd[:, f, half * 512:(half + 1) * 512],
                                    f == 0, f == 21, [act, wd])
                    self.tail(pso, la, rows["GA2" + sfx], rows["LNG1"], rows["LNB1"], B["tmp"], ou, B["st6"], B["mv"], B["rs"])
                    self.ffn_store(t, ou)
            em.barrier()
            em.release([wd, act] + wg + wu + h2 + sg + B["lat"] + B["outt"] + [B["tmp"], B["st6"], B["mv"], B["rs"]])

    def phase_h(self):
        em = self.em
        self.pair_gather(self.FX, self.FXG)
        with ExitStack() as st:
            a = [[em.sb(st, "ha%d_%d" % (s, q), [128, D], F32) for s in range(2)] for q in range(2)]
            ou = [em.sb(st, "ho%d" % q, [128, D], F32) for q in range(2)]
            k = 0
            for t in range(2 + self.NLT // 2, self.NT):
                pt = self.ptile(t)
                aa, o_ = a[k % 2], ou[k % 2]
                k += 1
                for s in range(2):
                    em.dma("sp", aa[s][:], self.FXG.ap()[s * self.HT + pt * 128:s * self.HT + (pt + 1) * 128, :], writes=[aa[s]])
                for half in range(2):
                    ps = self.pn()
                    for s in range(2):
                        self.mm(ps, ps[:, :], self.cf(C_J0 + s), aa[s][:, half * 512:(half + 1) * 512], s == 0, s == 1, [self.cmf, aa[s]])
                    em.op("act" if half else "dve", lambda e, ps=ps, half=half, o_=o_: (
                        e.activation(out=o_[:, half * 512:(half + 1) * 512], in_=ps[:, :], func=AF.Copy) if half else
                        e.tensor_copy(out=o_[:, half * 512:(half + 1) * 512], in_=ps[:, :])), reads=[ps], writes=[o_])
                em.dma("pool", self.LAT.ap()[t * 128:(t + 1) * 128, :], o_[:], reads=[o_])
            em.barrier()
            em.release(a[0] + a[1] + ou)

    def ret_alloc(self):
        NT = self.NT
        self.rQT2 = [self.dr("rQT%d" % z, [NT, 128, 1024], BF16) for z in range(2)]
        self.rKT2 = [self.dr("rKT%d" % z, [NT, 128, 1024], BF16) for z in range(2)]
        self.rKTM2 = [self.dr("rKTM%d" % z, [NT, 128, 1024], BF16) for z in range(2)]
        self.rV2 = [self.dr("rV%d" % z, [NT, 128, 2048], BF16) for z in range(2)]
        self.rG = self.dr("rG", [NT, 128, 2048], BF16)
        self.rope2 = [self.xin("rope%d" % z, [NT, 128, 256]) for z in range(2)]

    def ret_phase_a(self, j, modLC, z):
        em = self.em
        self.LAT, self.rope = self.LAT2[z], self.rope2[z]
        self.rQT, self.rKT, self.rKTM, self.rV = self.rQT2[z], self.rKT2[z], self.rKTM2[z], self.rV2[z]
        with ExitStack() as st:
            wb = [em.sb(st, "rw%d" % q, [128, 8, 512], BF16) for q in range(2)]
            hT = [em.sb(st, "rhT%d" % q, [128, 8, 512], BF16) for q in range(2)]
            lat = [em.sb(st, "rlat%d" % q, [128, D], F32) for q in range(2)]
            qr = [em.sb(st, "rqr%d" % q, [128, 1024], BF16) for q in range(4)]
            kr = [em.sb(st, "rkr%d" % q, [128, 1024], BF16) for q in range(4)]
            vv = [em.sb(st, "rvv%d" % q, [128, 2048], BF16) for q in range(4)]
            gg = [em.sb(st, "rgg%d" % q, [128, 2048], BF16) for q in range(4)]
            rp = [em.sb(st, "rrp%d" % q, [128, 256], F32) for q in range(4)]
            cos4 = [em.sb(st, "rcos%d" % q, [128, 4, 64], F32) for q in range(4)]
            sin4 = [em.sb(st, "rsin%d" % q, [128, 4, 64], F32) for q in range(4)]
            xf = [em.sb(st, "rxf%d" % q, [128, 512], F32) for q in range(2)]
            t1 = [em.sb(st, "rt1%d" % q, [128, 512], F32) for q in range(2)]
            t2 = [em.sb(st, "rt2%d" % q, [128, 4, 64], F32) for q in range(2)]
            t3 = [em.sb(st, "rt3%d" % q, [128, 4, 64], F32) for q in range(2)]
            qT = [em.sb(st, "rqT%d" % q, [128, 1024], BF16) for q in range(2)]
            fl = [em.sb(st, "rfl%d" % q, [128, 2048], BF16) for q in range(2)]
            kk = 0
            rk = 0
            for gi, (t0, ng, isctx) in enumerate(self.groups):
                h = hT[gi % 2]
                for ti in range(ng):
                    la = lat[kk % 2]
                    kk += 1
                    em.dma("sp", la[:], self.LAT.ap()[(t0 + ti) * 128:(t0 + ti + 1) * 128, :], writes=[la])
                    self.hT_tile(la, h, ti * 128, modLC, 0, isctx)
                    em.dma("sp", rp[ti][:], self.rope.ap()[t0 + ti], writes=[rp[ti]])
                    em.op("pool", lambda e, ti=ti: e.tensor_copy(out=cos4[ti][:].rearrange("p (a b) f -> p a (b f)", a=2),
                                                                 in_=rp[ti][:, 0:128].unsqueeze(1).broadcast_to([128, 2, 128])),
                          reads=[rp[ti]], writes=[cos4[ti]])
                    em.op("pool", lambda e, ti=ti: e.tensor_copy(out=sin4[ti][:].rearrange("p (a b) f -> p a (b f)", a=2),
                                                                 in_=rp[ti][:, 128:256].unsqueeze(1).broadcast_to([128, 2, 128])),
                          reads=[rp[ti]], writes=[sin4[ti]])
                for nb in range(12 if z == 0 else 8):
                    w = wb[nb % 2]
                    self.load_w(w, "retin%d" % j, 0, 8, nb * 512, 512)
                    for ti in range(ng):
                        ps = self.pn()
                        for kc in range(8):
                            self.mm(ps, ps[:, :], h[:, kc, ti * 128:(ti + 1) * 128], w[:, kc, :], kc == 0, kc == 7, [h, w])
                        if nb < 4:
                            dest = (qr if nb < 2 else kr)[ti]
                            dsl = dest[:, (nb % 2) * 512:(nb % 2 + 1) * 512].rearrange("p (a s f) -> p a s f", a=4, s=2)
                            x_, t1_, t2_, t3_ = xf[rk % 2], t1[rk % 2], t2[rk % 2], t3[rk % 2]
                            rk += 1
                            sc = 1.0 if nb < 2 else 1.0 / 16.0
                            em.op("act", lambda e, ps=ps, x_=x_, sc=sc: e.activation(out=x_[:], in_=ps[:, :], func=AF.Copy, scale=sc),
                                  reads=[ps], writes=[x_])
                            X = x_[:].rearrange("p (a s f) -> p a s f", a=4, s=2)
                            T1 = t1_[:].rearrange("p (a s f) -> p a s f", a=4, s=2)
                            em.op("dve", lambda e, X=X, T1=T1, ti=ti: e.tensor_tensor(
                                out=T1, in0=X, in1=cos4[ti][:].unsqueeze(2).broadcast_to([128, 4, 2, 64]), op=ALU.mult),
                                reads=[x_, cos4[ti]], writes=[t1_])
                            em.op("pool", lambda e, X=X, t2_=t2_, ti=ti: e.tensor_tensor(out=t2_[:], in0=X[:, :, 1, :], in1=sin4[ti][:], op=ALU.mult),
                                  reads=[x_, sin4[ti]], writes=[t2_])
                            em.op("pool", lambda e, X=X, t3_=t3_, ti=ti: e.tensor_tensor(out=t3_[:], in0=X[:, :, 0, :], in1=sin4[ti][:], op=ALU.mult),
                                  reads=[x_, sin4[ti]], writes=[t3_])
                            em.op("dve", lambda e, dsl=dsl, T1=T1, t2_=t2_: e.tensor_tensor(out=dsl[:, :, 0, :], in0=T1[:, :, 0, :], in1=t2_[:], op=ALU.subtract),
                                  reads=[t1_, t2_], writes=[dest])
                            em.op("dve", lambda e, dsl=dsl, T1=T1, t3_=t3_: e.tensor_tensor(out=dsl[:, :, 1, :], in0=T1[:, :, 1, :], in1=t3_[:], op=ALU.add),
                                  reads=[t1_, t3_], writes=[dest])
                        else:
                            dest = (vv if nb < 8 else gg)[ti]
                            c0 = (nb % 4) * 512
                            if (nb + ti) % 2:
                                em.op("act", lambda e, ps=ps, dest=dest, c0=c0: e.activation(out=dest[:, c0:c0 + 512], in_=ps[:, :], func=AF.Copy),
                                      reads=[ps], writes=[dest])
                            else:
                                em.op("dve", lambda e, ps=ps, dest=dest, c0=c0: e.tensor_copy(out=dest[:, c0:c0 + 512], in_=ps[:, :]),
                                      reads=[ps], writes=[dest])
                for ti in range(ng):
                    t = t0 + ti
                    em.dma("pool", self.rKTM.ap()[t], kr[ti][:], reads=[kr[ti]])
                    em.dma("pool", self.rV.ap()[t], vv[ti][:], reads=[vv[ti]])
                    if z == 0:
                        em.dma("pool", self.rG.ap()[t], gg[ti][:], reads=[gg[ti]])
                    for src, dstT in ((qr[ti], self.rQT), (kr[ti], self.rKT)):
                        ps = self.pn()
                        pb = ps[:].bitcast(BF16)
                        for c in range(8):
                            em.op("pe", lambda e, pb=pb, src=src, c=c: e.transpose(out=pb[:, c * 128:(c + 1) * 128], in_=src[:, c * 128:(c + 1) * 128],
                                                                                 identity=self.cb(C_ID)), reads=[src, self.cmb], writes=[ps])
                        q_ = qT[kk % 2]
                        kk += 1
                        em.op("act", lambda e, pb=pb, q_=q_: e.activation(out=q_[:], in_=pb[:, 0:1024], func=AF.Copy), reads=[ps], writes=[q_])
                        em.dma("pool", dstT.ap()[t], q_[:], reads=[q_])
                    if z == 0 and getattr(self, "ret_flip", True):
                        pt = self.ptile(t)
                        Jb = self.cb(C_J0)
                        for src, dstD, nblk in ((kr[ti], self.rKTM2[1], 2), (vv[ti], self.rV2[1], 4)):
                            f_ = fl[kk % 2]
                            kk += 1
                            for b_ in range(nblk):
                                ps = self.pn()
                                self.mm(ps, ps[:, :], Jb, src[:, b_ * 512:(b_ + 1) * 512], True, True, [self.cmb, src])
                                if b_ % 2:
                                    em.op("act", lambda e, ps=ps, f_=f_, b_=b_: e.activation(out=f_[:, b_ * 512:(b_ + 1) * 512], in_=ps[:, :], func=AF.Copy), reads=[ps], writes=[f_])
                                else:
                                    em.op("dve", lambda e, ps=ps, f_=f_, b_=b_: e.tensor_copy(out=f_[:, b_ * 512:(b_ + 1) * 512], in_=ps[:, :]), reads=[ps], writes=[f_])
                            em.dma("pool", dstD.ap()[pt], f_[:, 0:nblk * 512], reads=[f_])
                        for src, dstD in ((qr[ti], self.rQT2[1]), (kr[ti], self.rKT2[1])):
                            f_ = fl[kk % 2]
                            kk += 1
                            for hb in range(2):
                                ps = self.pn()
                                for c4 in range(4):
                                    c = hb * 4 + c4
                                    self.mm(ps, ps[:, c4 * 128:(c4 + 1) * 128], src[:, c * 128:(c + 1) * 128], Jb, True, True, [self.cmb, src])
                                if hb:
                                    em.op("act", lambda e, ps=ps, f_=f_, hb=hb: e.activation(out=f_[:, hb * 512:(hb + 1) * 512], in_=ps[:, :], func=AF.Copy), reads=[ps], writes=[f_])
                                else:
                                    em.op("dve", lambda e, ps=ps, f_=f_, hb=hb: e.tensor_copy(out=f_[:, hb * 512:(hb + 1) * 512], in_=ps[:, :]), reads=[ps], writes=[f_])
                            em.dma("pool", dstD.ap()[pt], f_[:, 0:1024], reads=[f_])
            em.barrier()
            em.release(wb + hT + lat + qr + kr + vv + gg + rp + cos4 + sin4 + xf + t1 + t2 + t3 + qT + fl)

    def ret_phase_s(self, j, z):
        em = self.em
        dec_in = self.xin("retdec%d_%d" % (j, z), [1, 4])
        self.rQT, self.rKT, self.rKTM, self.rV, self.O = self.rQT2[z], self.rKT2[z], self.rKTM2[z], self.rV2[z], self.O2[z]
        with ExitStack() as st:
            dec = em.sb(st, "sdec", [128, 4], F32)
            em.dma("sp", dec[:], dec_in.ap().partition_broadcast(128), writes=[dec])
            lsp = em.sb(st, "slsp", [128, 4], F32)
            em.op("act", lambda e: e.activation(out=lsp[:], in_=dec[:], func=AF.Exp, scale=-1.0), reads=[dec], writes=[lsp])
            em.op("act", lambda e: e.activation(out=lsp[:], in_=lsp[:], func=AF.Ln, bias=1.0, scale=1.0), reads=[lsp], writes=[lsp])
            outsc = em.sb(st, "soutsc", [128, 4], F32)
            scsc = em.sb(st, "sscsc", [128, 4], F32)
            kdec = em.sb(st, "skdec", [128, 4], F32)
            gC = em.sb(st, "sgC", [128, 4], F32)
            io = self.iota
            em.op("act", lambda e: e.activation(out=outsc[:], in_=lsp[:], func=AF.Exp, scale=io[:, 1:2]), reads=[lsp, io], writes=[outsc])
            em.op("act", lambda e: e.activation(out=scsc[:], in_=lsp[:], func=AF.Exp, scale=io[:, 0:1]), reads=[lsp, io], writes=[scsc])
            em.op("act", lambda e: e.activation(out=kdec[:], in_=lsp[:], func=AF.Exp, scale=io[:, 3:4]), reads=[lsp, io], writes=[kdec])
            em.op("act", lambda e: e.activation(out=gC[:], in_=lsp[:], func=AF.Exp, scale=-128.0), reads=[lsp], writes=[gC])
            S = [[em.sb(st, "sS%d_%d" % (h, dc), [128, 512], F32) for dc in range(2)] for h in range(4)]
            Sb = [[em.sb(st, "sSb%d_%d" % (h, dc), [128, 512], BF16) for dc in range(2)] for h in range(4)]
            for h in range(4):
                for dc in range(2):
                    em.op("pool", lambda e, h=h, dc=dc: e.memset(S[h][dc][:], 0.0), writes=[S[h][dc]])
                    em.op("pool", lambda e, h=h, dc=dc: e.memset(Sb[h][dc][:], 0.0), writes=[Sb[h][dc]])
            qt = [em.sb(st, "sqt%d" % q, [128, 8, 128], BF16) for q in range(2)]
            kt = [em.sb(st, "skt%d" % q, [128, 8, 128], BF16) for q in range(2)]
            ktm = [em.sb(st, "sktm%d" % q, [128, 1024], BF16) for q in range(2)]
            v = [em.sb(st, "sv%d" % q, [128, 2048], BF16) for q in range(2)]
            ot = [em.sb(st, "sot%d" % q, [128, 2048], F32) for q in range(2)]
            scb = [em.sb(st, "sscb%d" % q, [128, 128], BF16) for q in range(4)]
            kd = [em.sb(st, "skd%d" % q, [128, 256], BF16) for q in range(4)]
            scb4, kd4 = scb, kd
            n = 0
            for t in range(self.NT):
                q_, k_, km_, v_, o_ = qt[t % 2], kt[t % 2], ktm[t % 2], v[t % 2], ot[t % 2]
                em.dma("sp", q_[:], self.rQT.ap()[t].rearrange("p (c n) -> p c n", c=8), writes=[q_])
                em.dma("sp", k_[:], self.rKT.ap()[t].rearrange("p (c n) -> p c n", c=8), writes=[k_])
                em.dma("sp", km_[:], self.rKTM.ap()[t], writes=[km_])
                em.dma("sp", v_[:], self.rV.ap()[t], writes=[v_])
                ps = self.pn()
                for h in range(4):
                    for dc in range(2):
                        self.mm(ps, ps[:, h * 128:(h + 1) * 128], k_[:, 2 * h + dc, :], q_[:, 2 * h + dc, :], dc == 0, dc == 1, [k_, q_])
                for h in range(4):
                    em.op("dve", lambda e, h=h: e.scalar_tensor_tensor(
                        out=scb4[h][:], in0=ps[:, h * 128:(h + 1) * 128], scalar=scsc[:, h:h + 1], in1=self.cf(C_MRET), op0=ALU.mult, op1=ALU.mult),
                        reads=[ps, scsc, self.cmf], writes=[scb4[h]])
                    em.op("pool", lambda e, h=h: e.tensor_scalar(out=kd4[h][:], in0=km_[:, h * 256:(h + 1) * 256],
                                                                 scalar1=kdec[:, h:h + 1], scalar2=None, op0=ALU.mult),
                          reads=[km_, kdec], writes=[kd4[h]])
                pos = [self.pn() for h in range(4)]
                for h in range(4):
                    vh = v_[:, h * 512:(h + 1) * 512]
                    self.mm(pos[h], pos[h][:, :], scb4[h][:], vh, True, False, [scb4[h], v_])
                    for dc in range(2):
                        self.mm(pos[h], pos[h][:, :], q_[:, 2 * h + dc, :], Sb[h][dc][:], False, dc == 1, [q_, Sb[h][dc]])
                for h in range(4):
                    em.op("act", lambda e, h=h: e.activation(out=o_[:, h * 512:(h + 1) * 512], in_=pos[h][:, :], func=AF.Identity,
                                                             scale=outsc[:, h:h + 1]), reads=[pos[h], outsc], writes=[o_])
                for hp in range(2):
                    pus = {}
                    for h in (2 * hp, 2 * hp + 1):
                        vh = v_[:, h * 512:(h + 1) * 512]
                        for dc in range(2):
                            pu = self.pn()
                            pus[(h, dc)] = pu
                            self.mm(pu, pu[:, :], kd4[h][:, dc * 128:(dc + 1) * 128], vh, True, True, [kd4[h], v_])
                    for h in (2 * hp, 2 * hp + 1):
                        for dc in range(2):
                            S_, Sb_, pu = S[h][dc], Sb[h][dc], pus[(h, dc)]
                            em.op("dve", lambda e, pu=pu, S_=S_, h=h: e.scalar_tensor_tensor(
                                out=S_[:], in0=S_[:], scalar=gC[:, h:h + 1], in1=pu[:, :], op0=ALU.mult, op1=ALU.add),
                                reads=[pu, S_, gC], writes=[S_])
                            em.op("act", lambda e, S_=S_, Sb_=Sb_: e.activation(out=Sb_[:], in_=S_[:], func=AF.Copy), reads=[S_], writes=[Sb_])
                em.dma("pool", self.O.ap()[t * 128:(t + 1) * 128, :], o_[:], reads=[o_])
            em.barrier()
            em.release([dec, lsp, outsc, scsc, kdec, gC] + sum(S, []) + sum(Sb, []) + qt + kt + ktm + v + ot + scb + kd)

    def ret_factory(self, j):
        em = self.em
        gn_in = self.xin("retgn%d" % j, [1, 2048])

        def factory(st):
            gng = em.sb(st, "ygng", [128, 2048], F32)
            em.dma("sp", gng[:], gn_in.ap().partition_broadcast(128), writes=[gng])
            oo = [em.sb(st, "yoo%d" % q, [128, 512], F32) for q in range(2)]
            p0 = [em.sb(st, "yp0%d" % q, [128, 512], F32) for q in range(2)]
            p1 = [em.sb(st, "yp1%d" % q, [128, 512], F32) for q in range(2)]
            gt = [em.sb(st, "ygt%d" % q, [128, 512], BF16) for q in range(2)]
            osum = em.sb(st, "yosum", [128, 512], F32)
            sg = em.sb(st, "ysg", [128, 512], F32)
            y = em.sb(st, "yy", [128, 2048], BF16)
            st6 = em.sb(st, "yst6", [128, 6], F32)
            mv = em.sb(st, "ymv", [128, 2], F32)
            rs = em.sb(st, "yrs", [128, 1], F32)
            cnt = [0]
            mk4 = lambda n, sh, dt: [em.sb(st, "y4%s%d" % (n, h), sh, dt) for h in range(4)]
            oo4, p14, os4, sg4 = mk4("oo", [128, 512], F32), mk4("p1", [128, 512], F32), mk4("os", [128, 512], F32), mk4("sg", [128, 512], F32)
            gt4 = mk4("gt", [128, 512], BF16)
            st64, mv4, rs4 = mk4("st6", [128, 6], F32), mk4("mv", [128, 2], F32), mk4("rs", [128, 1], F32)

            def make_yT(t, isctx, yT):
                pt = self.ptile(t)
                H = range(4)
                css = [slice(h * 512, (h + 1) * 512) for h in H]
                for h in H:
                    em.dma("sp", oo4[h][:], self.O2[0].ap()[t * 128:(t + 1) * 128, css[h]], writes=[oo4[h]])
                    em.dma("sp", p14[h][:], self.O2[1].ap()[pt * 128:(pt + 1) * 128, css[h]], writes=[p14[h]])
                    em.dma("sp", gt4[h][:], self.rG.ap()[t][:, css[h]], writes=[gt4[h]])
                pss = [self.pn() for h in H]
                for h in H:
                    self.mm(pss[h], pss[h][:, :], self.cf(C_J0), p14[h][:], True, True, [self.cmf, p14[h]])
                for h in H:
                    em.op("dve", lambda e, h=h: e.tensor_tensor(out=os4[h][:], in0=pss[h][:, :], in1=oo4[h][:], op=ALU.add),
                          reads=[pss[h], oo4[h]], writes=[os4[h]])
                    em.op("act", lambda e, h=h: e.activation(out=sg4[h][:], in_=gt4[h][:], func=AF.Silu), reads=[gt4[h]], writes=[sg4[h]])
                for h in H:
                    em.op("dve", lambda e, h=h: e.bn_stats(out=st64[h][:], in_=os4[h][:]), reads=[os4[h]], writes=[st64[h]])
                for h in H:
                    em.op("dve", lambda e, h=h: e.bn_aggr(out=mv4[h][:], in_=st64[h][:]), reads=[st64[h]], writes=[mv4[h]])
                for h in H:
                    em.op("act", lambda e, h=h: e.activation(out=rs4[h][:], in_=mv4[h][:, 1:2], func=AF.Sqrt, bias=1e-5, scale=1.0), reads=[mv4[h]], writes=[rs4[h]])
                for h in H:
                    em.op("dve", lambda e, h=h: e.reciprocal(out=rs4[h][:], in_=rs4[h][:]), reads=[rs4[h]], writes=[rs4[h]])
                for h in H:
                    em.op("dve", lambda e, h=h: e.tensor_scalar(out=os4[h][:], in0=os4[h][:], scalar1=mv4[h][:, 0:1], scalar2=rs4[h][:, 0:1],
                                                                op0=ALU.subtract, op1=ALU.mult), reads=[os4[h], mv4[h], rs4[h]], writes=[os4[h]])
                for h in H:
                    em.op("pool", lambda e, h=h: e.tensor_tensor(out=os4[h][:], in0=os4[h][:], in1=gng[:, css[h]], op=ALU.mult),
                          reads=[os4[h], gng], writes=[os4[h]])
                for h in H:
                    em.op("pool", lambda e, h=h: e.tensor_tensor(out=y[:, css[h]], in0=os4[h][:], in1=sg4[h][:], op=ALU.mult),
                          reads=[os4[h], sg4[h]], writes=[y])
                for half in range(2):
                    ps = self.pn()
                    pb = ps[:].bitcast(BF16)
                    for c in range(8):
                        cc = half * 8 + c
                        em.op("pe", lambda e, pb=pb, c=c, cc=cc: e.transpose(out=pb[:, c * 128:(c + 1) * 128], in_=y[:, cc * 128:(cc + 1) * 128],
                                                                           identity=self.cb(C_ID)), reads=[y, self.cmb], writes=[ps])
                    em.op("act", lambda e, pb=pb, half=half: e.activation(
                        out=yT[:, half * 8:(half + 1) * 8, :].rearrange("p c n -> p (c n)"), in_=pb[:, 0:1024], func=AF.Copy),
                        reads=[ps], writes=[yT])
            return make_yT, [gng, osum, sg, y, st6, mv, rs] + oo + p0 + p1 + gt + oo4 + p14 + os4 + sg4 + gt4 + st64 + mv4 + rs4
        return factory

    def build(self):
        em = self.em
        self.setup()
        pend = []
        stop = getattr(self, "stop", 99)
        for i in self.layer_ids:
            pend.append(self.unit("modw%d" % i, 1024, 6144))
            kind, j = i % 3, i // 3
            if kind == 0:
                pend += [self.unit("retin%d" % j, 1024, 6144), self.unit("retout%d" % j, 2048, 1024)]
            elif kind == 1:
                pend += [self.unit("dnin", 1024, 6144), self.unit("dnout", 2048, 1024)]
            else:
                pend += [self.unit(n, 1024, 1024) for n in ("rkr", "rkk", "rkv", "rkout")]
                pend += [self.unit("rkg1", 1024, 128), self.unit("rkg2", 128, 1024)]
            if stop == 5 and i == self.layer_ids[-1]:
                pass
            elif i % 2 == 0 and not getattr(self, "force_moe", False):
                pend += [self.unit("ffngu%d" % (i // 2), 1024, 5632), self.unit("ffndn%d" % (i // 2), 2816, 1024)]
            else:
                for e_ in range(8):
                    pend += [self.unit("moegu%d_%d" % (i // 2, e_), 1024, 7168), self.unit("moedn%d_%d" % (i // 2, e_), 3584, 1024)]
        self.gather_units(pend)
        stop = getattr(self, "stop", 99)
        if self.layers >= 1:
            self.ret_alloc()
        if stop == 0:
            em.barrier()
            return self.nc
        for i in self.layer_ids:
            kind, j = i % 3, i // 3
            with ExitStack() as lst:
                modLC, rows = self.phase_mod(i, lst)
                if stop == 1:
                    return self.nc
                if kind == 0:
                    self.ret_phase_a(j, modLC, 0)
                    for z in range(2):
                        self.ret_phase_s(j, z)
                    if stop == 3:
                        import os
                        which = os.environ.get("MKDUMP", "O0")
                        dbg = self.nc.dram_tensor("dbg", [self.T, 2048], F32, kind="ExternalOutput")
                        srcs = {"O0": self.O2[0].ap(), "O1": self.O2[1].ap(),
                                "V0": self.rV2[0].ap().rearrange("t p n -> (t p) n"),
                                "G": self.rG.ap().rearrange("t p n -> (t p) n")}
                        if which in srcs:
                            em.dma("pool", dbg.ap(), srcs[which], sem=self.gsem)
                        elif which == "K0":
                            em.dma("pool", dbg.ap()[:, 0:1024], self.rKTM2[0].ap().rearrange("t p n -> (t p) n"), sem=self.gsem)
                        elif which == "QT0":
                            em.dma("pool", dbg.ap()[:, 0:1024], self.rQT2[0].ap().rearrange("t p n -> (t p) n"), sem=self.gsem)
                        em.barrier()
                        return self.nc
                    self.phase_f(i, modLC, rows, self.ret_factory(j), "retout%d" % j, 16)
                elif kind == 1:
                    self.dn_layer(i, modLC, rows)
                else:
                    self.rk_layer(i, modLC, rows)
                if stop == 5 and i == self.layer_ids[-1]:
                    em.dma("pool", self.OUT.ap(), self.LAT2[0].ap(), sem=self.gsem)
                    em.barrier()
                    return self.nc
                if i % 2 == 0 and not getattr(self, "force_moe", False):
                    self.ffn_dense(i // 2, modLC, rows)
                else:
                    self.ffn_moe(i // 2, modLC, rows)
                em.barrier()
                em.release([modLC] + list(rows.values()))
        em.barrier()
        return self.nc


def rope_tables(L, z):
    NT = 2 + L // 128
    tab = np.zeros((NT, 128, 256), np.float32)
    tab[:2, :, 0:128] = 1.0
    inv = (10000.0 ** (-np.arange(64, dtype=np.float32) / 64)).astype(np.float32)
    n = np.arange(L)
    if z:
        n = n[::-1]
    row = (n // 64).astype(np.float32)[:, None] * inv[None, :]
    col = (n % 64).astype(np.float32)[:, None] * inv[None, :]
    full = np.concatenate([np.cos(row), np.cos(col), np.sin(row), np.sin(col)], 1).astype(np.float32)
    tab[2:] = full.reshape(L // 128, 128, 256)
    return tab


def host_inputs(mk, inp, core):
    b, z = core, 0
    L = mk.L
    f = lambda a: np.ascontiguousarray(a, dtype=np.float32)
    x = inp["x"][b][:L]
    cx = inp["ctx"][b]
    m = {}
    m["xs"] = f(np.concatenate([cx, x], 0))
    m["xsf"] = f(np.concatenate([cx[::-1], x[::-1]], 0))
    cv = np.stack([inp["c"][b], inp["c_ctx"]], -1).reshape(8, 128, 2).transpose(1, 0, 2).reshape(128, 16)
    m["cvec"] = f(cv)
    m["modb"] = f(inp["mod_b"].reshape(4, 48, 128).transpose(0, 2, 1))
    m["modbf"] = f(inp["mod_b"])
    m["lng"] = f(inp["ln_g"])
    m["lnb"] = f(inp["ln_b"])
    m["cm"] = f(make_consts(z).transpose(1, 0, 2))
    p = np.arange(128, dtype=np.float32)
    m["iota"] = f(np.stack([p + 1, -(p + 1), 0 * p, p - 127], 1))
    m["rope0"] = rope_tables(L, 0)
    m["rope1"] = rope_tables(L, 1)
    W = {}
    for i in range(4):
        W["modw%d" % i] = inp["mod_w"][i]
    for j in range(2):
        W["retin%d" % j] = inp["ret_w_in"][j]
        W["retout%d" % j] = inp["ret_w_out"][j]
        for zz in range(2):
            m["retdec%d_%d" % (j, zz)] = f(inp["ret_decay"][j][zz][None, :])
        m["retgn%d" % j] = f(inp["ret_gn_g"][j][None, :])
        W["ffngu%d" % j] = inp["ffn_w_gu"][j]
        W["ffndn%d" % j] = inp["ffn_w_down"][j]
        for e_ in range(8):
            W["moegu%d_%d" % (j, e_)] = inp["moe_w_gu"][j][e_]
            W["moedn%d_%d" % (j, e_)] = inp["moe_w_down"][j][e_]
    for j in range(2):
        m["moer%d" % j] = f(inp["moe_router"][j].reshape(8, 128, 8).transpose(1, 0, 2).reshape(128, 64))
    W["dnin"] = inp["dn_w_in"][0][:, :6144]
    W["dnout"] = inp["dn_w_out"][0]
    for q, nme in enumerate(("rkr", "rkk", "rkv")):
        W[nme] = inp["rk_w_rkv"][0][q]
    W["rkout"] = inp["rk_w_out"][0]
    W["rkg1"] = inp["rk_g1"][0]
    W["rkg2"] = inp["rk_g2"][0]
    for name in mk.ext:
        if name.startswith("w_"):
            w = W[name[2:]]
            k8 = w.shape[0] // 8
            m[name] = f(w[core * k8:(core + 1) * k8])
        elif name.startswith("wf_"):
            m[name] = f(W[name[3:]])
    host_extra(mk, inp, core, m)
    out = {}
    for name, (shape, dt) in mk.ext.items():
        a = m[name]
        assert tuple(a.shape) == tuple(shape), (name, a.shape, shape)
        out[name] = a
    return out


def host_extra(mk, inp, core, m):
    f = lambda a: np.ascontiguousarray(a, dtype=np.float32)
    wab = inp["dn_w_in"][0][:, 6144:]
    cw = inp["dn_conv_w"][0]
    for z in range(2):
        a = wab[:, z * 32:(z + 1) * 32]
        m["dnab%d" % z] = f(a.reshape(8, 128, 32).transpose(1, 0, 2).reshape(128, 256))
        m["dnalog%d" % z] = f(inp["dn_a_log"][0][z][None, :])
        m["dndtb%d" % z] = f(inp["dn_dt_bias"][0][z][None, :])
        c = cw if z == 0 else cw[::-1]
        m["dnconv%d" % z] = f(c.T.reshape(32, 128, 5).transpose(1, 0, 2).reshape(128, 160))
    m["dnng"] = f(inp["dn_norm_g"][0][None, :])
    fm8 = lambda v_: v_.reshape(8, 128).T
    m["rkmix"] = f(np.concatenate([fm8(inp["rk_mix"][0][q]) for q in range(6)], 1))
    m["rkvec"] = f(np.concatenate([fm8(inp["rk_k_k"][0]), fm8(inp["rk_k_a"][0]), fm8(inp["rk_r_k"][0].reshape(-1)), fm8(inp["rk_lnx_g"][0])], 1))
    lo = lambda w_: w_.reshape(8, 128, 64).transpose(1, 0, 2).reshape(128, 512)
    for z in range(2):
        m["rkw0_%d" % z] = f(inp["rk_w0"][0][z][None, :])
        m["rka0_%d" % z] = f(fm8(inp["rk_a0"][0][z]))
        m["rkw1_%d" % z] = f(lo(inp["rk_w1"][0][z]))
        m["rka1_%d" % z] = f(lo(inp["rk_a1"][0][z]))
        m["rkw2_%d" % z] = f(inp["rk_w2"][0][z])
        m["rka2_%d" % z] = f(inp["rk_a2"][0][z])


_CACHE = {}


def run_model(inp, L=8192, layers=4):
    key = (L, layers)
    if key not in _CACHE:
        mk = MK(L, layers)
        mk.build()
        _CACHE[key] = mk
    mk = _CACHE[key]
    nco = mk.ncores
    in_maps = [host_inputs(mk, inp, c) for c in range(nco)]
    res = run_bass_kernel_spmd(mk.nc, in_maps, core_ids=list(range(nco)))
    lat = np.zeros((4, L, D), np.float32)
    cx = np.zeros((4, NCTX, D), np.float32)
    for c in range(nco):
        o = res.results[c]["out"]
        lat[c] = o[256:]
        cx[c] = o[:256]
    return lat, cx


def kernel(**inputs):
    inp = {k: np.asarray(v) for k, v in inputs.items()}
    lat, _ = run_model(inp, 8192, 4)
    return lat


def ffn_moe(self, li, modLC, rows):
    em = self.em
    rin = self.xin("moer%d" % li, [128, 64])
    with ExitStack() as st:
        wr = em.sb(st, "mwr", [128, 8, 8], BF16)
        em.dma("pool", wr[:].rearrange("p c e -> p (c e)"), rin.ap(), writes=[wr])
        h2 = em.sb(st, "mh2", [128, 8, 512], BF16)
        wg = [em.sb(st, "mwg%d" % q, [128, 8, 256], BF16) for q in range(2)]
        wu = [em.sb(st, "mwu%d" % q, [128, 8, 256], BF16) for q in range(2)]
        wd = [em.sb(st, "mwd%d" % q, [128, 28, 512], BF16) for q in range(2)]
        act = em.sb(st, "mact", [128, 28, 512], BF16)
        acc = em.sb(st, "macc", [128, 4, D], F32)
        sg = [em.sb(st, "msg%d" % q, [128, 512], F32) for q in range(2)]
        lg = em.sb(st, "mlg", [128, 8], F32)
        eq = em.sb(st, "meq", [128, 8], F32)
        l2 = em.sb(st, "ml2", [128, 8], F32)
        ex = em.sb(st, "mex", [128, 8], F32)
        m1 = em.sb(st, "mm1", [128, 4], F32)
        gate = em.sb(st, "mgate", [128, 4, 8], F32)
        la = em.sb(st, "mlat", [128, D], F32)
        ou = em.sb(st, "mout", [128, D], F32)
        tmp = em.sb(st, "mtmp", [128, D], F32)
        st6 = em.sb(st, "mst6", [128, 2, 6], F32)
        mv = em.sb(st, "mmv", [128, 2], F32)
        rs = em.sb(st, "mrs", [128, 1], F32)
        nf = 0
        for gi, (t0, ng, isctx) in enumerate(self.groups):
            N = ng * 128
            em.dma("sp", h2[:], self.H2T.ap()[gi].rearrange("p (c n) -> p c n", c=8), writes=[h2])
            for ti in range(ng):
                ps = self.pn()
                for kc in range(8):
                    self.mm(ps, ps[:, 0:8], h2[:, kc, ti * 128:(ti + 1) * 128], wr[:, kc, :], kc == 0, kc == 7, [h2, wr])
                em.op("dve", lambda e, ps=ps: e.tensor_copy(out=lg[:], in_=ps[:, 0:8]), reads=[ps], writes=[lg])
                em.op("dve", lambda e: e.tensor_reduce(out=m1[:, 0:1], in_=lg[:], axis=AX.X, op=ALU.max), reads=[lg], writes=[m1])
                em.op("dve", lambda e: e.tensor_scalar(out=eq[:], in0=lg[:], scalar1=m1[:, 0:1], scalar2=None, op0=ALU.is_equal),
                      reads=[lg, m1], writes=[eq])
                em.op("dve", lambda e: e.scalar_tensor_tensor(out=l2[:], in0=eq[:], scalar=-1e30, in1=lg[:], op0=ALU.mult, op1=ALU.add),
                      reads=[eq, lg], writes=[l2])
                em.op("dve", lambda e: e.tensor_reduce(out=m1[:, 1:2], in_=l2[:], axis=AX.X, op=ALU.max), reads=[l2], writes=[m1])
                em.op("dve", lambda e: e.tensor_scalar(out=eq[:], in0=lg[:], scalar1=m1[:, 1:2], scalar2=None, op0=ALU.is_ge),
                      reads=[lg, m1], writes=[eq])
                em.op("dve", lambda e: e.tensor_scalar(out=m1[:, 2:3], in0=m1[:, 0:1], scalar1=-1.0, scalar2=None, op0=ALU.mult),
                      reads=[m1], writes=[m1])
                em.op("act", lambda e: e.activation(out=ex[:], in_=lg[:], func=AF.Exp, bias=m1[:, 2:3], scale=1.0), reads=[lg, m1], writes=[ex])
                em.op("dve", lambda e: e.tensor_tensor(out=ex[:], in0=ex[:], in1=eq[:], op=ALU.mult), reads=[ex, eq], writes=[ex])
                em.op("dve", lambda e: e.tensor_reduce(out=m1[:, 3:4], in_=ex[:], axis=AX.X, op=ALU.add), reads=[ex], writes=[m1])
                em.op("dve", lambda e: e.reciprocal(out=m1[:, 3:4], in_=m1[:, 3:4]), reads=[m1], writes=[m1])
                em.op("dve", lambda e, ti=ti: e.tensor_scalar(out=gate[:, ti, :], in0=ex[:], scalar1=m1[:, 3:4], scalar2=None, op0=ALU.mult),
                      reads=[ex, m1], writes=[gate])
            for e_ in range(8):
                for half in range(2):
                    self.load_w(wd[half], "moedn%d_%d" % (li, e_), 0, 28, half * 512, 512)
                for s in range(14):
                    g_, u_ = wg[s % 2], wu[s % 2]
                    self.load_w(g_, "moegu%d_%d" % (li, e_), 0, 8, s * 256, 256)
                    self.load_w(u_, "moegu%d_%d" % (li, e_), 0, 8, 3584 + s * 256, 256)
                    for fc in range(2):
                        f = s * 2 + fc
                        pg, pu = self.pn(), self.pn()
                        for kc in range(8):
                            self.mm(pg, pg[:, :N], g_[:, kc, fc * 128:(fc + 1) * 128], h2[:, kc, :N], kc == 0, kc == 7, [g_, h2])
                        for kc in range(8):
                            self.mm(pu, pu[:, :N], u_[:, kc, fc * 128:(fc + 1) * 128], h2[:, kc, :N], kc == 0, kc == 7, [u_, h2])
                        s_ = sg[nf % 2]
                        nf += 1
                        em.op("act", lambda e, pg=pg, s_=s_: e.activation(out=s_[:, :N], in_=pg[:, :N], func=AF.Silu), reads=[pg], writes=[s_])
                        em.op("dve", lambda e, pu=pu, s_=s_, f=f: e.tensor_tensor(out=act[:, f, :N], in0=s_[:, :N], in1=pu[:, :N], op=ALU.mult),
                              reads=[pu, s_], writes=[act])
                for ti in range(ng):
                    for half in range(2):
                        ps = self.pn()
                        for f in range(28):
                            self.mm(ps, ps[:, :], act[:, f, ti * 128:(ti + 1) * 128], wd[half][:, f, :], f == 0, f == 27, [act, wd[half]])
                        asl = acc[:, ti, half * 512:(half + 1) * 512]
                        if e_ == 0:
                            em.op("dve", lambda e, ps=ps, asl=asl, ti=ti, e_=e_: e.tensor_scalar(
                                out=asl, in0=ps[:, :], scalar1=gate[:, ti, e_:e_ + 1], scalar2=None, op0=ALU.mult),
                                reads=[ps, gate], writes=[acc])
                        else:
                            em.op("dve", lambda e, ps=ps, asl=asl, ti=ti, e_=e_: e.scalar_tensor_tensor(
                                out=asl, in0=ps[:, :], scalar=gate[:, ti, e_:e_ + 1], in1=asl, op0=ALU.mult, op1=ALU.add),
                                reads=[ps, gate, acc], writes=[acc])
            sfx = "C" if isctx else "L"
            for ti in range(ng):
                t = t0 + ti
                em.dma("sp", la[:], self.LAT2[0].ap()[t * 128:(t + 1) * 128, :], writes=[la])
                srcs = [(acc, acc[:, ti, 0:512]), (acc, acc[:, ti, 512:1024])]
                self.tail(srcs, la, rows["GA2" + sfx], rows["LNG1"], rows["LNB1"], tmp, ou, st6, mv, rs)
                self.ffn_store(t, ou)
        em.barrier()
        em.release([wr, h2, act, acc, lg, eq, l2, ex, m1, gate, la, ou, tmp, st6, mv, rs] + wg + wu + wd + sg)


MK.ffn_moe = ffn_moe


def dn_alloc(self):
    NT, T = self.NT, self.T
    self.dPRE = self.dr("dPRE", [32, 128, T], F32)
    self.dQT = [self.dr("dQT%d" % z, [8, 128, T], BF16) for z in range(2)]
    self.dKT = [self.dr("dKT%d" % z, [8, 128, T], BF16) for z in range(2)]
    self.dKM = [self.dr("dKM%d" % z, [NT, 128, 1024], BF16) for z in range(2)]
    self.dV = [self.dr("dV%d" % z, [NT, 128, 2048], BF16) for z in range(2)]
    self.dGB = [self.dr("dGB%d" % z, [NT, 128, 48], F32) for z in range(2)]
    self.dn_in = dict(
        ab=[self.xin("dnab%d" % z, [128, 8 * 32]) for z in range(2)],
        alog=[self.xin("dnalog%d" % z, [1, 16]) for z in range(2)],
        dtb=[self.xin("dndtb%d" % z, [1, 16]) for z in range(2)],
        conv=[self.xin("dnconv%d" % z, [128, 32 * 5]) for z in range(2)],
        ng=self.xin("dnng", [1, 128]))


def dn_phase_a1(self, modLC, z):
    em = self.em
    LAT = self.LAT2[z]
    with ExitStack() as st:
        wb = [em.sb(st, "dw%d" % q, [128, 8, 512], BF16) for q in range(2)]
        hT = [em.sb(st, "dhT%d" % q, [128, 8, 512], BF16) for q in range(2)]
        lat = [em.sb(st, "dlat%d" % q, [128, D], F32) for q in range(2)]
        stg = [em.sb(st, "dstg%d" % q, [128, 512], F32) for q in range(3)]
        gg = [em.sb(st, "dgg%d" % q, [128, 2048], BF16) for q in range(4)]
        wab = em.sb(st, "dwab", [128, 8, 32], BF16)
        em.dma("pool", wab[:].rearrange("p c n -> p (c n)"), self.dn_in["ab"][z].ap(), writes=[wab])
        nega = em.sb(st, "dnega", [128, 16], F32)
        dtb = em.sb(st, "ddtb", [128, 16], F32)
        em.dma("sp", nega[:], self.dn_in["alog"][z].ap().partition_broadcast(128), writes=[nega])
        em.dma("sp", dtb[:], self.dn_in["dtb"][z].ap().partition_broadcast(128), writes=[dtb])
        em.op("act", lambda e: e.activation(out=nega[:], in_=nega[:], func=AF.Exp), reads=[nega], writes=[nega])
        em.op("dve", lambda e: e.tensor_scalar(out=nega[:], in0=nega[:], scalar1=-1.0, scalar2=None, op0=ALU.mult), reads=[nega], writes=[nega])
        gb = [em.sb(st, "dgb%d" % q, [128, 48], F32) for q in range(2)]
        tt = [em.sb(st, "dtt%d" % q, [128, 32], F32) for q in range(2)]
        kk = 0
        sk = 0
        for gi, (t0, ng, isctx) in enumerate(self.groups):
            N = ng * 128
            h = hT[gi % 2]
            for ti in range(ng):
                la = lat[kk % 2]
                kk += 1
                em.dma("sp", la[:], LAT.ap()[(t0 + ti) * 128:(t0 + ti + 1) * 128, :], writes=[la])
                self.hT_tile(la, h, ti * 128, modLC, 0, isctx)
            for nb in range(8):
                w = wb[nb % 2]
                self.load_w(w, "dnin", 0, 8, nb * 512, 512)
                for c4 in range(4):
                    c = nb * 4 + c4
                    ps = self.pn()
                    for kc in range(8):
                        self.mm(ps, ps[:, :N], w[:, kc, c4 * 128:(c4 + 1) * 128], h[:, kc, :N], kc == 0, kc == 7, [w, h])
                    s_ = stg[sk % 3]
                    sk += 1
                    if c % 2:
                        em.op("act", lambda e, ps=ps, s_=s_: e.activation(out=s_[:, :N], in_=ps[:, :N], func=AF.Copy), reads=[ps], writes=[s_])
                    else:
                        em.op("dve", lambda e, ps=ps, s_=s_: e.tensor_copy(out=s_[:, :N], in_=ps[:, :N]), reads=[ps], writes=[s_])
                    em.dma("pool", self.dPRE.ap()[c][:, t0 * 128:t0 * 128 + N], s_[:, :N], reads=[s_])
            if z == 0:
                for nb in range(8, 12):
                    w = wb[nb % 2]
                    self.load_w(w, "dnin", 0, 8, nb * 512, 512)
                    for ti in range(ng):
                        ps = self.pn()
                        for kc in range(8):
                            self.mm(ps, ps[:, :], h[:, kc, ti * 128:(ti + 1) * 128], w[:, kc, :], kc == 0, kc == 7, [h, w])
                        dest = gg[ti]
                        c0 = (nb - 8) * 512
                        em.op("act", lambda e, ps=ps, dest=dest, c0=c0: e.activation(out=dest[:, c0:c0 + 512], in_=ps[:, :], func=AF.Copy),
                              reads=[ps], writes=[dest])
                for ti in range(ng):
                    em.dma("pool", self.rG.ap()[t0 + ti], gg[ti][:], reads=[gg[ti]])
            for ti in range(ng):
                ps = self.pn()
                for kc in range(8):
                    self.mm(ps, ps[:, 0:32], h[:, kc, ti * 128:(ti + 1) * 128], wab[:, kc, :], kc == 0, kc == 7, [h, wab])
                g_, t_ = gb[ti % 2], tt[ti % 2]
                em.op("dve", lambda e, ps=ps, t_=t_: e.tensor_tensor(out=t_[:, 0:16], in0=ps[:, 0:16], in1=dtb[:], op=ALU.add), reads=[ps, dtb], writes=[t_])
                em.op("dve", lambda e, ps=ps, t_=t_: e.tensor_scalar(out=t_[:, 16:32], in0=ps[:, 16:32], scalar1=-1.0, scalar2=None, op0=ALU.mult),
                      reads=[ps], writes=[t_])
                em.op("act", lambda e, t_=t_: e.activation(out=t_[:], in_=t_[:], func=AF.Exp), reads=[t_], writes=[t_])
                em.op("act", lambda e, t_=t_, g_=g_: e.activation(out=g_[:, 0:16], in_=t_[:, 0:16], func=AF.Ln, bias=1.0, scale=1.0), reads=[t_], writes=[g_])
                em.op("act", lambda e, t_=t_, g_=g_: e.activation(out=g_[:, 32:48], in_=t_[:, 16:32], func=AF.Ln, bias=1.0, scale=1.0), reads=[t_], writes=[g_])
                em.op("dve", lambda e, g_=g_: e.tensor_tensor(out=g_[:, 0:16], in0=g_[:, 0:16], in1=nega[:], op=ALU.mult), reads=[g_, nega], writes=[g_])
                em.op("dve", lambda e, g_=g_: e.tensor_scalar(out=g_[:, 32:48], in0=g_[:, 32:48], scalar1=-1.0, scalar2=None, op0=ALU.mult), reads=[g_], writes=[g_])
                em.op("dve", lambda e, t_=t_: e.tensor_scalar(out=t_[:, 16:32], in0=t_[:, 16:32], scalar1=1.0, scalar2=None, op0=ALU.add), reads=[t_], writes=[t_])
                em.op("dve", lambda e, t_=t_, g_=g_: e.reciprocal(out=g_[:, 16:32], in_=t_[:, 16:32]), reads=[t_], writes=[g_])
                em.dma("pool", self.dGB[z].ap()[t0 + ti], g_[:], reads=[g_])
        em.barrier()
        em.release(wb + hT + lat + stg + gg + [wab, nega, dtb] + gb + tt)


def dn_phase_a2(self, z):
    em = self.em
    T, NT = self.T, self.NT
    with ExitStack() as st:
        cw = em.sb(st, "cw", [128, 32, 5], F32)
        em.dma("sp", cw[:].rearrange("p c k -> p (c k)"), self.dn_in["conv"][z].ap(), writes=[cw])
        x = [em.sb(st, "cx%d" % q, [128, T], F32) for q in range(2)]
        acc = [em.sb(st, "cacc0", [128, T], F32)] * 2
        yb = em.sb(st, "cyb", [128, T], BF16)
        sq = em.sb(st, "csq", [128, 512], BF16)
        ri = em.sb(st, "cri", [128, 512], F32)
        tp = [em.sb(st, "ctp%d" % q, [128, 8, 128], BF16) for q in range(2)]
        segs = [(0, 256), (256, T)]
        tk = 0
        for c in range(32):
            x_, a_ = x[c % 2], acc[c % 2]
            eng = "dve"
            em.dma("sp", x_[:], self.dPRE.ap()[c], writes=[x_])
            em.op(eng, lambda e, x_=x_, a_=a_, c=c: e.tensor_scalar(out=a_[:], in0=x_[:], scalar1=cw[:, c, 2:3], scalar2=None, op0=ALU.mult),
                  reads=[x_, cw], writes=[a_])
            for k in (0, 1, 3, 4):
                s = k - 2
                for (a, b) in segs:
                    lo, hi = max(a, a - s), min(b, b - s)
                    em.op(eng, lambda e, x_=x_, a_=a_, c=c, k=k, lo=lo, hi=hi, s=s: e.scalar_tensor_tensor(
                        out=a_[:, lo:hi], in0=x_[:, lo + s:hi + s], scalar=cw[:, c, k:k + 1], in1=a_[:, lo:hi], op0=ALU.mult, op1=ALU.add),
                        reads=[x_, a_, cw], writes=[a_])
            em.op("act", lambda e, a_=a_: e.activation(out=a_[:], in_=a_[:], func=AF.Silu), reads=[a_], writes=[a_])
            if c < 16:
                for b0 in range(0, T, 512):
                    n = min(512, T - b0)
                    em.op("dve", lambda e, a_=a_, b0=b0, n=n: e.tensor_tensor(out=sq[:, :n], in0=a_[:, b0:b0 + n], in1=a_[:, b0:b0 + n], op=ALU.mult),
                          reads=[a_], writes=[sq])
                    ps = self.pn()
                    self.mm(ps, ps[:, :n], self.cb(C_ONES), sq[:, :n], True, True, [self.cmb, sq])
                    em.op("act", lambda e, ps=ps, n=n: e.activation(out=ri[:, :n], in_=ps[:, :n], func=AF.Sqrt, bias=1e-6, scale=1.0), reads=[ps], writes=[ri])
                    em.op("dve", lambda e, n=n: e.reciprocal(out=ri[:, :n], in_=ri[:, :n]), reads=[ri], writes=[ri])
                    sc = (128.0 ** -0.5) if c < 8 else 1.0
                    em.op("dve", lambda e, a_=a_, b0=b0, n=n, sc=sc: e.scalar_tensor_tensor(
                        out=yb[:, b0:b0 + n], in0=a_[:, b0:b0 + n], scalar=sc, in1=ri[:, :n], op0=ALU.mult, op1=ALU.mult),
                        reads=[a_, ri], writes=[yb])
                dst = (self.dQT if c < 8 else self.dKT)[z]
                em.dma("pool", dst.ap()[c % 8], yb[:], reads=[yb])
            else:
                em.op("dve", lambda e, a_=a_: e.tensor_copy(out=yb[:], in_=a_[:]), reads=[a_], writes=[yb])
            if c >= 8:
                for t8 in range(0, NT, 8):
                    nt = min(8, NT - t8)
                    ps = self.pn()
                    pb = ps[:].bitcast(BF16)
                    for q in range(nt):
                        em.op("pe", lambda e, pb=pb, q=q, t8=t8: e.transpose(out=pb[:, q * 128:(q + 1) * 128], in_=yb[:, (t8 + q) * 128:(t8 + q + 1) * 128],
                                                                           identity=self.cb(C_ID)), reads=[yb, self.cmb], writes=[ps])
                    t_ = tp[tk % 2]
                    tk += 1
                    em.op("act", lambda e, pb=pb, t_=t_, nt=nt: e.activation(out=t_[:, 0:nt, :].rearrange("p a b -> p (a b)"), in_=pb[:, 0:nt * 128], func=AF.Copy),
                          reads=[ps], writes=[t_])
                    if c < 16:
                        dstap = self.dKM[z].ap()[t8:t8 + nt, :, (c - 8) * 128:(c - 7) * 128].rearrange("t p n -> p t n")
                    else:
                        dstap = self.dV[z].ap()[t8:t8 + nt, :, (c - 16) * 128:(c - 15) * 128].rearrange("t p n -> p t n")
                    em.dma("pool", dstap, t_[:, 0:nt, :], reads=[t_])
        em.barrier()
        em.release([cw, yb, sq, ri] + x + acc[:1] + tp)


MK.dn_alloc = dn_alloc
MK.dn_phase_a1 = dn_phase_a1
MK.dn_phase_a2 = dn_phase_a2


def tri_inverse(self, LT, INV, INVT, Tsb, tmpx, sign, ltf=None):
    em = self.em
    for q, dst in ((0, INV), (1, INVT)):
        em.op("pool", lambda e, dst=dst: e.tensor_copy(out=dst[:], in_=self.cb(C_ID).unsqueeze(1).broadcast_to([128, 4, 128])),
              reads=[self.cmb], writes=[dst])
    for lv in range(6):
        pT = self.pn()
        for h4 in range(4):
            self.mm(pT, pT[:, h4 * 128:(h4 + 1) * 128], LT[:, h4, :] if ltf is None else ltf(h4), INV[:, h4, :], True, True, [LT, INV])
        em.op("act", lambda e, pT=pT: e.activation(out=Tsb[:].rearrange("p a b -> p (a b)"), in_=pT[:, :], func=AF.Copy), reads=[pT], writes=[Tsb])
        pX, pXT = self.pn(), self.pn()
        for h4 in range(4):
            self.mm(pX, pX[:, h4 * 128:(h4 + 1) * 128], INVT[:, h4, :], Tsb[:, h4, :], True, True, [INVT, Tsb])
        for h4 in range(4):
            self.mm(pXT, pXT[:, h4 * 128:(h4 + 1) * 128], Tsb[:, h4, :], INVT[:, h4, :], True, True, [INVT, Tsb])
        for (pp, msk, dst, q) in ((pX, C_LV + lv, INV, 0), (pXT, C_LVT + lv, INVT, 1)):
            tx = tmpx[q]
            em.op("dve", lambda e, pp=pp, msk=msk, tx=tx: e.scalar_tensor_tensor(
                out=tx[:], in0=pp[:, :].rearrange("p (a b) -> p a b", a=4), scalar=-float(sign),
                in1=self.cb(msk).unsqueeze(1).broadcast_to([128, 4, 128]), op0=ALU.mult, op1=ALU.mult),
                reads=[pp, self.cmb], writes=[tx])
            em.op("pool", lambda e, dst=dst, tx=tx: e.tensor_tensor(out=dst[:], in0=dst[:], in1=tx[:], op=ALU.add), reads=[dst, tx], writes=[dst])


MK.tri_inverse = tri_inverse


def tri_inverse16(self, ltf, ltbufs, INV, INVT, Tsb, tmpx, sign):
    em = self.em
    for g in range(4):
        for dst in (INV[g], INVT[g]):
            em.op("pool", lambda e, dst=dst: e.tensor_copy(out=dst[:], in_=self.cb(C_ID).unsqueeze(1).broadcast_to([128, 4, 128])),
                  reads=[self.cmb], writes=[dst])
    f2 = lambda b: b[:].rearrange("p a b -> p (a b)")
    for lv in range(6):
        pT = [self.pn() for _ in range(4)]
        for g in range(4):
            for h4 in range(4):
                self.mm(pT[g], pT[g][:, h4 * 128:(h4 + 1) * 128], ltf(g * 4 + h4), INV[g][:, h4, :], True, True, [ltbufs[g], INV[g]])
        for g in range(4):
            if g % 2:
                em.op("dve", lambda e, g=g: e.tensor_copy(out=f2(Tsb[g]), in_=pT[g][:, :]), reads=[pT[g]], writes=[Tsb[g]])
            else:
                em.op("act", lambda e, g=g: e.activation(out=f2(Tsb[g]), in_=pT[g][:, :], func=AF.Copy), reads=[pT[g]], writes=[Tsb[g]])
        pX = [self.pn() for _ in range(4)]
        for g in range(4):
            for h4 in range(4):
                self.mm(pX[g], pX[g][:, h4 * 128:(h4 + 1) * 128], INVT[g][:, h4, :], Tsb[g][:, h4, :], True, True, [INVT[g], Tsb[g]])
        pXT = [self.pn() for _ in range(4)]
        for g in range(4):
            for h4 in range(4):
                self.mm(pXT[g], pXT[g][:, h4 * 128:(h4 + 1) * 128], Tsb[g][:, h4, :], INVT[g][:, h4, :], True, True, [INVT[g], Tsb[g]])
        for (pp, msk, dstl, q) in ((pX, C_LV + lv, INV, 0), (pXT, C_LVT + lv, INVT, 1)):
            for g in range(4):
                tx = tmpx[q][g]
                em.op("dve", lambda e, pp=pp, msk=msk, tx=tx, g=g: e.scalar_tensor_tensor(
                    out=tx[:], in0=pp[g][:, :].rearrange("p (a b) -> p a b", a=4), scalar=-float(sign),
                    in1=self.cb(msk).unsqueeze(1).broadcast_to([128, 4, 128]), op0=ALU.mult, op1=ALU.mult),
                    reads=[pp[g], self.cmb], writes=[tx])
                em.op("pool", lambda e, dstl=dstl, tx=tx, g=g: e.tensor_tensor(out=dstl[g][:], in0=dstl[g][:], in1=tx[:], op=ALU.add),
                      reads=[dstl[g], tx], writes=[dstl[g]])


MK.tri_inverse16 = tri_inverse16


def dn_phase_s(self, z):
    em = self.em
    T, NT = self.T, self.NT
    O = self.O2[z]
    with ExitStack() as st:
        B = lambda n, sh, dt: em.sb(st, "n" + n, sh, dt)
        qt = B("qt", [128, 8, 128], BF16)
        kt_ = B("ktf", [128, 8, 128], BF16)
        km = B("km", [128, 8, 128], BF16)
        v = B("v", [128, 16, 128], BF16)
        gb = B("gb", [128, 48], F32)
        sm = B("sm", [128, 64], F32)
        vec = B("vec", [128, 6, 16], F32)
        mneg = B("mneg", [128, 2, 4, 128], BF16)
        for q, cidx in ((0, C_MI), (1, C_MS)):
            em.op("dve", lambda e, q=q, cidx=cidx: e.tensor_scalar(
                out=mneg[:, q, :, :], in0=self.cb(cidx).unsqueeze(1).broadcast_to([128, 4, 128]), scalar1=-1.0, scalar2=30000.0,
                op0=ALU.add, op1=ALU.mult), reads=[self.cmb], writes=[mneg])
        DgA = [[B("Dg%d_%d" % (q, r), [128, 4, 128], F32) for q in range(2)] for r in range(2)]
        DeA = [B("De%d" % r, [128, 4, 128], BF16) for r in range(2)]
        EA = [[B("E%d_%d" % (q, r), [128, 4, 128], F32) for q in range(2)] for r in range(2)]
        LTg = [B("LT%d" % g, [128, 4, 128], BF16) for g in range(4)]
        INVg = [B("INV%d" % g, [128, 4, 128], BF16) for g in range(4)]
        INVTg = [B("INVT%d" % g, [128, 4, 128], BF16) for g in range(4)]
        Tsbg = [B("Tsb%d" % g, [128, 4, 128], BF16) for g in range(4)]
        tmpxg = [[B("tmpx%d_%d" % (q, g), [128, 4, 128], BF16) for g in range(4)] for q in range(2)]
        QKd = B("QKd", [128, 16, 128], BF16)
        qgT = B("qgT", [128, 16, 128], BF16)
        WT = B("WT", [128, 16, 128], BF16)
        U = B("U", [128, 16, 128], F32)
        VN = [B("VN%d" % q, [128, 8, 128], BF16) for q in range(2)]
        stmp2 = [B("stmp2_%d" % q, [128, 8, 128], F32) for q in range(2)]
        ktk = B("ktk", [128, 16, 128], BF16)
        vb = B("vb", [128, 16, 128], BF16)
        kbg = B("kbg", [128, 16, 128], BF16)
        OT = [B("OT%d" % q, [128, 2048], F32) for q in range(2)]
        S = [B("S%d" % q, [128, 8, 128], F32) for q in range(2)]
        Sb = [B("Sb%d" % q, [128, 8, 128], BF16) for q in range(2)]
        stmp = B("stmp", [128, 8, 128], F32)
        for q in range(2):
            em.op("pool", lambda e, q=q: e.memset(S[q][:], 0.0), writes=[S[q]])
            em.op("pool", lambda e, q=q: e.memset(Sb[q][:], 0.0), writes=[Sb[q]])
        bc16 = lambda ap: ap.unsqueeze(2).broadcast_to([128, 16, 128])
        for t in range(NT):
            c0 = t * 128
            em.dma("sp", qt[:], self.dQT[z].ap()[:, :, c0:c0 + 128].rearrange("h p n -> p h n"), writes=[qt])
            em.dma("sp", kt_[:], self.dKT[z].ap()[:, :, c0:c0 + 128].rearrange("h p n -> p h n"), writes=[kt_])
            em.dma("sp", km[:].rearrange("p a b -> p (a b)"), self.dKM[z].ap()[t], writes=[km])
            em.dma("sp", v[:].rearrange("p a b -> p (a b)"), self.dV[z].ap()[t], writes=[v])
            em.dma("sp", gb[:], self.dGB[z].ap()[t], writes=[gb])
            ps = self.pn()
            for q, cidx in enumerate((C_TRI, C_BLK, C_SEL0, C_SEL1)):
                self.mm(ps, ps[:, q * 16:(q + 1) * 16], self.cf(cidx), gb[:, 0:16], True, True, [self.cmf, gb])
            em.op("dve", lambda e, ps=ps: e.tensor_copy(out=sm[:], in_=ps[:, 0:64]), reads=[ps], writes=[sm])
            gc, gl = sm[:, 0:16], sm[:, 16:32]
            em.op("dve", lambda e: e.tensor_tensor(out=vec[:, 0, :], in0=gc, in1=gb[:, 32:48], op=ALU.add), reads=[sm, gb], writes=[vec])
            em.op("act", lambda e: e.activation(out=vec[:, 1, :], in_=gc, func=AF.Exp), reads=[sm], writes=[vec])
            em.op("dve", lambda e: e.tensor_tensor(out=vec[:, 2, :], in0=gl, in1=gc, op=ALU.subtract), reads=[sm], writes=[vec])
            em.op("act", lambda e: e.activation(out=vec[:, 2, :], in_=vec[:, 2, :], func=AF.Exp), reads=[vec], writes=[vec])
            em.op("dve", lambda e: e.tensor_tensor(out=vec[:, 3, :], in0=vec[:, 1, :], in1=gb[:, 16:32], op=ALU.mult), reads=[vec, gb], writes=[vec])
            em.op("act", lambda e: e.activation(out=vec[:, 4:6, :].rearrange("p a b -> p (a b)"), in_=sm[:, 32:64], func=AF.Exp), reads=[sm], writes=[vec])
            em.op("dve", lambda e: e.tensor_tensor(out=vb[:], in0=v[:], in1=bc16(gb[:, 16:32]), op=ALU.mult), reads=[v, gb], writes=[vb])
            for r in range(2):
                kmr = km[:]
                em.op("pool", lambda e, r=r: e.tensor_tensor(
                    out=kbg[:].rearrange("p (a r) n -> p a r n", r=2)[:, :, r, :], in0=km[:],
                    in1=vec[:, 3, :].rearrange("p (a r) -> p a r", r=2)[:, :, r].unsqueeze(2).broadcast_to([128, 8, 128]), op=ALU.mult),
                    reads=[km, vec], writes=[kbg])
                em.op("pool", lambda e, r=r: e.tensor_tensor(
                    out=ktk[:].rearrange("p (a r) n -> p a r n", r=2)[:, :, r, :], in0=km[:],
                    in1=vec[:, 2, :].rearrange("p (a r) -> p a r", r=2)[:, :, r].unsqueeze(2).broadcast_to([128, 8, 128]), op=ALU.mult),
                    reads=[km, vec], writes=[ktk])
            for hg in range(4):
                hs = slice(hg * 4, hg * 4 + 4)
                Dg, De, E, LT = DgA[hg % 2], DeA[hg % 2], EA[hg % 2], LTg[hg]
                bc4 = lambda ap: ap.unsqueeze(2).broadcast_to([128, 4, 128])
                idf = self.cf(C_ID).unsqueeze(1).broadcast_to([128, 4, 128])
                em.op("dve", lambda e, hs=hs: e.tensor_tensor(out=Dg[0][:], in0=idf, in1=bc4(sm[:, hs]), op=ALU.mult), reads=[self.cmf, sm], writes=[Dg[0]])
                em.op("dve", lambda e, hs=hs: e.tensor_tensor(out=Dg[1][:], in0=idf, in1=bc4(vec[:, 0, hs]), op=ALU.mult), reads=[self.cmf, vec], writes=[Dg[1]])
                em.op("pool", lambda e, hs=hs: e.tensor_tensor(out=De[:], in0=self.cb(C_ID).unsqueeze(1).broadcast_to([128, 4, 128]), in1=bc4(vec[:, 1, hs]), op=ALU.mult),
                      reads=[self.cmb, vec], writes=[De])
                for q in range(2):
                    p_ = self.pn()
                    self.mm(p_, p_[:, :], self.cf(C_ONES), Dg[q][:].rearrange("p a b -> p (a b)"), True, False, [self.cmf, Dg[q]])
                    self.mm(p_, p_[:, :], self.cb(C_ID), mneg[:, q, :, :].rearrange("p a b -> p (a b)"), False, True, [self.cmb, mneg])
                    em.op("dve", lambda e, p_=p_, q=q, hs=hs: e.tensor_tensor(out=E[q][:], in0=p_[:, :].rearrange("p (a b) -> p a b", a=4),
                                                                           in1=bc4(sm[:, hs]), op=ALU.subtract), reads=[p_, sm], writes=[E[q]])
                    em.op("act", lambda e, q=q: e.activation(out=E[q][:], in_=E[q][:], func=AF.Exp), reads=[E[q]], writes=[E[q]])
                pe_ = self.pn()
                self.mm(pe_, pe_[:, :], self.cb(C_ONES), De[:].rearrange("p a b -> p (a b)"), True, True, [self.cmb, De])
                pk = self.pn()
                for qh in range(2):
                    hq = hg * 2 + qh
                    self.mm(pk, pk[:, qh * 128:(qh + 1) * 128], kt_[:, hq, :], kt_[:, hq, :], True, True, [kt_])
                    self.mm(pk, pk[:, 256 + qh * 128:256 + (qh + 1) * 128], kt_[:, hq, :], qt[:, hq, :], True, True, [kt_, qt])
                rep = lambda ap: ap.rearrange("p (a b) -> p a b", a=2).unsqueeze(2).broadcast_to([128, 2, 2, 128])
                v4 = lambda ap: ap.rearrange("p (a r) b -> p a r b", r=2)
                em.op("dve", lambda e, pk=pk: e.tensor_tensor(out=v4(LT[:]), in0=rep(pk[:, 0:256]), in1=v4(E[1][:]), op=ALU.mult), reads=[pk, E[1]], writes=[LT])
                em.op("dve", lambda e, pk=pk, hs=hs: e.tensor_tensor(out=v4(QKd[:, hs, :]), in0=rep(pk[:, 256:512]), in1=v4(E[0][:]), op=ALU.mult),
                      reads=[pk, E[0]], writes=[QKd])
                em.op("dve", lambda e, pe_=pe_, hs=hs, hg=hg: e.tensor_tensor(
                    out=v4(qgT[:, hs, :]), in0=qt[:, hg * 2:hg * 2 + 2, :].unsqueeze(2).broadcast_to([128, 2, 2, 128]),
                    in1=v4(pe_[:, :].rearrange("p (a b) -> p a b", a=4)), op=ALU.mult), reads=[pe_, qt], writes=[qgT])
            self.tri_inverse16(lambda h: LTg[h // 4][:, h % 4, :], LTg, INVg, INVTg, Tsbg, tmpxg, 1.0)
            for hg in range(4):
                hs = slice(hg * 4, hg * 4 + 4)
                INVT = INVTg[hg]
                pu, pw = self.pn(), self.pn()
                for h4 in range(4):
                    h = hg * 4 + h4
                    self.mm(pu, pu[:, h4 * 128:(h4 + 1) * 128], INVT[:, h4, :], vb[:, h, :], True, True, [INVT, vb])
                    self.mm(pw, pw[:, h4 * 128:(h4 + 1) * 128], kbg[:, h, :], INVT[:, h4, :], True, True, [INVT, kbg])
                em.op("act", lambda e, pu=pu, hs=hs: e.activation(out=U[:, hs, :].rearrange("p a b -> p (a b)"), in_=pu[:, :], func=AF.Copy), reads=[pu], writes=[U])
                em.op("dve", lambda e, pw=pw, hs=hs: e.tensor_copy(out=WT[:, hs, :].rearrange("p a b -> p (a b)"), in_=pw[:, :]), reads=[pw], writes=[WT])
            o_ = OT[t % 2]
            for c in range(2):
                r0 = c * 64
                rs_ = slice(r0, r0 + 64)
                pws = [[self.pn(), self.pn()] for hh in range(2)]
                for hh in range(2):
                    for h8 in range(8):
                        h = hh * 8 + h8
                        pb_ = pws[hh][h8 // 4]
                        self.mm(pb_, pb_[rs_, (h8 % 4) * 128:(h8 % 4 + 1) * 128], WT[:, h, rs_], Sb[hh][:, h8, :], True, True, [WT, Sb[hh]])
                for hh in range(2):
                    for b2 in range(2):
                        hsl = slice(hh * 8 + b2 * 4, hh * 8 + b2 * 4 + 4)
                        em.op("dve", lambda e, b2=b2, hsl=hsl, hh=hh: e.tensor_tensor(
                            out=VN[hh][rs_, b2 * 4:b2 * 4 + 4, :].rearrange("p a b -> p (a b)"), in0=U[rs_, hsl, :].rearrange("p a b -> p (a b)"),
                            in1=pws[hh][b2][rs_, :], op=ALU.subtract), reads=[U, pws[hh][b2]], writes=[VN[hh]])
                for hh in range(2):
                    S_, Sb_ = S[hh], Sb[hh]
                    pos = [self.pn(), self.pn()]
                    pss = [self.pn(), self.pn()]
                    for h8 in range(8):
                        h = hh * 8 + h8
                        po_, ps_ = pos[h8 // 4], pss[h8 // 4]
                        cs_ = slice((h8 % 4) * 128, (h8 % 4 + 1) * 128)
                        self.mm(po_, po_[rs_, cs_], QKd[rs_, h, rs_], VN[hh][rs_, h8, :], True, False, [QKd, VN[hh]])
                        self.mm(po_, po_[rs_, cs_], qgT[:, h, rs_], Sb_[:, h8, :], False, True, [qgT, Sb_])
                        self.mm(ps_, ps_[:, cs_], ktk[rs_, h, :], VN[hh][rs_, h8, :], True, True, [ktk, VN[hh]])
                    for b2 in range(2):
                        oc0 = (hh * 8 + b2 * 4) * 128
                        em.op("act", lambda e, b2=b2, oc0=oc0, pos=pos: e.activation(out=o_[rs_, oc0:oc0 + 512], in_=pos[b2][rs_, :], func=AF.Copy),
                              reads=[pos[b2]], writes=[o_])
                    st_ = stmp2[hh]
                    em.op("dve", lambda e, hh=hh, c=c, st_=st_, S_=S_: e.tensor_tensor(
                        out=st_[:], in0=S_[:], in1=vec[:, 4 + c, hh * 8:hh * 8 + 8].unsqueeze(2).broadcast_to([128, 8, 128]), op=ALU.mult),
                        reads=[S_, vec], writes=[st_])
                    for b2 in range(2):
                        em.op("dve", lambda e, b2=b2, pss=pss, st_=st_, S_=S_: e.tensor_tensor(
                            out=S_[:, b2 * 4:b2 * 4 + 4, :].rearrange("p a b -> p (a b)"), in0=st_[:, b2 * 4:b2 * 4 + 4, :].rearrange("p a b -> p (a b)"),
                            in1=pss[b2][:, :], op=ALU.add), reads=[st_, pss[b2]], writes=[S_])
                    em.op("act", lambda e, S_=S_, Sb_=Sb_: e.activation(out=Sb_[:].rearrange("p a b -> p (a b)"), in_=S_[:].rearrange("p a b -> p (a b)"), func=AF.Copy),
                          reads=[S_], writes=[Sb_])
            em.dma("pool", O.ap()[t * 128:(t + 1) * 128, :], o_[:], reads=[o_])
        em.barrier()
        em.release([qt, kt_, km, v, gb, sm, vec, mneg, QKd, qgT, WT, U, ktk, vb, kbg, stmp] + VN + stmp2 + DgA[0] + DgA[1] + DeA + EA[0] + EA[1] + LTg + INVg + INVTg + Tsbg + tmpxg[0] + tmpxg[1] + OT + S + Sb)


MK.dn_phase_s = dn_phase_s


def dn_factory(self):
    em = self.em

    def factory(st):
        ngr = em.sb(st, "zng", [128, 128], F32)
        em.dma("sp", ngr[:], self.dn_in["ng"].ap().partition_broadcast(128), writes=[ngr])
        oo = [em.sb(st, "zoo%d" % q, [128, 4, 128], F32) for q in range(2)]
        p1 = [em.sb(st, "zp1%d" % q, [128, 512], F32) for q in range(2)]
        gt = [em.sb(st, "zgt%d" % q, [128, 512], BF16) for q in range(2)]
        osum = em.sb(st, "zosum", [128, 4, 128], F32)
        sq = em.sb(st, "zsq", [128, 4, 128], F32)
        sg = em.sb(st, "zsg", [128, 4, 128], F32)
        ss = em.sb(st, "zss", [128, 4], F32)
        y = em.sb(st, "zy", [128, 2048], BF16)
        cnt = [0]
        f2 = lambda b: b[:].rearrange("p a b -> p (a b)")

        def make_yT(t, isctx, yT):
            pt = self.ptile(t)
            for blk in range(4):
                k = cnt[0]
                cnt[0] += 1
                o_, a1, g_ = oo[k % 2], p1[k % 2], gt[k % 2]
                cs = slice(blk * 512, (blk + 1) * 512)
                em.dma("sp", f2(o_), self.O2[0].ap()[t * 128:(t + 1) * 128, cs], writes=[o_])
                em.dma("sp", a1[:], self.O2[1].ap()[pt * 128:(pt + 1) * 128, cs], writes=[a1])
                em.dma("sp", g_[:], self.rG.ap()[t][:, cs], writes=[g_])
                ps = self.pn()
                self.mm(ps, ps[:, :], self.cf(C_J0), a1[:], True, True, [self.cmf, a1])
                em.op("dve", lambda e: e.tensor_tensor(out=f2(osum), in0=ps[:, :], in1=f2(o_), op=ALU.add), reads=[ps, o_], writes=[osum])
                em.op("pool", lambda e: e.tensor_tensor(out=sq[:], in0=osum[:], in1=osum[:], op=ALU.mult), reads=[osum], writes=[sq])
                em.op("dve", lambda e: e.tensor_reduce(out=ss[:], in_=sq[:], axis=AX.X, op=ALU.add), reads=[sq], writes=[ss])
                em.op("act", lambda e: e.activation(out=ss[:], in_=ss[:], func=AF.Sqrt, bias=1e-6, scale=1.0 / 128.0), reads=[ss], writes=[ss])
                em.op("dve", lambda e: e.reciprocal(out=ss[:], in_=ss[:]), reads=[ss], writes=[ss])
                em.op("act", lambda e: e.activation(out=f2(sg), in_=g_[:], func=AF.Silu), reads=[g_], writes=[sg])
                em.op("dve", lambda e: e.tensor_tensor(out=osum[:], in0=osum[:], in1=ss[:].unsqueeze(2).broadcast_to([128, 4, 128]), op=ALU.mult),
                      reads=[osum, ss], writes=[osum])
                em.op("pool", lambda e: e.tensor_tensor(out=osum[:], in0=osum[:], in1=ngr[:].unsqueeze(1).broadcast_to([128, 4, 128]), op=ALU.mult),
                      reads=[osum, ngr], writes=[osum])
                em.op("pool", lambda e: e.tensor_tensor(out=y[:, cs], in0=f2(osum), in1=f2(sg), op=ALU.mult), reads=[osum, sg], writes=[y])
            for half in range(2):
                ps = self.pn()
                pb = ps[:].bitcast(BF16)
                for c in range(8):
                    cc = half * 8 + c
                    em.op("pe", lambda e: e.transpose(out=pb[:, c * 128:(c + 1) * 128], in_=y[:, cc * 128:(cc + 1) * 128], identity=self.cb(C_ID)),
                          reads=[y, self.cmb], writes=[ps])
                em.op("act", lambda e: e.activation(out=yT[:, half * 8:(half + 1) * 8, :].rearrange("p c n -> p (c n)"), in_=pb[:, 0:1024], func=AF.Copy),
                      reads=[ps], writes=[yT])
        return make_yT, [ngr, osum, sq, sg, ss, y] + oo + p1 + gt
    return factory


def dn_layer(self, i, modLC, rows):
    if not hasattr(self, "dPRE"):
        self.dn_alloc()
    import os
    dbg = int(os.environ.get("DNDBG", "99"))
    for z in range(2):
        self.dn_phase_a1(modLC, z)
        if dbg >= 2:
            self.dn_phase_a2(z)
        if dbg >= 3:
            self.dn_phase_s(z)
    if dbg >= 4:
        self.phase_f(i, modLC, rows, self.dn_factory(), "dnout", 16)


MK.dn_factory = dn_factory
MK.dn_layer = dn_layer


def rk_alloc(self):
    NT, T = self.NT, self.T
    self.kHT = self.dr("kHT", [8, 128, T], F32)
    fm = lambda n: [self.dr("k%s%d" % (n, z), [8, 128, T], F32) for z in range(2)]
    self.kR, self.kK, self.kA, self.kB = fm("R"), fm("K"), fm("A"), fm("B")
    self.kV = [self.dr("kV%d" % z, [NT, 128, 1024], BF16) for z in range(2)]
    self.kLW = [self.dr("kLW%d" % z, [NT, 128, 1024], F32) for z in range(2)]
    self.kBT = self.dr("kBT", [8, 128, T], BF16)
    self.kGT = self.dr("kGT", [8, 128, T], BF16)
    self.rk_in = dict(
        mix=self.xin("rkmix", [128, 48]), vec=self.xin("rkvec", [128, 32]),
        w0=[self.xin("rkw0_%d" % z, [1, 1024]) for z in range(2)],
        a0=[self.xin("rka0_%d" % z, [128, 8]) for z in range(2)],
        w1=[self.xin("rkw1_%d" % z, [128, 512]) for z in range(2)],
        a1=[self.xin("rka1_%d" % z, [128, 512]) for z in range(2)],
        w2=[self.xin("rkw2_%d" % z, [64, 1024]) for z in range(2)],
        a2=[self.xin("rka2_%d" % z, [64, 1024]) for z in range(2)])


def rk_phase_a1(self, modLC, z):
    em = self.em
    LAT = self.LAT2[z]
    with ExitStack() as st:
        hT = [em.sb(st, "khT%d" % q, [128, 8, 512], F32) for q in range(2)]
        lat = [em.sb(st, "klat%d" % q, [128, D], F32) for q in range(2)]
        kk = 0
        for gi, (t0, ng, isctx) in enumerate(self.groups):
            h = hT[gi % 2]
            for ti in range(ng):
                la = lat[kk % 2]
                kk += 1
                em.dma("sp", la[:], LAT.ap()[(t0 + ti) * 128:(t0 + ti + 1) * 128, :], writes=[la])
                self.hT_tile(la, h, ti * 128, modLC, 0, isctx)
            N = ng * 128
            em.dma("pool", self.kHT.ap()[:, :, t0 * 128:t0 * 128 + N].rearrange("c p n -> p c n"), h[:, :, 0:N], reads=[h])
        em.barrier()
        em.release(hT + lat)


def rk_phase_a2(self, z):
    em = self.em
    T, NT = self.T, self.NT
    I = self.rk_in
    N = 256
    with ExitStack() as st:
        B = lambda n, sh, dt: em.sb(st, "a" + n, sh, dt)
        mixv = B("mixv", [128, 48], F32)
        vecs = B("vecs", [128, 40], F32)
        em.dma("sp", mixv[:], I["mix"].ap(), writes=[mixv])
        em.dma("sp", vecs[:, 0:32], I["vec"].ap(), writes=[vecs])
        em.op("dve", lambda e: e.tensor_scalar(out=vecs[:, 32:40], in0=vecs[:, 8:16], scalar1=-1.0, scalar2=1.0, op0=ALU.mult, op1=ALU.add),
              reads=[vecs], writes=[vecs])
        zo = [z, 1 - z]
        a0 = [B("a0_%d" % q, [128, 8], F32) for q in range(2)]
        w1 = B("w1", [128, 8, 64], BF16)
        a1 = [B("a1_%d" % q, [128, 8, 64], BF16) for q in range(2)]
        w2 = B("w2", [64, 1024], BF16)
        a2 = [B("a2_%d" % q, [64, 1024], BF16) for q in range(2)]
        w0r = B("w0r", [128, 1024], F32)
        em.dma("sp", w0r[:], I["w0"][z].ap().partition_broadcast(128), writes=[w0r])
        em.dma("pool", w1[:].rearrange("p c n -> p (c n)"), I["w1"][z].ap(), writes=[w1])
        em.dma("pool", w2[:], I["w2"][z].ap(), writes=[w2])
        for q in range(2):
            em.dma("sp", a0[q][:], I["a0"][zo[q]].ap(), writes=[a0[q]])
            em.dma("pool", a1[q][:].rearrange("p c n -> p (c n)"), I["a1"][zo[q]].ap(), writes=[a1[q]])
            em.dma("pool", a2[q][:], I["a2"][zo[q]].ap(), writes=[a2[q]])
        Wr, Wk, Wv = [B("W%d" % q, [128, 8, 1024], BF16) for q in range(3)]
        for wb_, nm in ((Wr, "rkr"), (Wk, "rkk"), (Wv, "rkv")):
            self.load_w(wb_, nm, 0, 8, 0, 1024)
        g1 = B("g1", [128, 8, 128], BF16)
        g2 = B("g2", [128, 1, 1024], BF16)
        self.load_w(g1, "rkg1", 0, 8, 0, 128)
        self.load_w(g2, "rkg2", 0, 1, 0, 1024)
        hW = B("hW", [128, 8, N + 2], F32)
        xx = B("xx", [128, 8, N], F32)
        xm = [B("xm%d" % m, [128, 8, N], BF16) for m in range(6)]
        t1T = B("t1T", [64, N], BF16)
        u1T = [B("u1T%d" % q, [64, N], BF16) for q in range(2)]
        gsT = B("gsT", [128, N], BF16)
        ch = {n: B("c" + n, [128, N], F32) for n in ("r", "k", "v", "a", "ao", "kk", "t", "t2", "kd")}
        sqb = B("sqb", [128, N], BF16)
        obf = [B("obf%d" % q, [128, N], BF16) for q in range(2)]
        vtm = [B("vtm%d" % q, [128, 1024], BF16) for q in range(2)]
        lwt = [B("lwt%d" % q, [128, 1024], F32) for q in range(2)]
        groups = [(0, 256, 0, 256)] + [(256 + g * N, N, 256, T) for g in range((T - 256) // N)]
        nob = 0
        for (s0, n_, lo, hi) in groups:
            a_, b_ = max(lo, s0 - 1), min(hi, s0 + n_ + 1)
            if a_ > s0 - 1:
                em.op("pool", lambda e: e.memset(hW[:, :, 0:1], 0.0), writes=[hW])
            if b_ < s0 + n_ + 1:
                em.op("pool", lambda e: e.memset(hW[:, :, N + 1:N + 2], 0.0), writes=[hW])
            em.dma("sp", hW[:, :, a_ - (s0 - 1):b_ - (s0 - 1)], self.kHT.ap()[:, :, a_:b_].rearrange("c p n -> p c n"), writes=[hW])
            em.op("dve", lambda e: e.tensor_tensor(out=xx[:], in0=hW[:, :, 0:N], in1=hW[:, :, 2:N + 2], op=ALU.add), reads=[hW], writes=[xx])
            em.op("dve", lambda e: e.scalar_tensor_tensor(out=xx[:], in0=xx[:], scalar=0.5, in1=hW[:, :, 1:N + 1], op0=ALU.mult, op1=ALU.subtract),
                  reads=[xx, hW], writes=[xx])
            for m in range(6):
                if z == 1 and m == 5:
                    continue
                for c in range(8):
                    em.op("dve", lambda e, m=m, c=c: e.scalar_tensor_tensor(
                        out=xm[m][:, c, :], in0=xx[:, c, :], scalar=mixv[:, m * 8 + c:m * 8 + c + 1], in1=hW[:, c, 1:N + 1],
                        op0=ALU.mult, op1=ALU.add), reads=[xx, hW, mixv], writes=[xm[m]])
            ps = self.pn()
            for kc in range(8):
                self.mm(ps, ps[0:64, 0:N], w1[:, kc, :], xm[3][:, kc, :], kc == 0, kc == 7, [w1, xm[3]])
            em.op("act", lambda e, ps=ps: e.activation(out=t1T[:], in_=ps[0:64, 0:N], func=AF.Tanh), reads=[ps], writes=[t1T])
            for q in range(2 if z == 0 else 1):
                ps = self.pn()
                for kc in range(8):
                    self.mm(ps, ps[0:64, 0:N], a1[q][:, kc, :], xm[4][:, kc, :], kc == 0, kc == 7, [a1[q], xm[4]])
                em.op("act", lambda e, ps=ps, q=q: e.activation(out=u1T[q][:], in_=ps[0:64, 0:N], func=AF.Copy), reads=[ps], writes=[u1T[q]])
            if z == 0:
                ps = self.pn()
                for kc in range(8):
                    self.mm(ps, ps[:, 0:N], g1[:, kc, :], xm[5][:, kc, :], kc == 0, kc == 7, [g1, xm[5]])
                em.op("act", lambda e, ps=ps: e.activation(out=gsT[:], in_=ps[:, 0:N], func=AF.Sigmoid), reads=[ps], writes=[gsT])
            for ti in range(2):
                t = s0 // 128 + ti
                v_, l_ = vtm[ti], lwt[ti]
                for half in range(2):
                    ps = self.pn()
                    for kc in range(8):
                        self.mm(ps, ps[:, :], xm[2][:, kc, ti * 128:(ti + 1) * 128], Wv[:, kc, half * 512:(half + 1) * 512], kc == 0, kc == 7, [xm[2], Wv])
                    em.op("act", lambda e, ps=ps, half=half: e.activation(out=v_[:, half * 512:(half + 1) * 512], in_=ps[:, :], func=AF.Copy), reads=[ps], writes=[v_])
                    ps = self.pn()
                    self.mm(ps, ps[:, :], t1T[:, ti * 128:(ti + 1) * 128], w2[:, half * 512:(half + 1) * 512], True, True, [t1T, w2])
                    em.op("dve", lambda e, ps=ps, half=half: e.tensor_tensor(out=l_[:, half * 512:(half + 1) * 512], in0=ps[:, :], in1=w0r[:, half * 512:(half + 1) * 512], op=ALU.add),
                          reads=[ps, w0r], writes=[l_])
                em.op("act", lambda e: e.activation(out=l_[:], in_=l_[:], func=AF.Sigmoid), reads=[l_], writes=[l_])
                em.op("dve", lambda e: e.tensor_scalar(out=l_[:], in0=l_[:], scalar1=-float(np.exp(-0.5)), scalar2=None, op0=ALU.mult), reads=[l_], writes=[l_])
                em.dma("pool", self.kV[z].ap()[t], v_[:], reads=[v_])
                em.dma("pool", self.kLW[z].ap()[t], l_[:], reads=[l_])
            for c in range(8):
                cs = slice(c * 128, (c + 1) * 128)
                for nm, m, W_ in (("r", 0, Wr), ("k", 1, Wk), ("v", 2, Wv)):
                    ps = self.pn()
                    for kc in range(8):
                        self.mm(ps, ps[:, 0:N], W_[:, kc, cs], xm[m][:, kc, :], kc == 0, kc == 7, [W_, xm[m]])
                    em.op("act", lambda e, ps=ps, nm=nm: e.activation(out=ch[nm][:], in_=ps[:, 0:N], func=AF.Copy), reads=[ps], writes=[ch[nm]])
                for q, nm in ((0, "a"), (1, "ao")):
                    if q == 1 and z == 1:
                        continue
                    ps = self.pn()
                    self.mm(ps, ps[:, 0:N], a2[q][:, cs], u1T[q][:], True, True, [a2[q], u1T[q]])
                    em.op("act", lambda e, ps=ps, nm=nm, q=q: e.activation(out=ch[nm][:], in_=ps[:, 0:N], func=AF.Sigmoid, bias=a0[q][:, c:c + 1], scale=1.0),
                          reads=[ps, a0[q]], writes=[ch[nm]])
                em.op("dve", lambda e: e.tensor_scalar(out=ch["kk"][:], in0=ch["k"][:], scalar1=vecs[:, c:c + 1], scalar2=None, op0=ALU.mult), reads=[ch["k"], vecs], writes=[ch["kk"]])
                em.op("dve", lambda e: e.tensor_tensor(out=sqb[:], in0=ch["kk"][:], in1=ch["kk"][:], op=ALU.mult), reads=[ch["kk"]], writes=[sqb])
                ps = self.pn()
                self.mm(ps, ps[:, 0:N], self.cb(C_B64), sqb[:], True, True, [self.cmb, sqb])
                em.op("act", lambda e, ps=ps: e.activation(out=ch["t"][:], in_=ps[:, 0:N], func=AF.Sqrt, bias=1e-6, scale=1.0), reads=[ps], writes=[ch["t"]])
                em.op("dve", lambda e: e.reciprocal(out=ch["t"][:], in_=ch["t"][:]), reads=[ch["t"]], writes=[ch["t"]])
                em.op("dve", lambda e: e.tensor_tensor(out=ch["kk"][:], in0=ch["kk"][:], in1=ch["t"][:], op=ALU.mult), reads=[ch["kk"], ch["t"]], writes=[ch["kk"]])
                em.op("dve", lambda e: e.tensor_scalar(out=ch["t"][:], in0=ch["a"][:], scalar1=vecs[:, 8 + c:9 + c], scalar2=vecs[:, 32 + c:33 + c], op0=ALU.mult, op1=ALU.add),
                      reads=[ch["a"], vecs], writes=[ch["t"]])
                em.op("dve", lambda e: e.tensor_tensor(out=ch["kd"][:], in0=ch["k"][:], in1=ch["t"][:], op=ALU.mult), reads=[ch["k"], ch["t"]], writes=[ch["kd"]])
                em.op("dve", lambda e: e.tensor_tensor(out=ch["t"][:], in0=ch["kk"][:], in1=ch["a"][:], op=ALU.mult), reads=[ch["kk"], ch["a"]], writes=[ch["t"]])
                em.op("dve", lambda e: e.tensor_scalar(out=ch["kk"][:], in0=ch["kk"][:], scalar1=-1.0, scalar2=None, op0=ALU.mult), reads=[ch["kk"]], writes=[ch["kk"]])
                for dst, nm in ((self.kR, "r"), (self.kK, "kd"), (self.kA, "kk"), (self.kB, "t")):
                    em.dma("pool", dst[z].ap()[c][:, s0:s0 + n_], ch[nm][:, 0:n_], reads=[ch[nm]])
                if z == 0:
                    em.op("dve", lambda e: e.tensor_scalar(out=ch["t2"][:], in0=ch["ao"][:], scalar1=vecs[:, 8 + c:9 + c], scalar2=vecs[:, 32 + c:33 + c], op0=ALU.mult, op1=ALU.add),
                          reads=[ch["ao"], vecs], writes=[ch["t2"]])
                    em.op("dve", lambda e: e.tensor_tensor(out=ch["t2"][:], in0=ch["t2"][:], in1=ch["k"][:], op=ALU.mult), reads=[ch["t2"], ch["k"]], writes=[ch["t2"]])
                    em.op("dve", lambda e: e.tensor_tensor(out=ch["t2"][:], in0=ch["t2"][:], in1=ch["kd"][:], op=ALU.add), reads=[ch["t2"], ch["kd"]], writes=[ch["t2"]])
                    em.op("dve", lambda e: e.scalar_tensor_tensor(out=sqb[:], in0=ch["t2"][:], scalar=vecs[:, 16 + c:17 + c], in1=ch["r"][:], op0=ALU.mult, op1=ALU.mult),
                          reads=[ch["t2"], ch["r"], vecs], writes=[sqb])
                    ps = self.pn()
                    self.mm(ps, ps[:, 0:N], self.cb(C_B64), sqb[:], True, True, [self.cmb, sqb])
                    ob = obf[nob % 2]
                    nob += 1
                    em.op("dve", lambda e, ps=ps, ob=ob: e.tensor_tensor(out=ob[:], in0=ps[:, 0:N], in1=ch["v"][:], op=ALU.mult), reads=[ps, ch["v"]], writes=[ob])
                    em.dma("pool", self.kBT.ap()[c][:, s0:s0 + n_], ob[:, 0:n_], reads=[ob])
                    ps = self.pn()
                    self.mm(ps, ps[:, 0:N], g2[:, 0, cs], gsT[:], True, True, [g2, gsT])
                    ob = obf[nob % 2]
                    nob += 1
                    em.op("act", lambda e, ps=ps, ob=ob: e.activation(out=ob[:], in_=ps[:, 0:N], func=AF.Copy), reads=[ps], writes=[ob])
                    em.dma("pool", self.kGT.ap()[c][:, s0:s0 + n_], ob[:, 0:n_], reads=[ob])
        em.barrier()
        em.release([mixv, vecs, w1, w2, w0r, Wr, Wk, Wv, g1, g2, hW, xx, t1T, gsT, sqb] + a0 + a1 + a2 + xm + u1T + list(ch.values()) + obf + vtm + lwt)


MK.rk_alloc = rk_alloc
MK.rk_phase_a1 = rk_phase_a1
MK.rk_phase_a2 = rk_phase_a2


def rk_phase_s(self, z):
    em = self.em
    T, NT = self.T, self.NT
    O = self.O2[z]
    with ExitStack() as st:
        B = lambda n, sh, dt: em.sb(st, "s" + n, sh, dt)
        ld = {n: B("ld" + n, [128, 8, 128], F32) for n in "rkab"}
        v = B("v", [128, 1024], BF16)
        lw = B("lw", [128, 1024], F32)
        ex = {n: B("ex" + n, [128, 8, 128], F32) for n in ("g", "gx", "ng", "gl")}
        GL = B("GL", [128, 8, 2], F32)
        AR = B("AR", [128, 8, 2, 128], BF16)
        BK = B("BK", [128, 8, 2, 128], BF16)
        BKh = B("BKh", [128, 8, 2, 128], BF16)
        BhT = B("BhT", [128, 8, 128], BF16)
        KhT = B("KhT", [128, 8, 128], BF16)
        MASK4 = B("MASK4", [128, 4, 128], BF16)
        for q, cidx, sgn in ((0, C_MS, -1.0), (1, C_MI, 1.0), (2, C_MS, 1.0), (3, C_MI, 1.0)):
            em.op("dve", lambda e, q=q, cidx=cidx: e.tensor_copy(out=MASK4[:, q, :], in_=self.cb(cidx)), reads=[self.cmb], writes=[MASK4])
        AM = B("AM", [128, 4, 16, 128], BF16)
        INVg = [B("INV%d" % g, [128, 4, 128], BF16) for g in range(4)]
        INVTg = [B("INVT%d" % g, [128, 4, 128], BF16) for g in range(4)]
        Tsbg = [B("Tsb%d" % g, [128, 4, 128], BF16) for g in range(4)]
        tmpxg = [[B("tmpx%d_%d" % (q, g), [128, 4, 128], BF16) for g in range(4)] for q in range(2)]
        RHSb = B("RHSb", [128, 1024], BF16)
        Ub = B("Ub", [128, 1024], BF16)
        OT = [B("OT%d" % q, [128, 1024], F32) for q in range(2)]
        S = B("S", [128, 8, 2, 64], F32)
        Sb = B("Sb", [128, 8, 2, 64], BF16)
        stmp = B("stmp", [128, 8, 128], F32)
        t2m = B("t2m", [128, 4, 128], F32)
        em.op("pool", lambda e: e.memset(S[:], 0.0), writes=[S])
        em.op("pool", lambda e: e.memset(Sb[:], 0.0), writes=[Sb])
        srcs = dict(r=self.kR[z], k=self.kK[z], a=self.kA[z], b=self.kB[z])
        f2 = lambda ap: ap.rearrange("p a b -> p (a b)")
        for t in range(NT):
            c0 = t * 128
            for n in "rkab":
                em.dma("sp", ld[n][:], srcs[n].ap()[:, :, c0:c0 + 128].rearrange("c p n -> p c n"), writes=[ld[n]])
            em.dma("sp", v[:], self.kV[z].ap()[t], writes=[v])
            em.dma("sp", lw[:], self.kLW[z].ap()[t], writes=[lw])
            for c in range(8):
                ps = self.pn()
                for q, cidx in enumerate((C_TRI, C_TRIS, C_BLK)):
                    self.mm(ps, ps[:, q * 128:(q + 1) * 128], lw[:, c * 128:(c + 1) * 128], self.cf(cidx), True, True, [lw, self.cmf])
                em.op("act", lambda e, ps=ps, c=c: e.activation(out=ex["g"][:, c, :], in_=ps[:, 0:128], func=AF.Exp), reads=[ps], writes=[ex["g"]])
                em.op("act", lambda e, ps=ps, c=c: e.activation(out=ex["gx"][:, c, :], in_=ps[:, 128:256], func=AF.Exp), reads=[ps], writes=[ex["gx"]])
                em.op("act", lambda e, ps=ps, c=c: e.activation(out=ex["ng"][:, c, :], in_=ps[:, 0:128], func=AF.Exp, scale=-1.0), reads=[ps], writes=[ex["ng"]])
                em.op("act", lambda e, ps=ps, c=c: e.activation(out=ex["gl"][:, c, :], in_=ps[:, 256:384], func=AF.Exp), reads=[ps], writes=[ex["gl"]])
            em.op("dve", lambda e: e.tensor_copy(out=GL[:], in_=ex["gl"][:].rearrange("p c (a b) -> p c a b", a=2)[:, :, :, 0]), reads=[ex["gl"]], writes=[GL])
            em.op("dve", lambda e: e.tensor_tensor(out=ex["gl"][:], in0=ex["gl"][:], in1=ex["ng"][:], op=ALU.mult), reads=[ex["gl"], ex["ng"]], writes=[ex["gl"]])
            em.op("dve", lambda e: e.tensor_tensor(out=AR[:, :, 0, :], in0=ld["a"][:], in1=ex["gx"][:], op=ALU.mult), reads=[ld["a"], ex["gx"]], writes=[AR])
            em.op("dve", lambda e: e.tensor_tensor(out=AR[:, :, 1, :], in0=ld["r"][:], in1=ex["g"][:], op=ALU.mult), reads=[ld["r"], ex["g"]], writes=[AR])
            em.op("pool", lambda e: e.tensor_tensor(out=BK[:, :, 0, :], in0=ld["b"][:], in1=ex["ng"][:], op=ALU.mult), reads=[ld["b"], ex["ng"]], writes=[BK])
            em.op("pool", lambda e: e.tensor_tensor(out=BK[:, :, 1, :], in0=ld["k"][:], in1=ex["ng"][:], op=ALU.mult), reads=[ld["k"], ex["ng"]], writes=[BK])
            em.op("pool", lambda e: e.tensor_tensor(out=BKh[:, :, 0, :], in0=ld["b"][:], in1=ex["gl"][:], op=ALU.mult), reads=[ld["b"], ex["gl"]], writes=[BKh])
            em.op("dve", lambda e: e.tensor_tensor(out=BKh[:, :, 1, :], in0=ld["k"][:], in1=ex["gl"][:], op=ALU.mult), reads=[ld["k"], ex["gl"]], writes=[BKh])
            import os
            dS = int(os.environ.get("RKS", "99"))
            if dS <= 1:
                continue
            for q, dstb in ((0, BhT), (1, KhT)):
                ps = self.pn()
                pb = ps[:].bitcast(BF16)
                for c in range(8):
                    em.op("pe", lambda e, pb=pb, c=c, q=q: e.transpose(out=pb[:, c * 128:(c + 1) * 128], in_=BKh[:, c, q, :], identity=self.cb(C_ID)),
                          reads=[BKh, self.cmb], writes=[ps])
                em.op("act", lambda e, pb=pb, dstb=dstb: e.activation(out=f2(dstb[:]), in_=pb[:, 0:1024], func=AF.Copy), reads=[ps], writes=[dstb])
            if dS <= 2:
                continue
            for h in range(16):
                c, base = h // 2, (h % 2) * 64
                ps = self.pn()
                rhs = AR[base:base + 64, c, :, :].rearrange("p a b -> p (a b)")
                self.mm(ps, ps[:, 0:256], BK[base:base + 64, c, 0, :], rhs, True, True, [BK, AR])
                self.mm(ps, ps[:, 256:512], BK[base:base + 64, c, 1, :], rhs, True, True, [BK, AR])
                em.op("dve", lambda e, ps=ps, h=h: e.tensor_tensor(out=AM[:, :, h, :], in0=ps[:, :].rearrange("p (a b) -> p a b", a=4), in1=MASK4[:], op=ALU.mult),
                      reads=[ps, MASK4], writes=[AM])
            if dS <= 3:
                continue
            self.tri_inverse16(lambda h: AM[:, 0, h, :], [AM] * 4, INVg, INVTg, Tsbg, tmpxg, -1.0)
            if dS <= 4:
                continue
            o_ = OT[t % 2]
            for cc in range(2):
                r0 = cc * 64
                rs_ = slice(r0, r0 + 64)
                pr = [self.pn(), self.pn()]
                for c in range(8):
                    pb_ = pr[c // 4]
                    q0 = (c % 4) * 128
                    self.mm(pb_, pb_[rs_, q0:q0 + 128], AR[:, c, 0, rs_], Sb[:, c, :, :].rearrange("p a b -> p (a b)"), True, False, [AR, Sb])
                    for e_ in range(2):
                        h = 2 * c + e_
                        self.mm(pb_, pb_[rs_, q0 + e_ * 64:q0 + (e_ + 1) * 64], AM[rs_, 2, h, rs_], v[rs_, h * 64:(h + 1) * 64], False, True, [AM, v])
                for b2 in range(2):
                    em.op("act" if b2 else "dve", lambda e, b2=b2: (e.activation(out=RHSb[rs_, b2 * 512:(b2 + 1) * 512], in_=pr[b2][rs_, :], func=AF.Copy) if b2 else
                                                                   e.tensor_copy(out=RHSb[rs_, 0:512], in_=pr[0][rs_, :])), reads=[pr[b2]], writes=[RHSb])
                pu = [self.pn(), self.pn()]
                for h in range(16):
                    pb_ = pu[h // 8]
                    cs_ = slice((h % 8) * 64, (h % 8 + 1) * 64)
                    self.mm(pb_, pb_[rs_, cs_], INVTg[h // 4][rs_, h % 4, rs_], RHSb[rs_, h * 64:(h + 1) * 64], True, True, [INVTg[h // 4], RHSb])
                for b2 in range(2):
                    em.op("act" if b2 else "dve", lambda e, b2=b2: (e.activation(out=Ub[rs_, b2 * 512:(b2 + 1) * 512], in_=pu[b2][rs_, :], func=AF.Copy) if b2 else
                                                                   e.tensor_copy(out=Ub[rs_, 0:512], in_=pu[0][rs_, :])), reads=[pu[b2]], writes=[Ub])
                po = [self.pn(), self.pn()]
                pS = [self.pn(), self.pn()]
                for c in range(8):
                    pb_ = po[c // 4]
                    q0 = (c % 4) * 128
                    self.mm(pb_, pb_[rs_, q0:q0 + 128], AR[:, c, 1, rs_], Sb[:, c, :, :].rearrange("p a b -> p (a b)"), True, False, [AR, Sb])
                    for e_ in range(2):
                        h = 2 * c + e_
                        osl = pb_[rs_, q0 + e_ * 64:q0 + (e_ + 1) * 64]
                        self.mm(pb_, osl, AM[rs_, 1, h, rs_], Ub[rs_, h * 64:(h + 1) * 64], False, False, [AM, Ub])
                        self.mm(pb_, osl, AM[rs_, 3, h, rs_], v[rs_, h * 64:(h + 1) * 64], False, True, [AM, v])
                    ps_ = pS[c // 4]
                    self.mm(ps_, ps_[:, q0:q0 + 128], BhT[rs_, c, :], Ub[rs_, c * 128:(c + 1) * 128], True, False, [BhT, Ub])
                    self.mm(ps_, ps_[:, q0:q0 + 128], KhT[rs_, c, :], v[rs_, c * 128:(c + 1) * 128], False, True, [KhT, v])
                for b2 in range(2):
                    em.op("act", lambda e, b2=b2: e.activation(out=o_[rs_, b2 * 512:(b2 + 1) * 512], in_=po[b2][rs_, :], func=AF.Copy), reads=[po[b2]], writes=[o_])
                em.op("dve", lambda e, cc=cc: e.tensor_tensor(out=stmp[:], in0=S[:].rearrange("p c a b -> p c (a b)"),
                                                              in1=GL[:, :, cc].unsqueeze(2).broadcast_to([128, 8, 128]), op=ALU.mult),
                      reads=[S, GL], writes=[stmp])
                for b2 in range(2):
                    em.op("dve", lambda e, b2=b2: e.tensor_tensor(out=t2m[:], in0=pS[b2][:, :].rearrange("p (c n) -> p c n", c=4),
                                                                  in1=self.cb(C_B64).unsqueeze(1).broadcast_to([128, 4, 128]), op=ALU.mult),
                          reads=[pS[b2], self.cmb], writes=[t2m])
                    em.op("pool", lambda e, b2=b2: e.tensor_tensor(out=S[:, b2 * 4:(b2 + 1) * 4, :, :].rearrange("p c a b -> p c (a b)"),
                                                                   in0=stmp[:, b2 * 4:(b2 + 1) * 4, :], in1=t2m[:], op=ALU.add),
                          reads=[stmp, t2m], writes=[S])
                em.op("act", lambda e: e.activation(out=Sb[:].rearrange("p c a b -> p (c a b)"), in_=S[:].rearrange("p c a b -> p (c a b)"), func=AF.Copy),
                      reads=[S], writes=[Sb])
            em.dma("pool", O.ap()[t * 128:(t + 1) * 128, 0:1024], o_[:], reads=[o_])
        em.barrier()
        em.release(list(ld.values()) + list(ex.values()) + [v, lw, GL, AR, BK, BKh, BhT, KhT, MASK4, AM, RHSb, Ub, S, Sb, stmp, t2m] + INVg + INVTg + Tsbg + tmpxg[0] + tmpxg[1] + OT)


def rk_factory(self):
    em = self.em

    def factory(st):
        vecs = em.sb(st, "qvecs", [128, 32], F32)
        em.dma("sp", vecs[:], self.rk_in["vec"].ap(), writes=[vecs])
        oo = [em.sb(st, "qoo%d" % q, [128, 8, 64], F32) for q in range(2)]
        p1 = [em.sb(st, "qp1%d" % q, [128, 512], F32) for q in range(2)]
        bt = [em.sb(st, "qbt%d" % q, [128, 8, 128], BF16) for q in range(2)]
        gt = [em.sb(st, "qgt%d" % q, [128, 8, 128], BF16) for q in range(2)]
        osum = em.sb(st, "qosum", [128, 8, 64], F32)
        sq = em.sb(st, "qsq", [128, 8, 64], F32)
        ss = em.sb(st, "qss", [128, 8], F32)
        y = em.sb(st, "qy", [128, 1024], BF16)
        tt = em.sb(st, "qtt", [128, 128], F32)
        cnt = [0]
        f2 = lambda b: b[:].rearrange("p a b -> p (a b)")
        bc8 = lambda ap: ap.unsqueeze(2).broadcast_to([128, 8, 64])

        def make_yT(t, isctx, yT):
            pt = self.ptile(t)
            b_, g_ = bt[t % 2], gt[t % 2]
            em.dma("sp", b_[:], self.kBT.ap()[:, :, t * 128:(t + 1) * 128].rearrange("c p n -> p c n"), writes=[b_])
            em.dma("sp", g_[:], self.kGT.ap()[:, :, t * 128:(t + 1) * 128].rearrange("c p n -> p c n"), writes=[g_])
            for blk in range(2):
                k = cnt[0]
                cnt[0] += 1
                o_, a1 = oo[k % 2], p1[k % 2]
                cs = slice(blk * 512, (blk + 1) * 512)
                em.dma("sp", f2(o_), self.O2[0].ap()[t * 128:(t + 1) * 128, cs], writes=[o_])
                em.dma("sp", a1[:], self.O2[1].ap()[pt * 128:(pt + 1) * 128, cs], writes=[a1])
                ps = self.pn()
                self.mm(ps, ps[:, :], self.cf(C_J0), a1[:], True, True, [self.cmf, a1])
                em.op("dve", lambda e: e.tensor_tensor(out=f2(osum), in0=ps[:, :], in1=f2(o_), op=ALU.add), reads=[ps, o_], writes=[osum])
                em.op("dve", lambda e: e.tensor_reduce(out=ss[:], in_=osum[:], axis=AX.X, op=ALU.add), reads=[osum], writes=[ss])
                em.op("dve", lambda e: e.tensor_scalar(out=ss[:], in0=ss[:], scalar1=-1.0 / 64.0, scalar2=None, op0=ALU.mult), reads=[ss], writes=[ss])
                em.op("dve", lambda e: e.tensor_tensor(out=osum[:], in0=osum[:], in1=bc8(ss[:]), op=ALU.add), reads=[osum, ss], writes=[osum])
                em.op("pool", lambda e: e.tensor_tensor(out=sq[:], in0=osum[:], in1=osum[:], op=ALU.mult), reads=[osum], writes=[sq])
                em.op("dve", lambda e: e.tensor_reduce(out=ss[:], in_=sq[:], axis=AX.X, op=ALU.add), reads=[sq], writes=[ss])
                em.op("act", lambda e: e.activation(out=ss[:], in_=ss[:], func=AF.Sqrt, bias=64e-5, scale=1.0 / 64.0), reads=[ss], writes=[ss])
                em.op("dve", lambda e: e.reciprocal(out=ss[:], in_=ss[:]), reads=[ss], writes=[ss])
                em.op("dve", lambda e: e.tensor_tensor(out=y[:, cs].rearrange("p (a b) -> p a b", a=8), in0=osum[:], in1=bc8(ss[:]), op=ALU.mult),
                      reads=[osum, ss], writes=[y])
            ps = self.pn()
            pb = ps[:].bitcast(BF16)
            for c in range(8):
                em.op("pe", lambda e: e.transpose(out=pb[:, c * 128:(c + 1) * 128], in_=y[:, c * 128:(c + 1) * 128], identity=self.cb(C_ID)),
                      reads=[y, self.cmb], writes=[ps])
            for c in range(8):
                em.op("dve", lambda e: e.scalar_tensor_tensor(out=tt[:], in0=pb[:, c * 128:(c + 1) * 128], scalar=vecs[:, 24 + c:25 + c], in1=b_[:, c, :],
                                                              op0=ALU.mult, op1=ALU.add), reads=[ps, vecs, b_], writes=[tt])
                em.op("pool", lambda e: e.tensor_tensor(out=yT[:, c, :], in0=tt[:], in1=g_[:, c, :], op=ALU.mult), reads=[tt, g_], writes=[yT])
        return make_yT, [vecs, osum, sq, ss, y, tt] + oo + p1 + bt + gt
    return factory


def rk_layer(self, i, modLC, rows):
    if not hasattr(self, "kHT"):
        self.rk_alloc()
    import os
    dbg = int(os.environ.get("RKDBG", "99"))
    for z in range(2):
        self.rk_phase_a1(modLC, z)
        if dbg >= 2:
            self.rk_phase_a2(z)
        if dbg >= 3:
            self.rk_phase_s(z)
    if dbg >= 4:
        self.phase_f(i, modLC, rows, self.rk_factory(), "rkout", 8)


MK.rk_phase_s = rk_phase_s
MK.rk_factory = rk_factory
MK.rk_layer = rk_layer
```

```python
import numpy as np
from contextlib import ExitStack
import concourse.bass as bass
import concourse.mybir as mybir
from concourse.bass_utils import run_bass_kernel_spmd

F32 = mybir.dt.float32
BF16 = mybir.dt.bfloat16
AF = mybir.ActivationFunctionType
ALU = mybir.AluOpType
AX = mybir.AxisListType

D = 1024
NCTX = 256
ALPHA = 8.0 ** 0.25
LN_EPS = 1e-5


class Sem:
    __slots__ = ("h", "total")

    def __init__(self, h):
        self.h = h
        self.total = 0


class Buf:
    __slots__ = ("t", "w", "r", "sem", "name")

    def __init__(self, t, name):
        self.t = t
        self.w = None
        self.r = {}
        self.sem = None
        self.name = name

    def __getitem__(self, k):
        return self.t[k]


class Em:
    def __init__(self, nc):
        self.nc = nc
        self.eng = {"pe": nc.tensor, "act": nc.scalar, "dve": nc.vector, "pool": nc.gpsimd, "sp": nc.sync}
        self.semobj = {k: Sem(nc.alloc_semaphore(name="s_" + k)) for k in self.eng}
        self.waited = {k: {} for k in self.eng}
        self.dma_sems = []
        self.free_dma_sems = []
        self.bufs = []
        self.nins = 0

    def sb(self, stack, name, shape, dt):
        self.uid = getattr(self, "uid", 0) + 1
        t = stack.enter_context(self.nc.sbuf_tensor("sb%d_%s" % (self.uid, name), list(shape), dt))
        b = Buf(t, name)
        self.bufs.append(b)
        return b

    def ps(self, stack, name, shape=(128, 512), dt=F32):
        t = stack.enter_context(self.nc.psum_tensor("pp_" + name, list(shape), dt))
        b = Buf(t, name)
        self.bufs.append(b)
        return b

    def _dma_sem(self):
        if self.free_dma_sems:
            return self.free_dma_sems.pop()
        s = Sem(self.nc.alloc_semaphore(name="d%d" % len(self.dma_sems)))
        self.dma_sems.append(s)
        return s

    def release(self, bufs):
        for b in bufs:
            if b.sem is not None:
                self.free_dma_sems.append(b.sem)
                b.sem = None
            if b in self.bufs:
                self.bufs.remove(b)

    def _wait(self, en, toks):
        E = self.eng[en]
        w = self.waited[en]
        own = self.semobj[en]
        best = {}
        for (s, v) in toks:
            if s is own and en in ("pe", "sp"):
                continue
            if best.get(s, 0) < v:
                best[s] = v
        for s, v in best.items():
            if w.get(s, 0) < v:
                E.wait_ge(s.h, v)
                w[s] = v

    def _deps(self, reads, writes):
        toks = []
        for b in reads:
            if b.w is not None:
                toks.append(b.w)
        for b in writes:
            if b.w is not None:
                toks.append(b.w)
            toks.extend(b.r.items())
        return toks

    @staticmethod
    def _mark(tok, reads, writes):
        s, v = tok
        for b in reads:
            if b.r.get(s, 0) < v:
                b.r[s] = v
        for b in writes:
            b.w = tok
            b.r = {}

    def op(self, en, fn, reads=(), writes=()):
        self._wait(en, self._deps(reads, writes))
        ins = fn(self.eng[en])
        s = self.semobj[en]
        s.total += 1
        ins.then_inc(s.h, 1)
        self._mark((s, s.total), reads, writes)
        self.nins += 1

    def dma(self, qn, out, in_, reads=(), writes=(), sem=None):
        self._wait(qn, self._deps(reads, writes))
        ins = self.eng[qn].dma_start(out=out, in_=in_)
        if sem is None:
            b = writes[0] if writes else reads[0]
            if b.sem is None:
                b.sem = self._dma_sem()
            sem = b.sem
        sem.total += 16
        ins.then_inc(sem.h, 16)
        self._mark((sem, sem.total), reads, writes)
        self.nins += 1

    def barrier(self):
        allsems = list(self.semobj.values()) + self.dma_sems
        for en, E in self.eng.items():
            w = self.waited[en]
            for s in allsems:
                if s.total > 0 and w.get(s, 0) < s.total:
                    E.wait_ge(s.h, s.total)
                    w[s] = s.total
        for b in self.bufs:
            b.w = None
            b.r = {}


def bc(ap_t, offset_elems, dims):
    from concourse.ap import AP
    return AP(ap_t, offset_elems, dims)


C_ID, C_J0, C_J1, C_MRET, C_TRI, C_BLK, C_SEL0, C_SEL1, C_MS, C_MI = range(10)
C_TRIS, C_ONES, C_B64 = 10, 11, 12
C_LVT = 13
C_LV = 19
NCONST = 25
NF32 = 13


def make_consts(z):
    p = np.arange(128)[:, None]
    f = np.arange(128)[None, :]
    blk = (p // 64) == (f // 64)
    cm = np.zeros((NCONST, 128, 128), np.float32)
    cm[C_ID] = np.eye(128)
    J = np.eye(128)[::-1]
    cm[C_J0] = J
    cm[C_MRET] = (p <= f)
    cm[C_TRI] = (p <= f) & blk
    cm[C_TRIS] = (p < f) & blk
    cm[C_BLK] = blk
    cm[C_SEL0] = (p < 64) & (f >= 0)
    cm[C_SEL1] = (p >= 64) & (f >= 0)
    cm[C_MS] = (p < f) & blk
    cm[C_MI] = (p <= f) & blk
    for k in range(6):
        m = ((p >> (k + 1)) == (f >> (k + 1))) & (((p >> k) & 1) == 1) & (((f >> k) & 1) == 0)
        cm[C_LV + k] = m
        cm[C_LVT + k] = m.T
    cm[C_ONES] = 1.0
    cm[C_B64] = blk
    return cm


class MK:
    def __init__(self, L, layers, ncores=4, direct=True, layer_ids=None):
        self.layer_ids = list(range(layers)) if layer_ids is None else list(layer_ids)
        self.ncores = ncores
        self.direct = direct
        self.pairs = [[2 * p, 2 * p + 1] for p in range(max(1, ncores // 2))]
        self.L = L
        self.layers = layers
        self.NLT = L // 128
        self.NT = 2 + self.NLT
        self.T = 256 + L
        self.HT = 256 + L // 2
        self.groups = [(0, 2, True)] + [(2 + 4 * g, 4, False) for g in range(self.NLT // 4)]
        self.half_groups = self.groups
        self.nc = bass.Bass("TRN2", target_bir_lowering=False)
        self.em = Em(self.nc)
        self.ext = {}
        self.units = {}
        self.cc_sem = Sem(self.nc.alloc_semaphore(name="cc"))
        self.em.dma_sems.append(self.cc_sem)
        self.gsem = Sem(self.nc.alloc_semaphore(name="gdma"))
        self.em.dma_sems.append(self.gsem)

    def xin(self, name, shape, dt=F32):
        t = self.nc.dram_tensor(name, list(shape), dt, kind="ExternalInput")
        self.ext[name] = (tuple(shape), dt)
        return t

    def dr(self, name, shape, dt):
        return self.nc.dram_tensor(name, list(shape), dt, kind="Internal")

    def ptile(self, t):
        return 1 - t if t < 2 else 2 + (self.NLT - 1 - (t - 2))

    def unit(self, name, K, N):
        em = self.em
        g = self.dr("wg_" + name, [K, N], BF16)
        self.units[name] = (g, K, N)
        if self.direct:
            full = self.xin("wf_" + name, [K, N])
            em.dma("pool", g.ap(), full.ap(), sem=self.gsem)
            return None
        sh = self.xin("w_" + name, [K // 8, N])
        sb = self.dr("ws_" + name, [K // 8, N], BF16)
        gq = self.dr("wq_" + name, [K // 2, N], BF16)
        em.dma("pool", sb.ap(), sh.ap(), sem=self.gsem)
        return (sb, gq, g)

    def gather_units(self, pend):
        em = self.em
        em.barrier()
        if self.direct:
            return
        for stage in range(2):
            for sb, gq, g in pend:
                rg = [[0, 1, 2, 3], [4, 5, 6, 7]] if stage == 0 else [[0, 4], [1, 5], [2, 6], [3, 7]]
                src, dst = (sb, gq) if stage == 0 else (gq, g)
                ins = self.nc.gpsimd.collective_compute("AllGather", ALU.bypass, replica_groups=rg,
                                                        ins=[src.ap()], outs=[dst.ap()])
                self.cc_sem.total += 1
                ins.then_inc(self.cc_sem.h, 1)
                self.nc.gpsimd.wait_ge(self.cc_sem.h, self.cc_sem.total)
            em.barrier()

    def pair_gather(self, src, dst):
        em = self.em
        em.barrier()
        ins = self.nc.gpsimd.collective_compute("AllGather", ALU.bypass, replica_groups=self.pairs,
                                                ins=[src.ap()], outs=[dst.ap()])
        self.cc_sem.total += 1
        ins.then_inc(self.cc_sem.h, 1)
        em.barrier()

    def wsrc(self, name, kc0, nkc, n0, nw):
        g, K, N = self.units[name]
        return g.ap()[kc0 * 128:(kc0 + nkc) * 128, n0:n0 + nw].rearrange("(c p) n -> p c n", p=128)

    def load_w(self, buf, name, kc0, nkc, n0, nw):
        self.em.dma("sp", buf[:, 0:nkc, 0:nw], self.wsrc(name, kc0, nkc, n0, nw), writes=[buf])

    def setup(self):
        nc, em = self.nc, self.em
        self.pst = ExitStack()
        st = self.pst
        self.cmf = em.sb(st, "cmf", [128, NF32, 128], F32)
        self.cmb = em.sb(st, "cmb", [128, NCONST, 128], BF16)
        cm = self.xin("cm", [128, NCONST, 128])
        em.dma("sp", self.cmf[:], cm.ap()[:, 0:NF32, :], writes=[self.cmf])
        em.dma("pool", self.cmb[:], cm.ap(), writes=[self.cmb])
        self.iota = em.sb(st, "iota", [128, 4], F32)
        em.dma("sp", self.iota[:], self.xin("iota", [128, 4]).ap(), writes=[self.iota])
        self.psb = [em.ps(st, "ps%d" % i) for i in range(8)]
        self.psk = 0
        self.xs = self.xin("xs", [self.T, D])
        self.cvec = self.xin("cvec", [128, 16])
        self.modb = self.xin("modb", [4, 128, 48])
        self.modbf = self.xin("modbf", [4, 6144])
        self.lng = self.xin("lng", [4, 2, D])
        self.lnb = self.xin("lnb", [4, 2, D])
        self.xsf = self.xin("xsf", [self.T, D])
        self.LAT2 = [self.dr("LAT0", [self.T, D], F32), self.dr("LAT1", [self.T, D], F32)]
        self.O2 = [self.dr("O0", [self.T, 2048], F32), self.dr("O1", [self.T, 2048], F32)]
        self.H2T = self.dr("H2T", [len(self.half_groups), 128, 8 * 512], BF16)
        em.dma("pool", self.LAT2[0].ap(), self.xs.ap(), sem=self.gsem)
        em.dma("pool", self.LAT2[1].ap(), self.xsf.ap(), sem=self.gsem)
        self.OUT = self.nc.dram_tensor("out", [self.T, D], F32, kind="ExternalOutput")
        self.flt = [em.sb(st, "flt%d" % q, [128, D], F32) for q in range(2)]
        self.fltk = 0

    def cf(self, i):
        return self.cmf[:, i, :]

    def store_lat(self, t, ou, out=False):
        em = self.em
        em.dma("pool", self.LAT2[0].ap()[t * 128:(t + 1) * 128, :], ou[:], reads=[ou])
        if out:
            em.dma("pool", self.OUT.ap()[t * 128:(t + 1) * 128, :], ou[:], reads=[ou])
        fl = self.flt[self.fltk % 2]
        self.fltk += 1
        for half in range(2):
            ps = self.pn()
            self.mm(ps, ps[:, :], self.cf(C_J0), ou[:, half * 512:(half + 1) * 512], True, True, [self.cmf, ou])
            if half:
                em.op("act", lambda e, ps=ps, fl=fl: e.activation(out=fl[:, 512:1024], in_=ps[:, :], func=AF.Copy), reads=[ps], writes=[fl])
            else:
                em.op("dve", lambda e, ps=ps, fl=fl: e.tensor_copy(out=fl[:, 0:512], in_=ps[:, :]), reads=[ps], writes=[fl])
        pt = self.ptile(t)
        em.dma("pool", self.LAT2[1].ap()[pt * 128:(pt + 1) * 128, :], fl[:], reads=[fl])

    def cb(self, i):
        return self.cmb[:, i, :]

    def pn(self):
        while True:
            b = self.psb[self.psk % 8]
            self.psk += 1
            if b not in getattr(self, "reserved", ()):
                return b

    def mm(self, ps, out, lhsT, rhs, start, stop, reads):
        self.em.op("pe", lambda e: e.matmul(out, lhsT=lhsT, rhs=rhs, start=start, stop=stop), reads=reads, writes=[ps])

    def phase_mod(self, i, st):
        em = self.em
        modLC = em.sb(st, "modLC%d" % i, [128, 48, 2], F32)
        rows = {n: em.sb(st, "%s_%d" % (n, i), [128, D], F32)
                for n in ("GA1L", "GA1C", "GA2L", "GA2C", "LNG0", "LNB0", "LNG1", "LNB1")}
        import os
        for s in range(2 if os.environ.get("MKA", "0") == "0" else 0):
            em.dma("sp", rows["LNG%d" % s][:], self.lng.ap()[i, s:s + 1, :].partition_broadcast(128), writes=[rows["LNG%d" % s]])
            em.dma("sp", rows["LNB%d" % s][:], self.lnb.ap()[i, s:s + 1, :].partition_broadcast(128), writes=[rows["LNB%d" % s]])
        import os
        dbg = int(os.environ.get("MKDBG", "99"))
        if dbg == 0:
            em.barrier()
            return modLC, rows
        with ExitStack() as ts:
            cv = em.sb(ts, "cv", [128, 16], F32)
            em.dma("sp", cv[:], self.cvec.ap(), writes=[cv])
            sT = em.sb(ts, "sT", [128, 8, 2], BF16)
            em.op("act", lambda e: e.activation(out=sT[:].rearrange("p c t -> p (c t)"), in_=cv[:], func=AF.Silu),
                  reads=[cv], writes=[sT])
            sBC = em.sb(ts, "sBC", [128, 8, 2, 128], BF16)
            em.op("dve", lambda e: e.tensor_copy(out=sBC[:], in_=sT[:].unsqueeze(3).broadcast_to([128, 8, 2, 128])),
                  reads=[sT], writes=[sBC])
            mb = em.sb(ts, "mb", [128, 48], F32)
            em.dma("sp", mb[:], self.modb.ap()[i], writes=[mb])
            brow = [em.sb(ts, "brow%d" % q, [128, D], F32) for q in range(2)]
            em.dma("sp", brow[0][:], self.modbf.ap()[i:i + 1, 2048:3072].partition_broadcast(128), writes=[brow[0]])
            em.dma("sp", brow[1][:], self.modbf.ap()[i:i + 1, 5120:6144].partition_broadcast(128), writes=[brow[1]])
            wb = [em.sb(ts, "wm%d" % q, [128, 8, 1536], BF16) for q in range(2)]
            psm = self.pn()
            self.reserved = [psm]
            if dbg == 1:
                em.barrier()
                return modLC, rows
            for piece in range(4):
                w = wb[piece % 2]
                self.load_w(w, "modw%d" % i, 0, 8, piece * 1536, 1536)
                if dbg == 2:
                    continue
                for oc in range(12):
                    g = piece * 12 + oc
                    for kc in range(8):
                        self.mm(psm, psm[:, g * 2:g * 2 + 2], w[:, kc, oc * 128:(oc + 1) * 128], sT[:, kc, :],
                                kc == 0, kc == 7, [w, sT])
                if piece in (1, 3):
                    for t, nm in ((0, "L"), (1, "C")):
                        dest = rows[("GA1" if piece == 1 else "GA2") + nm]
                        br = brow[0 if piece == 1 else 1]
                        for half in range(2):
                            pb = self.pn()
                            for kc in range(8):
                                self.mm(pb, pb[:, :], sBC[:, kc, t, :], w[:, kc, 512 + half * 512:1024 + half * 512],
                                        kc == 0, kc == 7, [w, sBC])
                            em.op("dve", lambda e, pb=pb, dest=dest, br=br, half=half: e.scalar_tensor_tensor(
                                out=dest[:, half * 512:(half + 1) * 512], in0=pb[:, :], scalar=1.0,
                                in1=br[:, half * 512:(half + 1) * 512], op0=ALU.add, op1=ALU.add),
                                reads=[pb, br], writes=[dest])
            em.op("dve", lambda e: e.tensor_tensor(out=modLC[:], in0=psm[:, 0:96].rearrange("p (c t) -> p c t", t=2),
                                                   in1=mb[:].unsqueeze(2).broadcast_to([128, 48, 2]), op=ALU.add),
                  reads=[psm, mb], writes=[modLC])
            self.reserved = []
            for c0 in (8, 32):
                em.op("dve", lambda e, c0=c0: e.tensor_scalar_add(out=modLC[:, c0:c0 + 8, :], in0=modLC[:, c0:c0 + 8, :], scalar1=1.0),
                      reads=[modLC], writes=[modLC])
            em.barrier()
            em.release([cv, sT, sBC, mb] + brow + wb)
        return modLC, rows

    def hT_tile(self, lat_tile, hT, col0, modLC, sub, isctx):
        em = self.em
        t = 1 if isctx else 0
        sh0, sc0 = (0, 8) if sub == 0 else (24, 32)
        for half in range(2):
            ps = self.pn()
            for c4 in range(4):
                c = half * 4 + c4
                em.op("pe", lambda e, ps=ps, c=c, c4=c4: e.transpose(out=ps[:, c4 * 128:(c4 + 1) * 128],
                                                                     in_=lat_tile[:, c * 128:(c + 1) * 128],
                                                                     identity=self.cf(C_ID)),
                      reads=[lat_tile, self.cmf], writes=[ps])
            for c4 in range(4):
                c = half * 4 + c4
                em.op("act", lambda e, ps=ps, c=c, c4=c4: e.activation(
                    out=hT[:, c, col0:col0 + 128], in_=ps[:, c4 * 128:(c4 + 1) * 128], func=AF.Identity,
                    scale=modLC[:, sc0 + c, t:t + 1], bias=modLC[:, sh0 + c, t:t + 1]),
                    reads=[ps, modLC], writes=[hT])

    def tail(self, pso, lat_tile, GA, LNG, LNB, tmp, outt, st6, mv, rs):
        em = self.em
        for half in range(2):
            sl = slice(half * 512, (half + 1) * 512)
            sb_, sap = pso[half] if isinstance(pso[half], tuple) else (pso[half], pso[half][:, :])
            em.op("dve", lambda e, sap=sap, sl=sl: e.tensor_tensor(out=tmp[:, sl], in0=sap, in1=GA[:, sl], op=ALU.mult),
                  reads=[sb_, GA], writes=[tmp])
        em.op("dve", lambda e: e.scalar_tensor_tensor(out=tmp[:], in0=lat_tile[:], scalar=ALPHA, in1=tmp[:], op0=ALU.mult, op1=ALU.add),
              reads=[lat_tile, tmp], writes=[tmp])
        self.layer_norm(tmp, LNG, LNB, outt, st6, mv, rs)

    def layer_norm(self, tmp, LNG, LNB, outt, st6, mv, rs):
        em = self.em
        for half in range(2):
            em.op("dve", lambda e, half=half: e.bn_stats(out=st6[:, half, :], in_=tmp[:, half * 512:(half + 1) * 512]),
                  reads=[tmp], writes=[st6])
        em.op("dve", lambda e: e.bn_aggr(out=mv[:], in_=st6[:]), reads=[st6], writes=[mv])
        em.op("act", lambda e: e.activation(out=rs[:], in_=mv[:, 1:2], func=AF.Sqrt, bias=LN_EPS, scale=1.0), reads=[mv], writes=[rs])
        em.op("dve", lambda e: e.reciprocal(out=rs[:], in_=rs[:]), reads=[rs], writes=[rs])
        em.op("dve", lambda e: e.tensor_scalar(out=tmp[:], in0=tmp[:], scalar1=mv[:, 0:1], scalar2=rs[:, 0:1],
                                               op0=ALU.subtract, op1=ALU.mult), reads=[tmp, mv, rs], writes=[tmp])
        em.op("pool", lambda e: e.tensor_tensor(out=tmp[:], in0=tmp[:], in1=LNG[:], op=ALU.mult), reads=[tmp, LNG], writes=[tmp])
        em.op("pool", lambda e: e.tensor_tensor(out=outt[:], in0=tmp[:], in1=LNB[:], op=ALU.add), reads=[tmp, LNB], writes=[outt])

    def phase_f(self, i, modLC, rows, factory, wout_name, KC):
        em = self.em
        with ExitStack() as st:
            make_yT, mbufs = factory(st)
            wout = em.sb(st, "wout", [128, KC, D], BF16)
            self.load_w(wout, wout_name, 0, KC, 0, D)
            yT = [em.sb(st, "yT%d" % q, [128, KC, 128], BF16) for q in range(2)]
            lat = [em.sb(st, "flat%d" % q, [128, D], F32) for q in range(2)]
            outt = [em.sb(st, "fout%d" % q, [128, D], F32) for q in range(2)]
            tmp = em.sb(st, "ftmp", [128, D], F32)
            st6 = em.sb(st, "fst6", [128, 2, 6], F32)
            mv = em.sb(st, "fmv", [128, 2], F32)
            rs = em.sb(st, "frs", [128, 1], F32)
            h2g = [em.sb(st, "h2g%d" % q, [128, 8, 512], BF16) for q in range(2)]
            k = 0
            for gi, (t0, ng, isctx) in enumerate(self.groups):
                own = gi < len(self.half_groups)
                sfx = "C" if isctx else "L"
                for ti in range(ng):
                    t = t0 + ti
                    y = yT[k % 2]
                    la = lat[k % 2]
                    ou = outt[k % 2]
                    k += 1
                    make_yT(t, isctx, y)
                    em.dma("sp", la[:], self.LAT2[0].ap()[t * 128:(t + 1) * 128, :], writes=[la])
                    pso = [self.pn(), self.pn()]
                    for half in range(2):
                        for kc in range(KC):
                            self.mm(pso[half], pso[half][:, :], y[:, kc, :], wout[:, kc, half * 512:(half + 1) * 512],
                                    kc == 0, kc == KC - 1, [y, wout])
                    self.tail(pso, la, rows["GA1" + sfx], rows["LNG0"], rows["LNB0"], tmp, ou, st6, mv, rs)
                    self.store_lat(t, ou)
                    if own:
                        self.hT_tile(ou, h2g[gi % 2], ti * 128, modLC, 1, isctx)
                if own:
                    em.dma("pool", self.H2T.ap()[gi].rearrange("p (c n) -> p c n", c=8), h2g[gi % 2][:], reads=[h2g[gi % 2]])
            em.barrier()
            em.release([wout, tmp, st6, mv, rs] + yT + lat + outt + h2g + mbufs)

    def ffn_tail_bufs(self, st):
        em = self.em
        d = dict(lat=[em.sb(st, "glat%d" % q, [128, D], F32) for q in range(2)],
                 outt=[em.sb(st, "gout%d" % q, [128, D], F32) for q in range(2)],
                 tmp=em.sb(st, "gtmp", [128, D], F32), st6=em.sb(st, "gst6", [128, 2, 6], F32),
                 mv=em.sb(st, "gmv", [128, 2], F32), rs=em.sb(st, "grs", [128, 1], F32))
        return d

    def ffn_store(self, t, ou):
        self.store_lat(t, ou, out=True)

    def ffn_dense(self, li, modLC, rows):
        em = self.em
        with ExitStack() as st:
            wd = em.sb(st, "wd", [128, 22, D], BF16)
            self.load_w(wd, "ffndn%d" % li, 0, 22, 0, D)
            wg = [em.sb(st, "wg%d" % q, [128, 8, 256], BF16) for q in range(2)]
            wu = [em.sb(st, "wu%d" % q, [128, 8, 256], BF16) for q in range(2)]
            h2 = [em.sb(st, "h2_%d" % q, [128, 8, 512], BF16) for q in range(2)]
            act = em.sb(st, "act", [128, 22, 512], BF16)
            sg = [em.sb(st, "sg%d" % q, [128, 512], F32) for q in range(2)]
            B = self.ffn_tail_bufs(st)
            k = 0
            for gi, (t0, ng, isctx) in enumerate(self.half_groups):
                N = ng * 128
                h = h2[gi % 2]
                em.dma("sp", h[:], self.H2T.ap()[gi].rearrange("p (c n) -> p c n", c=8), writes=[h])
                for s in range(11):
                    g_, u_ = wg[s % 2], wu[s % 2]
                    self.load_w(g_, "ffngu%d" % li, 0, 8, s * 256, 256)
                    self.load_w(u_, "ffngu%d" % li, 0, 8, 2816 + s * 256, 256)
                    for fc in range(2):
                        f = s * 2 + fc
                        pg, pu = self.pn(), self.pn()
                        for kc in range(8):
                            self.mm(pg, pg[:, :N], g_[:, kc, fc * 128:(fc + 1) * 128], h[:, kc, :N], kc == 0, kc == 7, [g_, h])
                        for kc in range(8):
                            self.mm(pu, pu[:, :N], u_[:, kc, fc * 128:(fc + 1) * 128], h[:, kc, :N], kc == 0, kc == 7, [u_, h])
                        s_ = sg[f % 2]
                        em.op("act", lambda e, pg=pg, s_=s_: e.activation(out=s_[:, :N], in_=pg[:, :N], func=AF.Silu), reads=[pg], writes=[s_])
                        em.op("dve", lambda e, pu=pu, s_=s_, f=f: e.tensor_tensor(out=act[:, f, :N], in0=s_[:, :N], in1=pu[:, :N], op=ALU.mult),
                              reads=[pu, s_], writes=[act])
                sfx = "C" if isctx else "L"
                for ti in range(ng):
                    t = t0 + ti
                    la, ou = B["lat"][k % 2], B["outt"][k % 2]
                    k += 1
                    em.dma("sp", la[:], self.LAT2[0].ap()[t * 128:(t + 1) * 128, :], writes=[la])
                    pso = [self.pn(), self.pn()]
                    for half in range(2):
                        for f in range(22):
                            self.mm(pso[half], pso[half][:, :], act[:, f, ti * 128:(ti + 1) * 128], wd[:, f, half * 512:(half + 1) * 512],
                                    f == 0, f == 21, [act, wd])
                    self.tail(pso, la, rows["GA2" + sfx], rows["LNG1"], rows["LNB1"], B["tmp"], ou, B["st6"], B["mv"], B["rs"])
                    self.ffn_store(t, ou)
            em.barrier()
            em.release([wd, act] + wg + wu + h2 + sg + B["lat"] + B["outt"] + [B["tmp"], B["st6"], B["mv"], B["rs"]])

    def phase_h(self):
        em = self.em
        self.pair_gather(self.FX, self.FXG)
        with ExitStack() as st:
            a = [[em.sb(st, "ha%d_%d" % (s, q), [128, D], F32) for s in range(2)] for q in range(2)]
            ou = [em.sb(st, "ho%d" % q, [128, D], F32) for q in range(2)]
            k = 0
            for t in range(2 + self.NLT // 2, self.NT):
                pt = self.ptile(t)
                aa, o_ = a[k % 2], ou[k % 2]
                k += 1
                for s in range(2):
                    em.dma("sp", aa[s][:], self.FXG.ap()[s * self.HT + pt * 128:s * self.HT + (pt + 1) * 128, :], writes=[aa[s]])
                for half in range(2):
                    ps = self.pn()
                    for s in range(2):
                        self.mm(ps, ps[:, :], self.cf(C_J0 + s), aa[s][:, half * 512:(half + 1) * 512], s == 0, s == 1, [self.cmf, aa[s]])
                    em.op("act" if half else "dve", lambda e, ps=ps, half=half, o_=o_: (
                        e.activation(out=o_[:, half * 512:(half + 1) * 512], in_=ps[:, :], func=AF.Copy) if half else
                        e.tensor_copy(out=o_[:, half * 512:(half + 1) * 512], in_=ps[:, :])), reads=[ps], writes=[o_])
                em.dma("pool", self.LAT.ap()[t * 128:(t + 1) * 128, :], o_[:], reads=[o_])
            em.barrier()
            em.release(a[0] + a[1] + ou)

    def ret_alloc(self):
        NT = self.NT
        self.rQT2 = [self.dr("rQT%d" % z, [NT, 128, 1024], BF16) for z in range(2)]
        self.rKT2 = [self.dr("rKT%d" % z, [NT, 128, 1024], BF16) for z in range(2)]
        self.rKTM2 = [self.dr("rKTM%d" % z, [NT, 128, 1024], BF16) for z in range(2)]
        self.rV2 = [self.dr("rV%d" % z, [NT, 128, 2048], BF16) for z in range(2)]
        self.rG = self.dr("rG", [NT, 128, 2048], BF16)
        self.rope2 = [self.xin("rope%d" % z, [NT, 128, 256]) for z in range(2)]

    def ret_phase_a(self, j, modLC, z):
        em = self.em
        self.LAT, self.rope = self.LAT2[z], self.rope2[z]
        self.rQT, self.rKT, self.rKTM, self.rV = self.rQT2[z], self.rKT2[z], self.rKTM2[z], self.rV2[z]
        with ExitStack() as st:
            wb = [em.sb(st, "rw%d" % q, [128, 8, 512], BF16) for q in range(2)]
            hT = [em.sb(st, "rhT%d" % q, [128, 8, 512], BF16) for q in range(2)]
            lat = [em.sb(st, "rlat%d" % q, [128, D], F32) for q in range(2)]
            qr = [em.sb(st, "rqr%d" % q, [128, 1024], BF16) for q in range(4)]
            kr = [em.sb(st, "rkr%d" % q, [128, 1024], BF16) for q in range(4)]
            vv = [em.sb(st, "rvv%d" % q, [128, 2048], BF16) for q in range(4)]
            gg = [em.sb(st, "rgg%d" % q, [128, 2048], BF16) for q in range(4)]
            rp = [em.sb(st, "rrp%d" % q, [128, 256], F32) for q in range(4)]
            cos4 = [em.sb(st, "rcos%d" % q, [128, 4, 64], F32) for q in range(4)]
            sin4 = [em.sb(st, "rsin%d" % q, [128, 4, 64], F32) for q in range(4)]
            xf = [em.sb(st, "rxf%d" % q, [128, 512], F32) for q in range(2)]
            t1 = [em.sb(st, "rt1%d" % q, [128, 512], F32) for q in range(2)]
            t2 = [em.sb(st, "rt2%d" % q, [128, 4, 64], F32) for q in range(2)]
            t3 = [em.sb(st, "rt3%d" % q, [128, 4, 64], F32) for q in range(2)]
            qT = [em.sb(st, "rqT%d" % q, [128, 1024], BF16) for q in range(2)]
            fl = [em.sb(st, "rfl%d" % q, [128, 2048], BF16) for q in range(2)]
            kk = 0
            rk = 0
            for gi, (t0, ng, isctx) in enumerate(self.groups):
                h = hT[gi % 2]
                for ti in range(ng):
                    la = lat[kk % 2]
                    kk += 1
                    em.dma("sp", la[:], self.LAT.ap()[(t0 + ti) * 128:(t0 + ti + 1) * 128, :], writes=[la])
                    self.hT_tile(la, h, ti * 128, modLC, 0, isctx)
                    em.dma("sp", rp[ti][:], self.rope.ap()[t0 + ti], writes=[rp[ti]])
                    em.op("pool", lambda e, ti=ti: e.tensor_copy(out=cos4[ti][:].rearrange("p (a b) f -> p a (b f)", a=2),
                                                                 in_=rp[ti][:, 0:128].unsqueeze(1).broadcast_to([128, 2, 128])),
                          reads=[rp[ti]], writes=[cos4[ti]])
                    em.op("pool", lambda e, ti=ti: e.tensor_copy(out=sin4[ti][:].rearrange("p (a b) f -> p a (b f)", a=2),
                                                                 in_=rp[ti][:, 128:256].unsqueeze(1).broadcast_to([128, 2, 128])),
                          reads=[rp[ti]], writes=[sin4[ti]])
                for nb in range(12 if z == 0 else 8):
                    w = wb[nb % 2]
                    self.load_w(w, "retin%d" % j, 0, 8, nb * 512, 512)
                    for ti in range(ng):
                        ps = self.pn()
                        for kc in range(8):
                            self.mm(ps, ps[:, :], h[:, kc, ti * 128:(ti + 1) * 128], w[:, kc, :], kc == 0, kc == 7, [h, w])
                        if nb < 4:
                            dest = (qr if nb < 2 else kr)[ti]
                            dsl = dest[:, (nb % 2) * 512:(nb % 2 + 1) * 512].rearrange("p (a s f) -> p a s f", a=4, s=2)
                            x_, t1_, t2_, t3_ = xf[rk % 2], t1[rk % 2], t2[rk % 2], t3[rk % 2]
                            rk += 1
                            sc = 1.0 if nb < 2 else 1.0 / 16.0
                            em.op("act", lambda e, ps=ps, x_=x_, sc=sc: e.activation(out=x_[:], in_=ps[:, :], func=AF.Copy, scale=sc),
                                  reads=[ps], writes=[x_])
                            X = x_[:].rearrange("p (a s f) -> p a s f", a=4, s=2)
                            T1 = t1_[:].rearrange("p (a s f) -> p a s f", a=4, s=2)
                            em.op("dve", lambda e, X=X, T1=T1, ti=ti: e.tensor_tensor(
                                out=T1, in0=X, in1=cos4[ti][:].unsqueeze(2).broadcast_to([128, 4, 2, 64]), op=ALU.mult),
                                reads=[x_, cos4[ti]], writes=[t1_])
                            em.op("pool", lambda e, X=X, t2_=t2_, ti=ti: e.tensor_tensor(out=t2_[:], in0=X[:, :, 1, :], in1=sin4[ti][:], op=ALU.mult),
                                  reads=[x_, sin4[ti]], writes=[t2_])
                            em.op("pool", lambda e, X=X, t3_=t3_, ti=ti: e.tensor_tensor(out=t3_[:], in0=X[:, :, 0, :], in1=sin4[ti][:], op=ALU.mult),
                                  reads=[x_, sin4[ti]], writes=[t3_])
                            em.op("dve", lambda e, dsl=dsl, T1=T1, t2_=t2_: e.tensor_tensor(out=dsl[:, :, 0, :], in0=T1[:, :, 0, :], in1=t2_[:], op=ALU.subtract),
                                  reads=[t1_, t2_], writes=[dest])
                            em.op("dve", lambda e, dsl=dsl, T1=T1, t3_=t3_: e.tensor_tensor(out=dsl[:, :, 1, :], in0=T1[:, :, 1, :], in1=t3_[:], op=ALU.add),
                                  reads=[t1_, t3_], writes=[dest])
                        else:
                            dest = (vv if nb < 8 else gg)[ti]
                            c0 = (nb % 4) * 512
                            if (nb + ti) % 2:
                                em.op("act", lambda e, ps=ps, dest=dest, c0=c0: e.activation(out=dest[:, c0:c0 + 512], in_=ps[:, :], func=AF.Copy),
                                      reads=[ps], writes=[dest])
                            else:
                                em.op("dve", lambda e, ps=ps, dest=dest, c0=c0: e.tensor_copy(out=dest[:, c0:c0 + 512], in_=ps[:, :]),
                                      reads=[ps], writes=[dest])
                for ti in range(ng):
                    t = t0 + ti
                    em.dma("pool", self.rKTM.ap()[t], kr[ti][:], reads=[kr[ti]])
                    em.dma("pool", self.rV.ap()[t], vv[ti][:], reads=[vv[ti]])
                    if z == 0:
                        em.dma("pool", self.rG.ap()[t], gg[ti][:], reads=[gg[ti]])
                    for src, dstT in ((qr[ti], self.rQT), (kr[ti], self.rKT)):
                        ps = self.pn()
                        pb = ps[:].bitcast(BF16)
                        for c in range(8):
                            em.op("pe", lambda e, pb=pb, src=src, c=c: e.transpose(out=pb[:, c * 128:(c + 1) * 128], in_=src[:, c * 128:(c + 1) * 128],
                                                                                 identity=self.cb(C_ID)), reads=[src, self.cmb], writes=[ps])
                        q_ = qT[kk % 2]
                        kk += 1
                        em.op("act", lambda e, pb=pb, q_=q_: e.activation(out=q_[:], in_=pb[:, 0:1024], func=AF.Copy), reads=[ps], writes=[q_])
                        em.dma("pool", dstT.ap()[t], q_[:], reads=[q_])
                    if z == 0 and getattr(self, "ret_flip", True):
                        pt = self.ptile(t)
                        Jb = self.cb(C_J0)
                        for src, dstD, nblk in ((kr[ti], self.rKTM2[1], 2), (vv[ti], self.rV2[1], 4)):
                            f_ = fl[kk % 2]
                            kk += 1
                            for b_ in range(nblk):
                                ps = self.pn()
                                self.mm(ps, ps[:, :], Jb, src[:, b_ * 512:(b_ + 1) * 512], True, True, [self.cmb, src])
                                if b_ % 2:
                                    em.op("act", lambda e, ps=ps, f_=f_, b_=b_: e.activation(out=f_[:, b_ * 512:(b_ + 1) * 512], in_=ps[:, :], func=AF.Copy), reads=[ps], writes=[f_])
                                else:
                                    em.op("dve", lambda e, ps=ps, f_=f_, b_=b_: e.tensor_copy(out=f_[:, b_ * 512:(b_ + 1) * 512], in_=ps[:, :]), reads=[ps], writes=[f_])
                            em.dma("pool", dstD.ap()[pt], f_[:, 0:nblk * 512], reads=[f_])
                        for src, dstD in ((qr[ti], self.rQT2[1]), (kr[ti], self.rKT2[1])):
                            f_ = fl[kk % 2]
                            kk += 1
                            for hb in range(2):
                                ps = self.pn()
                                for c4 in range(4):
                                    c = hb * 4 + c4
                                    self.mm(ps, ps[:, c4 * 128:(c4 + 1) * 128], src[:, c * 128:(c + 1) * 128], Jb, True, True, [self.cmb, src])
                                if hb:
                                    em.op("act", lambda e, ps=ps, f_=f_, hb=hb: e.activation(out=f_[:, hb * 512:(hb + 1) * 512], in_=ps[:, :], func=AF.Copy), reads=[ps], writes=[f_])
                                else:
                                    em.op("dve", lambda e, ps=ps, f_=f_, hb=hb: e.tensor_copy(out=f_[:, hb * 512:(hb + 1) * 512], in_=ps[:, :]), reads=[ps], writes=[f_])
                            em.dma("pool", dstD.ap()[pt], f_[:, 0:1024], reads=[f_])
            em.barrier()
            em.release(wb + hT + lat + qr + kr + vv + gg + rp + cos4 + sin4 + xf + t1 + t2 + t3 + qT + fl)

    def ret_phase_s(self, j, z):
        em = self.em
        dec_in = self.xin("retdec%d_%d" % (j, z), [1, 4])
        self.rQT, self.rKT, self.rKTM, self.rV, self.O = self.rQT2[z], self.rKT2[z], self.rKTM2[z], self.rV2[z], self.O2[z]
        with ExitStack() as st:
            dec = em.sb(st, "sdec", [128, 4], F32)
            em.dma("sp", dec[:], dec_in.ap().partition_broadcast(128), writes=[dec])
            lsp = em.sb(st, "slsp", [128, 4], F32)
            em.op("act", lambda e: e.activation(out=lsp[:], in_=dec[:], func=AF.Exp, scale=-1.0), reads=[dec], writes=[lsp])
            em.op("act", lambda e: e.activation(out=lsp[:], in_=lsp[:], func=AF.Ln, bias=1.0, scale=1.0), reads=[lsp], writes=[lsp])
            outsc = em.sb(st, "soutsc", [128, 4], F32)
            scsc = em.sb(st, "sscsc", [128, 4], F32)
            kdec = em.sb(st, "skdec", [128, 4], F32)
            gC = em.sb(st, "sgC", [128, 4], F32)
            io = self.iota
            em.op("act", lambda e: e.activation(out=outsc[:], in_=lsp[:], func=AF.Exp, scale=io[:, 1:2]), reads=[lsp, io], writes=[outsc])
            em.op("act", lambda e: e.activation(out=scsc[:], in_=lsp[:], func=AF.Exp, scale=io[:, 0:1]), reads=[lsp, io], writes=[scsc])
            em.op("act", lambda e: e.activation(out=kdec[:], in_=lsp[:], func=AF.Exp, scale=io[:, 3:4]), reads=[lsp, io], writes=[kdec])
            em.op("act", lambda e: e.activation(out=gC[:], in_=lsp[:], func=AF.Exp, scale=-128.0), reads=[lsp], writes=[gC])
            S = [[em.sb(st, "sS%d_%d" % (h, dc), [128, 512], F32) for dc in range(2)] for h in range(4)]
            Sb = [[em.sb(st, "sSb%d_%d" % (h, dc), [128, 512], BF16) for dc in range(2)] for h in range(4)]
            for h in range(4):
                for dc in range(2):
                    em.op("pool", lambda e, h=h, dc=dc: e.memset(S[h][dc][:], 0.0), writes=[S[h][dc]])
                    em.op("pool", lambda e, h=h, dc=dc: e.memset(Sb[h][dc][:], 0.0), writes=[Sb[h][dc]])
            qt = [em.sb(st, "sqt%d" % q, [128, 8, 128], BF16) for q in range(2)]
            kt = [em.sb(st, "skt%d" % q, [128, 8, 128], BF16) for q in range(2)]
            ktm = [em.sb(st, "sktm%d" % q, [128, 1024], BF16) for q in range(2)]
            v = [em.sb(st, "sv%d" % q, [128, 2048], BF16) for q in range(2)]
            ot = [em.sb(st, "sot%d" % q, [128, 2048], F32) for q in range(2)]
            scb = [em.sb(st, "sscb%d" % q, [128, 128], BF16) for q in range(4)]
            kd = [em.sb(st, "skd%d" % q, [128, 256], BF16) for q in range(4)]
            scb4, kd4 = scb, kd
            n = 0
            for t in range(self.NT):
                q_, k_, km_, v_, o_ = qt[t % 2], kt[t % 2], ktm[t % 2], v[t % 2], ot[t % 2]
                em.dma("sp", q_[:], self.rQT.ap()[t].rearrange("p (c n) -> p c n", c=8), writes=[q_])
                em.dma("sp", k_[:], self.rKT.ap()[t].rearrange("p (c n) -> p c n", c=8), writes=[k_])
                em.dma("sp", km_[:], self.rKTM.ap()[t], writes=[km_])
                em.dma("sp", v_[:], self.rV.ap()[t], writes=[v_])
                ps = self.pn()
                for h in range(4):
                    for dc in range(2):
                        self.mm(ps, ps[:, h * 128:(h + 1) * 128], k_[:, 2 * h + dc, :], q_[:, 2 * h + dc, :], dc == 0, dc == 1, [k_, q_])
                for h in range(4):
                    em.op("dve", lambda e, h=h: e.scalar_tensor_tensor(
                        out=scb4[h][:], in0=ps[:, h * 128:(h + 1) * 128], scalar=scsc[:, h:h + 1], in1=self.cf(C_MRET), op0=ALU.mult, op1=ALU.mult),
                        reads=[ps, scsc, self.cmf], writes=[scb4[h]])
                    em.op("pool", lambda e, h=h: e.tensor_scalar(out=kd4[h][:], in0=km_[:, h * 256:(h + 1) * 256],
                                                                 scalar1=kdec[:, h:h + 1], scalar2=None, op0=ALU.mult),
                          reads=[km_, kdec], writes=[kd4[h]])
                pos = [self.pn() for h in range(4)]
                for h in range(4):
                    vh = v_[:, h * 512:(h + 1) * 512]
                    self.mm(pos[h], pos[h][:, :], scb4[h][:], vh, True, False, [scb4[h], v_])
                    for dc in range(2):
                        self.mm(pos[h], pos[h][:, :], q_[:, 2 * h + dc, :], Sb[h][dc][:], False, dc == 1, [q_, Sb[h][dc]])
                for h in range(4):
                    em.op("act", lambda e, h=h: e.activation(out=o_[:, h * 512:(h + 1) * 512], in_=pos[h][:, :], func=AF.Identity,
                                                             scale=outsc[:, h:h + 1]), reads=[pos[h], outsc], writes=[o_])
                for hp in range(2):
                    pus = {}
                    for h in (2 * hp, 2 * hp + 1):
                        vh = v_[:, h * 512:(h + 1) * 512]
                        for dc in range(2):
                            pu = self.pn()
                            pus[(h, dc)] = pu
                            self.mm(pu, pu[:, :], kd4[h][:, dc * 128:(dc + 1) * 128], vh, True, True, [kd4[h], v_])
                    for h in (2 * hp, 2 * hp + 1):
                        for dc in range(2):
                            S_, Sb_, pu = S[h][dc], Sb[h][dc], pus[(h, dc)]
                            em.op("dve", lambda e, pu=pu, S_=S_, h=h: e.scalar_tensor_tensor(
                                out=S_[:], in0=S_[:], scalar=gC[:, h:h + 1], in1=pu[:, :], op0=ALU.mult, op1=ALU.add),
                                reads=[pu, S_, gC], writes=[S_])
                            em.op("act", lambda e, S_=S_, Sb_=Sb_: e.activation(out=Sb_[:], in_=S_[:], func=AF.Copy), reads=[S_], writes=[Sb_])
                em.dma("pool", self.O.ap()[t * 128:(t + 1) * 128, :], o_[:], reads=[o_])
            em.barrier()
            em.release([dec, lsp, outsc, scsc, kdec, gC] + sum(S, []) + sum(Sb, []) + qt + kt + ktm + v + ot + scb + kd)

    def ret_factory(self, j):
        em = self.em
        gn_in = self.xin("retgn%d" % j, [1, 2048])

        def factory(st):
            gng = em.sb(st, "ygng", [128, 2048], F32)
            em.dma("sp", gng[:], gn_in.ap().partition_broadcast(128), writes=[gng])
            oo = [em.sb(st, "yoo%d" % q, [128, 512], F32) for q in range(2)]
            p0 = [em.sb(st, "yp0%d" % q, [128, 512], F32) for q in range(2)]
            p1 = [em.sb(st, "yp1%d" % q, [128, 512], F32) for q in range(2)]
            gt = [em.sb(st, "ygt%d" % q, [128, 512], BF16) for q in range(2)]
            osum = em.sb(st, "yosum", [128, 512], F32)
            sg = em.sb(st, "ysg", [128, 512], F32)
            y = em.sb(st, "yy", [128, 2048], BF16)
            st6 = em.sb(st, "yst6", [128, 6], F32)
            mv = em.sb(st, "ymv", [128, 2], F32)
            rs = em.sb(st, "yrs", [128, 1], F32)
            cnt = [0]
            mk4 = lambda n, sh, dt: [em.sb(st, "y4%s%d" % (n, h), sh, dt) for h in range(4)]
            oo4, p14, os4, sg4 = mk4("oo", [128, 512], F32), mk4("p1", [128, 512], F32), mk4("os", [128, 512], F32), mk4("sg", [128, 512], F32)
            gt4 = mk4("gt", [128, 512], BF16)
            st64, mv4, rs4 = mk4("st6", [128, 6], F32), mk4("mv", [128, 2], F32), mk4("rs", [128, 1], F32)

            def make_yT(t, isctx, yT):
                pt = self.ptile(t)
                H = range(4)
                css = [slice(h * 512, (h + 1) * 512) for h in H]
                for h in H:
                    em.dma("sp", oo4[h][:], self.O2[0].ap()[t * 128:(t + 1) * 128, css[h]], writes=[oo4[h]])
                    em.dma("sp", p14[h][:], self.O2[1].ap()[pt * 128:(pt + 1) * 128, css[h]], writes=[p14[h]])
                    em.dma("sp", gt4[h][:], self.rG.ap()[t][:, css[h]], writes=[gt4[h]])
                pss = [self.pn() for h in H]
                for h in H:
                    self.mm(pss[h], pss[h][:, :], self.cf(C_J0), p14[h][:], True, True, [self.cmf, p14[h]])
                for h in H:
                    em.op("dve", lambda e, h=h: e.tensor_tensor(out=os4[h][:], in0=pss[h][:, :], in1=oo4[h][:], op=ALU.add),
                          reads=[pss[h], oo4[h]], writes=[os4[h]])
                    em.op("act", lambda e, h=h: e.activation(out=sg4[h][:], in_=gt4[h][:], func=AF.Silu), reads=[gt4[h]], writes=[sg4[h]])
                for h in H:
                    em.op("dve", lambda e, h=h: e.bn_stats(out=st64[h][:], in_=os4[h][:]), reads=[os4[h]], writes=[st64[h]])
                for h in H:
                    em.op("dve", lambda e, h=h: e.bn_aggr(out=mv4[h][:], in_=st64[h][:]), reads=[st64[h]], writes=[mv4[h]])
                for h in H:
                    em.op("act", lambda e, h=h: e.activation(out=rs4[h][:], in_=mv4[h][:, 1:2], func=AF.Sqrt, bias=1e-5, scale=1.0), reads=[mv4[h]], writes=[rs4[h]])
                for h in H:
                    em.op("dve", lambda e, h=h: e.reciprocal(out=rs4[h][:], in_=rs4[h][:]), reads=[rs4[h]], writes=[rs4[h]])
                for h in H:
                    em.op("dve", lambda e, h=h: e.tensor_scalar(out=os4[h][:], in0=os4[h][:], scalar1=mv4[h][:, 0:1], scalar2=rs4[h][:, 0:1],
                                                                op0=ALU.subtract, op1=ALU.mult), reads=[os4[h], mv4[h], rs4[h]], writes=[os4[h]])
                for h in H:
                    em.op("pool", lambda e, h=h: e.tensor_tensor(out=os4[h][:], in0=os4[h][:], in1=gng[:, css[h]], op=ALU.mult),
                          reads=[os4[h], gng], writes=[os4[h]])
                for h in H:
                    em.op("pool", lambda e, h=h: e.tensor_tensor(out=y[:, css[h]], in0=os4[h][:], in1=sg4[h][:], op=ALU.mult),
                          reads=[os4[h], sg4[h]], writes=[y])
                for half in range(2):
                    ps = self.pn()
                    pb = ps[:].bitcast(BF16)
                    for c in range(8):
                        cc = half * 8 + c
                        em.op("pe", lambda e, pb=pb, c=c, cc=cc: e.transpose(out=pb[:, c * 128:(c + 1) * 128], in_=y[:, cc * 128:(cc + 1) * 128],
                                                                           identity=self.cb(C_ID)), reads=[y, self.cmb], writes=[ps])
                    em.op("act", lambda e, pb=pb, half=half: e.activation(
                        out=yT[:, half * 8:(half + 1) * 8, :].rearrange("p c n -> p (c n)"), in_=pb[:, 0:1024], func=AF.Copy),
                        reads=[ps], writes=[yT])
            return make_yT, [gng, osum, sg, y, st6, mv, rs] + oo + p0 + p1 + gt + oo4 + p14 + os4 + sg4 + gt4 + st64 + mv4 + rs4
        return factory

    def build(self):
        em = self.em
        self.setup()
        pend = []
        stop = getattr(self, "stop", 99)
        for i in self.layer_ids:
            pend.append(self.unit("modw%d" % i, 1024, 6144))
            kind, j = i % 3, i // 3
            if kind == 0:
                pend += [self.unit("retin%d" % j, 1024, 6144), self.unit("retout%d" % j, 2048, 1024)]
            elif kind == 1:
                pend += [self.unit("dnin", 1024, 6144), self.unit("dnout", 2048, 1024)]
            else:
                pend += [self.unit(n, 1024, 1024) for n in ("rkr", "rkk", "rkv", "rkout")]
                pend += [self.unit("rkg1", 1024, 128), self.unit("rkg2", 128, 1024)]
            if stop == 5 and i == self.layer_ids[-1]:
                pass
            elif i % 2 == 0 and not getattr(self, "force_moe", False):
                pend += [self.unit("ffngu%d" % (i // 2), 1024, 5632), self.unit("ffndn%d" % (i // 2), 2816, 1024)]
            else:
                for e_ in range(8):
                    pend += [self.unit("moegu%d_%d" % (i // 2, e_), 1024, 7168), self.unit("moedn%d_%d" % (i // 2, e_), 3584, 1024)]
        self.gather_units(pend)
        stop = getattr(self, "stop", 99)
        if self.layers >= 1:
            self.ret_alloc()
        if stop == 0:
            em.barrier()
            return self.nc
        for i in self.layer_ids:
            kind, j = i % 3, i // 3
            with ExitStack() as lst:
                modLC, rows = self.phase_mod(i, lst)
                if stop == 1:
                    return self.nc
                if kind == 0:
                    self.ret_phase_a(j, modLC, 0)
                    for z in range(2):
                        self.ret_phase_s(j, z)
                    if stop == 3:
                        import os
                        which = os.environ.get("MKDUMP", "O0")
                        dbg = self.nc.dram_tensor("dbg", [self.T, 2048], F32, kind="ExternalOutput")
                        srcs = {"O0": self.O2[0].ap(), "O1": self.O2[1].ap(),
                                "V0": self.rV2[0].ap().rearrange("t p n -> (t p) n"),
                                "G": self.rG.ap().rearrange("t p n -> (t p) n")}
                        if which in srcs:
                            em.dma("pool", dbg.ap(), srcs[which], sem=self.gsem)
                        elif which == "K0":
                            em.dma("pool", dbg.ap()[:, 0:1024], self.rKTM2[0].ap().rearrange("t p n -> (t p) n"), sem=self.gsem)
                        elif which == "QT0":
                            em.dma("pool", dbg.ap()[:, 0:1024], self.rQT2[0].ap().rearrange("t p n -> (t p) n"), sem=self.gsem)
                        em.barrier()
                        return self.nc
                    self.phase_f(i, modLC, rows, self.ret_factory(j), "retout%d" % j, 16)
                elif kind == 1:
                    self.dn_layer(i, modLC, rows)
                else:
                    self.rk_layer(i, modLC, rows)
                if stop == 5 and i == self.layer_ids[-1]:
                    em.dma("pool", self.OUT.ap(), self.LAT2[0].ap(), sem=self.gsem)
                    em.barrier()
                    return self.nc
                self.skip_ctx_ffn = (i == 3)
                if i % 2 == 0 and not getattr(self, "force_moe", False):
                    self.ffn_dense(i // 2, modLC, rows)
                else:
                    self.ffn_moe(i // 2, modLC, rows)
                em.barrier()
                em.release([modLC] + list(rows.values()))
        em.barrier()
        return self.nc


def rope_tables(L, z):
    NT = 2 + L // 128
    tab = np.zeros((NT, 128, 256), np.float32)
    tab[:2, :, 0:128] = 1.0
    inv = (10000.0 ** (-np.arange(64, dtype=np.float32) / 64)).astype(np.float32)
    n = np.arange(L)
    if z:
        n = n[::-1]
    row = (n // 64).astype(np.float32)[:, None] * inv[None, :]
    col = (n % 64).astype(np.float32)[:, None] * inv[None, :]
    full = np.concatenate([np.cos(row), np.cos(col), np.sin(row), np.sin(col)], 1).astype(np.float32)
    tab[2:] = full.reshape(L // 128, 128, 256)
    return tab


def host_inputs(mk, inp, core):
    b, z = core, 0
    L = mk.L
    f = lambda a: np.ascontiguousarray(a, dtype=np.float32)
    x = inp["x"][b][:L]
    cx = inp["ctx"][b]
    m = {}
    m["xs"] = f(np.concatenate([cx, x], 0))
    m["xsf"] = f(np.concatenate([cx[::-1], x[::-1]], 0))
    cv = np.stack([inp["c"][b], inp["c_ctx"]], -1).reshape(8, 128, 2).transpose(1, 0, 2).reshape(128, 16)
    m["cvec"] = f(cv)
    m["modb"] = f(inp["mod_b"].reshape(4, 48, 128).transpose(0, 2, 1))
    m["modbf"] = f(inp["mod_b"])
    m["lng"] = f(inp["ln_g"])
    m["lnb"] = f(inp["ln_b"])
    m["cm"] = f(make_consts(z).transpose(1, 0, 2))
    p = np.arange(128, dtype=np.float32)
    m["iota"] = f(np.stack([p + 1, -(p + 1), 0 * p, p - 127], 1))
    m["rope0"] = rope_tables(L, 0)
    m["rope1"] = rope_tables(L, 1)
    W = {}
    for i in range(4):
        W["modw%d" % i] = inp["mod_w"][i]
    for j in range(2):
        W["retin%d" % j] = inp["ret_w_in"][j]
        W["retout%d" % j] = inp["ret_w_out"][j]
        for zz in range(2):
            m["retdec%d_%d" % (j, zz)] = f(inp["ret_decay"][j][zz][None, :])
        m["retgn%d" % j] = f(inp["ret_gn_g"][j][None, :])
        W["ffngu%d" % j] = inp["ffn_w_gu"][j]
        W["ffndn%d" % j] = inp["ffn_w_down"][j]
        for e_ in range(8):
            W["moegu%d_%d" % (j, e_)] = inp["moe_w_gu"][j][e_]
            W["moedn%d_%d" % (j, e_)] = inp["moe_w_down"][j][e_]
    for j in range(2):
        m["moer%d" % j] = f(inp["moe_router"][j].reshape(8, 128, 8).transpose(1, 0, 2).reshape(128, 64))
    W["dnin"] = inp["dn_w_in"][0][:, :6144]
    W["dnout"] = inp["dn_w_out"][0]
    for q, nme in enumerate(("rkr", "rkk", "rkv")):
        W[nme] = inp["rk_w_rkv"][0][q]
    W["rkout"] = inp["rk_w_out"][0]
    W["rkg1"] = inp["rk_g1"][0]
    W["rkg2"] = inp["rk_g2"][0]
    for name in mk.ext:
        if name.startswith("w_"):
            w = W[name[2:]]
            k8 = w.shape[0] // 8
            m[name] = f(w[core * k8:(core + 1) * k8])
        elif name.startswith("wf_"):
            m[name] = f(W[name[3:]])
    host_extra(mk, inp, core, m)
    out = {}
    for name, (shape, dt) in mk.ext.items():
        a = m[name]
        assert tuple(a.shape) == tuple(shape), (name, a.shape, shape)
        out[name] = a
    return out


def host_extra(mk, inp, core, m):
    f = lambda a: np.ascontiguousarray(a, dtype=np.float32)
    wab = inp["dn_w_in"][0][:, 6144:]
    cw = inp["dn_conv_w"][0]
    for z in range(2):
        a = wab[:, z * 32:(z + 1) * 32]
        m["dnab%d" % z] = f(a.reshape(8, 128, 32).transpose(1, 0, 2).reshape(128, 256))
        m["dnalog%d" % z] = f(inp["dn_a_log"][0][z][None, :])
        m["dndtb%d" % z] = f(inp["dn_dt_bias"][0][z][None, :])
        c = cw if z == 0 else cw[::-1]
        m["dnconv%d" % z] = f(c.T.reshape(32, 128, 5).transpose(1, 0, 2).reshape(128, 160))
    m["dnng"] = f(inp["dn_norm_g"][0][None, :])
    fm8 = lambda v_: v_.reshape(8, 128).T
    m["rkmix"] = f(np.concatenate([fm8(inp["rk_mix"][0][q]) for q in range(6)], 1))
    m["rkvec"] = f(np.concatenate([fm8(inp["rk_k_k"][0]), fm8(inp["rk_k_a"][0]), fm8(inp["rk_r_k"][0].reshape(-1)), fm8(inp["rk_lnx_g"][0])], 1))
    lo = lambda w_: w_.reshape(8, 128, 64).transpose(1, 0, 2).reshape(128, 512)
    for z in range(2):
        m["rkw0_%d" % z] = f(inp["rk_w0"][0][z][None, :])
        m["rka0_%d" % z] = f(fm8(inp["rk_a0"][0][z]))
        m["rkw1_%d" % z] = f(lo(inp["rk_w1"][0][z]))
        m["rka1_%d" % z] = f(lo(inp["rk_a1"][0][z]))
        m["rkw2_%d" % z] = f(inp["rk_w2"][0][z])
        m["rka2_%d" % z] = f(inp["rk_a2"][0][z])


_CACHE = {}


def run_model(inp, L=8192, layers=4):
    key = (L, layers)
    if key not in _CACHE:
        mk = MK(L, layers)
        mk.build()
        _CACHE[key] = mk
    mk = _CACHE[key]
    nco = mk.ncores
    in_maps = [host_inputs(mk, inp, c) for c in range(nco)]
    res = run_bass_kernel_spmd(mk.nc, in_maps, core_ids=list(range(nco)))
    lat = np.zeros((4, L, D), np.float32)
    cx = np.zeros((4, NCTX, D), np.float32)
    for c in range(nco):
        o = res.results[c]["out"]
        lat[c] = o[256:]
        cx[c] = o[:256]
    return lat, cx


def kernel(**inputs):
    inp = {k: np.asarray(v) for k, v in inputs.items()}
    lat, _ = run_model(inp, 8192, 4)
    return lat


def ffn_moe(self, li, modLC, rows):
    em = self.em
    rin = self.xin("moer%d" % li, [128, 64])
    with ExitStack() as st:
        wr = em.sb(st, "mwr", [128, 8, 8], BF16)
        em.dma("pool", wr[:].rearrange("p c e -> p (c e)"), rin.ap(), writes=[wr])
        h2 = em.sb(st, "mh2", [128, 8, 512], BF16)
        wg = [em.sb(st, "mwg%d" % q, [128, 8, 256], BF16) for q in range(2)]
        wu = [em.sb(st, "mwu%d" % q, [128, 8, 256], BF16) for q in range(2)]
        wd = [em.sb(st, "mwd%d" % q, [128, 28, 512], BF16) for q in range(2)]
        act = em.sb(st, "mact", [128, 28, 512], BF16)
        acc = em.sb(st, "macc", [128, 4, D], F32)
        sg = [em.sb(st, "msg%d" % q, [128, 512], F32) for q in range(2)]
        lg = em.sb(st, "mlg", [128, 8], F32)
        eq = em.sb(st, "meq", [128, 8], F32)
        l2 = em.sb(st, "ml2", [128, 8], F32)
        ex = em.sb(st, "mex", [128, 8], F32)
        m1 = em.sb(st, "mm1", [128, 4], F32)
        lg4 = [em.sb(st, "mlg4%d" % q, [128, 8], F32) for q in range(4)]
        eq4 = [em.sb(st, "meq4%d" % q, [128, 8], F32) for q in range(4)]
        l24 = [em.sb(st, "ml24%d" % q, [128, 8], F32) for q in range(4)]
        ex4 = [em.sb(st, "mex4%d" % q, [128, 8], F32) for q in range(4)]
        m14 = [em.sb(st, "mm14%d" % q, [128, 4], F32) for q in range(4)]
        gate = em.sb(st, "mgate", [128, 4, 8], F32)
        la = em.sb(st, "mlat", [128, D], F32)
        ou = em.sb(st, "mout", [128, D], F32)
        tmp = em.sb(st, "mtmp", [128, D], F32)
        st6 = em.sb(st, "mst6", [128, 2, 6], F32)
        mv = em.sb(st, "mmv", [128, 2], F32)
        rs = em.sb(st, "mrs", [128, 1], F32)
        nf = 0
        for gi, (t0, ng, isctx) in enumerate(self.groups):
            if isctx and getattr(self, "skip_ctx_ffn", False):
                continue
            N = ng * 128
            em.dma("sp", h2[:], self.H2T.ap()[gi].rearrange("p (c n) -> p c n", c=8), writes=[h2])
            TI = range(ng)
            pr_ = [self.pn() for ti in TI]
            for ti in TI:
                for kc in range(8):
                    self.mm(pr_[ti], pr_[ti][:, 0:8], h2[:, kc, ti * 128:(ti + 1) * 128], wr[:, kc, :], kc == 0, kc == 7, [h2, wr])
            stages = [
                lambda e, ti: e.tensor_copy(out=lg4[ti][:], in_=pr_[ti][:, 0:8]),
                lambda e, ti: e.tensor_reduce(out=m14[ti][:, 0:1], in_=lg4[ti][:], axis=AX.X, op=ALU.max),
                lambda e, ti: e.tensor_scalar(out=eq4[ti][:], in0=lg4[ti][:], scalar1=m14[ti][:, 0:1], scalar2=None, op0=ALU.is_equal),
                lambda e, ti: e.scalar_tensor_tensor(out=l24[ti][:], in0=eq4[ti][:], scalar=-1e30, in1=lg4[ti][:], op0=ALU.mult, op1=ALU.add),
                lambda e, ti: e.tensor_reduce(out=m14[ti][:, 1:2], in_=l24[ti][:], axis=AX.X, op=ALU.max),
                lambda e, ti: e.tensor_scalar(out=eq4[ti][:], in0=lg4[ti][:], scalar1=m14[ti][:, 1:2], scalar2=None, op0=ALU.is_ge),
                lambda e, ti: e.tensor_scalar(out=m14[ti][:, 2:3], in0=m14[ti][:, 0:1], scalar1=-1.0, scalar2=None, op0=ALU.mult),
                ("act", lambda e, ti: e.activation(out=ex4[ti][:], in_=lg4[ti][:], func=AF.Exp, bias=m14[ti][:, 2:3], scale=1.0)),
                lambda e, ti: e.tensor_tensor(out=ex4[ti][:], in0=ex4[ti][:], in1=eq4[ti][:], op=ALU.mult),
                lambda e, ti: e.tensor_reduce(out=m14[ti][:, 3:4], in_=ex4[ti][:], axis=AX.X, op=ALU.add),
                lambda e, ti: e.reciprocal(out=m14[ti][:, 3:4], in_=m14[ti][:, 3:4]),
                lambda e, ti: e.tensor_scalar(out=gate[:, ti, :], in0=ex4[ti][:], scalar1=m14[ti][:, 3:4], scalar2=None, op0=ALU.mult),
            ]
            for sidx, stg in enumerate(stages):
                en, fn = stg if isinstance(stg, tuple) else ("dve", stg)
                for ti in TI:
                    allb = [lg4[ti], m14[ti], eq4[ti], l24[ti], ex4[ti]]
                    wr_ = allb + ([gate] if sidx == len(stages) - 1 else [])
                    em.op(en, lambda e, fn=fn, ti=ti: fn(e, ti), reads=[pr_[ti]] + allb, writes=wr_)
            for e_ in range(8):
                for half in range(2):
                    self.load_w(wd[half], "moedn%d_%d" % (li, e_), 0, 28, half * 512, 512)
                for s in range(14):
                    g_, u_ = wg[s % 2], wu[s % 2]
                    self.load_w(g_, "moegu%d_%d" % (li, e_), 0, 8, s * 256, 256)
                    self.load_w(u_, "moegu%d_%d" % (li, e_), 0, 8, 3584 + s * 256, 256)
                    for fc in range(2):
                        f = s * 2 + fc
                        pg, pu = self.pn(), self.pn()
                        for kc in range(8):
                            self.mm(pg, pg[:, :N], g_[:, kc, fc * 128:(fc + 1) * 128], h2[:, kc, :N], kc == 0, kc == 7, [g_, h2])
                        for kc in range(8):
                            self.mm(pu, pu[:, :N], u_[:, kc, fc * 128:(fc + 1) * 128], h2[:, kc, :N], kc == 0, kc == 7, [u_, h2])
                        s_ = sg[nf % 2]
                        nf += 1
                        em.op("act", lambda e, pg=pg, s_=s_: e.activation(out=s_[:, :N], in_=pg[:, :N], func=AF.Silu), reads=[pg], writes=[s_])
                        em.op("dve", lambda e, pu=pu, s_=s_, f=f: e.tensor_tensor(out=act[:, f, :N], in0=s_[:, :N], in1=pu[:, :N], op=ALU.mult),
                              reads=[pu, s_], writes=[act])
                for ti in range(ng):
                    for half in range(2):
                        ps = self.pn()
                        for f in range(28):
                            self.mm(ps, ps[:, :], act[:, f, ti * 128:(ti + 1) * 128], wd[half][:, f, :], f == 0, f == 27, [act, wd[half]])
                        asl = acc[:, ti, half * 512:(half + 1) * 512]
                        if e_ == 0:
                            em.op("dve", lambda e, ps=ps, asl=asl, ti=ti, e_=e_: e.tensor_scalar(
                                out=asl, in0=ps[:, :], scalar1=gate[:, ti, e_:e_ + 1], scalar2=None, op0=ALU.mult),
                                reads=[ps, gate], writes=[acc])
                        else:
                            em.op("dve", lambda e, ps=ps, asl=asl, ti=ti, e_=e_: e.scalar_tensor_tensor(
                                out=asl, in0=ps[:, :], scalar=gate[:, ti, e_:e_ + 1], in1=asl, op0=ALU.mult, op1=ALU.add),
                                reads=[ps, gate, acc], writes=[acc])
            sfx = "C" if isctx else "L"
            for ti in range(ng):
                t = t0 + ti
                em.dma("sp", la[:], self.LAT2[0].ap()[t * 128:(t + 1) * 128, :], writes=[la])
                srcs = [(acc, acc[:, ti, 0:512]), (acc, acc[:, ti, 512:1024])]
                self.tail(srcs, la, rows["GA2" + sfx], rows["LNG1"], rows["LNB1"], tmp, ou, st6, mv, rs)
                self.ffn_store(t, ou)
        em.barrier()
        em.release([wr, h2, act, acc, lg, eq, l2, ex, m1, gate, la, ou, tmp, st6, mv, rs] + wg + wu + wd + sg + lg4 + eq4 + l24 + ex4 + m14)


MK.ffn_moe = ffn_moe


def dn_alloc(self):
    NT, T = self.NT, self.T
    self.dPRE = self.dr("dPRE", [32, 128, T], F32)
    self.dQT = [self.dr("dQT%d" % z, [8, 128, T], BF16) for z in range(2)]
    self.dKT = [self.dr("dKT%d" % z, [8, 128, T], BF16) for z in range(2)]
    self.dKM = [self.dr("dKM%d" % z, [NT, 128, 1024], BF16) for z in range(2)]
    self.dV = [self.dr("dV%d" % z, [NT, 128, 2048], BF16) for z in range(2)]
    self.dGB = [self.dr("dGB%d" % z, [NT, 128, 48], F32) for z in range(2)]
    self.dn_in = dict(
        ab=[self.xin("dnab%d" % z, [128, 8 * 32]) for z in range(2)],
        alog=[self.xin("dnalog%d" % z, [1, 16]) for z in range(2)],
        dtb=[self.xin("dndtb%d" % z, [1, 16]) for z in range(2)],
        conv=[self.xin("dnconv%d" % z, [128, 32 * 5]) for z in range(2)],
        ng=self.xin("dnng", [1, 128]))


def dn_phase_a1(self, modLC, z):
    em = self.em
    LAT = self.LAT2[z]
    with ExitStack() as st:
        wb = [em.sb(st, "dw%d" % q, [128, 8, 512], BF16) for q in range(2)]
        hT = [em.sb(st, "dhT%d" % q, [128, 8, 512], BF16) for q in range(2)]
        lat = [em.sb(st, "dlat%d" % q, [128, D], F32) for q in range(2)]
        stg = [em.sb(st, "dstg%d" % q, [128, 512], F32) for q in range(3)]
        gg = [em.sb(st, "dgg%d" % q, [128, 2048], BF16) for q in range(4)]
        wab = em.sb(st, "dwab", [128, 8, 32], BF16)
        em.dma("pool", wab[:].rearrange("p c n -> p (c n)"), self.dn_in["ab"][z].ap(), writes=[wab])
        nega = em.sb(st, "dnega", [128, 16], F32)
        dtb = em.sb(st, "ddtb", [128, 16], F32)
        em.dma("sp", nega[:], self.dn_in["alog"][z].ap().partition_broadcast(128), writes=[nega])
        em.dma("sp", dtb[:], self.dn_in["dtb"][z].ap().partition_broadcast(128), writes=[dtb])
        em.op("act", lambda e: e.activation(out=nega[:], in_=nega[:], func=AF.Exp), reads=[nega], writes=[nega])
        em.op("dve", lambda e: e.tensor_scalar(out=nega[:], in0=nega[:], scalar1=-1.0, scalar2=None, op0=ALU.mult), reads=[nega], writes=[nega])
        gb = [em.sb(st, "dgb%d" % q, [128, 48], F32) for q in range(2)]
        tt = [em.sb(st, "dtt%d" % q, [128, 32], F32) for q in range(2)]
        kk = 0
        sk = 0
        for gi, (t0, ng, isctx) in enumerate(self.groups):
            N = ng * 128
            h = hT[gi % 2]
            for ti in range(ng):
                la = lat[kk % 2]
                kk += 1
                em.dma("sp", la[:], LAT.ap()[(t0 + ti) * 128:(t0 + ti + 1) * 128, :], writes=[la])
                self.hT_tile(la, h, ti * 128, modLC, 0, isctx)
            for nb in range(8):
                w = wb[nb % 2]
                self.load_w(w, "dnin", 0, 8, nb * 512, 512)
                for c4 in range(4):
                    c = nb * 4 + c4
                    ps = self.pn()
                    for kc in range(8):
                        self.mm(ps, ps[:, :N], w[:, kc, c4 * 128:(c4 + 1) * 128], h[:, kc, :N], kc == 0, kc == 7, [w, h])
                    s_ = stg[sk % 3]
                    sk += 1
                    if c % 2:
                        em.op("act", lambda e, ps=ps, s_=s_: e.activation(out=s_[:, :N], in_=ps[:, :N], func=AF.Copy), reads=[ps], writes=[s_])
                    else:
                        em.op("dve", lambda e, ps=ps, s_=s_: e.tensor_copy(out=s_[:, :N], in_=ps[:, :N]), reads=[ps], writes=[s_])
                    em.dma("pool", self.dPRE.ap()[c][:, t0 * 128:t0 * 128 + N], s_[:, :N], reads=[s_])
            if z == 0:
                for nb in range(8, 12):
                    w = wb[nb % 2]
                    self.load_w(w, "dnin", 0, 8, nb * 512, 512)
                    for ti in range(ng):
                        ps = self.pn()
                        for kc in range(8):
                            self.mm(ps, ps[:, :], h[:, kc, ti * 128:(ti + 1) * 128], w[:, kc, :], kc == 0, kc == 7, [h, w])
                        dest = gg[ti]
                        c0 = (nb - 8) * 512
                        em.op("act", lambda e, ps=ps, dest=dest, c0=c0: e.activation(out=dest[:, c0:c0 + 512], in_=ps[:, :], func=AF.Copy),
                              reads=[ps], writes=[dest])
                for ti in range(ng):
                    em.dma("pool", self.rG.ap()[t0 + ti], gg[ti][:], reads=[gg[ti]])
            for ti in range(ng):
                ps = self.pn()
                for kc in range(8):
                    self.mm(ps, ps[:, 0:32], h[:, kc, ti * 128:(ti + 1) * 128], wab[:, kc, :], kc == 0, kc == 7, [h, wab])
                g_, t_ = gb[ti % 2], tt[ti % 2]
                em.op("dve", lambda e, ps=ps, t_=t_: e.tensor_tensor(out=t_[:, 0:16], in0=ps[:, 0:16], in1=dtb[:], op=ALU.add), reads=[ps, dtb], writes=[t_])
                em.op("dve", lambda e, ps=ps, t_=t_: e.tensor_scalar(out=t_[:, 16:32], in0=ps[:, 16:32], scalar1=-1.0, scalar2=None, op0=ALU.mult),
                      reads=[ps], writes=[t_])
                em.op("act", lambda e, t_=t_: e.activation(out=t_[:], in_=t_[:], func=AF.Exp), reads=[t_], writes=[t_])
                em.op("act", lambda e, t_=t_, g_=g_: e.activation(out=g_[:, 0:16], in_=t_[:, 0:16], func=AF.Ln, bias=1.0, scale=1.0), reads=[t_], writes=[g_])
                em.op("act", lambda e, t_=t_, g_=g_: e.activation(out=g_[:, 32:48], in_=t_[:, 16:32], func=AF.Ln, bias=1.0, scale=1.0), reads=[t_], writes=[g_])
                em.op("dve", lambda e, g_=g_: e.tensor_tensor(out=g_[:, 0:16], in0=g_[:, 0:16], in1=nega[:], op=ALU.mult), reads=[g_, nega], writes=[g_])
                em.op("dve", lambda e, g_=g_: e.tensor_scalar(out=g_[:, 32:48], in0=g_[:, 32:48], scalar1=-1.0, scalar2=None, op0=ALU.mult), reads=[g_], writes=[g_])
                em.op("dve", lambda e, t_=t_: e.tensor_scalar(out=t_[:, 16:32], in0=t_[:, 16:32], scalar1=1.0, scalar2=None, op0=ALU.add), reads=[t_], writes=[t_])
                em.op("dve", lambda e, t_=t_, g_=g_: e.reciprocal(out=g_[:, 16:32], in_=t_[:, 16:32]), reads=[t_], writes=[g_])
                em.dma("pool", self.dGB[z].ap()[t0 + ti], g_[:], reads=[g_])
        em.barrier()
        em.release(wb + hT + lat + stg + gg + [wab, nega, dtb] + gb + tt)


def dn_phase_a2(self, z):
    em = self.em
    T, NT = self.T, self.NT
    with ExitStack() as st:
        cw = em.sb(st, "cw", [128, 32, 5], F32)
        em.dma("sp", cw[:].rearrange("p c k -> p (c k)"), self.dn_in["conv"][z].ap(), writes=[cw])
        x = [em.sb(st, "cx%d" % q, [128, T], F32) for q in range(2)]
        acc = [em.sb(st, "cacc0", [128, T], F32)] * 2
        yb = em.sb(st, "cyb", [128, T], BF16)
        sq = em.sb(st, "csq", [128, 512], BF16)
        ri = em.sb(st, "cri", [128, 512], F32)
        tp = [em.sb(st, "ctp%d" % q, [128, 8, 128], BF16) for q in range(2)]
        segs = [(0, 256), (256, T)]
        tk = 0
        for c in range(32):
            x_, a_ = x[c % 2], acc[c % 2]
            eng = "dve"
            em.dma("sp", x_[:], self.dPRE.ap()[c], writes=[x_])
            em.op(eng, lambda e, x_=x_, a_=a_, c=c: e.tensor_scalar(out=a_[:], in0=x_[:], scalar1=cw[:, c, 2:3], scalar2=None, op0=ALU.mult),
                  reads=[x_, cw], writes=[a_])
            for k in (0, 1, 3, 4):
                s = k - 2
                for (a, b) in segs:
                    lo, hi = max(a, a - s), min(b, b - s)
                    em.op(eng, lambda e, x_=x_, a_=a_, c=c, k=k, lo=lo, hi=hi, s=s: e.scalar_tensor_tensor(
                        out=a_[:, lo:hi], in0=x_[:, lo + s:hi + s], scalar=cw[:, c, k:k + 1], in1=a_[:, lo:hi], op0=ALU.mult, op1=ALU.add),
                        reads=[x_, a_, cw], writes=[a_])
            em.op("act", lambda e, a_=a_: e.activation(out=a_[:], in_=a_[:], func=AF.Silu), reads=[a_], writes=[a_])
            if c < 16:
                for b0 in range(0, T, 512):
                    n = min(512, T - b0)
                    em.op("dve", lambda e, a_=a_, b0=b0, n=n: e.tensor_tensor(out=sq[:, :n], in0=a_[:, b0:b0 + n], in1=a_[:, b0:b0 + n], op=ALU.mult),
                          reads=[a_], writes=[sq])
                    ps = self.pn()
                    self.mm(ps, ps[:, :n], self.cb(C_ONES), sq[:, :n], True, True, [self.cmb, sq])
                    em.op("act", lambda e, ps=ps, n=n: e.activation(out=ri[:, :n], in_=ps[:, :n], func=AF.Sqrt, bias=1e-6, scale=1.0), reads=[ps], writes=[ri])
                    em.op("dve", lambda e, n=n: e.reciprocal(out=ri[:, :n], in_=ri[:, :n]), reads=[ri], writes=[ri])
                    sc = (128.0 ** -0.5) if c < 8 else 1.0
                    em.op("dve", lambda e, a_=a_, b0=b0, n=n, sc=sc: e.scalar_tensor_tensor(
                        out=yb[:, b0:b0 + n], in0=a_[:, b0:b0 + n], scalar=sc, in1=ri[:, :n], op0=ALU.mult, op1=ALU.mult),
                        reads=[a_, ri], writes=[yb])
                dst = (self.dQT if c < 8 else self.dKT)[z]
                em.dma("pool", dst.ap()[c % 8], yb[:], reads=[yb])
            else:
                em.op("dve", lambda e, a_=a_: e.tensor_copy(out=yb[:], in_=a_[:]), reads=[a_], writes=[yb])
            if c >= 8:
                for t8 in range(0, NT, 8):
                    nt = min(8, NT - t8)
                    ps = self.pn()
                    pb = ps[:].bitcast(BF16)
                    for q in range(nt):
                        em.op("pe", lambda e, pb=pb, q=q, t8=t8: e.transpose(out=pb[:, q * 128:(q + 1) * 128], in_=yb[:, (t8 + q) * 128:(t8 + q + 1) * 128],
                                                                           identity=self.cb(C_ID)), reads=[yb, self.cmb], writes=[ps])
                    t_ = tp[tk % 2]
                    tk += 1
                    em.op("act", lambda e, pb=pb, t_=t_, nt=nt: e.activation(out=t_[:, 0:nt, :].rearrange("p a b -> p (a b)"), in_=pb[:, 0:nt * 128], func=AF.Copy),
                          reads=[ps], writes=[t_])
                    if c < 16:
                        dstap = self.dKM[z].ap()[t8:t8 + nt, :, (c - 8) * 128:(c - 7) * 128].rearrange("t p n -> p t n")
                    else:
                        dstap = self.dV[z].ap()[t8:t8 + nt, :, (c - 16) * 128:(c - 15) * 128].rearrange("t p n -> p t n")
                    em.dma("pool", dstap, t_[:, 0:nt, :], reads=[t_])
        em.barrier()
        em.release([cw, yb, sq, ri] + x + acc[:1] + tp)


MK.dn_alloc = dn_alloc
MK.dn_phase_a1 = dn_phase_a1
MK.dn_phase_a2 = dn_phase_a2


def tri_inverse(self, LT, INV, INVT, Tsb, tmpx, sign, ltf=None):
    em = self.em
    for q, dst in ((0, INV), (1, INVT)):
        em.op("pool", lambda e, dst=dst: e.tensor_copy(out=dst[:], in_=self.cb(C_ID).unsqueeze(1).broadcast_to([128, 4, 128])),
              reads=[self.cmb], writes=[dst])
    for lv in range(6):
        pT = self.pn()
        for h4 in range(4):
            self.mm(pT, pT[:, h4 * 128:(h4 + 1) * 128], LT[:, h4, :] if ltf is None else ltf(h4), INV[:, h4, :], True, True, [LT, INV])
        em.op("act", lambda e, pT=pT: e.activation(out=Tsb[:].rearrange("p a b -> p (a b)"), in_=pT[:, :], func=AF.Copy), reads=[pT], writes=[Tsb])
        pX, pXT = self.pn(), self.pn()
        for h4 in range(4):
            self.mm(pX, pX[:, h4 * 128:(h4 + 1) * 128], INVT[:, h4, :], Tsb[:, h4, :], True, True, [INVT, Tsb])
        for h4 in range(4):
            self.mm(pXT, pXT[:, h4 * 128:(h4 + 1) * 128], Tsb[:, h4, :], INVT[:, h4, :], True, True, [INVT, Tsb])
        for (pp, msk, dst, q) in ((pX, C_LV + lv, INV, 0), (pXT, C_LVT + lv, INVT, 1)):
            tx = tmpx[q]
            em.op("dve", lambda e, pp=pp, msk=msk, tx=tx: e.scalar_tensor_tensor(
                out=tx[:], in0=pp[:, :].rearrange("p (a b) -> p a b", a=4), scalar=-float(sign),
                in1=self.cb(msk).unsqueeze(1).broadcast_to([128, 4, 128]), op0=ALU.mult, op1=ALU.mult),
                reads=[pp, self.cmb], writes=[tx])
            em.op("pool", lambda e, dst=dst, tx=tx: e.tensor_tensor(out=dst[:], in0=dst[:], in1=tx[:], op=ALU.add), reads=[dst, tx], writes=[dst])


MK.tri_inverse = tri_inverse


def tri_inverse16(self, ltf, ltbufs, INV, INVT, Tsb, tmpx, sign):
    em = self.em
    for g in range(4):
        for dst in (INV[g], INVT[g]):
            em.op("pool", lambda e, dst=dst: e.tensor_copy(out=dst[:], in_=self.cb(C_ID).unsqueeze(1).broadcast_to([128, 4, 128])),
                  reads=[self.cmb], writes=[dst])
    f2 = lambda b: b[:].rearrange("p a b -> p (a b)")
    for lv in range(6):
        pT = [self.pn() for _ in range(4)]
        for g in range(4):
            for h4 in range(4):
                self.mm(pT[g], pT[g][:, h4 * 128:(h4 + 1) * 128], ltf(g * 4 + h4), INV[g][:, h4, :], True, True, [ltbufs[g], INV[g]])
        for g in range(4):
            if g % 2:
                em.op("dve", lambda e, g=g: e.tensor_copy(out=f2(Tsb[g]), in_=pT[g][:, :]), reads=[pT[g]], writes=[Tsb[g]])
            else:
                em.op("act", lambda e, g=g: e.activation(out=f2(Tsb[g]), in_=pT[g][:, :], func=AF.Copy), reads=[pT[g]], writes=[Tsb[g]])
        pX = [self.pn() for _ in range(4)]
        for g in range(4):
            for h4 in range(4):
                self.mm(pX[g], pX[g][:, h4 * 128:(h4 + 1) * 128], INVT[g][:, h4, :], Tsb[g][:, h4, :], True, True, [INVT[g], Tsb[g]])
        pXT = [self.pn() for _ in range(4)]
        for g in range(4):
            for h4 in range(4):
                self.mm(pXT[g], pXT[g][:, h4 * 128:(h4 + 1) * 128], Tsb[g][:, h4, :], INVT[g][:, h4, :], True, True, [INVT[g], Tsb[g]])
        for (pp, msk, dstl, q) in ((pX, C_LV + lv, INV, 0), (pXT, C_LVT + lv, INVT, 1)):
            for g in range(4):
                tx = tmpx[q][g]
                em.op("dve", lambda e, pp=pp, msk=msk, tx=tx, g=g: e.scalar_tensor_tensor(
                    out=tx[:], in0=pp[g][:, :].rearrange("p (a b) -> p a b", a=4), scalar=-float(sign),
                    in1=self.cb(msk).unsqueeze(1).broadcast_to([128, 4, 128]), op0=ALU.mult, op1=ALU.mult),
                    reads=[pp[g], self.cmb], writes=[tx])
                em.op("pool", lambda e, dstl=dstl, tx=tx, g=g: e.tensor_tensor(out=dstl[g][:], in0=dstl[g][:], in1=tx[:], op=ALU.add),
                      reads=[dstl[g], tx], writes=[dstl[g]])


MK.tri_inverse16 = tri_inverse16


def dn_phase_s(self, z):
    em = self.em
    T, NT = self.T, self.NT
    O = self.O2[z]
    with ExitStack() as st:
        B = lambda n, sh, dt: em.sb(st, "n" + n, sh, dt)
        qt = B("qt", [128, 8, 128], BF16)
        kt_ = B("ktf", [128, 8, 128], BF16)
        km = B("km", [128, 8, 128], BF16)
        v = B("v", [128, 16, 128], BF16)
        gb = B("gb", [128, 48], F32)
        sm = B("sm", [128, 64], F32)
        vec = B("vec", [128, 6, 16], F32)
        mneg = B("mneg", [128, 2, 4, 128], BF16)
        for q, cidx in ((0, C_MI), (1, C_MS)):
            em.op("dve", lambda e, q=q, cidx=cidx: e.tensor_scalar(
                out=mneg[:, q, :, :], in0=self.cb(cidx).unsqueeze(1).broadcast_to([128, 4, 128]), scalar1=-1.0, scalar2=30000.0,
                op0=ALU.add, op1=ALU.mult), reads=[self.cmb], writes=[mneg])
        DgA = [[B("Dg%d_%d" % (q, r), [128, 4, 128], F32) for q in range(2)] for r in range(2)]
        DeA = [B("De%d" % r, [128, 4, 128], BF16) for r in range(2)]
        EA = [[B("E%d_%d" % (q, r), [128, 4, 128], F32) for q in range(2)] for r in range(2)]
        LTg = [B("LT%d" % g, [128, 4, 128], BF16) for g in range(4)]
        INVg = [B("INV%d" % g, [128, 4, 128], BF16) for g in range(4)]
        INVTg = [B("INVT%d" % g, [128, 4, 128], BF16) for g in range(4)]
        Tsbg = [B("Tsb%d" % g, [128, 4, 128], BF16) for g in range(4)]
        tmpxg = [[B("tmpx%d_%d" % (q, g), [128, 4, 128], BF16) for g in range(4)] for q in range(2)]
        QKd = B("QKd", [128, 16, 128], BF16)
        qgT = B("qgT", [128, 16, 128], BF16)
        WT = B("WT", [128, 16, 128], BF16)
        U = B("U", [128, 16, 128], F32)
        VN = [B("VN%d" % q, [128, 8, 128], BF16) for q in range(2)]
        stmp2 = [B("stmp2_%d" % q, [128, 8, 128], F32) for q in range(2)]
        ktk = B("ktk", [128, 16, 128], BF16)
        vb = B("vb", [128, 16, 128], BF16)
        kbg = B("kbg", [128, 16, 128], BF16)
        OT = [B("OT%d" % q, [128, 2048], F32) for q in range(2)]
        S = [B("S%d" % q, [128, 8, 128], F32) for q in range(2)]
        Sb = [B("Sb%d" % q, [128, 8, 128], BF16) for q in range(2)]
        stmp = B("stmp", [128, 8, 128], F32)
        for q in range(2):
            em.op("pool", lambda e, q=q: e.memset(S[q][:], 0.0), writes=[S[q]])
            em.op("pool", lambda e, q=q: e.memset(Sb[q][:], 0.0), writes=[Sb[q]])
        bc16 = lambda ap: ap.unsqueeze(2).broadcast_to([128, 16, 128])
        for t in range(NT):
            c0 = t * 128
            em.dma("sp", qt[:], self.dQT[z].ap()[:, :, c0:c0 + 128].rearrange("h p n -> p h n"), writes=[qt])
            em.dma("sp", kt_[:], self.dKT[z].ap()[:, :, c0:c0 + 128].rearrange("h p n -> p h n"), writes=[kt_])
            em.dma("sp", km[:].rearrange("p a b -> p (a b)"), self.dKM[z].ap()[t], writes=[km])
            em.dma("sp", v[:].rearrange("p a b -> p (a b)"), self.dV[z].ap()[t], writes=[v])
            em.dma("sp", gb[:], self.dGB[z].ap()[t], writes=[gb])
            ps = self.pn()
            for q, cidx in enumerate((C_TRI, C_BLK, C_SEL0, C_SEL1)):
                self.mm(ps, ps[:, q * 16:(q + 1) * 16], self.cf(cidx), gb[:, 0:16], True, True, [self.cmf, gb])
            em.op("dve", lambda e, ps=ps: e.tensor_copy(out=sm[:], in_=ps[:, 0:64]), reads=[ps], writes=[sm])
            gc, gl = sm[:, 0:16], sm[:, 16:32]
            em.op("dve", lambda e: e.tensor_tensor(out=vec[:, 0, :], in0=gc, in1=gb[:, 32:48], op=ALU.add), reads=[sm, gb], writes=[vec])
            em.op("act", lambda e: e.activation(out=vec[:, 1, :], in_=gc, func=AF.Exp), reads=[sm], writes=[vec])
            em.op("dve", lambda e: e.tensor_tensor(out=vec[:, 2, :], in0=gl, in1=gc, op=ALU.subtract), reads=[sm], writes=[vec])
            em.op("act", lambda e: e.activation(out=vec[:, 2, :], in_=vec[:, 2, :], func=AF.Exp), reads=[vec], writes=[vec])
            em.op("dve", lambda e: e.tensor_tensor(out=vec[:, 3, :], in0=vec[:, 1, :], in1=gb[:, 16:32], op=ALU.mult), reads=[vec, gb], writes=[vec])
            em.op("act", lambda e: e.activation(out=vec[:, 4:6, :].rearrange("p a b -> p (a b)"), in_=sm[:, 32:64], func=AF.Exp), reads=[sm], writes=[vec])
            em.op("dve", lambda e: e.tensor_tensor(out=vb[:], in0=v[:], in1=bc16(gb[:, 16:32]), op=ALU.mult), reads=[v, gb], writes=[vb])
            for r in range(2):
                kmr = km[:]
                em.op("pool", lambda e, r=r: e.tensor_tensor(
                    out=kbg[:].rearrange("p (a r) n -> p a r n", r=2)[:, :, r, :], in0=km[:],
                    in1=vec[:, 3, :].rearrange("p (a r) -> p a r", r=2)[:, :, r].unsqueeze(2).broadcast_to([128, 8, 128]), op=ALU.mult),
                    reads=[km, vec], writes=[kbg])
                em.op("pool", lambda e, r=r: e.tensor_tensor(
                    out=ktk[:].rearrange("p (a r) n -> p a r n", r=2)[:, :, r, :], in0=km[:],
                    in1=vec[:, 2, :].rearrange("p (a r) -> p a r", r=2)[:, :, r].unsqueeze(2).broadcast_to([128, 8, 128]), op=ALU.mult),
                    reads=[km, vec], writes=[ktk])
            for hg in range(4):
                hs = slice(hg * 4, hg * 4 + 4)
                Dg, De, E, LT = DgA[hg % 2], DeA[hg % 2], EA[hg % 2], LTg[hg]
                bc4 = lambda ap: ap.unsqueeze(2).broadcast_to([128, 4, 128])
                idf = self.cf(C_ID).unsqueeze(1).broadcast_to([128, 4, 128])
                em.op("dve", lambda e, hs=hs: e.tensor_tensor(out=Dg[0][:], in0=idf, in1=bc4(sm[:, hs]), op=ALU.mult), reads=[self.cmf, sm], writes=[Dg[0]])
                em.op("dve", lambda e, hs=hs: e.tensor_tensor(out=Dg[1][:], in0=idf, in1=bc4(vec[:, 0, hs]), op=ALU.mult), reads=[self.cmf, vec], writes=[Dg[1]])
                em.op("pool", lambda e, hs=hs: e.tensor_tensor(out=De[:], in0=self.cb(C_ID).unsqueeze(1).broadcast_to([128, 4, 128]), in1=bc4(vec[:, 1, hs]), op=ALU.mult),
                      reads=[self.cmb, vec], writes=[De])
                for q in range(2):
                    p_ = self.pn()
                    self.mm(p_, p_[:, :], self.cf(C_ONES), Dg[q][:].rearrange("p a b -> p (a b)"), True, False, [self.cmf, Dg[q]])
                    self.mm(p_, p_[:, :], self.cb(C_ID), mneg[:, q, :, :].rearrange("p a b -> p (a b)"), False, True, [self.cmb, mneg])
                    em.op("dve", lambda e, p_=p_, q=q, hs=hs: e.tensor_tensor(out=E[q][:], in0=p_[:, :].rearrange("p (a b) -> p a b", a=4),
                                                                           in1=bc4(sm[:, hs]), op=ALU.subtract), reads=[p_, sm], writes=[E[q]])
                    em.op("act", lambda e, q=q: e.activation(out=E[q][:], in_=E[q][:], func=AF.Exp), reads=[E[q]], writes=[E[q]])
                pe_ = self.pn()
                self.mm(pe_, pe_[:, :], self.cb(C_ONES), De[:].rearrange("p a b -> p (a b)"), True, True, [self.cmb, De])
                pk = self.pn()
                for qh in range(2):
                    hq = hg * 2 + qh
                    self.mm(pk, pk[:, qh * 128:(qh + 1) * 128], kt_[:, hq, :], kt_[:, hq, :], True, True, [kt_])
                    self.mm(pk, pk[:, 256 + qh * 128:256 + (qh + 1) * 128], kt_[:, hq, :], qt[:, hq, :], True, True, [kt_, qt])
                rep = lambda ap: ap.rearrange("p (a b) -> p a b", a=2).unsqueeze(2).broadcast_to([128, 2, 2, 128])
                v4 = lambda ap: ap.rearrange("p (a r) b -> p a r b", r=2)
                em.op("dve", lambda e, pk=pk: e.tensor_tensor(out=v4(LT[:]), in0=rep(pk[:, 0:256]), in1=v4(E[1][:]), op=ALU.mult), reads=[pk, E[1]], writes=[LT])
                em.op("dve", lambda e, pk=pk, hs=hs: e.tensor_tensor(out=v4(QKd[:, hs, :]), in0=rep(pk[:, 256:512]), in1=v4(E[0][:]), op=ALU.mult),
                      reads=[pk, E[0]], writes=[QKd])
                em.op("dve", lambda e, pe_=pe_, hs=hs, hg=hg: e.tensor_tensor(
                    out=v4(qgT[:, hs, :]), in0=qt[:, hg * 2:hg * 2 + 2, :].unsqueeze(2).broadcast_to([128, 2, 2, 128]),
                    in1=v4(pe_[:, :].rearrange("p (a b) -> p a b", a=4)), op=ALU.mult), reads=[pe_, qt], writes=[qgT])
            self.tri_inverse16(lambda h: LTg[h // 4][:, h % 4, :], LTg, INVg, INVTg, Tsbg, tmpxg, 1.0)
            for hg in range(4):
                hs = slice(hg * 4, hg * 4 + 4)
                INVT = INVTg[hg]
                pu, pw = self.pn(), self.pn()
                for h4 in range(4):
                    h = hg * 4 + h4
                    self.mm(pu, pu[:, h4 * 128:(h4 + 1) * 128], INVT[:, h4, :], vb[:, h, :], True, True, [INVT, vb])
                    self.mm(pw, pw[:, h4 * 128:(h4 + 1) * 128], kbg[:, h, :], INVT[:, h4, :], True, True, [INVT, kbg])
                em.op("act", lambda e, pu=pu, hs=hs: e.activation(out=U[:, hs, :].rearrange("p a b -> p (a b)"), in_=pu[:, :], func=AF.Copy), reads=[pu], writes=[U])
                em.op("dve", lambda e, pw=pw, hs=hs: e.tensor_copy(out=WT[:, hs, :].rearrange("p a b -> p (a b)"), in_=pw[:, :]), reads=[pw], writes=[WT])
            o_ = OT[t % 2]
            for c in range(2):
                r0 = c * 64
                rs_ = slice(r0, r0 + 64)
                pws = [[self.pn(), self.pn()] for hh in range(2)]
                for hh in range(2):
                    for h8 in range(8):
                        h = hh * 8 + h8
                        pb_ = pws[hh][h8 // 4]
                        self.mm(pb_, pb_[rs_, (h8 % 4) * 128:(h8 % 4 + 1) * 128], WT[:, h, rs_], Sb[hh][:, h8, :], True, True, [WT, Sb[hh]])
                for hh in range(2):
                    for b2 in range(2):
                        hsl = slice(hh * 8 + b2 * 4, hh * 8 + b2 * 4 + 4)
                        em.op("dve", lambda e, b2=b2, hsl=hsl, hh=hh: e.tensor_tensor(
                            out=VN[hh][rs_, b2 * 4:b2 * 4 + 4, :].rearrange("p a b -> p (a b)"), in0=U[rs_, hsl, :].rearrange("p a b -> p (a b)"),
                            in1=pws[hh][b2][rs_, :], op=ALU.subtract), reads=[U, pws[hh][b2]], writes=[VN[hh]])
                for hh in range(2):
                    S_, Sb_ = S[hh], Sb[hh]
                    pos = [self.pn(), self.pn()]
                    pss = [self.pn(), self.pn()]
                    for h8 in range(8):
                        h = hh * 8 + h8
                        po_, ps_ = pos[h8 // 4], pss[h8 // 4]
                        cs_ = slice((h8 % 4) * 128, (h8 % 4 + 1) * 128)
                        self.mm(po_, po_[rs_, cs_], QKd[rs_, h, rs_], VN[hh][rs_, h8, :], True, False, [QKd, VN[hh]])
                        self.mm(po_, po_[rs_, cs_], qgT[:, h, rs_], Sb_[:, h8, :], False, True, [qgT, Sb_])
                        self.mm(ps_, ps_[:, cs_], ktk[rs_, h, :], VN[hh][rs_, h8, :], True, True, [ktk, VN[hh]])
                    for b2 in range(2):
                        oc0 = (hh * 8 + b2 * 4) * 128
                        em.op("act", lambda e, b2=b2, oc0=oc0, pos=pos: e.activation(out=o_[rs_, oc0:oc0 + 512], in_=pos[b2][rs_, :], func=AF.Copy),
                              reads=[pos[b2]], writes=[o_])
                    st_ = stmp2[hh]
                    em.op("dve", lambda e, hh=hh, c=c, st_=st_, S_=S_: e.tensor_tensor(
                        out=st_[:], in0=S_[:], in1=vec[:, 4 + c, hh * 8:hh * 8 + 8].unsqueeze(2).broadcast_to([128, 8, 128]), op=ALU.mult),
                        reads=[S_, vec], writes=[st_])
                    for b2 in range(2):
                        em.op("dve", lambda e, b2=b2, pss=pss, st_=st_, S_=S_: e.tensor_tensor(
                            out=S_[:, b2 * 4:b2 * 4 + 4, :].rearrange("p a b -> p (a b)"), in0=st_[:, b2 * 4:b2 * 4 + 4, :].rearrange("p a b -> p (a b)"),
                            in1=pss[b2][:, :], op=ALU.add), reads=[st_, pss[b2]], writes=[S_])
                    em.op("act", lambda e, S_=S_, Sb_=Sb_: e.activation(out=Sb_[:].rearrange("p a b -> p (a b)"), in_=S_[:].rearrange("p a b -> p (a b)"), func=AF.Copy),
                          reads=[S_], writes=[Sb_])
            em.dma("pool", O.ap()[t * 128:(t + 1) * 128, :], o_[:], reads=[o_])
        em.barrier()
        em.release([qt, kt_, km, v, gb, sm, vec, mneg, QKd, qgT, WT, U, ktk, vb, kbg, stmp] + VN + stmp2 + DgA[0] + DgA[1] + DeA + EA[0] + EA[1] + LTg + INVg + INVTg + Tsbg + tmpxg[0] + tmpxg[1] + OT + S + Sb)


MK.dn_phase_s = dn_phase_s


def dn_factory(self):
    em = self.em

    def factory(st):
        ngr = em.sb(st, "zng", [128, 128], F32)
        em.dma("sp", ngr[:], self.dn_in["ng"].ap().partition_broadcast(128), writes=[ngr])
        oo = [em.sb(st, "zoo%d" % q, [128, 4, 128], F32) for q in range(2)]
        p1 = [em.sb(st, "zp1%d" % q, [128, 512], F32) for q in range(2)]
        gt = [em.sb(st, "zgt%d" % q, [128, 512], BF16) for q in range(2)]
        osum = em.sb(st, "zosum", [128, 4, 128], F32)
        sq = em.sb(st, "zsq", [128, 4, 128], F32)
        sg = em.sb(st, "zsg", [128, 4, 128], F32)
        ss = em.sb(st, "zss", [128, 4], F32)
        y = em.sb(st, "zy", [128, 2048], BF16)
        cnt = [0]
        f2 = lambda b: b[:].rearrange("p a b -> p (a b)")

        def make_yT(t, isctx, yT):
            pt = self.ptile(t)
            for blk in range(4):
                k = cnt[0]
                cnt[0] += 1
                o_, a1, g_ = oo[k % 2], p1[k % 2], gt[k % 2]
                cs = slice(blk * 512, (blk + 1) * 512)
                em.dma("sp", f2(o_), self.O2[0].ap()[t * 128:(t + 1) * 128, cs], writes=[o_])
                em.dma("sp", a1[:], self.O2[1].ap()[pt * 128:(pt + 1) * 128, cs], writes=[a1])
                em.dma("sp", g_[:], self.rG.ap()[t][:, cs], writes=[g_])
                ps = self.pn()
                self.mm(ps, ps[:, :], self.cf(C_J0), a1[:], True, True, [self.cmf, a1])
                em.op("dve", lambda e: e.tensor_tensor(out=f2(osum), in0=ps[:, :], in1=f2(o_), op=ALU.add), reads=[ps, o_], writes=[osum])
                em.op("pool", lambda e: e.tensor_tensor(out=sq[:], in0=osum[:], in1=osum[:], op=ALU.mult), reads=[osum], writes=[sq])
                em.op("dve", lambda e: e.tensor_reduce(out=ss[:], in_=sq[:], axis=AX.X, op=ALU.add), reads=[sq], writes=[ss])
                em.op("act", lambda e: e.activation(out=ss[:], in_=ss[:], func=AF.Sqrt, bias=1e-6, scale=1.0 / 128.0), reads=[ss], writes=[ss])
                em.op("dve", lambda e: e.reciprocal(out=ss[:], in_=ss[:]), reads=[ss], writes=[ss])
                em.op("act", lambda e: e.activation(out=f2(sg), in_=g_[:], func=AF.Silu), reads=[g_], writes=[sg])
                em.op("dve", lambda e: e.tensor_tensor(out=osum[:], in0=osum[:], in1=ss[:].unsqueeze(2).broadcast_to([128, 4, 128]), op=ALU.mult),
                      reads=[osum, ss], writes=[osum])
                em.op("pool", lambda e: e.tensor_tensor(out=osum[:], in0=osum[:], in1=ngr[:].unsqueeze(1).broadcast_to([128, 4, 128]), op=ALU.mult),
                      reads=[osum, ngr], writes=[osum])
                em.op("pool", lambda e: e.tensor_tensor(out=y[:, cs], in0=f2(osum), in1=f2(sg), op=ALU.mult), reads=[osum, sg], writes=[y])
            for half in range(2):
                ps = self.pn()
                pb = ps[:].bitcast(BF16)
                for c in range(8):
                    cc = half * 8 + c
                    em.op("pe", lambda e: e.transpose(out=pb[:, c * 128:(c + 1) * 128], in_=y[:, cc * 128:(cc + 1) * 128], identity=self.cb(C_ID)),
                          reads=[y, self.cmb], writes=[ps])
                em.op("act", lambda e: e.activation(out=yT[:, half * 8:(half + 1) * 8, :].rearrange("p c n -> p (c n)"), in_=pb[:, 0:1024], func=AF.Copy),
                      reads=[ps], writes=[yT])
        return make_yT, [ngr, osum, sq, sg, ss, y] + oo + p1 + gt
    return factory


def dn_layer(self, i, modLC, rows):
    if not hasattr(self, "dPRE"):
        self.dn_alloc()
    import os
    dbg = int(os.environ.get("DNDBG", "99"))
    for z in range(2):
        self.dn_phase_a1(modLC, z)
        if dbg >= 2:
            self.dn_phase_a2(z)
        if dbg >= 3:
            self.dn_phase_s(z)
    if dbg >= 4:
        self.phase_f(i, modLC, rows, self.dn_factory(), "dnout", 16)


MK.dn_factory = dn_factory
MK.dn_layer = dn_layer


def rk_alloc(self):
    NT, T = self.NT, self.T
    self.kHT = self.dr("kHT", [8, 128, T], F32)
    fm = lambda n: [self.dr("k%s%d" % (n, z), [8, 128, T], F32) for z in range(2)]
    self.kR, self.kK, self.kA, self.kB = fm("R"), fm("K"), fm("A"), fm("B")
    self.kV = [self.dr("kV%d" % z, [NT, 128, 1024], BF16) for z in range(2)]
    self.kLW = [self.dr("kLW%d" % z, [NT, 128, 1024], F32) for z in range(2)]
    self.kBT = self.dr("kBT", [8, 128, T], BF16)
    self.kGT = self.dr("kGT", [8, 128, T], BF16)
    self.rk_in = dict(
        mix=self.xin("rkmix", [128, 48]), vec=self.xin("rkvec", [128, 32]),
        w0=[self.xin("rkw0_%d" % z, [1, 1024]) for z in range(2)],
        a0=[self.xin("rka0_%d" % z, [128, 8]) for z in range(2)],
        w1=[self.xin("rkw1_%d" % z, [128, 512]) for z in range(2)],
        a1=[self.xin("rka1_%d" % z, [128, 512]) for z in range(2)],
        w2=[self.xin("rkw2_%d" % z, [64, 1024]) for z in range(2)],
        a2=[self.xin("rka2_%d" % z, [64, 1024]) for z in range(2)])


def rk_phase_a1(self, modLC, z):
    em = self.em
    LAT = self.LAT2[z]
    with ExitStack() as st:
        hT = [em.sb(st, "khT%d" % q, [128, 8, 512], F32) for q in range(2)]
        lat = [em.sb(st, "klat%d" % q, [128, D], F32) for q in range(2)]
        kk = 0
        for gi, (t0, ng, isctx) in enumerate(self.groups):
            h = hT[gi % 2]
            for ti in range(ng):
                la = lat[kk % 2]
                kk += 1
                em.dma("sp", la[:], LAT.ap()[(t0 + ti) * 128:(t0 + ti + 1) * 128, :], writes=[la])
                self.hT_tile(la, h, ti * 128, modLC, 0, isctx)
            N = ng * 128
            em.dma("pool", self.kHT.ap()[:, :, t0 * 128:t0 * 128 + N].rearrange("c p n -> p c n"), h[:, :, 0:N], reads=[h])
        em.barrier()
        em.release(hT + lat)


def rk_phase_a2(self, z):
    em = self.em
    T, NT = self.T, self.NT
    I = self.rk_in
    N = 256
    with ExitStack() as st:
        B = lambda n, sh, dt: em.sb(st, "a" + n, sh, dt)
        mixv = B("mixv", [128, 48], F32)
        vecs = B("vecs", [128, 40], F32)
        em.dma("sp", mixv[:], I["mix"].ap(), writes=[mixv])
        em.dma("sp", vecs[:, 0:32], I["vec"].ap(), writes=[vecs])
        em.op("dve", lambda e: e.tensor_scalar(out=vecs[:, 32:40], in0=vecs[:, 8:16], scalar1=-1.0, scalar2=1.0, op0=ALU.mult, op1=ALU.add),
              reads=[vecs], writes=[vecs])
        zo = [z, 1 - z]
        a0 = [B("a0_%d" % q, [128, 8], F32) for q in range(2)]
        w1 = B("w1", [128, 8, 64], BF16)
        a1 = [B("a1_%d" % q, [128, 8, 64], BF16) for q in range(2)]
        w2 = B("w2", [64, 1024], BF16)
        a2 = [B("a2_%d" % q, [64, 1024], BF16) for q in range(2)]
        w0r = B("w0r", [128, 1024], F32)
        em.dma("sp", w0r[:], I["w0"][z].ap().partition_broadcast(128), writes=[w0r])
        em.dma("pool", w1[:].rearrange("p c n -> p (c n)"), I["w1"][z].ap(), writes=[w1])
        em.dma("pool", w2[:], I["w2"][z].ap(), writes=[w2])
        for q in range(2):
            em.dma("sp", a0[q][:], I["a0"][zo[q]].ap(), writes=[a0[q]])
            em.dma("pool", a1[q][:].rearrange("p c n -> p (c n)"), I["a1"][zo[q]].ap(), writes=[a1[q]])
            em.dma("pool", a2[q][:], I["a2"][zo[q]].ap(), writes=[a2[q]])
        Wr, Wk, Wv = [B("W%d" % q, [128, 8, 1024], BF16) for q in range(3)]
        for wb_, nm in ((Wr, "rkr"), (Wk, "rkk"), (Wv, "rkv")):
            self.load_w(wb_, nm, 0, 8, 0, 1024)
        g1 = B("g1", [128, 8, 128], BF16)
        g2 = B("g2", [128, 1, 1024], BF16)
        self.load_w(g1, "rkg1", 0, 8, 0, 128)
        self.load_w(g2, "rkg2", 0, 1, 0, 1024)
        hW = B("hW", [128, 8, N + 2], F32)
        xx = B("xx", [128, 8, N], F32)
        xm = [B("xm%d" % m, [128, 8, N], BF16) for m in range(6)]
        t1T = B("t1T", [64, N], BF16)
        u1T = [B("u1T%d" % q, [64, N], BF16) for q in range(2)]
        gsT = B("gsT", [128, N], BF16)
        chs = [{n: B("c%d%s" % (q, n), [128, N], F32) for n in ("r", "k", "v", "a", "ao", "kk", "t", "t2", "kd")} for q in range(2)]
        sqb = B("sqb", [128, N], BF16)
        obf = [B("obf%d" % q, [128, N], BF16) for q in range(2)]
        vtm = [B("vtm%d" % q, [128, 1024], BF16) for q in range(2)]
        lwt = [B("lwt%d" % q, [128, 1024], F32) for q in range(2)]
        groups = [(0, 256, 0, 256)] + [(256 + g * N, N, 256, T) for g in range((T - 256) // N)]
        nob = 0
        for (s0, n_, lo, hi) in groups:
            a_, b_ = max(lo, s0 - 1), min(hi, s0 + n_ + 1)
            if a_ > s0 - 1:
                em.op("pool", lambda e: e.memset(hW[:, :, 0:1], 0.0), writes=[hW])
            if b_ < s0 + n_ + 1:
                em.op("pool", lambda e: e.memset(hW[:, :, N + 1:N + 2], 0.0), writes=[hW])
            em.dma("sp", hW[:, :, a_ - (s0 - 1):b_ - (s0 - 1)], self.kHT.ap()[:, :, a_:b_].rearrange("c p n -> p c n"), writes=[hW])
            em.op("dve", lambda e: e.tensor_tensor(out=xx[:], in0=hW[:, :, 0:N], in1=hW[:, :, 2:N + 2], op=ALU.add), reads=[hW], writes=[xx])
            em.op("dve", lambda e: e.scalar_tensor_tensor(out=xx[:], in0=xx[:], scalar=0.5, in1=hW[:, :, 1:N + 1], op0=ALU.mult, op1=ALU.subtract),
                  reads=[xx, hW], writes=[xx])
            for m in range(6):
                if z == 1 and m == 5:
                    continue
                for c in range(8):
                    em.op("dve", lambda e, m=m, c=c: e.scalar_tensor_tensor(
                        out=xm[m][:, c, :], in0=xx[:, c, :], scalar=mixv[:, m * 8 + c:m * 8 + c + 1], in1=hW[:, c, 1:N + 1],
                        op0=ALU.mult, op1=ALU.add), reads=[xx, hW, mixv], writes=[xm[m]])
            ps = self.pn()
            for kc in range(8):
                self.mm(ps, ps[0:64, 0:N], w1[:, kc, :], xm[3][:, kc, :], kc == 0, kc == 7, [w1, xm[3]])
            em.op("act", lambda e, ps=ps: e.activation(out=t1T[:], in_=ps[0:64, 0:N], func=AF.Tanh), reads=[ps], writes=[t1T])
            for q in range(2 if z == 0 else 1):
                ps = self.pn()
                for kc in range(8):
                    self.mm(ps, ps[0:64, 0:N], a1[q][:, kc, :], xm[4][:, kc, :], kc == 0, kc == 7, [a1[q], xm[4]])
                em.op("act", lambda e, ps=ps, q=q: e.activation(out=u1T[q][:], in_=ps[0:64, 0:N], func=AF.Copy), reads=[ps], writes=[u1T[q]])
            if z == 0:
                ps = self.pn()
                for kc in range(8):
                    self.mm(ps, ps[:, 0:N], g1[:, kc, :], xm[5][:, kc, :], kc == 0, kc == 7, [g1, xm[5]])
                em.op("act", lambda e, ps=ps: e.activation(out=gsT[:], in_=ps[:, 0:N], func=AF.Sigmoid), reads=[ps], writes=[gsT])
            for ti in range(2):
                t = s0 // 128 + ti
                v_, l_ = vtm[ti], lwt[ti]
                for half in range(2):
                    ps = self.pn()
                    for kc in range(8):
                        self.mm(ps, ps[:, :], xm[2][:, kc, ti * 128:(ti + 1) * 128], Wv[:, kc, half * 512:(half + 1) * 512], kc == 0, kc == 7, [xm[2], Wv])
                    em.op("act", lambda e, ps=ps, half=half: e.activation(out=v_[:, half * 512:(half + 1) * 512], in_=ps[:, :], func=AF.Copy), reads=[ps], writes=[v_])
                    ps = self.pn()
                    self.mm(ps, ps[:, :], t1T[:, ti * 128:(ti + 1) * 128], w2[:, half * 512:(half + 1) * 512], True, True, [t1T, w2])
                    em.op("dve", lambda e, ps=ps, half=half: e.tensor_tensor(out=l_[:, half * 512:(half + 1) * 512], in0=ps[:, :], in1=w0r[:, half * 512:(half + 1) * 512], op=ALU.add),
                          reads=[ps, w0r], writes=[l_])
                em.op("act", lambda e: e.activation(out=l_[:], in_=l_[:], func=AF.Sigmoid), reads=[l_], writes=[l_])
                em.op("dve", lambda e: e.tensor_scalar(out=l_[:], in0=l_[:], scalar1=-float(np.exp(-0.5)), scalar2=None, op0=ALU.mult), reads=[l_], writes=[l_])
                em.dma("pool", self.kV[z].ap()[t], v_[:], reads=[v_])
                em.dma("pool", self.kLW[z].ap()[t], l_[:], reads=[l_])
            for c in range(8):
                ch = chs[c % 2]
                cs = slice(c * 128, (c + 1) * 128)
                for nm, m, W_ in (("r", 0, Wr), ("k", 1, Wk), ("v", 2, Wv)):
                    ps = self.pn()
                    for kc in range(8):
                        self.mm(ps, ps[:, 0:N], W_[:, kc, cs], xm[m][:, kc, :], kc == 0, kc == 7, [W_, xm[m]])
                    em.op("act", lambda e, ps=ps, nm=nm: e.activation(out=ch[nm][:], in_=ps[:, 0:N], func=AF.Copy), reads=[ps], writes=[ch[nm]])
                for q, nm in ((0, "a"), (1, "ao")):
                    if q == 1 and z == 1:
                        continue
                    ps = self.pn()
                    self.mm(ps, ps[:, 0:N], a2[q][:, cs], u1T[q][:], True, True, [a2[q], u1T[q]])
                    em.op("act", lambda e, ps=ps, nm=nm, q=q: e.activation(out=ch[nm][:], in_=ps[:, 0:N], func=AF.Sigmoid, bias=a0[q][:, c:c + 1], scale=1.0),
                          reads=[ps, a0[q]], writes=[ch[nm]])
                em.op("dve", lambda e: e.tensor_scalar(out=ch["kk"][:], in0=ch["k"][:], scalar1=vecs[:, c:c + 1], scalar2=None, op0=ALU.mult), reads=[ch["k"], vecs], writes=[ch["kk"]])
                em.op("dve", lambda e: e.tensor_tensor(out=sqb[:], in0=ch["kk"][:], in1=ch["kk"][:], op=ALU.mult), reads=[ch["kk"]], writes=[sqb])
                ps = self.pn()
                self.mm(ps, ps[:, 0:N], self.cb(C_B64), sqb[:], True, True, [self.cmb, sqb])
                em.op("act", lambda e, ps=ps: e.activation(out=ch["t"][:], in_=ps[:, 0:N], func=AF.Sqrt, bias=1e-6, scale=1.0), reads=[ps], writes=[ch["t"]])
                em.op("dve", lambda e: e.reciprocal(out=ch["t"][:], in_=ch["t"][:]), reads=[ch["t"]], writes=[ch["t"]])
                em.op("dve", lambda e: e.tensor_tensor(out=ch["kk"][:], in0=ch["kk"][:], in1=ch["t"][:], op=ALU.mult), reads=[ch["kk"], ch["t"]], writes=[ch["kk"]])
                em.op("dve", lambda e: e.tensor_scalar(out=ch["t"][:], in0=ch["a"][:], scalar1=vecs[:, 8 + c:9 + c], scalar2=vecs[:, 32 + c:33 + c], op0=ALU.mult, op1=ALU.add),
                      reads=[ch["a"], vecs], writes=[ch["t"]])
                em.op("dve", lambda e: e.tensor_tensor(out=ch["kd"][:], in0=ch["k"][:], in1=ch["t"][:], op=ALU.mult), reads=[ch["k"], ch["t"]], writes=[ch["kd"]])
                em.op("dve", lambda e: e.tensor_tensor(out=ch["t"][:], in0=ch["kk"][:], in1=ch["a"][:], op=ALU.mult), reads=[ch["kk"], ch["a"]], writes=[ch["t"]])
                em.op("dve", lambda e: e.tensor_scalar(out=ch["kk"][:], in0=ch["kk"][:], scalar1=-1.0, scalar2=None, op0=ALU.mult), reads=[ch["kk"]], writes=[ch["kk"]])
                for dst, nm in ((self.kR, "r"), (self.kK, "kd"), (self.kA, "kk"), (self.kB, "t")):
                    em.dma("pool", dst[z].ap()[c][:, s0:s0 + n_], ch[nm][:, 0:n_], reads=[ch[nm]])
                if z == 0:
                    em.op("dve", lambda e: e.tensor_scalar(out=ch["t2"][:], in0=ch["ao"][:], scalar1=vecs[:, 8 + c:9 + c], scalar2=vecs[:, 32 + c:33 + c], op0=ALU.mult, op1=ALU.add),
                          reads=[ch["ao"], vecs], writes=[ch["t2"]])
                    em.op("dve", lambda e: e.tensor_tensor(out=ch["t2"][:], in0=ch["t2"][:], in1=ch["k"][:], op=ALU.mult), reads=[ch["t2"], ch["k"]], writes=[ch["t2"]])
                    em.op("dve", lambda e: e.tensor_tensor(out=ch["t2"][:], in0=ch["t2"][:], in1=ch["kd"][:], op=ALU.add), reads=[ch["t2"], ch["kd"]], writes=[ch["t2"]])
                    em.op("dve", lambda e: e.scalar_tensor_tensor(out=sqb[:], in0=ch["t2"][:], scalar=vecs[:, 16 + c:17 + c], in1=ch["r"][:], op0=ALU.mult, op1=ALU.mult),
                          reads=[ch["t2"], ch["r"], vecs], writes=[sqb])
                    ps = self.pn()
                    self.mm(ps, ps[:, 0:N], self.cb(C_B64), sqb[:], True, True, [self.cmb, sqb])
                    ob = obf[nob % 2]
                    nob += 1
                    em.op("dve", lambda e, ps=ps, ob=ob: e.tensor_tensor(out=ob[:], in0=ps[:, 0:N], in1=ch["v"][:], op=ALU.mult), reads=[ps, ch["v"]], writes=[ob])
                    em.dma("pool", self.kBT.ap()[c][:, s0:s0 + n_], ob[:, 0:n_], reads=[ob])
                    ps = self.pn()
                    self.mm(ps, ps[:, 0:N], g2[:, 0, cs], gsT[:], True, True, [g2, gsT])
                    ob = obf[nob % 2]
                    nob += 1
                    em.op("act", lambda e, ps=ps, ob=ob: e.activation(out=ob[:], in_=ps[:, 0:N], func=AF.Copy), reads=[ps], writes=[ob])
                    em.dma("pool", self.kGT.ap()[c][:, s0:s0 + n_], ob[:, 0:n_], reads=[ob])
        em.barrier()
        em.release([mixv, vecs, w1, w2, w0r, Wr, Wk, Wv, g1, g2, hW, xx, t1T, gsT, sqb] + a0 + a1 + a2 + xm + u1T + list(chs[0].values()) + list(chs[1].values()) + obf + vtm + lwt)


MK.rk_alloc = rk_alloc
MK.rk_phase_a1 = rk_phase_a1
MK.rk_phase_a2 = rk_phase_a2


def rk_phase_s(self, z):
    em = self.em
    T, NT = self.T, self.NT
    O = self.O2[z]
    with ExitStack() as st:
        B = lambda n, sh, dt: em.sb(st, "s" + n, sh, dt)
        ld = {n: B("ld" + n, [128, 8, 128], F32) for n in "rkab"}
        v = B("v", [128, 1024], BF16)
        lw = B("lw", [128, 1024], F32)
        ex = {n: B("ex" + n, [128, 8, 128], F32) for n in ("g", "gx", "ng", "gl")}
        GL = B("GL", [128, 8, 2], F32)
        AR = B("AR", [128, 8, 2, 128], BF16)
        BK = B("BK", [128, 8, 2, 128], BF16)
        BKh = B("BKh", [128, 8, 2, 128], BF16)
        BhT = B("BhT", [128, 8, 128], BF16)
        KhT = B("KhT", [128, 8, 128], BF16)
        MASK4 = B("MASK4", [128, 4, 128], BF16)
        for q, cidx, sgn in ((0, C_MS, -1.0), (1, C_MI, 1.0), (2, C_MS, 1.0), (3, C_MI, 1.0)):
            em.op("dve", lambda e, q=q, cidx=cidx: e.tensor_copy(out=MASK4[:, q, :], in_=self.cb(cidx)), reads=[self.cmb], writes=[MASK4])
        AM = B("AM", [128, 4, 16, 128], BF16)
        INVg = [B("INV%d" % g, [128, 4, 128], BF16) for g in range(4)]
        INVTg = [B("INVT%d" % g, [128, 4, 128], BF16) for g in range(4)]
        Tsbg = [B("Tsb%d" % g, [128, 4, 128], BF16) for g in range(4)]
        tmpxg = [[B("tmpx%d_%d" % (q, g), [128, 4, 128], BF16) for g in range(4)] for q in range(2)]
        RHSb = B("RHSb", [128, 1024], BF16)
        Ub = B("Ub", [128, 1024], BF16)
        OT = [B("OT%d" % q, [128, 1024], F32) for q in range(2)]
        S = B("S", [128, 8, 2, 64], F32)
        Sb = B("Sb", [128, 8, 2, 64], BF16)
        stmp = B("stmp", [128, 8, 128], F32)
        t2m = B("t2m", [128, 4, 128], F32)
        em.op("pool", lambda e: e.memset(S[:], 0.0), writes=[S])
        em.op("pool", lambda e: e.memset(Sb[:], 0.0), writes=[Sb])
        srcs = dict(r=self.kR[z], k=self.kK[z], a=self.kA[z], b=self.kB[z])
        f2 = lambda ap: ap.rearrange("p a b -> p (a b)")
        for t in range(NT):
            c0 = t * 128
            for n in "rkab":
                em.dma("sp", ld[n][:], srcs[n].ap()[:, :, c0:c0 + 128].rearrange("c p n -> p c n"), writes=[ld[n]])
            em.dma("sp", v[:], self.kV[z].ap()[t], writes=[v])
            em.dma("sp", lw[:], self.kLW[z].ap()[t], writes=[lw])
            for c in range(8):
                ps = self.pn()
                for q, cidx in enumerate((C_TRI, C_TRIS, C_BLK)):
                    self.mm(ps, ps[:, q * 128:(q + 1) * 128], lw[:, c * 128:(c + 1) * 128], self.cf(cidx), True, True, [lw, self.cmf])
                em.op("act", lambda e, ps=ps, c=c: e.activation(out=ex["g"][:, c, :], in_=ps[:, 0:128], func=AF.Exp), reads=[ps], writes=[ex["g"]])
                em.op("act", lambda e, ps=ps, c=c: e.activation(out=ex["gx"][:, c, :], in_=ps[:, 128:256], func=AF.Exp), reads=[ps], writes=[ex["gx"]])
                em.op("act", lambda e, ps=ps, c=c: e.activation(out=ex["ng"][:, c, :], in_=ps[:, 0:128], func=AF.Exp, scale=-1.0), reads=[ps], writes=[ex["ng"]])
                em.op("act", lambda e, ps=ps, c=c: e.activation(out=ex["gl"][:, c, :], in_=ps[:, 256:384], func=AF.Exp), reads=[ps], writes=[ex["gl"]])
            em.op("dve", lambda e: e.tensor_copy(out=GL[:], in_=ex["gl"][:].rearrange("p c (a b) -> p c a b", a=2)[:, :, :, 0]), reads=[ex["gl"]], writes=[GL])
            em.op("dve", lambda e: e.tensor_tensor(out=ex["gl"][:], in0=ex["gl"][:], in1=ex["ng"][:], op=ALU.mult), reads=[ex["gl"], ex["ng"]], writes=[ex["gl"]])
            em.op("dve", lambda e: e.tensor_tensor(out=AR[:, :, 0, :], in0=ld["a"][:], in1=ex["gx"][:], op=ALU.mult), reads=[ld["a"], ex["gx"]], writes=[AR])
            em.op("dve", lambda e: e.tensor_tensor(out=AR[:, :, 1, :], in0=ld["r"][:], in1=ex["g"][:], op=ALU.mult), reads=[ld["r"], ex["g"]], writes=[AR])
            em.op("pool", lambda e: e.tensor_tensor(out=BK[:, :, 0, :], in0=ld["b"][:], in1=ex["ng"][:], op=ALU.mult), reads=[ld["b"], ex["ng"]], writes=[BK])
            em.op("pool", lambda e: e.tensor_tensor(out=BK[:, :, 1, :], in0=ld["k"][:], in1=ex["ng"][:], op=ALU.mult), reads=[ld["k"], ex["ng"]], writes=[BK])
            em.op("pool", lambda e: e.tensor_tensor(out=BKh[:, :, 0, :], in0=ld["b"][:], in1=ex["gl"][:], op=ALU.mult), reads=[ld["b"], ex["gl"]], writes=[BKh])
            em.op("dve", lambda e: e.tensor_tensor(out=BKh[:, :, 1, :], in0=ld["k"][:], in1=ex["gl"][:], op=ALU.mult), reads=[ld["k"], ex["gl"]], writes=[BKh])
            import os
            dS = int(os.environ.get("RKS", "99"))
            if dS <= 1:
                continue
            for q, dstb in ((0, BhT), (1, KhT)):
                ps = self.pn()
                pb = ps[:].bitcast(BF16)
                for c in range(8):
                    em.op("pe", lambda e, pb=pb, c=c, q=q: e.transpose(out=pb[:, c * 128:(c + 1) * 128], in_=BKh[:, c, q, :], identity=self.cb(C_ID)),
                          reads=[BKh, self.cmb], writes=[ps])
                em.op("act", lambda e, pb=pb, dstb=dstb: e.activation(out=f2(dstb[:]), in_=pb[:, 0:1024], func=AF.Copy), reads=[ps], writes=[dstb])
            if dS <= 2:
                continue
            for h in range(16):
                c, base = h // 2, (h % 2) * 64
                ps = self.pn()
                rhs = AR[base:base + 64, c, :, :].rearrange("p a b -> p (a b)")
                self.mm(ps, ps[:, 0:256], BK[base:base + 64, c, 0, :], rhs, True, True, [BK, AR])
                self.mm(ps, ps[:, 256:512], BK[base:base + 64, c, 1, :], rhs, True, True, [BK, AR])
                em.op("dve", lambda e, ps=ps, h=h: e.tensor_tensor(out=AM[:, :, h, :], in0=ps[:, :].rearrange("p (a b) -> p a b", a=4), in1=MASK4[:], op=ALU.mult),
                      reads=[ps, MASK4], writes=[AM])
            if dS <= 3:
                continue
            self.tri_inverse16(lambda h: AM[:, 0, h, :], [AM] * 4, INVg, INVTg, Tsbg, tmpxg, -1.0)
            if dS <= 4:
                continue
            o_ = OT[t % 2]
            for cc in range(2):
                r0 = cc * 64
                rs_ = slice(r0, r0 + 64)
                pr = [self.pn(), self.pn()]
                for c in range(8):
                    pb_ = pr[c // 4]
                    q0 = (c % 4) * 128
                    self.mm(pb_, pb_[rs_, q0:q0 + 128], AR[:, c, 0, rs_], Sb[:, c, :, :].rearrange("p a b -> p (a b)"), True, False, [AR, Sb])
                    for e_ in range(2):
                        h = 2 * c + e_
                        self.mm(pb_, pb_[rs_, q0 + e_ * 64:q0 + (e_ + 1) * 64], AM[rs_, 2, h, rs_], v[rs_, h * 64:(h + 1) * 64], False, True, [AM, v])
                for b2 in range(2):
                    em.op("act" if b2 else "dve", lambda e, b2=b2: (e.activation(out=RHSb[rs_, b2 * 512:(b2 + 1) * 512], in_=pr[b2][rs_, :], func=AF.Copy) if b2 else
                                                                   e.tensor_copy(out=RHSb[rs_, 0:512], in_=pr[0][rs_, :])), reads=[pr[b2]], writes=[RHSb])
                pu = [self.pn(), self.pn()]
                for h in range(16):
                    pb_ = pu[h // 8]
                    cs_ = slice((h % 8) * 64, (h % 8 + 1) * 64)
                    self.mm(pb_, pb_[rs_, cs_], INVTg[h // 4][rs_, h % 4, rs_], RHSb[rs_, h * 64:(h + 1) * 64], True, True, [INVTg[h // 4], RHSb])
                for b2 in range(2):
                    em.op("act" if b2 else "dve", lambda e, b2=b2: (e.activation(out=Ub[rs_, b2 * 512:(b2 + 1) * 512], in_=pu[b2][rs_, :], func=AF.Copy) if b2 else
                                                                   e.tensor_copy(out=Ub[rs_, 0:512], in_=pu[0][rs_, :])), reads=[pu[b2]], writes=[Ub])
                po = [self.pn(), self.pn()]
                pS = [self.pn(), self.pn()]
                for c in range(8):
                    pb_ = po[c // 4]
                    q0 = (c % 4) * 128
                    self.mm(pb_, pb_[rs_, q0:q0 + 128], AR[:, c, 1, rs_], Sb[:, c, :, :].rearrange("p a b -> p (a b)"), True, False, [AR, Sb])
                    for e_ in range(2):
                        h = 2 * c + e_
                        osl = pb_[rs_, q0 + e_ * 64:q0 + (e_ + 1) * 64]
                        self.mm(pb_, osl, AM[rs_, 1, h, rs_], Ub[rs_, h * 64:(h + 1) * 64], False, False, [AM, Ub])
                        self.mm(pb_, osl, AM[rs_, 3, h, rs_], v[rs_, h * 64:(h + 1) * 64], False, True, [AM, v])
                    ps_ = pS[c // 4]
                    self.mm(ps_, ps_[:, q0:q0 + 128], BhT[rs_, c, :], Ub[rs_, c * 128:(c + 1) * 128], True, False, [BhT, Ub])
                    self.mm(ps_, ps_[:, q0:q0 + 128], KhT[rs_, c, :], v[rs_, c * 128:(c + 1) * 128], False, True, [KhT, v])
                for b2 in range(2):
                    em.op("act", lambda e, b2=b2: e.activation(out=o_[rs_, b2 * 512:(b2 + 1) * 512], in_=po[b2][rs_, :], func=AF.Copy), reads=[po[b2]], writes=[o_])
                em.op("dve", lambda e, cc=cc: e.tensor_tensor(out=stmp[:], in0=S[:].rearrange("p c a b -> p c (a b)"),
                                                              in1=GL[:, :, cc].unsqueeze(2).broadcast_to([128, 8, 128]), op=ALU.mult),
                      reads=[S, GL], writes=[stmp])
                for b2 in range(2):
                    em.op("dve", lambda e, b2=b2: e.tensor_tensor(out=t2m[:], in0=pS[b2][:, :].rearrange("p (c n) -> p c n", c=4),
                                                                  in1=self.cb(C_B64).unsqueeze(1).broadcast_to([128, 4, 128]), op=ALU.mult),
                          reads=[pS[b2], self.cmb], writes=[t2m])
                    em.op("pool", lambda e, b2=b2: e.tensor_tensor(out=S[:, b2 * 4:(b2 + 1) * 4, :, :].rearrange("p c a b -> p c (a b)"),
                                                                   in0=stmp[:, b2 * 4:(b2 + 1) * 4, :], in1=t2m[:], op=ALU.add),
                          reads=[stmp, t2m], writes=[S])
                em.op("act", lambda e: e.activation(out=Sb[:].rearrange("p c a b -> p (c a b)"), in_=S[:].rearrange("p c a b -> p (c a b)"), func=AF.Copy),
                      reads=[S], writes=[Sb])
            em.dma("pool", O.ap()[t * 128:(t + 1) * 128, 0:1024], o_[:], reads=[o_])
        em.barrier()
        em.release(list(ld.values()) + list(ex.values()) + [v, lw, GL, AR, BK, BKh, BhT, KhT, MASK4, AM, RHSb, Ub, S, Sb, stmp, t2m] + INVg + INVTg + Tsbg + tmpxg[0] + tmpxg[1] + OT)


def rk_factory(self):
    em = self.em

    def factory(st):
        vecs = em.sb(st, "qvecs", [128, 32], F32)
        em.dma("sp", vecs[:], self.rk_in["vec"].ap(), writes=[vecs])
        oo = [em.sb(st, "qoo%d" % q, [128, 8, 64], F32) for q in range(2)]
        p1 = [em.sb(st, "qp1%d" % q, [128, 512], F32) for q in range(2)]
        bt = [em.sb(st, "qbt%d" % q, [128, 8, 128], BF16) for q in range(2)]
        gt = [em.sb(st, "qgt%d" % q, [128, 8, 128], BF16) for q in range(2)]
        osum = em.sb(st, "qosum", [128, 8, 64], F32)
        sq = em.sb(st, "qsq", [128, 8, 64], F32)
        ss = em.sb(st, "qss", [128, 8], F32)
        y = em.sb(st, "qy", [128, 1024], BF16)
        tt = em.sb(st, "qtt", [128, 128], F32)
        cnt = [0]
        f2 = lambda b: b[:].rearrange("p a b -> p (a b)")
        bc8 = lambda ap: ap.unsqueeze(2).broadcast_to([128, 8, 64])

        def make_yT(t, isctx, yT):
            pt = self.ptile(t)
            b_, g_ = bt[t % 2], gt[t % 2]
            em.dma("sp", b_[:], self.kBT.ap()[:, :, t * 128:(t + 1) * 128].rearrange("c p n -> p c n"), writes=[b_])
            em.dma("sp", g_[:], self.kGT.ap()[:, :, t * 128:(t + 1) * 128].rearrange("c p n -> p c n"), writes=[g_])
            for blk in range(2):
                k = cnt[0]
                cnt[0] += 1
                o_, a1 = oo[k % 2], p1[k % 2]
                cs = slice(blk * 512, (blk + 1) * 512)
                em.dma("sp", f2(o_), self.O2[0].ap()[t * 128:(t + 1) * 128, cs], writes=[o_])
                em.dma("sp", a1[:], self.O2[1].ap()[pt * 128:(pt + 1) * 128, cs], writes=[a1])
                ps = self.pn()
                self.mm(ps, ps[:, :], self.cf(C_J0), a1[:], True, True, [self.cmf, a1])
                em.op("dve", lambda e: e.tensor_tensor(out=f2(osum), in0=ps[:, :], in1=f2(o_), op=ALU.add), reads=[ps, o_], writes=[osum])
                em.op("dve", lambda e: e.tensor_reduce(out=ss[:], in_=osum[:], axis=AX.X, op=ALU.add), reads=[osum], writes=[ss])
                em.op("dve", lambda e: e.tensor_scalar(out=ss[:], in0=ss[:], scalar1=-1.0 / 64.0, scalar2=None, op0=ALU.mult), reads=[ss], writes=[ss])
                em.op("dve", lambda e: e.tensor_tensor(out=osum[:], in0=osum[:], in1=bc8(ss[:]), op=ALU.add), reads=[osum, ss], writes=[osum])
                em.op("pool", lambda e: e.tensor_tensor(out=sq[:], in0=osum[:], in1=osum[:], op=ALU.mult), reads=[osum], writes=[sq])
                em.op("dve", lambda e: e.tensor_reduce(out=ss[:], in_=sq[:], axis=AX.X, op=ALU.add), reads=[sq], writes=[ss])
                em.op("act", lambda e: e.activation(out=ss[:], in_=ss[:], func=AF.Sqrt, bias=64e-5, scale=1.0 / 64.0), reads=[ss], writes=[ss])
                em.op("dve", lambda e: e.reciprocal(out=ss[:], in_=ss[:]), reads=[ss], writes=[ss])
                em.op("dve", lambda e: e.tensor_tensor(out=y[:, cs].rearrange("p (a b) -> p a b", a=8), in0=osum[:], in1=bc8(ss[:]), op=ALU.mult),
                      reads=[osum, ss], writes=[y])
            ps = self.pn()
            pb = ps[:].bitcast(BF16)
            for c in range(8):
                em.op("pe", lambda e: e.transpose(out=pb[:, c * 128:(c + 1) * 128], in_=y[:, c * 128:(c + 1) * 128], identity=self.cb(C_ID)),
                      reads=[y, self.cmb], writes=[ps])
            for c in range(8):
                em.op("dve", lambda e: e.scalar_tensor_tensor(out=tt[:], in0=pb[:, c * 128:(c + 1) * 128], scalar=vecs[:, 24 + c:25 + c], in1=b_[:, c, :],
                                                              op0=ALU.mult, op1=ALU.add), reads=[ps, vecs, b_], writes=[tt])
                em.op("pool", lambda e: e.tensor_tensor(out=yT[:, c, :], in0=tt[:], in1=g_[:, c, :], op=ALU.mult), reads=[tt, g_], writes=[yT])
        return make_yT, [vecs, osum, sq, ss, y, tt] + oo + p1 + bt + gt
    return factory


def rk_layer(self, i, modLC, rows):
    if not hasattr(self, "kHT"):
        self.rk_alloc()
    import os
    dbg = int(os.environ.get("RKDBG", "99"))
    for z in range(2):
        self.rk_phase_a1(modLC, z)
        if dbg >= 2:
            self.rk_phase_a2(z)
        if dbg >= 3:
            self.rk_phase_s(z)
    if dbg >= 4:
        self.phase_f(i, modLC, rows, self.rk_factory(), "rkout", 8)


MK.rk_phase_s = rk_phase_s
MK.rk_factory = rk_factory
MK.rk_layer = rk_layer
```
